# Optimizing a Trainium2 kernel written in Bass

```python
import math
import jax, jax.numpy as jnp
from jax import lax
import numpy as np

D_MODEL = 1024
BATCH = 8
SEQ = 4096
DEPTH = 2

D_MIX = D_MODEL
W_CONF = D_MIX // 4
W_POOL = D_MIX // 4
W_DIFF = D_MIX // 4
W_SCONV = D_MIX // 4

CONF_KERNEL = 31
POOL_WINDOWS = (2, 4, 8, 16)
POOL_GROUPS = len(POOL_WINDOWS)
POOL_GROUP_DIM = W_POOL // POOL_GROUPS
DIFF_HEADS = 4
DIFF_HEAD_DIM = W_DIFF // (2 * DIFF_HEADS)
DIFF_V_DIM = 2 * DIFF_HEAD_DIM
ROPE_THETA = 10000.0
Q_BLOCK = 128
SCONV_KERNEL = 3

N_GROUPS = 4
EXPERTS_PER_GROUP = 4
N_EXPERTS = N_GROUPS * EXPERTS_PER_GROUP
TOP_K_IN_GROUP = 2
D_EXPERT = 256

PLE_DIM = 256
EPS = 1e-6

IN_WIDTHS = (W_CONF, W_CONF,
             W_POOL,
             W_DIFF, W_DIFF, W_DIFF,
             W_SCONV, W_SCONV, W_SCONV)
IN_COLS = sum(IN_WIDTHS)
IN_SPLITS = tuple(int(s) for s in np.cumsum(IN_WIDTHS)[:-1])

kernel_name = "hybrid_parallel_heads_hmoe_block"


def rmsnorm(x, g):
    xf = x.astype(jnp.float32)
    y = xf * lax.rsqrt(jnp.mean(xf * xf, axis=-1, keepdims=True) + EPS)
    return (y * g.astype(jnp.float32)).astype(x.dtype)


def layernorm(x, g, b):
    xf = x.astype(jnp.float32)
    mu = jnp.mean(xf, axis=-1, keepdims=True)
    var = jnp.mean(jnp.square(xf - mu), axis=-1, keepdims=True)
    y = (xf - mu) * lax.rsqrt(var + EPS)
    return (y * g.astype(jnp.float32) + b.astype(jnp.float32)).astype(x.dtype)


def causal_dwconv(u, w):
    k, c = w.shape
    return lax.conv_general_dilated(
        u, w.astype(u.dtype)[:, None, :], window_strides=(1,),
        padding=[(k - 1, 0)], dimension_numbers=("NWC", "WIO", "NWC"),
        feature_group_count=c)


def rope_tables(seq, dim):
    pos = jnp.arange(seq, dtype=jnp.float32)
    inv = ROPE_THETA ** (-jnp.arange(0, dim, 2, dtype=jnp.float32) / dim)
    ang = pos[:, None] * inv[None, :]
    ang = jnp.concatenate([ang, ang], axis=-1)
    return jnp.cos(ang), jnp.sin(ang)


def apply_rope(x, cos, sin):
    xf = x.astype(jnp.float32)
    half = xf.shape[-1] // 2
    rot = jnp.concatenate([-xf[..., half:], xf[..., :half]], axis=-1)
    c = cos[None, :, None, None, :]
    s = sin[None, :, None, None, :]
    return (xf * c + rot * s).astype(x.dtype)


def conformer_conv(a_val, a_gate, conv_w, conv_b, ln_g, ln_b):
    glu = a_val * jax.nn.sigmoid(a_gate)
    y = causal_dwconv(glu, conv_w) + conv_b.astype(glu.dtype)
    return jax.nn.silu(layernorm(y, ln_g, ln_b))


def pool_mixer(u, w, b, scale):
    bsz, s, _ = u.shape
    cs = jnp.cumsum(u.astype(jnp.float32), axis=1)
    t = jnp.arange(s)
    outs = []
    for g, win in enumerate(POOL_WINDOWS):
        sl = slice(g * POOL_GROUP_DIM, (g + 1) * POOL_GROUP_DIM)
        csg = cs[..., sl]
        shifted = jnp.pad(csg, ((0, 0), (win, 0), (0, 0)))[:, :s]
        cnt = jnp.minimum(t + 1, win).astype(jnp.float32)[None, :, None]
        outs.append((csg - shifted) / cnt)
    pooled = jnp.concatenate(outs, axis=-1).astype(u.dtype) - u
    pg = pooled.reshape(bsz, s, POOL_GROUPS, POOL_GROUP_DIM)
    y = jnp.einsum("bsgc,gcd->bsgd", pg, w) + b
    return y.reshape(bsz, s, W_POOL) * scale


def diff_attention(q, k, v, lam, subln_g, lam_init, cos, sin):
    bsz, s = q.shape[0], q.shape[1]
    q = apply_rope(q, cos, sin)
    k = apply_rope(k, cos, sin)
    scale = DIFF_HEAD_DIM ** -0.5
    key_pos = jnp.arange(s)

    def block(i):
        start = i * Q_BLOCK
        qb = lax.dynamic_slice_in_dim(q, start, Q_BLOCK, axis=1)
        sc = jnp.einsum("bqhcd,bkhcd->bhcqk", qb, k,
                        preferred_element_type=jnp.float32) * scale
        qpos = start + jnp.arange(Q_BLOCK)
        mask = key_pos[None, :] <= qpos[:, None]
        a = jax.nn.softmax(jnp.where(mask, sc, -jnp.inf), axis=-1)
        a = a[:, :, 0] - lam * a[:, :, 1]
        return jnp.einsum("bhqk,bkhe->bqhe", a.astype(v.dtype), v)

    o = lax.map(block, jnp.arange(s // Q_BLOCK))
    o = jnp.moveaxis(o, 0, 1).reshape(bsz, s, DIFF_HEADS, DIFF_V_DIM)
    o = rmsnorm(o, subln_g) * (1.0 - lam_init)
    return o.reshape(bsz, s, W_DIFF)


def short_gated_conv(gb, gc, val, w):
    return gb * causal_dwconv(gc * val, w)


def hier_moe(x, wg, bg, we, be, w_gate, w_up, w_down):
    bsz, s, _ = x.shape
    gl = jnp.einsum("bsd,dg->bsg", x, wg, preferred_element_type=jnp.float32) + bg
    pg = jax.nn.softmax(gl, axis=-1)
    c = jnp.argmax(gl, axis=-1)
    el = jnp.einsum("bsd,de->bse", x, we, preferred_element_type=jnp.float32) + be
    el = el.reshape(bsz, s, N_GROUPS, EXPERTS_PER_GROUP)
    el_c = jnp.take_along_axis(el, c[..., None, None], axis=2)[..., 0, :]
    pe = jax.nn.softmax(el_c, axis=-1)
    tv, ti = lax.top_k(pe, TOP_K_IN_GROUP)
    tv = tv / jnp.sum(tv, axis=-1, keepdims=True)
    gc = jnp.take_along_axis(pg, c[..., None], axis=-1)
    wts = gc * tv
    idx = c[..., None] * EXPERTS_PER_GROUP + ti
    combine = jnp.sum(jax.nn.one_hot(idx, N_EXPERTS, dtype=jnp.float32)
                      * wts[..., None], axis=-2)
    y = jnp.zeros(x.shape, jnp.float32)
    for e in range(N_EXPERTS):
        hdn = jax.nn.silu(x @ w_gate[e]) * (x @ w_up[e])
        y = y + combine[..., e:e + 1] * (hdn @ w_down[e]).astype(jnp.float32)
    return y.astype(x.dtype)


def setup_inputs(seed: int = 0) -> dict:
    key = jax.random.key(seed)
    ks = iter(jax.random.split(key, 40))

    def nrm(shape, scale):
        return scale * jax.random.normal(next(ks), shape, jnp.float32)

    D = D_MODEL
    return {
        "x": nrm((BATCH, SEQ, D), 1.0),
        "p": nrm((DEPTH, BATCH, SEQ, PLE_DIM), 1.0),
        "mix_norm": 1.0 + nrm((DEPTH, D), 0.05),
        "w_in": nrm((DEPTH, D, IN_COLS), D ** -0.5),
        "conf_conv_w": nrm((DEPTH, CONF_KERNEL, W_CONF), CONF_KERNEL ** -0.5),
        "conf_conv_b": nrm((DEPTH, W_CONF), 0.02),
        "conf_ln_g": 1.0 + nrm((DEPTH, W_CONF), 0.05),
        "conf_ln_b": nrm((DEPTH, W_CONF), 0.02),
        "pool_w": nrm((DEPTH, POOL_GROUPS, POOL_GROUP_DIM, POOL_GROUP_DIM), POOL_GROUP_DIM ** -0.5),
        "pool_b": nrm((DEPTH, POOL_GROUPS, POOL_GROUP_DIM), 0.02),
        "pool_scale": 1.0 + nrm((DEPTH, W_POOL), 0.05),
        "diff_lam_q1": nrm((DEPTH, DIFF_HEAD_DIM), 0.1),
        "diff_lam_k1": nrm((DEPTH, DIFF_HEAD_DIM), 0.1),
        "diff_lam_q2": nrm((DEPTH, DIFF_HEAD_DIM), 0.1),
        "diff_lam_k2": nrm((DEPTH, DIFF_HEAD_DIM), 0.1),
        "diff_subln_g": 1.0 + nrm((DEPTH, DIFF_V_DIM), 0.05),
        "sconv_w": nrm((DEPTH, SCONV_KERNEL, W_SCONV), SCONV_KERNEL ** -0.5),
        "w_out": nrm((DEPTH, D_MIX, D), D_MIX ** -0.5),
        "ffn_norm": 1.0 + nrm((DEPTH, D), 0.05),
        "router_group_w": nrm((DEPTH, D, N_GROUPS), D ** -0.5),
        "router_group_b": nrm((DEPTH, N_GROUPS), 0.01),
        "router_expert_w": nrm((DEPTH, D, N_EXPERTS), D ** -0.5),
        "router_expert_b": nrm((DEPTH, N_EXPERTS), 0.01),
        "expert_w_gate": nrm((DEPTH, N_EXPERTS, D, D_EXPERT), D ** -0.5),
        "expert_w_up": nrm((DEPTH, N_EXPERTS, D, D_EXPERT), D ** -0.5),
        "expert_w_down": nrm((DEPTH, N_EXPERTS, D_EXPERT, D), D_EXPERT ** -0.5),
        "ple_norm": 1.0 + nrm((DEPTH, D), 0.05),
        "ple_gate_w": nrm((DEPTH, D, D), D ** -0.5),
        "ple_gate_b": nrm((DEPTH, D), 0.02),
        "ple_proj": nrm((DEPTH, PLE_DIM, D), PLE_DIM ** -0.5),
        "final_norm": 1.0 + nrm((D,), 0.05),
    }


def reference(x, p, mix_norm, w_in, conf_conv_w, conf_conv_b, conf_ln_g, conf_ln_b,
              pool_w, pool_b, pool_scale, diff_lam_q1, diff_lam_k1, diff_lam_q2,
              diff_lam_k2, diff_subln_g, sconv_w, w_out, ffn_norm, router_group_w,
              router_group_b, router_expert_w, router_expert_b, expert_w_gate,
              expert_w_up, expert_w_down, ple_norm, ple_gate_w, ple_gate_b, ple_proj,
              final_norm):
    bsz, s, _ = x.shape
    cos, sin = rope_tables(s, DIFF_HEAD_DIM)
    h = x
    for i in range(DEPTH):
        n = rmsnorm(h, mix_norm[i])
        u = n @ w_in[i]
        a_val, a_gate, pool_in, q, k, v, gb, gc, sv = jnp.split(u, IN_SPLITS, axis=-1)

        y_a = conformer_conv(a_val, a_gate, conf_conv_w[i], conf_conv_b[i],
                             conf_ln_g[i], conf_ln_b[i])
        y_b = pool_mixer(pool_in, pool_w[i], pool_b[i], pool_scale[i])

        lam_init = 0.8 - 0.6 * math.exp(-0.3 * i)
        lam = (jnp.exp(jnp.sum(diff_lam_q1[i].astype(jnp.float32) * diff_lam_k1[i].astype(jnp.float32)))
               - jnp.exp(jnp.sum(diff_lam_q2[i].astype(jnp.float32) * diff_lam_k2[i].astype(jnp.float32)))
               + lam_init)
        y_c = diff_attention(q.reshape(bsz, s, DIFF_HEADS, 2, DIFF_HEAD_DIM),
                             k.reshape(bsz, s, DIFF_HEADS, 2, DIFF_HEAD_DIM),
                             v.reshape(bsz, s, DIFF_HEADS, DIFF_V_DIM),
                             lam, diff_subln_g[i], lam_init, cos, sin)
        y_d = short_gated_conv(gb, gc, sv, sconv_w[i])

        mixed = jnp.concatenate([y_a, y_b, y_c, y_d], axis=-1)
        h = h + mixed @ w_out[i]

        h = h + hier_moe(rmsnorm(h, ffn_norm[i]), router_group_w[i], router_group_b[i],
                         router_expert_w[i], router_expert_b[i], expert_w_gate[i],
                         expert_w_up[i], expert_w_down[i])

        gate = jax.nn.sigmoid(rmsnorm(h, ple_norm[i]) @ ple_gate_w[i] + ple_gate_b[i])
        h = h + gate * (p[i] @ ple_proj[i])
    return rmsnorm(h, final_norm)
```

```python
import math
import contextlib
import numpy as np
import ml_dtypes
import concourse.bass as bass
import concourse.mybir as mybir
from concourse.bass_utils import run_bass_kernel_spmd

F32 = mybir.dt.float32
BF16 = mybir.dt.bfloat16
ALU = mybir.AluOpType
AF = mybir.ActivationFunctionType
AX = mybir.AxisListType

S = 4096
D = 1024
DEPTH = 2
NT = 8
TS = 512
INC = 2304
NE = 16
EPS = 1e-6
CK = 31
SCALE = 32 ** -0.5
SEM_CHUNK = 30000

PC_MIXG, PC_FFNG, PC_PLEG, PC_FING = 0, 8, 16, 24
PC_CW = 32
PC_CB = PC_CW + 62
PC_LNG = PC_CB + 2
PC_LNB = PC_LNG + 2
PC_PB = PC_LNB + 2
PC_PS = PC_PB + 2
PC_SW = PC_PS + 2
PC_SUB = PC_SW + 6
PC_IW = PC_SUB + 1
PC_N = PC_IW + 2


class Tok:
    __slots__ = ("sem", "val", "eng")

    def __init__(self, eng):
        self.sem = None
        self.val = None
        self.eng = eng


class Res:
    __slots__ = ("name", "w", "r", "excl")

    def __init__(self, name, excl=False):
        self.name = name
        self.w = None
        self.r = []
        self.excl = excl


class Prog:
    def __init__(self, nc, es):
        self.nc = nc
        self.es = es
        self.eobj = {"pe": nc.tensor, "act": nc.scalar, "dve": nc.vector,
                     "pool": nc.gpsimd, "sp": nc.sync}
        self.sems = {e: [] for e in self.eobj}
        self.count = {e: 0 for e in self.eobj}
        self.known = {e: {} for e in self.eobj}
        self.pending = {e: None for e in self.eobj}
        self.last_tok = {e: None for e in self.eobj}
        self.nsem = 0
        self.dma_sems = []
        self.dma_cnt = []
        self.dma_i = 0
        self.dma_ip = 0
        for i in range(24):
            self.dma_sems.append(self._new_sem("dq%d" % i))
            self.dma_cnt.append(0)
        self.dma_toks = []
        self.n_ops = 0
        self.n_waits = 0
        self.limit = None
        self.n_all = 0
        self.skip = False

    def _skipping(self):
        if self.skip:
            return True
        if self.limit is not None and self.n_all > self.limit and all(v is None for v in self.pending.values()):
            self.skip = True
            return True
        return False

    def _new_sem(self, name):
        self.nsem += 1
        return self.es.enter_context(self.nc.semaphore(name))

    def _eng_tok(self, eng, tok):
        c = self.count[eng]
        idx = c // SEM_CHUNK
        while len(self.sems[eng]) <= idx:
            self.sems[eng].append(self._new_sem("%s%d" % (eng, len(self.sems[eng]))))
        tok.sem = self.sems[eng][idx]
        tok.val = c % SEM_CHUNK + 1
        self.count[eng] = c + 1
        return tok

    def _wait(self, eng, tok):
        if tok is None:
            return
        if tok.sem is None:
            assert tok.eng == eng, "dependency on unsignaled op of %s from %s" % (tok.eng, eng)
            return
        k = self.known[eng]
        sid = id(tok.sem)
        if k.get(sid, 0) >= tok.val:
            return
        k[sid] = tok.val
        self.eobj[eng].wait_ge(tok.sem, tok.val)
        self.n_waits += 1

    def _deps(self, eng, reads, writes, is_dma=False):
        for r in reads:
            if r.w is not None:
                if r.w.eng == eng and not is_dma and eng == "pe":
                    continue
                self._wait(eng, r.w)
        same_ok = (eng == "pe") and not is_dma
        for w in writes:
            if w.w is not None and not (same_ok and w.w.eng == eng):
                self._wait(eng, w.w)
            for t in w.r:
                if not (same_ok and t.eng == eng):
                    self._wait(eng, t)

    def op(self, eng, fn, reads=(), writes=(), signal=True):
        self.n_all += 1
        if self._skipping():
            return None
        if any(r.excl for r in reads):
            writes = list(writes) + [r for r in reads if r.excl and r not in writes]
            reads = [r for r in reads if not r.excl]
        self._deps(eng, reads, writes)
        ins = fn(self.eobj[eng])
        self.n_ops += 1
        tok = self.pending[eng]
        if tok is None:
            tok = Tok(eng)
            self.pending[eng] = tok
        if signal:
            self._eng_tok(eng, tok)
            ins.then_inc(tok.sem, 1)
            self.pending[eng] = None
            self.last_tok[eng] = tok
        for r in reads:
            r.r.append(tok)
        for w in writes:
            w.w = tok
            w.r = []
        return tok

    def dma(self, q, out, in_, reads=(), writes=()):
        self.n_all += 1
        if self._skipping():
            return None
        self._deps(q, reads, writes, is_dma=True)
        if q == "pool":
            i = 16 + self.dma_ip % 8
            self.dma_ip += 1
        else:
            i = self.dma_i % 16
            self.dma_i += 1
        sem = self.dma_sems[i]
        if self.dma_cnt[i] > 0:
            prev = Tok("dma")
            prev.sem = sem
            prev.val = self.dma_cnt[i]
            self._wait(q, prev)
        self.eobj[q].dma_start(out=out, in_=in_).then_inc(sem, 16)
        self.dma_cnt[i] += 16
        tok = Tok("dma")
        tok.sem = sem
        tok.val = self.dma_cnt[i]
        self.dma_toks.append(tok)
        for r in reads:
            r.r.append(tok)
        for w in writes:
            w.w = tok
            w.r = []
        return tok

    def barrier(self):
        toks = [t for t in self.last_tok.values() if t is not None]
        for i, s in enumerate(self.dma_sems):
            if self.dma_cnt[i] > 0:
                t = Tok("dma")
                t.sem = s
                t.val = self.dma_cnt[i]
                toks.append(t)
        for e in self.eobj:
            assert self.pending[e] is None
            for t in toks:
                if t.eng == e:
                    continue
                self._wait(e, t)

    def final_wait(self):
        for i, s in enumerate(self.dma_sems):
            if self.dma_cnt[i] > 0:
                t = Tok("dma")
                t.sem = s
                t.val = self.dma_cnt[i]
                self._wait("sp", t)


class Buf:
    def __init__(self, t, name, nslots=1):
        self.t = t
        self.res = [Res("%s.%d" % (name, i)) for i in range(nslots)]

    @property
    def r(self):
        return self.res[0]


def build(nc, dbg=None, limit=None):
    P = None
    with contextlib.ExitStack() as es:
        P = Prog(nc, es)
        P.limit = limit

        def dram_in(name, shape, dt=F32):
            return nc.dram_tensor(name, list(shape), dt, kind="ExternalInput").ap()

        def dram_scr(name, shape, dt, kind="Internal"):
            return nc.dram_tensor(name, list(shape), dt, kind=kind).ap()

        x_d = dram_in("x", [S, D])
        p_d = dram_in("p", [DEPTH, S, 256])
        w_in_d = dram_in("w_in", [DEPTH, D, INC])
        w_out_d = dram_in("w_out", [DEPTH, D, D])
        wg_d = dram_in("expert_w_gate", [DEPTH, NE, D, 256])
        wu_d = dram_in("expert_w_up", [DEPTH, NE, D, 256])
        wd_d = dram_in("expert_w_down", [DEPTH, NE, 256, D])
        pgw_d = dram_in("ple_gate_w", [DEPTH, D, D])
        pgb_d = dram_in("ple_gate_b", [DEPTH, D])
        ppj_d = dram_in("ple_proj", [DEPTH, 256, D])
        prm_d = dram_in("prm", [DEPTH, 128, PC_N])
        wr_d = dram_in("wr", [DEPTH, 128, 8, 20])
        rb_d = dram_in("rbias", [DEPTH, 128, 20])
        lam_d = dram_in("lamv", [DEPTH, 128, 4, 32])
        pw_d = dram_in("poolw", [DEPTH, 128, 2, 128])
        gfin_d = dram_in("gfin", [128, D])
        cidf_d = dram_in("c_identf", [128, 128])
        crp_d = dram_in("c_rperm", [128, 128])
        ctri_d = dram_in("c_tri", [128, 128])
        cb64_d = dram_in("c_blk64", [128, 128])
        cones_d = dram_in("c_ones256", [128, 128])
        csel_d = dram_in("c_sel", [16, NE, 128])
        ccos_d = dram_in("c_cos", [128, S])
        csin_d = dram_in("c_sin", [128, S])
        ccorr_d = dram_in("c_corr", [128, 2, 16])
        out_d = nc.dram_tensor("out", [S, D], F32, kind="ExternalOutput").ap()

        dkind = "ExternalOutput" if dbg else "Internal"
        h_d = dram_scr("h_scr", [S, D], F32, dkind)
        glu_d = dram_scr("glu_scr", [256, S], BF16, dkind)
        pin_d = dram_scr("pin_scr", [256, S], F32, dkind)
        q_d = dram_scr("q_scr", [256, S], BF16, dkind)
        k_d = dram_scr("k_scr", [256, S], BF16, dkind)
        v_d = dram_scr("v_scr", [S, 512], BF16, dkind)
        gb_d = dram_scr("gb_scr", [256, S], F32, dkind)
        gcv_d = dram_scr("gcv_scr", [256, S], F32, dkind)
        mix_d = dram_scr("mix_scr", [D, S], BF16, dkind)
        xT_d = dram_scr("xT_scr", [D, S], BF16, dkind)
        cmb_d = dram_scr("cmb_scr", [16, S], F32, dkind)

        def tiles(name):
            return [Res("%s%d" % (name, i)) for i in range(NT)]
        R_h = tiles("h")
        R_glu, R_pin, R_q, R_k, R_v = tiles("glu"), tiles("pin"), tiles("q"), tiles("k"), tiles("v")
        R_gb, R_gcv, R_mixA, R_mixB, R_xT, R_cmb = (tiles("gb"), tiles("gcv"), tiles("mixA"),
                                                    tiles("mixB"), tiles("xT"), tiles("cmb"))
        R_out = tiles("out")

        def fm(ap, i, lo=0, hi=TS):
            return ap.rearrange("(c p) t -> p c t", p=128)[:, :, i * TS + lo:i * TS + hi]

        def tm(ap, i):
            return ap[i * TS:(i + 1) * TS, :].rearrange("(j p) f -> p j f", p=128)

        uid = [0]
        def sbuf(stack, name, shape, dt, nslots=1):
            uid[0] += 1
            t = stack.enter_context(nc.sbuf_tensor("%s_u%d" % (name, uid[0]), list(shape), dt))
            return Buf(t, name, nslots)

        def psum(stack, name, shape, dt=F32):
            t = stack.enter_context(nc.psum_tensor(name, list(shape), dt))
            b = Buf(t, name, 1)
            b.res[0].excl = True
            return b

        identf = sbuf(es, "identf", [128, 128], F32)
        identb = sbuf(es, "identb", [128, 128], BF16)
        rperm = sbuf(es, "rperm", [128, 128], F32)
        trib = sbuf(es, "trib", [128, 128], BF16)
        blk64 = sbuf(es, "blk64", [128, 128], F32)
        ones256 = sbuf(es, "ones256", [128, 128], F32)
        sel = sbuf(es, "sel", [16, NE, 128], F32)
        prm = sbuf(es, "prm_sb", [128, PC_N], F32)
        corr = sbuf(es, "corr", [128, 2, 16], F32)
        gfin = sbuf(es, "gfin_sb", [128, D], F32)
        neglam = sbuf(es, "neglam", [128, 1], F32)
        pbs = sbuf(es, "pbs", [128, 2], F32)
        onesrow = sbuf(es, "onesrow", [1, 128], BF16)
        epsc = sbuf(es, "epsc", [128, 1], F32)

        pf = []
        pb = []
        pf_i = [0]
        pb_i = [0]

        def set_psum(stack, nf, nb):
            uid[0] += 1
            pf[:] = [psum(stack, "pf%d_%d" % (i, uid[0]), [128, 512], F32) for i in range(nf)]
            pb[:] = [psum(stack, "pb%d_%d" % (i, uid[0]), [128, 1024], BF16) for i in range(nb)]

        def next_pf():
            b = pf[pf_i[0] % len(pf)]
            pf_i[0] += 1
            return b

        def next_pb():
            b = pb[pb_i[0] % len(pb)]
            pb_i[0] += 1
            return b

        P.dma("sp", identf.t[:], cidf_d, writes=[identf.r])
        P.dma("sp", rperm.t[:], crp_d, writes=[rperm.r])
        P.dma("sp", blk64.t[:], cb64_d, writes=[blk64.r])
        P.dma("sp", ones256.t[:], cones_d, writes=[ones256.r])
        P.dma("sp", sel.t[:], csel_d, writes=[sel.r])
        P.dma("sp", corr.t[:], ccorr_d, writes=[corr.r])
        P.dma("sp", gfin.t[:], gfin_d, writes=[gfin.r])
        P.dma("pool", identb.t[:], cidf_d, writes=[identb.r])
        P.dma("pool", trib.t[:], ctri_d, writes=[trib.r])
        P.op("dve", lambda e: e.memset(onesrow.t[:], 1.0), writes=[onesrow.r])
        P.op("dve", lambda e: e.memset(epsc.t[:], EPS), writes=[epsc.r])

        def norm_stats(st, ht, slot, ss, rstd, sqj):
            P.op("dve", lambda e: e.memset(ss.t[:], 0.0), writes=[ss.r])
            for j in range(4):
                P.op("act", lambda e, j=j: e.activation(out=sqj.t[:], in_=ht.t[:, j, :], func=AF.Square,
                                                        accum_out=ss.t[:, j:j + 1]),
                     reads=[ht.res[slot], ss.r], writes=[sqj.r, ss.r])
            P.op("dve", lambda e: e.tensor_scalar(out=rstd.t[:], in0=ss.t[:], scalar1=1.0 / D, scalar2=EPS,
                                                  op0=ALU.mult, op1=ALU.add), reads=[ss.r], writes=[rstd.r])
            P.op("act", lambda e: e.activation(out=rstd.t[:], in_=rstd.t[:], func=AF.Sqrt),
                 reads=[rstd.r], writes=[rstd.r])
            P.op("dve", lambda e: e.reciprocal(out=rstd.t[:], in_=rstd.t[:]), reads=[rstd.r], writes=[rstd.r])

        def norm_transpose(ht, slot, rstd, xn, xT, gcol):
            for j in range(4):
                P.op("dve", lambda e, j=j: e.tensor_scalar(out=xn.t[:, j, :], in0=ht.t[:, j, :],
                                                           scalar1=rstd.t[:, j:j + 1], scalar2=None, op0=ALU.mult),
                     reads=[ht.res[slot], rstd.r], writes=[xn.res[j]])
            for c2 in range(4):
                bank = next_pb()
                for cc in range(2):
                    c = c2 * 2 + cc
                    for j in range(4):
                        last = (cc == 1 and j == 3)
                        P.op("pe", lambda e, c=c, cc=cc, j=j: e.transpose(
                            bank.t[:, cc * 512 + j * 128: cc * 512 + (j + 1) * 128],
                            xn.t[:, j, c * 128:(c + 1) * 128], identb.t[:]),
                            reads=[xn.res[j], identb.r], writes=[bank.r], signal=last)
                for cc in range(2):
                    c = c2 * 2 + cc
                    if cc == 0:
                        P.op("act", lambda e, c=c, cc=cc: e.activation(
                            out=xT.t[:, c, :], in_=bank.t[:, cc * 512:(cc + 1) * 512], func=AF.Identity,
                            scale=prm.t[:, gcol + c:gcol + c + 1]),
                            reads=[bank.r, prm.r], writes=[xT.res[c]])
                    else:
                        P.op("dve", lambda e, c=c, cc=cc: e.tensor_scalar(
                            out=xT.t[:, c, :], in0=bank.t[:, cc * 512:(cc + 1) * 512],
                            scalar1=prm.t[:, gcol + c:gcol + c + 1], scalar2=None, op0=ALU.mult),
                            reads=[bank.r, prm.r], writes=[xT.res[c]])

        def load_h(i, ht, slot, src):
            P.dma("sp", ht.t[:], tm(src, i), reads=[R_h[i]], writes=[ht.res[slot]])

        def mm_group(bank, cols, lhs_list, rhs_list, reads, tile_position=None):
            n = len(lhs_list)
            for k in range(n):
                P.op("pe", lambda e, k=k: e.matmul(bank.t[:, cols[0]:cols[1]], lhs_list[k], rhs_list[k],
                                                   start=(k == 0), stop=(k == n - 1)),
                     reads=reads[k], writes=[bank.r], signal=(k == n - 1))

        def load_w_in(l_):
            stw = contextlib.ExitStack()
            uid[0] += 1
            t_ = stw.enter_context(nc.sbuf_tensor("w_in_sb_u%d" % uid[0], [128, 8, INC], BF16, side="right"))
            b_ = Buf(t_, "w_in_sb", 1)
            for c in range(8):
                P.dma("pool", b_.t[:, c, :], w_in_d[l_, c * 128:(c + 1) * 128, :], writes=[b_.r])
            return stw, b_

        st_win, w_in_next = load_w_in(0)
        for l in range(DEPTH):
            lam_init = 0.8 - 0.6 * math.exp(-0.3 * l)
            h_src = x_d if l == 0 else h_d

            P.barrier()
            P.dma("sp", prm.t[:], prm_d[l], writes=[prm.r])
            with contextlib.ExitStack() as st:
                lamv = sbuf(st, "lamv", [128, 4, 32], F32)
                lt = sbuf(st, "lt", [128, 2, 32], F32)
                ls = sbuf(st, "ls", [128, 2], F32)
                P.dma("sp", lamv.t[:], lam_d[l], writes=[lamv.r])
                P.op("dve", lambda e: e.tensor_tensor(out=lt.t[:, 0, :], in0=lamv.t[:, 0, :], in1=lamv.t[:, 1, :],
                                                      op=ALU.mult), reads=[lamv.r], writes=[lt.r])
                P.op("dve", lambda e: e.tensor_tensor(out=lt.t[:, 1, :], in0=lamv.t[:, 2, :], in1=lamv.t[:, 3, :],
                                                      op=ALU.mult), reads=[lamv.r, lt.r], writes=[lt.r])
                P.op("dve", lambda e: e.reduce_sum(out=ls.t[:], in_=lt.t[:], axis=AX.X), reads=[lt.r], writes=[ls.r])
                P.op("act", lambda e: e.activation(out=ls.t[:], in_=ls.t[:], func=AF.Exp), reads=[ls.r], writes=[ls.r])
                P.op("dve", lambda e: e.scalar_tensor_tensor(out=neglam.t[:], in0=ls.t[:, 1:2], scalar=-lam_init,
                                                             in1=ls.t[:, 0:1], op0=ALU.add, op1=ALU.subtract),
                     reads=[ls.r], writes=[neglam.r])
                P.op("dve", lambda e: e.tensor_tensor(out=pbs.t[:], in0=prm.t[:, PC_PB:PC_PB + 2],
                                                      in1=prm.t[:, PC_PS:PC_PS + 2], op=ALU.mult),
                     reads=[prm.r], writes=[pbs.r])
                P.barrier()

            st_dg = contextlib.ExitStack()
            dg = sbuf(st_dg, "s2_dg", [128, 2, CK, 128], BF16)
            pw_sb = sbuf(st_dg, "s2_pw", [128, 2, 128], BF16)
            with contextlib.ExitStack() as st:
                set_psum(st, 6, 2)
                w_in_sb = w_in_next
                ht = sbuf(st, "s1_ht", [128, 4, D], F32, 2)
                hts = [ht, sbuf(st, "s1_ht2", [128, 4, D], F32, 2)]
                ss2 = [sbuf(st, "s1_ss%d" % k, [128, 4], F32) for k in range(2)]
                rstd2 = [sbuf(st, "s1_rstd%d" % k, [128, 4], F32) for k in range(2)]
                sqj = sbuf(st, "s1_sqj", [128, D], BF16)
                xn2 = [sbuf(st, "s1_xn%d" % k, [128, 4, D], BF16, 4) for k in range(2)]
                nT2 = [sbuf(st, "s1_nT%d" % k, [128, 8, TS], BF16, 8) for k in range(2)]
                sig = sbuf(st, "s1_sig", [128, TS], F32)
                gcs = sbuf(st, "s1_gc", [128, TS], F32)
                glu_st = sbuf(st, "s1_glu", [128, 2, TS], BF16)
                pin_st = sbuf(st, "s1_pin", [128, 2, TS], F32)
                qk_st = sbuf(st, "s1_qk", [128, 4, TS], F32, 4)
                qkr_st = sbuf(st, "s1_qkr", [128, 4, TS], BF16, 4)
                gb_st = sbuf(st, "s1_gb", [128, 2, TS], F32)
                gcv_st = sbuf(st, "s1_gcv", [128, 2, TS], F32)
                v_st = sbuf(st, "s1_v", [128, 4, 512], BF16)
                cos_t = sbuf(st, "s1_cos", [128, TS], F32)
                sin_t = sbuf(st, "s1_sin", [128, TS], F32)
                t1 = sbuf(st, "s1_t1", [128, TS], F32)
                t2 = sbuf(st, "s1_t2", [128, TS], F32)

                P.op("dve", lambda e: e.memset(v_st.t[:], 1.0), writes=[v_st.r])

                def s1_prologue(i_):
                    norm_stats(st, hts[i_ % 2], 0, ss2[i_ % 2], rstd2[i_ % 2], sqj)
                    norm_transpose(hts[i_ % 2], 0, rstd2[i_ % 2], xn2[i_ % 2], nT2[i_ % 2], PC_MIXG)

                P.dma("sp", hts[0].t[:], tm(h_src, 0), reads=[R_h[0]], writes=[hts[0].r])
                s1_prologue(0)
                P.dma("pool", pw_sb.t[:], pw_d[l], writes=[pw_sb.r])
                for cc in range(2):
                    for j in range(CK):
                        col = PC_CW + cc * CK + j
                        P.op("dve", lambda e, cc=cc, j=j, col=col: e.tensor_scalar(
                            out=dg.t[:, cc, j, :], in0=identf.t[:], scalar1=prm.t[:, col:col + 1], scalar2=None,
                            op0=ALU.mult), reads=[identf.r, prm.r], writes=[dg.r])
                for i in range(NT):
                    if i + 1 < NT:
                        P.dma("sp", hts[(i + 1) % 2].t[:], tm(h_src, i + 1), reads=[R_h[i + 1]],
                              writes=[hts[(i + 1) % 2].r])
                    P.dma("sp", cos_t.t[:], ccos_d[:, i * TS:(i + 1) * TS], writes=[cos_t.r])
                    P.dma("sp", sin_t.t[:], csin_d[:, i * TS:(i + 1) * TS], writes=[sin_t.r])
                    nT = nT2[i % 2]

                    def proj(col0):
                        bank = next_pf()
                        mm_group(bank, (0, TS), [w_in_sb.t[:, c, col0:col0 + 128] for c in range(8)],
                                 [nT.t[:, c, :] for c in range(8)],
                                 [[w_in_sb.r, nT.res[c]] for c in range(8)])
                        return bank

                    for cc in range(2):
                        bg_ = proj(256 + cc * 128)
                        P.op("act", lambda e: e.activation(out=sig.t[:], in_=bg_.t[:], func=AF.Sigmoid),
                             reads=[bg_.r], writes=[sig.r])
                        bv_ = proj(0 + cc * 128)
                        P.op("dve", lambda e: e.tensor_tensor(out=glu_st.t[:, cc, :], in0=bv_.t[:], in1=sig.t[:],
                                                              op=ALU.mult),
                             reads=[bv_.r, sig.r], writes=[glu_st.r])
                    for cc in range(2):
                        bp_ = proj(512 + cc * 128)
                        P.op("act", lambda e: e.copy(out=pin_st.t[:, cc, :], in_=bp_.t[:]),
                             reads=[bp_.r], writes=[pin_st.r])
                    if i + 1 < NT:
                        s1_prologue(i + 1)
                    for m in range(4):
                        bq_ = proj(768 + m * 128)
                        if m % 2 == 0:
                            P.op("act", lambda e: e.copy(out=qk_st.t[:, m, :], in_=bq_.t[:]),
                                 reads=[bq_.r], writes=[qk_st.res[m]])
                        else:
                            P.op("dve", lambda e: e.tensor_copy(out=qk_st.t[:, m, :], in_=bq_.t[:]),
                                 reads=[bq_.r], writes=[qk_st.res[m]])
                    for cc in range(2):
                        bb_ = proj(1536 + cc * 128)
                        P.op("act", lambda e: e.copy(out=gb_st.t[:, cc, :], in_=bb_.t[:]),
                             reads=[bb_.r], writes=[gb_st.r])
                    for cc in range(2):
                        bc_ = proj(1792 + cc * 128)
                        P.op("act", lambda e: e.copy(out=gcs.t[:], in_=bc_.t[:]), reads=[bc_.r], writes=[gcs.r])
                        bs_ = proj(2048 + cc * 128)
                        P.op("dve", lambda e: e.tensor_tensor(out=gcv_st.t[:, cc, :], in0=bs_.t[:], in1=gcs.t[:],
                                                              op=ALU.mult),
                             reads=[bs_.r, gcs.r], writes=[gcv_st.r])
                    for j in range(4):
                        bank = next_pf()
                        mm_group(bank, (0, 256), [nT.t[:, c, j * 128:(j + 1) * 128] for c in range(8)],
                                 [w_in_sb.t[:, c, 1280:1536] for c in range(8)],
                                 [[w_in_sb.r, nT.res[c]] for c in range(8)])
                        for hh in range(4):
                            off = hh * 128 + (0 if hh % 2 == 0 else 64)
                            eng = "act" if hh % 2 == 0 else "dve"
                            if eng == "act":
                                P.op("act", lambda e: e.copy(out=v_st.t[:, j, off:off + 64],
                                                             in_=bank.t[:, hh * 64:(hh + 1) * 64]),
                                     reads=[bank.r], writes=[v_st.r])
                            else:
                                P.op("dve", lambda e: e.tensor_copy(out=v_st.t[:, j, off:off + 64],
                                                                    in_=bank.t[:, hh * 64:(hh + 1) * 64]),
                                     reads=[bank.r], writes=[v_st.r])
                    for m in range(4):
                        bank = next_pf()
                        P.op("pe", lambda e: e.matmul(bank.t[:], rperm.t[:], qk_st.t[:, m, :], start=True, stop=True),
                             reads=[rperm.r, qk_st.res[m]], writes=[bank.r])
                        P.op("dve", lambda e: e.tensor_tensor(out=t1.t[:], in0=qk_st.t[:, m, :], in1=cos_t.t[:],
                                                              op=ALU.mult),
                             reads=[qk_st.res[m], cos_t.r], writes=[t1.r])
                        P.op("dve", lambda e: e.tensor_tensor(out=t2.t[:], in0=bank.t[:], in1=sin_t.t[:],
                                                              op=ALU.mult),
                             reads=[bank.r, sin_t.r], writes=[t2.r])
                        P.op("dve", lambda e: e.tensor_tensor(out=qkr_st.t[:, m, :], in0=t1.t[:], in1=t2.t[:],
                                                              op=ALU.add),
                             reads=[t1.r, t2.r], writes=[qkr_st.res[m]])
                    P.dma("sp", fm(glu_d, i), glu_st.t[:], reads=[glu_st.r], writes=[R_glu[i]])
                    P.dma("sp", fm(pin_d, i), pin_st.t[:], reads=[pin_st.r], writes=[R_pin[i]])
                    P.dma("sp", fm(q_d, i), qkr_st.t[:, 0:2, :], reads=[qkr_st.res[0], qkr_st.res[1]], writes=[R_q[i]])
                    P.dma("sp", fm(k_d, i), qkr_st.t[:, 2:4, :], reads=[qkr_st.res[2], qkr_st.res[3]], writes=[R_k[i]])
                    P.dma("sp", tm(v_d, i), v_st.t[:], reads=[v_st.r], writes=[R_v[i]])
                    P.dma("sp", fm(gb_d, i), gb_st.t[:], reads=[gb_st.r], writes=[R_gb[i]])
                    P.dma("sp", fm(gcv_d, i), gcv_st.t[:], reads=[gcv_st.r], writes=[R_gcv[i]])
                P.barrier()
            st_win.close()
            if dbg == "s1":
                st_dg.close()
                break

            st_wout = contextlib.ExitStack()
            uid[0] += 1
            w_out_sb = Buf(st_wout.enter_context(nc.sbuf_tensor("w_out_sb_u%d" % uid[0], [128, 8, D], BF16,
                                                                side="right")), "w_out_sb", 1)
            for c in range(8):
                P.dma("pool", w_out_sb.t[:, c, :], w_out_d[l, c * 128:(c + 1) * 128, :], writes=[w_out_sb.r])
            st_kv = contextlib.ExitStack()
            kT = sbuf(st_kv, "at_kT", [128, 2, S], BF16)
            Vs = sbuf(st_kv, "at_V", [128, 32, 512], BF16)
            P.dma("sp", kT.t[:], k_d.rearrange("(c p) t -> p c t", p=128), reads=R_k, writes=[kT.r])
            for i8 in range(NT):
                P.dma("sp", Vs.t[:, i8 * 4:(i8 + 1) * 4, :], tm(v_d, i8), reads=[R_v[i8]], writes=[Vs.r])
            with contextlib.ExitStack() as st:
                set_psum(st, 8, 0)
                glu_in = [sbuf(st, "s2_glu%d" % k, [128, 2, 30 + TS], BF16) for k in range(2)]
                pin_in = [sbuf(st, "s2_pin%d" % k, [128, 2, 16 + TS], F32) for k in range(2)]
                gcv_in = [sbuf(st, "s2_gcv%d" % k, [128, 2, 2 + TS], F32) for k in range(2)]
                gb_in = [sbuf(st, "s2_gb%d" % k, [128, 2, TS], F32) for k in range(2)]
                yc = sbuf(st, "s2_y", [128, 2, TS], F32, 2)
                ysq = sbuf(st, "s2_ysq", [128, 2, TS], F32, 2)
                m2 = sbuf(st, "s2_m2", [128, TS], F32)
                var = sbuf(st, "s2_var", [128, TS], F32)
                dd = sbuf(st, "s2_dd", [128, TS], F32)
                sA = sbuf(st, "s2_sA", [128, 16 + TS], F32)
                sB = sbuf(st, "s2_sB", [128, 16 + TS], F32)
                pooled = sbuf(st, "s2_pooled", [128, TS], BF16)
                acc3 = sbuf(st, "s2_acc3", [128, TS], F32)
                mixA = [sbuf(st, "s2_mixA%d" % k, [128, 6, TS], BF16) for k in range(2)]


                def s2_load(i):
                    k = i % 2
                    if i == 0:
                        P.op("dve", lambda e: e.memset(glu_in[k].t[:, :, 0:30], 0.0), writes=[glu_in[k].r])
                        P.op("dve", lambda e: e.memset(pin_in[k].t[:, :, 0:16], 0.0), writes=[pin_in[k].r])
                        P.op("dve", lambda e: e.memset(gcv_in[k].t[:, :, 0:2], 0.0), writes=[gcv_in[k].r])
                        P.dma("sp", glu_in[k].t[:, :, 30:30 + TS], fm(glu_d, 0), reads=[R_glu[0]], writes=[glu_in[k].r])
                        P.dma("sp", pin_in[k].t[:, :, 16:16 + TS], fm(pin_d, 0), reads=[R_pin[0]], writes=[pin_in[k].r])
                        P.dma("sp", gcv_in[k].t[:, :, 2:2 + TS], fm(gcv_d, 0), reads=[R_gcv[0]], writes=[gcv_in[k].r])
                    else:
                        P.dma("sp", glu_in[k].t[:], fm(glu_d, i, -30, TS), reads=[R_glu[i - 1], R_glu[i]],
                              writes=[glu_in[k].r])
                        P.dma("sp", pin_in[k].t[:], fm(pin_d, i, -16, TS), reads=[R_pin[i - 1], R_pin[i]],
                              writes=[pin_in[k].r])
                        P.dma("sp", gcv_in[k].t[:], fm(gcv_d, i, -2, TS), reads=[R_gcv[i - 1], R_gcv[i]],
                              writes=[gcv_in[k].r])
                    P.dma("sp", gb_in[k].t[:], fm(gb_d, i), reads=[R_gb[i]], writes=[gb_in[k].r])

                s2_load(0)
                for i in range(NT):
                    k = i % 2
                    if i + 1 < NT:
                        s2_load(i + 1)
                    mx = mixA[k]
                    for cc in range(2):
                        bank = next_pf()
                        mm_group(bank, (0, TS), [dg.t[:, cc, j, :] for j in range(CK)],
                                 [glu_in[k].t[:, cc, j:j + TS] for j in range(CK)],
                                 [[dg.r, glu_in[k].r]] * CK)
                        P.op("act", lambda e: e.activation(out=yc.t[:, cc, :], in_=bank.t[:], func=AF.Identity,
                                                           bias=prm.t[:, PC_CB + cc:PC_CB + cc + 1]),
                             reads=[bank.r, prm.r], writes=[yc.res[cc]])
                        P.op("act", lambda e: e.activation(out=ysq.t[:, cc, :], in_=bank.t[:], func=AF.Square,
                                                           bias=prm.t[:, PC_CB + cc:PC_CB + cc + 1]),
                             reads=[bank.r, prm.r], writes=[ysq.res[cc]])
                    bm = next_pf()
                    mm_group(bm, (0, TS), [ones256.t[:], ones256.t[:]], [yc.t[:, 0, :], yc.t[:, 1, :]],
                             [[ones256.r, yc.res[0]], [ones256.r, yc.res[1]]])
                    bq = next_pf()
                    mm_group(bq, (0, TS), [ones256.t[:], ones256.t[:]], [ysq.t[:, 0, :], ysq.t[:, 1, :]],
                             [[ones256.r, ysq.res[0]], [ones256.r, ysq.res[1]]])
                    for cc in range(2):
                        u = pin_in[k].t[:, cc, :]
                        W_ = 16 + TS
                        P.op("dve", lambda e: e.memset(sA.t[:, 0:1], 0.0), writes=[sA.r])
                        P.op("dve", lambda e: e.tensor_tensor(out=sA.t[:, 1:W_], in0=pin_in[k].t[:, cc, 1:W_],
                                                              in1=pin_in[k].t[:, cc, 0:W_ - 1], op=ALU.add),
                             reads=[pin_in[k].r], writes=[sA.r])
                        if cc == 0:
                            P.op("dve", lambda e: e.tensor_tensor(out=sB.t[64:128, 3:W_], in0=sA.t[64:128, 3:W_],
                                                                  in1=sA.t[64:128, 1:W_ - 2], op=ALU.add),
                                 reads=[sA.r], writes=[sB.r])
                            P.op("dve", lambda e: e.tensor_copy(out=sB.t[0:64, 3:W_], in_=sA.t[0:64, 3:W_]),
                                 reads=[sA.r, sB.r], writes=[sB.r])
                            fin = sB
                        else:
                            P.op("dve", lambda e: e.tensor_tensor(out=sB.t[:, 3:W_], in0=sA.t[:, 3:W_],
                                                                  in1=sA.t[:, 1:W_ - 2], op=ALU.add),
                                 reads=[sA.r], writes=[sB.r])
                            P.op("dve", lambda e: e.tensor_tensor(out=sA.t[:, 7:W_], in0=sB.t[:, 7:W_],
                                                                  in1=sB.t[:, 3:W_ - 4], op=ALU.add),
                                 reads=[sB.r, sA.r], writes=[sA.r])
                            P.op("dve", lambda e: e.tensor_tensor(out=sB.t[64:128, 15:W_], in0=sA.t[64:128, 15:W_],
                                                                  in1=sA.t[64:128, 7:W_ - 8], op=ALU.add),
                                 reads=[sA.r, sB.r], writes=[sB.r])
                            P.op("dve", lambda e: e.tensor_copy(out=sB.t[0:64, 15:W_], in_=sA.t[0:64, 15:W_]),
                                 reads=[sA.r, sB.r], writes=[sB.r])
                            fin = sB
                        if i == 0:
                            P.op("dve", lambda e: e.tensor_tensor(out=fin.t[:, 16:32], in0=fin.t[:, 16:32],
                                                                  in1=corr.t[:, cc, :], op=ALU.mult),
                                 reads=[fin.r, corr.r], writes=[fin.r])
                        P.op("dve", lambda e: e.scalar_tensor_tensor(
                            out=pooled.t[:], in0=fin.t[:, 16:16 + TS], scalar=prm.t[:, PC_IW + cc:PC_IW + cc + 1],
                            in1=pin_in[k].t[:, cc, 16:16 + TS], op0=ALU.mult, op1=ALU.subtract),
                            reads=[fin.r, prm.r, pin_in[k].r], writes=[pooled.r])
                        bank = next_pf()
                        P.op("pe", lambda e: e.matmul(bank.t[:], pw_sb.t[:, cc, :], pooled.t[:], start=True, stop=True),
                             reads=[pw_sb.r, pooled.r], writes=[bank.r])
                        P.op("act", lambda e: e.activation(out=mx.t[:, 2 + cc, :], in_=bank.t[:], func=AF.Identity,
                                                           scale=prm.t[:, PC_PS + cc:PC_PS + cc + 1],
                                                           bias=pbs.t[:, cc:cc + 1]),
                             reads=[bank.r, prm.r, pbs.r], writes=[mx.r])
                    for cc in range(2):
                        g_ = gcv_in[k]
                        P.op("dve", lambda e: e.tensor_scalar(out=acc3.t[:], in0=g_.t[:, cc, 0:TS],
                                                              scalar1=prm.t[:, PC_SW + cc * 3:PC_SW + cc * 3 + 1],
                                                              scalar2=None, op0=ALU.mult),
                             reads=[g_.r, prm.r], writes=[acc3.r])
                        for j in (1, 2):
                            P.op("dve", lambda e, j=j: e.scalar_tensor_tensor(
                                out=acc3.t[:], in0=g_.t[:, cc, j:j + TS],
                                scalar=prm.t[:, PC_SW + cc * 3 + j:PC_SW + cc * 3 + j + 1], in1=acc3.t[:],
                                op0=ALU.mult, op1=ALU.add), reads=[g_.r, prm.r, acc3.r], writes=[acc3.r])
                        P.op("dve", lambda e: e.tensor_tensor(out=mx.t[:, 4 + cc, :], in0=acc3.t[:],
                                                              in1=gb_in[k].t[:, cc, :], op=ALU.mult),
                             reads=[acc3.r, gb_in[k].r], writes=[mx.r])
                    P.op("act", lambda e: e.activation(out=m2.t[:], in_=bm.t[:], func=AF.Square),
                         reads=[bm.r], writes=[m2.r])
                    P.op("dve", lambda e: e.tensor_tensor(out=var.t[:], in0=bq.t[:], in1=m2.t[:], op=ALU.subtract),
                         reads=[bq.r, m2.r], writes=[var.r])
                    P.op("dve", lambda e: e.tensor_scalar(out=var.t[:], in0=var.t[:], scalar1=0.0, scalar2=EPS,
                                                          op0=ALU.max, op1=ALU.add), reads=[var.r], writes=[var.r])
                    P.op("act", lambda e: e.activation(out=var.t[:], in_=var.t[:], func=AF.Ln),
                         reads=[var.r], writes=[var.r])
                    P.op("act", lambda e: e.activation(out=var.t[:], in_=var.t[:], func=AF.Exp, scale=-0.5),
                         reads=[var.r], writes=[var.r])
                    for cc in range(2):
                        P.op("dve", lambda e: e.tensor_tensor(out=dd.t[:], in0=bm.t[:], in1=yc.t[:, cc, :],
                                                              op=ALU.subtract),
                             reads=[bm.r, yc.res[cc]], writes=[dd.r])
                        P.op("dve", lambda e: e.tensor_tensor(out=dd.t[:], in0=dd.t[:], in1=var.t[:], op=ALU.mult),
                             reads=[dd.r, var.r], writes=[dd.r])
                        P.op("dve", lambda e: e.tensor_scalar(out=dd.t[:], in0=dd.t[:],
                                                              scalar1=prm.t[:, PC_LNG + cc:PC_LNG + cc + 1],
                                                              scalar2=-1.0, op0=ALU.mult, op1=ALU.mult),
                             reads=[dd.r, prm.r], writes=[dd.r])
                        P.op("act", lambda e: e.activation(out=mx.t[:, cc, :], in_=dd.t[:], func=AF.Silu,
                                                           bias=prm.t[:, PC_LNB + cc:PC_LNB + cc + 1]),
                             reads=[dd.r, prm.r], writes=[mx.r])
                    mv = mix_d.rearrange("(c p) t -> p c t", p=128)
                    P.dma("sp", mv[:, 0:4, i * TS:(i + 1) * TS], mx.t[:, 0:4, :], reads=[mx.r], writes=[R_mixA[i]])
                    P.dma("sp", mv[:, 6:8, i * TS:(i + 1) * TS], mx.t[:, 4:6, :], reads=[mx.r], writes=[R_mixA[i]])
                P.barrier()
            if dbg == "s2a":
                st_kv.close()
                st_wout.close()
                st_dg.close()
                break

            with contextlib.ExitStack() as st:
                qTs = [sbuf(st, "at_q%d" % k, [128, 2, TS], BF16) for k in range(2)]
                pts = [sbuf(st, "at_pt%d" % k, [128, TS], BF16) for k in range(4)]
                rz = [sbuf(st, "at_rz%d" % k, [128, TS], F32) for k in range(2)]
                o12 = [sbuf(st, "at_o%d" % k, [128, TS], F32) for k in range(2)]
                od = sbuf(st, "at_od", [128, TS], F32)
                osq = sbuf(st, "at_osq", [128, TS], F32)
                rs = sbuf(st, "at_rs", [128, TS], F32)
                mixB = [sbuf(st, "at_mix%d" % k, [128, 2, TS], BF16) for k in range(2)]
                set_psum(st, 8, 0)
                accb = pf[0:4]
                scb = pf[4:8]
                pts8 = pts + [sbuf(st, "at_ptx%d" % k, [128, TS], BF16) for k in range(4)]
                P.dma("sp", qTs[0].t[:], fm(q_d, 0), reads=[R_q[0]], writes=[qTs[0].r])
                mv = mix_d.rearrange("(c p) t -> p c t", p=128)

                groups = []
                for i in range(NT):
                    nkt = 4 * i + 4
                    for hp in range(2):
                        for kt in range(nkt):
                            groups.append((i, hp, kt, nkt))

                def front(g):
                    i, hp, kt, nkt = groups[g]
                    if hp == 0 and kt == 0 and i + 1 < NT:
                        P.dma("sp", qTs[(i + 1) % 2].t[:], fm(q_d, i + 1), reads=[R_q[i + 1]],
                              writes=[qTs[(i + 1) % 2].r])
                    qT = qTs[i % 2]
                    jd = kt - 4 * i
                    qs = 128 * jd if jd > 0 else 0
                    n = TS - qs
                    for s_ in range(4):
                        po = s_ * 32
                        sc = scb[s_]
                        P.op("pe", lambda e: e.matmul(sc.t[:, 0:n], kT.t[po:po + 32, hp, kt * 128:(kt + 1) * 128],
                                                      qT.t[po:po + 32, hp, qs:TS], start=True, stop=True,
                                                      tile_position=(po, 0)),
                             reads=[kT.r, qT.r], writes=[sc.r])
                    for s_ in range(4):
                        sc = scb[s_]
                        pt = pts8[(g % 2) * 4 + s_]
                        P.op("act", lambda e: e.activation(out=pt.t[:, 0:n], in_=sc.t[:, 0:n], func=AF.Exp, scale=SCALE),
                             reads=[sc.r], writes=[pt.r])
                        if jd >= 0:
                            P.op("dve", lambda e: e.tensor_tensor(out=pt.t[:, 0:128], in0=pt.t[:, 0:128], in1=trib.t[:],
                                                                  op=ALU.mult),
                                 reads=[pt.r, trib.r], writes=[pt.r])

                def back(g):
                    i, hp, kt, nkt = groups[g]
                    jd = kt - 4 * i
                    qs = 128 * jd if jd > 0 else 0
                    n = TS - qs
                    for s_ in range(4):
                        h = 2 * hp + s_ // 2
                        pt = pts8[(g % 2) * 4 + s_]
                        acc = accb[s_]
                        P.op("pe", lambda e: e.matmul(acc.t[:, qs:TS], Vs.t[:, kt, h * 128:(h + 1) * 128], pt.t[:, 0:n],
                                                      start=(kt == 0), stop=(kt == nkt - 1)),
                             reads=[Vs.r, pt.r], writes=[acc.r])
                    if kt == nkt - 1:
                        finalize(i, 2 * hp)
                        finalize(i, 2 * hp + 1)

                def finalize(i, h):
                    ch = h // 2
                    mb = mixB[i % 2]
                    lo, hi = (0, 64) if h % 2 == 0 else (64, 128)
                    zlo, zhi = (64, 128) if h % 2 == 0 else (0, 64)
                    accs = [accb[(h % 2) * 2], accb[(h % 2) * 2 + 1]]
                    for comp in range(2):
                        P.op("act", lambda e: e.activation(out=rz[comp].t[zlo:zhi, :], in_=accs[comp].t[zlo:zhi, :],
                                                           func=AF.Ln),
                             reads=[accs[comp].r], writes=[rz[comp].r])
                        P.op("act", lambda e: e.activation(out=rz[comp].t[zlo:zhi, :], in_=rz[comp].t[zlo:zhi, :],
                                                           func=AF.Exp, scale=-1.0),
                             reads=[rz[comp].r], writes=[rz[comp].r])
                        P.op("dve", lambda e: e.tensor_tensor(out=o12[comp].t[lo:hi, :], in0=accs[comp].t[lo:hi, :],
                                                              in1=rz[comp].t[zlo:zhi, :], op=ALU.mult),
                             reads=[accs[comp].r, rz[comp].r], writes=[o12[comp].r])
                    P.op("dve", lambda e: e.scalar_tensor_tensor(out=od.t[lo:hi, :], in0=o12[1].t[lo:hi, :],
                                                                 scalar=neglam.t[lo:hi, 0:1], in1=o12[0].t[lo:hi, :],
                                                                 op0=ALU.mult, op1=ALU.add),
                         reads=[o12[0].r, o12[1].r, neglam.r], writes=[od.r])
                    if h % 2 == 1:
                        P.op("act", lambda e: e.activation(out=osq.t[:], in_=od.t[:], func=AF.Square),
                             reads=[od.r], writes=[osq.r])
                        bms = scb[3]
                        P.op("pe", lambda e: e.matmul(bms.t[:], blk64.t[:], osq.t[:], start=True, stop=True),
                             reads=[blk64.r, osq.r], writes=[bms.r])
                        P.op("act", lambda e: e.activation(out=rs.t[:], in_=bms.t[:], func=AF.Ln, bias=epsc.t[:, 0:1]),
                             reads=[bms.r, epsc.r], writes=[rs.r])
                        P.op("act", lambda e: e.activation(out=rs.t[:], in_=rs.t[:], func=AF.Exp, scale=-0.5),
                             reads=[rs.r], writes=[rs.r])
                        P.op("dve", lambda e: e.tensor_tensor(out=rs.t[:], in0=rs.t[:], in1=od.t[:], op=ALU.mult),
                             reads=[rs.r, od.r], writes=[rs.r])
                        P.op("dve", lambda e: e.tensor_scalar(out=mb.t[:, ch, :], in0=rs.t[:],
                                                              scalar1=prm.t[:, PC_SUB:PC_SUB + 1],
                                                              scalar2=1.0 - lam_init, op0=ALU.mult, op1=ALU.mult),
                             reads=[rs.r, prm.r], writes=[mb.r])
                    if h == 3:
                        P.dma("sp", mv[:, 4:6, i * TS:(i + 1) * TS], mb.t[:], reads=[mb.r], writes=[R_mixB[i]])

                LA = 1
                for g in range(len(groups) + LA):
                    if g < len(groups):
                        front(g)
                    if g >= LA:
                        back(g - LA)
                P.barrier()
            st_kv.close()
            st_dg.close()
            if dbg == "s2c":
                st_wout.close()
                break

            st_ple = contextlib.ExitStack()
            wpg = sbuf(st_ple, "s5_wpg", [128, 8, D], BF16)
            wpp = sbuf(st_ple, "s5_wpp", [128, 2, D], BF16)
            stm = contextlib.ExitStack()
            wgs = [sbuf(stm, "wgs0", [128, 4, 8, 256], BF16)]
            wus = [sbuf(stm, "wus0", [128, 4, 8, 256], BF16)]
            wds = [sbuf(stm, "wds0", [128, 4, 2, D], BF16)]
            def load_experts(pz, slot):
                for el in range(4):
                    eidx = pz * 4 + el
                    P.dma("pool", wgs[slot].t[:, el, :, :], wg_d[l, eidx].rearrange("(c p) n -> p c n", p=128),
                          writes=[wgs[slot].r])
                    P.dma("pool", wus[slot].t[:, el, :, :], wu_d[l, eidx].rearrange("(c p) n -> p c n", p=128),
                          writes=[wus[slot].r])
                    P.dma("pool", wds[slot].t[:, el, :, :], wd_d[l, eidx].rearrange("(c p) n -> p c n", p=128),
                          writes=[wds[slot].r])

            with contextlib.ExitStack() as st:
                set_psum(st, 6, 2)
                wrg = sbuf(st, "s3_wrg", [128, 8, 20], F32)
                rbias = sbuf(st, "s3_rb", [128, 20], F32)
                hts = [sbuf(st, "s3_ht%d" % k, [128, 4, D], F32) for k in range(2)]
                mxs = [sbuf(st, "s3_mx%d" % k, [128, 8, TS], BF16) for k in range(2)]
                ss2 = [sbuf(st, "s3_ss%d" % k, [128, 4], F32) for k in range(2)]
                rstd2 = [sbuf(st, "s3_rstd%d" % k, [128, 4], F32) for k in range(2)]
                sqj = sbuf(st, "s3_sqj", [128, D], BF16)
                xn2 = [sbuf(st, "s3_xn%d" % k, [128, 4, D], BF16, 4) for k in range(2)]
                xT2 = [sbuf(st, "s3_xT%d" % k, [128, 8, TS], BF16, 8) for k in range(2)]
                hTf = sbuf(st, "s3_hTf", [128, 8, 128], F32, 2)
                lg = sbuf(st, "s3_lg", [128, 20], F32)
                lg4 = sbuf(st, "s3_lg4", [128, 4, 20], F32)
                r4 = sbuf(st, "s3_r4", [128, 8, 4], F32)
                mg4 = sbuf(st, "s3_mg4", [128, 4, 4], F32)
                t4 = sbuf(st, "s3_t4", [128, 4, 4], F32)
                es4 = sbuf(st, "s3_es4", [128, 4, 4], F32)
                eq4 = sbuf(st, "s3_eq4", [128, 4, 4], F32)
                pr4 = sbuf(st, "s3_pr4", [128, 4, 4, 4], F32)
                sm = sbuf(st, "s3_sm", [128, 16], F32)
                mg = sbuf(st, "s3_mg", [128, 4], F32)
                ge = sbuf(st, "s3_ge", [128, 4], F32)
                esel = sbuf(st, "s3_esel", [128, 4], F32)
                eq = sbuf(st, "s3_eq", [128, 4], F32)
                em2 = sbuf(st, "s3_em2", [128, 4], F32)
                ee = sbuf(st, "s3_ee", [128, 4], F32)
                wsel = sbuf(st, "s3_wsel", [128, 4], F32)
                comb = sbuf(st, "s3_comb", [128, 4, 16], F32)
                cmbT = sbuf(st, "s3_cmbT", [16, TS], F32)

                load_experts(0, 0)
                P.dma("sp", wrg.t[:], wr_d[l], writes=[wrg.r])
                P.dma("sp", rbias.t[:], rb_d[l], writes=[rbias.r])
                for c in range(8):
                    P.op("dve", lambda e, c=c: e.tensor_scalar(out=wrg.t[:, c, :], in0=wrg.t[:, c, :],
                                                               scalar1=prm.t[:, PC_FFNG + c:PC_FFNG + c + 1],
                                                               scalar2=None, op0=ALU.mult),
                         reads=[wrg.r, prm.r], writes=[wrg.r])

                def s3_load(i):
                    k = i % 2
                    P.dma("sp", hts[k].t[:], tm(h_src, i), reads=[R_h[i]], writes=[hts[k].r])
                    P.dma("sp", mxs[k].t[:], fm(mix_d, i), reads=[R_mixA[i], R_mixB[i]], writes=[mxs[k].r])

                def s3_A(i):
                    k = i % 2
                    ht = hts[k]
                    mx = mxs[k]
                    ss, rstd, xn, xT = ss2[k], rstd2[k], xn2[k], xT2[k]
                    for j in range(4):
                        for half in range(2):
                            bank = next_pf()
                            mm_group(bank, (0, 512), [mx.t[:, c, j * 128:(j + 1) * 128] for c in range(8)],
                                     [w_out_sb.t[:, c, half * 512:(half + 1) * 512] for c in range(8)],
                                     [[mx.r, w_out_sb.r]] * 8)
                            P.op("dve", lambda e: e.tensor_tensor(out=ht.t[:, j, half * 512:(half + 1) * 512],
                                                                  in0=bank.t[:], in1=ht.t[:, j, half * 512:(half + 1) * 512],
                                                                  op=ALU.add),
                                 reads=[bank.r, ht.r], writes=[ht.r])
                    P.dma("sp", tm(h_d, i), ht.t[:], reads=[ht.r], writes=[R_h[i]])
                    norm_stats(st, ht, 0, ss, rstd, sqj)
                    norm_transpose(ht, 0, rstd, xn, xT, PC_FFNG)
                    P.dma("sp", fm(xT_d, i), xT.t[:], reads=xT.res, writes=[R_xT[i]])

                def s3_B(i):
                    k = i % 2
                    ht = hts[k]
                    ss, rstd, xn, xT = ss2[k], rstd2[k], xn2[k], xT2[k]
                    bct = next_pf()
                    for j in range(4):
                        for c2 in range(2):
                            bank = next_pf()
                            while bank is bct:
                                bank = next_pf()
                            for cq in range(4):
                                c = c2 * 4 + cq
                                P.op("pe", lambda e: e.transpose(bank.t[:, cq * 128:(cq + 1) * 128],
                                                                 ht.t[:, j, c * 128:(c + 1) * 128], identf.t[:]),
                                     reads=[ht.r, identf.r], writes=[bank.r], signal=(cq == 3))
                            if c2 == 0:
                                P.op("act", lambda e: e.copy(out=hTf.t[:, 0:4, :], in_=bank.t[:].rearrange("p (a b) -> p a b", a=4)),
                                     reads=[bank.r], writes=[hTf.res[0]])
                            else:
                                P.op("dve", lambda e: e.tensor_copy(out=hTf.t[:, 4:8, :], in_=bank.t[:].rearrange("p (a b) -> p a b", a=4)),
                                     reads=[bank.r], writes=[hTf.res[1]])
                        bl = next_pf()
                        while bl is bct:
                            bl = next_pf()
                        mm_group(bl, (0, 20), [hTf.t[:, c, :] for c in range(8)], [wrg.t[:, c, :] for c in range(8)],
                                 [[hTf.res[c // 4], wrg.r] for c in range(8)])
                        P.op("dve", lambda e: e.scalar_tensor_tensor(out=lg4.t[:, j, :], in0=bl.t[:, 0:20],
                                                                     scalar=rstd.t[:, j:j + 1], in1=rbias.t[:],
                                                                     op0=ALU.mult, op1=ALU.add),
                             reads=[bl.r, rstd.r, rbias.r], writes=[lg4.r])
                    V = lambda fn, rd, wr: P.op("dve", fn, reads=rd, writes=wr)
                    S3 = [128, 4, 4]
                    bc = lambda ap_: ap_.unsqueeze(2).to_broadcast(S3)
                    glg = lg4.t[:, :, 0:4]
                    V(lambda e: e.reduce_max(out=r4.t[:, 0, :], in_=glg, axis=AX.X), [lg4.r], [r4.r])
                    V(lambda e: e.tensor_tensor(out=mg4.t[:], in0=glg, in1=bc(r4.t[:, 0, :]), op=ALU.is_ge),
                      [lg4.r, r4.r], [mg4.r])
                    V(lambda e: e.tensor_tensor(out=t4.t[:], in0=glg, in1=bc(r4.t[:, 0, :]), op=ALU.subtract),
                      [lg4.r, r4.r], [t4.r])
                    P.op("act", lambda e: e.activation(out=t4.t[:], in_=t4.t[:], func=AF.Exp), reads=[t4.r], writes=[t4.r])
                    V(lambda e: e.reduce_sum(out=r4.t[:, 1, :], in_=t4.t[:], axis=AX.X), [t4.r, r4.r], [r4.r])
                    V(lambda e: e.reciprocal(out=r4.t[:, 2, :], in_=r4.t[:, 1, :]), [r4.r], [r4.r])
                    el4 = lg4.t[:, :, 4:20].rearrange("p j (g i) -> p j g i", g=4)
                    V(lambda e: e.tensor_tensor(out=pr4.t[:], in0=el4,
                                                in1=mg4.t[:].unsqueeze(3).to_broadcast([128, 4, 4, 4]), op=ALU.mult),
                      [lg4.r, mg4.r], [pr4.r])
                    V(lambda e: e.reduce_sum(out=es4.t[:], in_=pr4.t[:].rearrange("p j g i -> p j i g"), axis=AX.X),
                      [pr4.r], [es4.r])
                    V(lambda e: e.reduce_max(out=r4.t[:, 3, :], in_=es4.t[:], axis=AX.X), [es4.r, r4.r], [r4.r])
                    V(lambda e: e.tensor_tensor(out=eq4.t[:], in0=es4.t[:], in1=bc(r4.t[:, 3, :]), op=ALU.is_ge),
                      [es4.r, r4.r], [eq4.r])
                    V(lambda e: e.scalar_tensor_tensor(out=t4.t[:], in0=eq4.t[:], scalar=-1e30, in1=es4.t[:],
                                                       op0=ALU.mult, op1=ALU.add), [eq4.r, es4.r, t4.r], [t4.r])
                    V(lambda e: e.reduce_max(out=r4.t[:, 4, :], in_=t4.t[:], axis=AX.X), [t4.r, r4.r], [r4.r])
                    V(lambda e: e.tensor_tensor(out=eq4.t[:], in0=es4.t[:], in1=bc(r4.t[:, 4, :]), op=ALU.is_ge),
                      [es4.r, r4.r, eq4.r], [eq4.r])
                    V(lambda e: e.tensor_tensor(out=t4.t[:], in0=es4.t[:], in1=bc(r4.t[:, 3, :]), op=ALU.subtract),
                      [es4.r, r4.r, t4.r], [t4.r])
                    P.op("act", lambda e: e.activation(out=t4.t[:], in_=t4.t[:], func=AF.Exp), reads=[t4.r], writes=[t4.r])
                    V(lambda e: e.tensor_tensor(out=t4.t[:], in0=t4.t[:], in1=eq4.t[:], op=ALU.mult),
                      [t4.r, eq4.r], [t4.r])
                    V(lambda e: e.reduce_sum(out=r4.t[:, 5, :], in_=t4.t[:], axis=AX.X), [t4.r, r4.r], [r4.r])
                    V(lambda e: e.reciprocal(out=r4.t[:, 6, :], in_=r4.t[:, 5, :]), [r4.r], [r4.r])
                    V(lambda e: e.tensor_tensor(out=r4.t[:, 6, :], in0=r4.t[:, 6, :], in1=r4.t[:, 2, :], op=ALU.mult),
                      [r4.r], [r4.r])
                    V(lambda e: e.tensor_tensor(out=t4.t[:], in0=t4.t[:], in1=bc(r4.t[:, 6, :]), op=ALU.mult),
                      [t4.r, r4.r], [t4.r])
                    V(lambda e: e.tensor_tensor(out=comb.t[:].rearrange("p j (g i) -> p j g i", g=4),
                                                in0=t4.t[:].unsqueeze(2).to_broadcast([128, 4, 4, 4]),
                                                in1=mg4.t[:].unsqueeze(3).to_broadcast([128, 4, 4, 4]), op=ALU.mult),
                      [t4.r, mg4.r], [comb.r])
                    for j in range(4):
                        P.op("pe", lambda e: e.transpose(bct.t[0:16, j * 128:(j + 1) * 128], comb.t[:, j, :], identf.t[:]),
                             reads=[comb.r, identf.r], writes=[bct.r])
                    P.op("act", lambda e: e.copy(out=cmbT.t[:], in_=bct.t[0:16, :]), reads=[bct.r], writes=[cmbT.r])
                    P.dma("sp", cmb_d[:, i * TS:(i + 1) * TS], cmbT.t[:], reads=[cmbT.r], writes=[R_cmb[i]])

                s3_load(0)
                if NT > 1:
                    s3_load(1)
                s3_A(0)
                for i in range(NT):
                    if i + 1 < NT:
                        s3_A(i + 1)
                    s3_B(i)
                    if i + 2 < NT:
                        s3_load(i + 2)
                P.barrier()
            st_wout.close()
            if dbg == "s3":
                stm.close()
                st_ple.close()
                break

            wgs.append(sbuf(stm, "wgs1", [128, 4, 8, 256], BF16))
            wus.append(sbuf(stm, "wus1", [128, 4, 8, 256], BF16))
            wds.append(sbuf(stm, "wds1", [128, 4, 2, D], BF16))
            with contextlib.ExitStack() as st:
                set_psum(st, 8, 0)
                hts = [sbuf(st, "s4_ht%d" % k, [128, 4, D], F32) for k in range(2)]
                xTs = [sbuf(st, "s4_xT%d" % k, [128, 8, TS], BF16) for k in range(2)]
                cms = [sbuf(st, "s4_cm%d" % k, [16, TS], F32) for k in range(2)]
                cbs = sbuf(st, "s4_cb", [128, TS], F32)
                sg_ = sbuf(st, "s4_sg", [128, TS], F32)
                tt_ = sbuf(st, "s4_tt", [128, TS], F32)
                hdn = sbuf(st, "s4_hdn", [128, 4, 2, TS], BF16)

                def s4_load(i):
                    k = i % 2
                    P.dma("sp", hts[k].t[:], tm(h_d, i), reads=[R_h[i]], writes=[hts[k].r])
                    P.dma("sp", xTs[k].t[:], fm(xT_d, i), reads=[R_xT[i]], writes=[xTs[k].r])
                    P.dma("sp", cms[k].t[:], cmb_d[:, i * TS:(i + 1) * TS], reads=[R_cmb[i]], writes=[cms[k].r])

                s4_load(0)
                for pz in range(4):
                    slot = pz % 2
                    if pz + 1 < 4:
                        load_experts(pz + 1, (pz + 1) % 2)
                    if pz == 0:
                        for c in range(8):
                            P.dma("pool", wpg.t[:, c, :], pgw_d[l, c * 128:(c + 1) * 128, :], writes=[wpg.r])
                        P.dma("pool", wpp.t[:], ppj_d[l].rearrange("(c p) n -> p c n", p=128), writes=[wpp.r])
                    for i in range(NT):
                        k = i % 2
                        if i + 1 < NT:
                            s4_load(i + 1)
                        elif pz + 1 < 4:
                            s4_load(0)
                        ht, xT_, cm = hts[k], xTs[k], cms[k]
                        for el in range(4):
                            eidx = pz * 4 + el
                            bcb = next_pf()
                            P.op("pe", lambda e: e.matmul(bcb.t[:], sel.t[0:16, eidx, :], cm.t[0:16, :],
                                                          start=True, stop=True),
                                 reads=[sel.r, cm.r], writes=[bcb.r])
                            P.op("act", lambda e: e.copy(out=cbs.t[:], in_=bcb.t[:]), reads=[bcb.r], writes=[cbs.r])
                            for hc in range(2):
                                bg_ = next_pf()
                                mm_group(bg_, (0, TS), [wgs[slot].t[:, el, c, hc * 128:(hc + 1) * 128] for c in range(8)],
                                         [xT_.t[:, c, :] for c in range(8)], [[wgs[slot].r, xT_.r]] * 8)
                                bu_ = next_pf()
                                mm_group(bu_, (0, TS), [wus[slot].t[:, el, c, hc * 128:(hc + 1) * 128] for c in range(8)],
                                         [xT_.t[:, c, :] for c in range(8)], [[wus[slot].r, xT_.r]] * 8)
                                P.op("act", lambda e: e.activation(out=sg_.t[:], in_=bg_.t[:], func=AF.Silu),
                                     reads=[bg_.r], writes=[sg_.r])
                                P.op("dve", lambda e: e.tensor_tensor(out=tt_.t[:], in0=bu_.t[:], in1=cbs.t[:],
                                                                      op=ALU.mult),
                                     reads=[bu_.r, cbs.r], writes=[tt_.r])
                                P.op("dve", lambda e: e.tensor_tensor(out=hdn.t[:, el, hc, :], in0=sg_.t[:], in1=tt_.t[:],
                                                                      op=ALU.mult),
                                     reads=[sg_.r, tt_.r], writes=[hdn.r])
                        for j in range(4):
                            for half in range(2):
                                by = next_pf()
                                mm_group(by, (0, 512),
                                         [hdn.t[:, el, hc, j * 128:(j + 1) * 128] for el in range(4) for hc in range(2)],
                                         [wds[slot].t[:, el, hc, half * 512:(half + 1) * 512]
                                          for el in range(4) for hc in range(2)],
                                         [[hdn.r, wds[slot].r]] * 8)
                                P.op("dve", lambda e: e.tensor_tensor(out=ht.t[:, j, half * 512:(half + 1) * 512],
                                                                      in0=by.t[:],
                                                                      in1=ht.t[:, j, half * 512:(half + 1) * 512],
                                                                      op=ALU.add),
                                     reads=[by.r, ht.r], writes=[ht.r])
                        P.dma("sp", tm(h_d, i), ht.t[:], reads=[ht.r], writes=[R_h[i]])
                P.barrier()
            stm.close()
            if dbg == "s4":
                st_ple.close()
                break

            with contextlib.ExitStack() as st:
                set_psum(st, 6, 2)
                gbf = sbuf(st, "s5_gbf", [1, D], F32)
                gbb = sbuf(st, "s5_gbb", [1, D], BF16)
                hts = [sbuf(st, "s5_ht%d" % k, [128, 4, D], F32) for k in range(2)]
                pts_ = [sbuf(st, "s5_p%d" % k, [128, 4, 256], F32) for k in range(2)]
                ss2 = [sbuf(st, "s5_ss%d" % k, [128, 4], F32) for k in range(2)]
                rstd2 = [sbuf(st, "s5_rstd%d" % k, [128, 4], F32) for k in range(2)]
                sqj = sbuf(st, "s5_sqj", [128, D], BF16)
                xn2 = [sbuf(st, "s5_xn%d" % k, [128, 4, D], BF16, 4) for k in range(2)]
                xT2 = [sbuf(st, "s5_xT%d" % k, [128, 8, TS], BF16, 8) for k in range(2)]
                pT2 = [sbuf(st, "s5_pT%d" % k, [128, 2, TS], BF16) for k in range(2)]
                gts = [sbuf(st, "s5_gt%d" % k, [128, 512], F32) for k in range(3)]
                tqs = [sbuf(st, "s5_tq%d" % k, [128, 512], F32) for k in range(3)]
                if l + 1 < DEPTH:
                    st_win, w_in_next = load_w_in(l + 1)
                P.dma("sp", gbf.t[:], pgb_d[l:l + 1, :], writes=[gbf.r])
                P.op("dve", lambda e: e.tensor_copy(out=gbb.t[:], in_=gbf.t[:]), reads=[gbf.r], writes=[gbb.r])

                def s5_load(i):
                    k = i % 2
                    P.dma("sp", hts[k].t[:], tm(h_d, i), reads=[R_h[i]], writes=[hts[k].r])
                    P.dma("sp", pts_[k].t[:], tm(p_d[l], i), writes=[pts_[k].r])

                def s5_P(i):
                    k = i % 2
                    ht, pt_ = hts[k], pts_[k]
                    ss, rstd, xn, xT, pT = ss2[k], rstd2[k], xn2[k], xT2[k], pT2[k]
                    norm_stats(st, ht, 0, ss, rstd, sqj)
                    norm_transpose(ht, 0, rstd, xn, xT, PC_PLEG)
                    for c in range(2):
                        bank = next_pf()
                        for j in range(4):
                            P.op("pe", lambda e: e.transpose(bank.t[:, j * 128:(j + 1) * 128],
                                                             pt_.t[:, j, c * 128:(c + 1) * 128], identf.t[:]),
                                 reads=[pt_.r, identf.r], writes=[bank.r], signal=(j == 3))
                        P.op("act", lambda e: e.copy(out=pT.t[:, c, :], in_=bank.t[:]), reads=[bank.r], writes=[pT.r])

                def s5_M(i):
                    k = i % 2
                    ht, pt_ = hts[k], pts_[k]
                    ss, rstd, xn, xT, pT = ss2[k], rstd2[k], xn2[k], xT2[k], pT2[k]
                    for j in range(4):
                        for half in range(2):
                            hs = slice(half * 512, (half + 1) * 512)
                            gt = gts[(j * 2 + half) % 3]
                            tq = tqs[(j * 2 + half) % 3]
                            bg_ = next_pf()
                            mm_group(bg_, (0, 512),
                                     [xT.t[:, c, j * 128:(j + 1) * 128] for c in range(8)] + [onesrow.t[0:1, :]],
                                     [wpg.t[:, c, hs] for c in range(8)] + [gbb.t[0:1, hs]],
                                     [[xT.res[c], wpg.r] for c in range(8)] + [[onesrow.r, gbb.r]])
                            bp_ = next_pf()
                            mm_group(bp_, (0, 512), [pT.t[:, c, j * 128:(j + 1) * 128] for c in range(2)],
                                     [wpp.t[:, c, hs] for c in range(2)], [[pT.r, wpp.r]] * 2)
                            P.op("act", lambda e: e.activation(out=gt.t[:], in_=bg_.t[:], func=AF.Sigmoid),
                                 reads=[bg_.r], writes=[gt.r])
                            P.op("dve", lambda e: e.tensor_tensor(out=tq.t[:], in0=bp_.t[:], in1=gt.t[:], op=ALU.mult),
                                 reads=[bp_.r, gt.r], writes=[tq.r])
                            P.op("dve", lambda e: e.tensor_tensor(out=ht.t[:, j, hs], in0=ht.t[:, j, hs], in1=tq.t[:],
                                                                  op=ALU.add),
                                 reads=[ht.r, tq.r], writes=[ht.r])
                    if l < DEPTH - 1:
                        P.dma("sp", tm(h_d, i), ht.t[:], reads=[ht.r], writes=[R_h[i]])
                    else:
                        norm_stats(st, ht, 0, ss, rstd, sqj)
                        for j in range(4):
                            P.op("dve", lambda e: e.scalar_tensor_tensor(out=ht.t[:, j, :], in0=ht.t[:, j, :],
                                                                         scalar=rstd.t[:, j:j + 1], in1=gfin.t[:],
                                                                         op0=ALU.mult, op1=ALU.mult),
                                 reads=[ht.r, rstd.r, gfin.r], writes=[ht.r])
                        P.dma("sp", tm(out_d, i), ht.t[:], reads=[ht.r], writes=[R_out[i]])

                s5_load(0)
                if NT > 1:
                    s5_load(1)
                s5_P(0)
                for i in range(NT):
                    if i + 1 < NT:
                        s5_P(i + 1)
                    s5_M(i)
                    if i + 2 < NT:
                        s5_load(i + 2)
                P.barrier()
            st_ple.close()

        P.barrier()
        P.final_wait()
    return P


def _host_consts():
    c = {}
    c["c_identf"] = np.eye(128, dtype=np.float32)
    rp = np.zeros((128, 128), np.float32)
    for b in range(4):
        for d in range(16):
            rp[b * 32 + d + 16, b * 32 + d] = -1.0
            rp[b * 32 + d, b * 32 + d + 16] = 1.0
    c["c_rperm"] = rp
    kk = np.arange(128)[:, None]
    qq = np.arange(128)[None, :]
    c["c_tri"] = (qq >= kk).astype(np.float32)
    b64 = np.zeros((128, 128), np.float32)
    b64[0:64, 0:64] = 1.0 / 64
    b64[64:128, 64:128] = 1.0 / 64
    c["c_blk64"] = b64
    c["c_ones256"] = np.full((128, 128), 1.0 / 256, np.float32)
    sel = np.zeros((16, NE, 128), np.float32)
    for e in range(NE):
        sel[e, e, :] = 1.0
    c["c_sel"] = sel
    pos = np.arange(S, dtype=np.float32)
    inv = (10000.0 ** (-np.arange(0, 32, 2, dtype=np.float32) / np.float32(32))).astype(np.float32)
    ang = (pos[:, None] * inv[None, :]).astype(np.float32)
    ang = np.concatenate([ang, ang], axis=-1)
    cosT = np.cos(ang.astype(np.float64)).astype(np.float32).T
    sinT = np.sin(ang.astype(np.float64)).astype(np.float32).T
    c["c_cos"] = np.ascontiguousarray(np.tile(cosT, (4, 1)))
    c["c_sin"] = np.ascontiguousarray(np.tile(sinT, (4, 1)))
    wins = np.array([2, 4, 8, 16])
    corr = np.zeros((128, 2, 16), np.float32)
    for cc in range(2):
        for p in range(128):
            w = wins[cc * 2 + p // 64]
            for t in range(16):
                corr[p, cc, t] = w / min(t + 1, w)
    c["c_corr"] = corr
    iw = np.zeros((128, 2), np.float32)
    for cc in range(2):
        for p in range(128):
            iw[p, cc] = 1.0 / wins[cc * 2 + p // 64]
    c["_iw"] = iw
    return c


def _fmcols(v, nch):
    return np.ascontiguousarray(np.asarray(v, np.float32).reshape(nch, 128).T)


def _layout_inputs(inp):
    c = _host_consts()
    iw = c.pop("_iw")
    shared = dict(c)
    prm = np.zeros((DEPTH, 128, PC_N), np.float32)
    wr = np.zeros((DEPTH, 128, 8, 20), np.float32)
    rb = np.zeros((DEPTH, 128, 20), np.float32)
    lamv = np.zeros((DEPTH, 128, 4, 32), np.float32)
    pw = np.zeros((DEPTH, 128, 2, 128), np.float32)
    for l in range(DEPTH):
        prm[l, :, PC_MIXG:PC_MIXG + 8] = _fmcols(inp["mix_norm"][l], 8)
        prm[l, :, PC_FFNG:PC_FFNG + 8] = _fmcols(inp["ffn_norm"][l], 8)
        prm[l, :, PC_PLEG:PC_PLEG + 8] = _fmcols(inp["ple_norm"][l], 8)
        prm[l, :, PC_FING:PC_FING + 8] = _fmcols(inp["final_norm"], 8)
        cw = np.asarray(inp["conf_conv_w"][l], np.float32)
        for cc in range(2):
            prm[l, :, PC_CW + cc * CK:PC_CW + (cc + 1) * CK] = cw[:, cc * 128:(cc + 1) * 128].T
        prm[l, :, PC_CB:PC_CB + 2] = _fmcols(inp["conf_conv_b"][l], 2)
        prm[l, :, PC_LNG:PC_LNG + 2] = _fmcols(inp["conf_ln_g"][l], 2)
        prm[l, :, PC_LNB:PC_LNB + 2] = _fmcols(inp["conf_ln_b"][l], 2)
        prm[l, :, PC_PB:PC_PB + 2] = _fmcols(np.asarray(inp["pool_b"][l]).reshape(256), 2)
        prm[l, :, PC_PS:PC_PS + 2] = _fmcols(inp["pool_scale"][l], 2)
        sw = np.asarray(inp["sconv_w"][l], np.float32)
        for cc in range(2):
            prm[l, :, PC_SW + cc * 3:PC_SW + (cc + 1) * 3] = sw[:, cc * 128:(cc + 1) * 128].T
        prm[l, :, PC_SUB] = np.tile(np.asarray(inp["diff_subln_g"][l], np.float32), 2)
        prm[l, :, PC_IW:PC_IW + 2] = iw
        wcat = np.concatenate([np.asarray(inp["router_group_w"][l], np.float32),
                               np.asarray(inp["router_expert_w"][l], np.float32)], axis=1)
        wr[l] = wcat.reshape(8, 128, 20).transpose(1, 0, 2)
        bcat = np.concatenate([np.asarray(inp["router_group_b"][l], np.float32),
                               np.asarray(inp["router_expert_b"][l], np.float32)])
        rb[l] = np.tile(bcat[None, :], (128, 1))
        for n_, key in enumerate(["diff_lam_q1", "diff_lam_k1", "diff_lam_q2", "diff_lam_k2"]):
            lamv[l, :, n_, :] = np.tile(np.asarray(inp[key][l], np.float32)[None, :], (128, 1))
        pwl = np.asarray(inp["pool_w"][l], np.float32)
        for g in range(4):
            cc, hh = g // 2, g % 2
            pw[l, hh * 64:(hh + 1) * 64, cc, hh * 64:(hh + 1) * 64] = pwl[g]
    shared.update({
        "prm": prm, "wr": wr, "rbias": rb, "lamv": lamv, "poolw": pw,
        "gfin": np.ascontiguousarray(np.tile(np.asarray(inp["final_norm"], np.float32)[None, :], (128, 1))),
    })
    for key in ["w_in", "w_out", "expert_w_gate", "expert_w_up", "expert_w_down", "ple_gate_w", "ple_gate_b",
                "ple_proj"]:
        shared[key] = np.ascontiguousarray(np.asarray(inp[key], np.float32))
    return shared


_NC_CACHE = {}


def _get_nc():
    if "nc" not in _NC_CACHE:
        nc = bass.Bass("TRN2", target_bir_lowering=False)
        build(nc)
        _NC_CACHE["nc"] = nc
    return _NC_CACHE["nc"]


def kernel(**inputs):
    shared = _layout_inputs(inputs)
    x = np.asarray(inputs["x"], np.float32)
    p = np.asarray(inputs["p"], np.float32)
    n = x.shape[0]
    in_maps = []
    for b in range(n):
        m = dict(shared)
        m["x"] = np.ascontiguousarray(x[b])
        m["p"] = np.ascontiguousarray(p[:, b])
        in_maps.append(m)
    nc = _get_nc()
    res = run_bass_kernel_spmd(nc, in_maps, core_ids=list(range(n)))
    return np.stack([np.asarray(r["out"], np.float32) for r in res.results], axis=0)
```

```python
import math
import contextlib
import numpy as np
import ml_dtypes
import concourse.bass as bass
import concourse.mybir as mybir
from concourse.bass_utils import run_bass_kernel_spmd

F32 = mybir.dt.float32
BF16 = mybir.dt.bfloat16
ALU = mybir.AluOpType
AF = mybir.ActivationFunctionType
AX = mybir.AxisListType

S = 4096
D = 1024
DEPTH = 2
NT = 8
TS = 512
INC = 2304
NE = 16
EPS = 1e-6
CK = 31
SCALE = 32 ** -0.5
SEM_CHUNK = 30000

PC_MIXG, PC_FFNG, PC_PLEG, PC_FING = 0, 8, 16, 24
PC_CW = 32
PC_CB = PC_CW + 62
PC_LNG = PC_CB + 2
PC_LNB = PC_LNG + 2
PC_PB = PC_LNB + 2
PC_PS = PC_PB + 2
PC_SW = PC_PS + 2
PC_SUB = PC_SW + 6
PC_IW = PC_SUB + 1
PC_N = PC_IW + 2


class Tok:
    __slots__ = ("sem", "val", "eng")

    def __init__(self, eng):
        self.sem = None
        self.val = None
        self.eng = eng


class Res:
    __slots__ = ("name", "w", "r", "excl")

    def __init__(self, name, excl=False):
        self.name = name
        self.w = None
        self.r = []
        self.excl = excl


class Prog:
    def __init__(self, nc, es):
        self.nc = nc
        self.es = es
        self.eobj = {"pe": nc.tensor, "act": nc.scalar, "dve": nc.vector,
                     "pool": nc.gpsimd, "sp": nc.sync}
        self.sems = {e: [] for e in self.eobj}
        self.count = {e: 0 for e in self.eobj}
        self.known = {e: {} for e in self.eobj}
        self.pending = {e: None for e in self.eobj}
        self.last_tok = {e: None for e in self.eobj}
        self.nsem = 0
        self.dma_sems = []
        self.dma_cnt = []
        self.dma_i = 0
        self.dma_ip = 0
        for i in range(24):
            self.dma_sems.append(self._new_sem("dq%d" % i))
            self.dma_cnt.append(0)
        self.dma_toks = []
        self.n_ops = 0
        self.n_waits = 0
        self.limit = None
        self.stores_on_pool = False
        self.n_all = 0
        self.skip = False

    def _skipping(self):
        if self.skip:
            return True
        if self.limit is not None and self.n_all > self.limit and all(v is None for v in self.pending.values()):
            self.skip = True
            return True
        return False

    def _new_sem(self, name):
        self.nsem += 1
        return self.es.enter_context(self.nc.semaphore(name))

    def _eng_tok(self, eng, tok):
        c = self.count[eng]
        idx = c // SEM_CHUNK
        while len(self.sems[eng]) <= idx:
            self.sems[eng].append(self._new_sem("%s%d" % (eng, len(self.sems[eng]))))
        tok.sem = self.sems[eng][idx]
        tok.val = c % SEM_CHUNK + 1
        self.count[eng] = c + 1
        return tok

    def _wait(self, eng, tok):
        if tok is None:
            return
        if tok.sem is None:
            assert tok.eng == eng, "dependency on unsignaled op of %s from %s" % (tok.eng, eng)
            return
        k = self.known[eng]
        sid = id(tok.sem)
        if k.get(sid, 0) >= tok.val:
            return
        k[sid] = tok.val
        self.eobj[eng].wait_ge(tok.sem, tok.val)
        self.n_waits += 1

    def _deps(self, eng, reads, writes, is_dma=False):
        for r in reads:
            if r.w is not None:
                if r.w.eng == eng and not is_dma and eng == "pe":
                    continue
                self._wait(eng, r.w)
        same_ok = (eng == "pe") and not is_dma
        for w in writes:
            if w.w is not None and not (same_ok and w.w.eng == eng):
                self._wait(eng, w.w)
            for t in w.r:
                if not (same_ok and t.eng == eng):
                    self._wait(eng, t)

    def op(self, eng, fn, reads=(), writes=(), signal=True):
        self.n_all += 1
        if self._skipping():
            return None
        if any(r.excl for r in reads):
            writes = list(writes) + [r for r in reads if r.excl and r not in writes]
            reads = [r for r in reads if not r.excl]
        self._deps(eng, reads, writes)
        ins = fn(self.eobj[eng])
        self.n_ops += 1
        tok = self.pending[eng]
        if tok is None:
            tok = Tok(eng)
            self.pending[eng] = tok
        if signal:
            self._eng_tok(eng, tok)
            ins.then_inc(tok.sem, 1)
            self.pending[eng] = None
            self.last_tok[eng] = tok
        for r in reads:
            r.r.append(tok)
        for w in writes:
            w.w = tok
            w.r = []
        return tok

    def dma(self, q, out, in_, reads=(), writes=()):
        if q == "sp" and self.stores_on_pool and str(out.space).endswith("DRAM"):
            q = "pool"
        self.n_all += 1
        if self._skipping():
            return None
        self._deps(q, reads, writes, is_dma=True)
        if q == "pool":
            i = 16 + self.dma_ip % 8
            self.dma_ip += 1
        else:
            i = self.dma_i % 16
            self.dma_i += 1
        sem = self.dma_sems[i]
        if self.dma_cnt[i] > 0:
            prev = Tok("dma")
            prev.sem = sem
            prev.val = self.dma_cnt[i]
            self._wait(q, prev)
        self.eobj[q].dma_start(out=out, in_=in_).then_inc(sem, 16)
        self.dma_cnt[i] += 16
        tok = Tok("dma")
        tok.sem = sem
        tok.val = self.dma_cnt[i]
        self.dma_toks.append(tok)
        for r in reads:
            r.r.append(tok)
        for w in writes:
            w.w = tok
            w.r = []
        return tok

    def barrier(self):
        toks = [t for t in self.last_tok.values() if t is not None]
        for i, s in enumerate(self.dma_sems):
            if self.dma_cnt[i] > 0:
                t = Tok("dma")
                t.sem = s
                t.val = self.dma_cnt[i]
                toks.append(t)
        for e in self.eobj:
            assert self.pending[e] is None
            for t in toks:
                if t.eng == e:
                    continue
                self._wait(e, t)

    def final_wait(self):
        for i, s in enumerate(self.dma_sems):
            if self.dma_cnt[i] > 0:
                t = Tok("dma")
                t.sem = s
                t.val = self.dma_cnt[i]
                self._wait("sp", t)


class Buf:
    def __init__(self, t, name, nslots=1):
        self.t = t
        self.res = [Res("%s.%d" % (name, i)) for i in range(nslots)]

    @property
    def r(self):
        return self.res[0]


def build(nc, dbg=None, limit=None):
    P = None
    with contextlib.ExitStack() as es:
        P = Prog(nc, es)
        P.limit = limit

        def dram_in(name, shape, dt=F32):
            return nc.dram_tensor(name, list(shape), dt, kind="ExternalInput").ap()

        def dram_scr(name, shape, dt, kind="Internal"):
            return nc.dram_tensor(name, list(shape), dt, kind=kind).ap()

        x_d = dram_in("x", [S, D])
        p_d = dram_in("p", [DEPTH, S, 256])
        w_in_d = dram_in("w_in", [DEPTH, D, INC])
        w_out_d = dram_in("w_out", [DEPTH, D, D])
        wg_d = dram_in("expert_w_gate", [DEPTH, NE, D, 256])
        wu_d = dram_in("expert_w_up", [DEPTH, NE, D, 256])
        wd_d = dram_in("expert_w_down", [DEPTH, NE, 256, D])
        pgw_d = dram_in("ple_gate_w", [DEPTH, D, D])
        pgb_d = dram_in("ple_gate_b", [DEPTH, D])
        ppj_d = dram_in("ple_proj", [DEPTH, 256, D])
        prm_d = dram_in("prm", [DEPTH, 128, PC_N])
        wr_d = dram_in("wr", [DEPTH, 128, 8, 20])
        rb_d = dram_in("rbias", [DEPTH, 128, 20])
        lam_d = dram_in("lamv", [DEPTH, 128, 4, 32])
        pw_d = dram_in("poolw", [DEPTH, 128, 2, 128])
        gfin_d = dram_in("gfin", [128, D])
        cidf_d = dram_in("c_identf", [128, 128])
        crp_d = dram_in("c_rperm", [128, 128])
        ctri_d = dram_in("c_tri", [128, 128])
        cb64_d = dram_in("c_blk64", [128, 128])
        cones_d = dram_in("c_ones256", [128, 128])
        csel_d = dram_in("c_sel", [16, NE, 128])
        ccos_d = dram_in("c_cos", [128, S])
        csin_d = dram_in("c_sin", [128, S])
        ccorr_d = dram_in("c_corr", [128, 2, 16])
        out_d = nc.dram_tensor("out", [S, D], F32, kind="ExternalOutput").ap()

        dkind = "ExternalOutput" if dbg else "Internal"
        h_d = dram_scr("h_scr", [S, D], F32, dkind)
        glu_d = dram_scr("glu_scr", [256, S], BF16, dkind)
        pin_d = dram_scr("pin_scr", [256, S], F32, dkind)
        q_d = dram_scr("q_scr", [256, S], BF16, dkind)
        k_d = dram_scr("k_scr", [256, S], BF16, dkind)
        v_d = dram_scr("v_scr", [S, 512], BF16, dkind)
        gb_d = dram_scr("gb_scr", [256, S], F32, dkind)
        gcv_d = dram_scr("gcv_scr", [256, S], F32, dkind)
        mix_d = dram_scr("mix_scr", [D, S], BF16, dkind)
        xT_d = dram_scr("xT_scr", [D, S], BF16, dkind)
        cmb_d = dram_scr("cmb_scr", [16, S], F32, dkind)

        def tiles(name):
            return [Res("%s%d" % (name, i)) for i in range(NT)]
        R_h = tiles("h")
        R_glu, R_pin, R_q, R_k, R_v = tiles("glu"), tiles("pin"), tiles("q"), tiles("k"), tiles("v")
        R_gb, R_gcv, R_mixA, R_mixB, R_xT, R_cmb = (tiles("gb"), tiles("gcv"), tiles("mixA"),
                                                    tiles("mixB"), tiles("xT"), tiles("cmb"))
        R_out = tiles("out")

        def fm(ap, i, lo=0, hi=TS):
            return ap.rearrange("(c p) t -> p c t", p=128)[:, :, i * TS + lo:i * TS + hi]

        def tm(ap, i):
            return ap[i * TS:(i + 1) * TS, :].rearrange("(j p) f -> p j f", p=128)

        uid = [0]
        def sbuf(stack, name, shape, dt, nslots=1):
            uid[0] += 1
            t = stack.enter_context(nc.sbuf_tensor("%s_u%d" % (name, uid[0]), list(shape), dt))
            return Buf(t, name, nslots)

        def psum(stack, name, shape, dt=F32):
            t = stack.enter_context(nc.psum_tensor(name, list(shape), dt))
            b = Buf(t, name, 1)
            b.res[0].excl = True
            return b

        identf = sbuf(es, "identf", [128, 128], F32)
        identb = sbuf(es, "identb", [128, 128], BF16)
        rperm = sbuf(es, "rperm", [128, 128], F32)
        trib = sbuf(es, "trib", [128, 128], BF16)
        blk64 = sbuf(es, "blk64", [128, 128], F32)
        ones256 = sbuf(es, "ones256", [128, 128], F32)
        sel = sbuf(es, "sel", [16, NE, 128], F32)
        prm = sbuf(es, "prm_sb", [128, PC_N], F32)
        corr = sbuf(es, "corr", [128, 2, 16], F32)
        gfin = sbuf(es, "gfin_sb", [128, D], F32)
        neglam = sbuf(es, "neglam", [128, 1], F32)
        pbs = sbuf(es, "pbs", [128, 2], F32)
        onesrow = sbuf(es, "onesrow", [1, 128], BF16)
        epsc = sbuf(es, "epsc", [128, 1], F32)

        pf = []
        pb = []
        pf_i = [0]
        pb_i = [0]

        def set_psum(stack, nf, nb):
            uid[0] += 1
            pf[:] = [psum(stack, "pf%d_%d" % (i, uid[0]), [128, 512], F32) for i in range(nf)]
            pb[:] = [psum(stack, "pb%d_%d" % (i, uid[0]), [128, 1024], BF16) for i in range(nb)]

        def next_pf():
            b = pf[pf_i[0] % len(pf)]
            pf_i[0] += 1
            return b

        def next_pb():
            b = pb[pb_i[0] % len(pb)]
            pb_i[0] += 1
            return b

        P.dma("sp", identf.t[:], cidf_d, writes=[identf.r])
        P.dma("sp", rperm.t[:], crp_d, writes=[rperm.r])
        P.dma("sp", blk64.t[:], cb64_d, writes=[blk64.r])
        P.dma("sp", ones256.t[:], cones_d, writes=[ones256.r])
        P.dma("sp", sel.t[:], csel_d, writes=[sel.r])
        P.dma("sp", corr.t[:], ccorr_d, writes=[corr.r])
        P.dma("sp", gfin.t[:], gfin_d, writes=[gfin.r])
        P.dma("pool", identb.t[:], cidf_d, writes=[identb.r])
        P.dma("pool", trib.t[:], ctri_d, writes=[trib.r])
        P.op("dve", lambda e: e.memset(onesrow.t[:], 1.0), writes=[onesrow.r])
        P.op("dve", lambda e: e.memset(epsc.t[:], EPS), writes=[epsc.r])

        def norm_stats(st, ht, slot, ss, rstd, sqj):
            P.op("dve", lambda e: e.memset(ss.t[:], 0.0), writes=[ss.r])
            for j in range(4):
                P.op("act", lambda e, j=j: e.activation(out=sqj.t[:], in_=ht.t[:, j, :], func=AF.Square,
                                                        accum_out=ss.t[:, j:j + 1]),
                     reads=[ht.res[slot], ss.r], writes=[sqj.r, ss.r])
            P.op("dve", lambda e: e.tensor_scalar(out=rstd.t[:], in0=ss.t[:], scalar1=1.0 / D, scalar2=EPS,
                                                  op0=ALU.mult, op1=ALU.add), reads=[ss.r], writes=[rstd.r])
            P.op("act", lambda e: e.activation(out=rstd.t[:], in_=rstd.t[:], func=AF.Sqrt),
                 reads=[rstd.r], writes=[rstd.r])
            P.op("dve", lambda e: e.reciprocal(out=rstd.t[:], in_=rstd.t[:]), reads=[rstd.r], writes=[rstd.r])

        def norm_transpose(ht, slot, rstd, xn, xT, gcol):
            for j in range(4):
                P.op("dve", lambda e, j=j: e.tensor_scalar(out=xn.t[:, j, :], in0=ht.t[:, j, :],
                                                           scalar1=rstd.t[:, j:j + 1], scalar2=None, op0=ALU.mult),
                     reads=[ht.res[slot], rstd.r], writes=[xn.res[j]])
            for c2 in range(4):
                bank = next_pb()
                for cc in range(2):
                    c = c2 * 2 + cc
                    for j in range(4):
                        last = (cc == 1 and j == 3)
                        P.op("pe", lambda e, c=c, cc=cc, j=j: e.transpose(
                            bank.t[:, cc * 512 + j * 128: cc * 512 + (j + 1) * 128],
                            xn.t[:, j, c * 128:(c + 1) * 128], identb.t[:]),
                            reads=[xn.res[j], identb.r], writes=[bank.r], signal=last)
                for cc in range(2):
                    c = c2 * 2 + cc
                    if cc == 0:
                        P.op("act", lambda e, c=c, cc=cc: e.activation(
                            out=xT.t[:, c, :], in_=bank.t[:, cc * 512:(cc + 1) * 512], func=AF.Identity,
                            scale=prm.t[:, gcol + c:gcol + c + 1]),
                            reads=[bank.r, prm.r], writes=[xT.res[c]])
                    else:
                        P.op("dve", lambda e, c=c, cc=cc: e.tensor_scalar(
                            out=xT.t[:, c, :], in0=bank.t[:, cc * 512:(cc + 1) * 512],
                            scalar1=prm.t[:, gcol + c:gcol + c + 1], scalar2=None, op0=ALU.mult),
                            reads=[bank.r, prm.r], writes=[xT.res[c]])

        def load_h(i, ht, slot, src):
            P.dma("sp", ht.t[:], tm(src, i), reads=[R_h[i]], writes=[ht.res[slot]])

        def mm_group(bank, cols, lhs_list, rhs_list, reads, tile_position=None):
            n = len(lhs_list)
            for k in range(n):
                P.op("pe", lambda e, k=k: e.matmul(bank.t[:, cols[0]:cols[1]], lhs_list[k], rhs_list[k],
                                                   start=(k == 0), stop=(k == n - 1)),
                     reads=reads[k], writes=[bank.r], signal=(k == n - 1))

        def load_w_in(l_):
            stw = contextlib.ExitStack()
            uid[0] += 1
            t_ = stw.enter_context(nc.sbuf_tensor("w_in_sb_u%d" % uid[0], [128, 8, INC], BF16, side="right"))
            b_ = Buf(t_, "w_in_sb", 1)
            for c in range(8):
                P.dma("pool", b_.t[:, c, :], w_in_d[l_, c * 128:(c + 1) * 128, :], writes=[b_.r])
            return stw, b_

        st_win, w_in_next = load_w_in(0)
        for l in range(DEPTH):
            lam_init = 0.8 - 0.6 * math.exp(-0.3 * l)
            h_src = x_d if l == 0 else h_d

            P.barrier()
            P.dma("sp", prm.t[:], prm_d[l], writes=[prm.r])
            with contextlib.ExitStack() as st:
                lamv = sbuf(st, "lamv", [128, 4, 32], F32)
                lt = sbuf(st, "lt", [128, 2, 32], F32)
                ls = sbuf(st, "ls", [128, 2], F32)
                P.dma("sp", lamv.t[:], lam_d[l], writes=[lamv.r])
                P.op("dve", lambda e: e.tensor_tensor(out=lt.t[:, 0, :], in0=lamv.t[:, 0, :], in1=lamv.t[:, 1, :],
                                                      op=ALU.mult), reads=[lamv.r], writes=[lt.r])
                P.op("dve", lambda e: e.tensor_tensor(out=lt.t[:, 1, :], in0=lamv.t[:, 2, :], in1=lamv.t[:, 3, :],
                                                      op=ALU.mult), reads=[lamv.r, lt.r], writes=[lt.r])
                P.op("dve", lambda e: e.reduce_sum(out=ls.t[:], in_=lt.t[:], axis=AX.X), reads=[lt.r], writes=[ls.r])
                P.op("act", lambda e: e.activation(out=ls.t[:], in_=ls.t[:], func=AF.Exp), reads=[ls.r], writes=[ls.r])
                P.op("dve", lambda e: e.scalar_tensor_tensor(out=neglam.t[:], in0=ls.t[:, 1:2], scalar=-lam_init,
                                                             in1=ls.t[:, 0:1], op0=ALU.add, op1=ALU.subtract),
                     reads=[ls.r], writes=[neglam.r])
                P.op("dve", lambda e: e.tensor_tensor(out=pbs.t[:], in0=prm.t[:, PC_PB:PC_PB + 2],
                                                      in1=prm.t[:, PC_PS:PC_PS + 2], op=ALU.mult),
                     reads=[prm.r], writes=[pbs.r])
                P.barrier()

            st_dg = contextlib.ExitStack()
            dg = sbuf(st_dg, "s2_dg", [128, 2, CK, 128], BF16)
            pw_sb = sbuf(st_dg, "s2_pw", [128, 2, 128], BF16)
            with contextlib.ExitStack() as st:
                set_psum(st, 6, 2)
                w_in_sb = w_in_next
                ht = sbuf(st, "s1_ht", [128, 4, D], F32, 2)
                hts = [ht, sbuf(st, "s1_ht2", [128, 4, D], F32, 2)]
                ss2 = [sbuf(st, "s1_ss%d" % k, [128, 4], F32) for k in range(2)]
                rstd2 = [sbuf(st, "s1_rstd%d" % k, [128, 4], F32) for k in range(2)]
                sqj = sbuf(st, "s1_sqj", [128, D], BF16)
                xn2 = [sbuf(st, "s1_xn%d" % k, [128, 4, D], BF16, 4) for k in range(2)]
                nT2 = [sbuf(st, "s1_nT%d" % k, [128, 8, TS], BF16, 8) for k in range(2)]
                sig = sbuf(st, "s1_sig", [128, TS], F32)
                gcs = sbuf(st, "s1_gc", [128, TS], F32)
                glu_st = sbuf(st, "s1_glu", [128, 2, TS], BF16)
                pin_st = sbuf(st, "s1_pin", [128, 2, TS], F32)
                qk_st = sbuf(st, "s1_qk", [128, 4, TS], F32, 4)
                qkr_st = sbuf(st, "s1_qkr", [128, 4, TS], BF16, 4)
                gb_st = sbuf(st, "s1_gb", [128, 2, TS], F32)
                gcv_st = sbuf(st, "s1_gcv", [128, 2, TS], F32)
                v_st = sbuf(st, "s1_v", [128, 4, 512], BF16)
                cos_t = sbuf(st, "s1_cos", [128, TS], F32)
                sin_t = sbuf(st, "s1_sin", [128, TS], F32)
                t1 = sbuf(st, "s1_t1", [128, TS], F32)
                t2 = sbuf(st, "s1_t2", [128, TS], F32)

                P.op("dve", lambda e: e.memset(v_st.t[:], 1.0), writes=[v_st.r])

                def s1_prologue(i_):
                    norm_stats(st, hts[i_ % 2], 0, ss2[i_ % 2], rstd2[i_ % 2], sqj)
                    norm_transpose(hts[i_ % 2], 0, rstd2[i_ % 2], xn2[i_ % 2], nT2[i_ % 2], PC_MIXG)

                P.dma("sp", hts[0].t[:], tm(h_src, 0), reads=[R_h[0]], writes=[hts[0].r])
                P.dma("sp", hts[1].t[:], tm(h_src, 1), reads=[R_h[1]], writes=[hts[1].r])
                s1_prologue(0)
                P.dma("pool", pw_sb.t[:], pw_d[l], writes=[pw_sb.r])
                for cc in range(2):
                    for j in range(CK):
                        col = PC_CW + cc * CK + j
                        P.op("dve", lambda e, cc=cc, j=j, col=col: e.tensor_scalar(
                            out=dg.t[:, cc, j, :], in0=identf.t[:], scalar1=prm.t[:, col:col + 1], scalar2=None,
                            op0=ALU.mult), reads=[identf.r, prm.r], writes=[dg.r])
                for i in range(NT):
                    P.dma("sp", cos_t.t[:], ccos_d[:, i * TS:(i + 1) * TS], writes=[cos_t.r])
                    P.dma("sp", sin_t.t[:], csin_d[:, i * TS:(i + 1) * TS], writes=[sin_t.r])
                    nT = nT2[i % 2]

                    def proj(col0):
                        bank = next_pf()
                        mm_group(bank, (0, TS), [w_in_sb.t[:, c, col0:col0 + 128] for c in range(8)],
                                 [nT.t[:, c, :] for c in range(8)],
                                 [[w_in_sb.r, nT.res[c]] for c in range(8)])
                        return bank

                    for cc in range(2):
                        bg_ = proj(256 + cc * 128)
                        P.op("act", lambda e: e.activation(out=sig.t[:], in_=bg_.t[:], func=AF.Sigmoid),
                             reads=[bg_.r], writes=[sig.r])
                        bv_ = proj(0 + cc * 128)
                        P.op("dve", lambda e: e.tensor_tensor(out=glu_st.t[:, cc, :], in0=bv_.t[:], in1=sig.t[:],
                                                              op=ALU.mult),
                             reads=[bv_.r, sig.r], writes=[glu_st.r])
                    for cc in range(2):
                        bp_ = proj(512 + cc * 128)
                        P.op("act", lambda e: e.copy(out=pin_st.t[:, cc, :], in_=bp_.t[:]),
                             reads=[bp_.r], writes=[pin_st.r])
                    if i + 1 < NT:
                        s1_prologue(i + 1)
                    for m in range(4):
                        bq_ = proj(768 + m * 128)
                        if m % 2 == 0:
                            P.op("act", lambda e: e.copy(out=qk_st.t[:, m, :], in_=bq_.t[:]),
                                 reads=[bq_.r], writes=[qk_st.res[m]])
                        else:
                            P.op("dve", lambda e: e.tensor_copy(out=qk_st.t[:, m, :], in_=bq_.t[:]),
                                 reads=[bq_.r], writes=[qk_st.res[m]])
                    for cc in range(2):
                        bb_ = proj(1536 + cc * 128)
                        P.op("act", lambda e: e.copy(out=gb_st.t[:, cc, :], in_=bb_.t[:]),
                             reads=[bb_.r], writes=[gb_st.r])
                    for cc in range(2):
                        bc_ = proj(1792 + cc * 128)
                        P.op("act", lambda e: e.copy(out=gcs.t[:], in_=bc_.t[:]), reads=[bc_.r], writes=[gcs.r])
                        bs_ = proj(2048 + cc * 128)
                        P.op("dve", lambda e: e.tensor_tensor(out=gcv_st.t[:, cc, :], in0=bs_.t[:], in1=gcs.t[:],
                                                              op=ALU.mult),
                             reads=[bs_.r, gcs.r], writes=[gcv_st.r])
                    for j in range(4):
                        bank = next_pf()
                        mm_group(bank, (0, 256), [nT.t[:, c, j * 128:(j + 1) * 128] for c in range(8)],
                                 [w_in_sb.t[:, c, 1280:1536] for c in range(8)],
                                 [[w_in_sb.r, nT.res[c]] for c in range(8)])
                        for hh in range(4):
                            off = hh * 128 + (0 if hh % 2 == 0 else 64)
                            eng = "act" if hh % 2 == 0 else "dve"
                            if eng == "act":
                                P.op("act", lambda e: e.copy(out=v_st.t[:, j, off:off + 64],
                                                             in_=bank.t[:, hh * 64:(hh + 1) * 64]),
                                     reads=[bank.r], writes=[v_st.r])
                            else:
                                P.op("dve", lambda e: e.tensor_copy(out=v_st.t[:, j, off:off + 64],
                                                                    in_=bank.t[:, hh * 64:(hh + 1) * 64]),
                                     reads=[bank.r], writes=[v_st.r])
                    for m in range(4):
                        bank = next_pf()
                        P.op("pe", lambda e: e.matmul(bank.t[:], rperm.t[:], qk_st.t[:, m, :], start=True, stop=True),
                             reads=[rperm.r, qk_st.res[m]], writes=[bank.r])
                        P.op("dve", lambda e: e.tensor_tensor(out=t1.t[:], in0=qk_st.t[:, m, :], in1=cos_t.t[:],
                                                              op=ALU.mult),
                             reads=[qk_st.res[m], cos_t.r], writes=[t1.r])
                        P.op("dve", lambda e: e.tensor_tensor(out=t2.t[:], in0=bank.t[:], in1=sin_t.t[:],
                                                              op=ALU.mult),
                             reads=[bank.r, sin_t.r], writes=[t2.r])
                        P.op("dve", lambda e: e.tensor_tensor(out=qkr_st.t[:, m, :], in0=t1.t[:], in1=t2.t[:],
                                                              op=ALU.add),
                             reads=[t1.r, t2.r], writes=[qkr_st.res[m]])
                    if i + 2 < NT:
                        P.dma("sp", hts[i % 2].t[:], tm(h_src, i + 2), reads=[R_h[i + 2]], writes=[hts[i % 2].r])
                    P.dma("sp", fm(glu_d, i), glu_st.t[:], reads=[glu_st.r], writes=[R_glu[i]])
                    P.dma("sp", fm(pin_d, i), pin_st.t[:], reads=[pin_st.r], writes=[R_pin[i]])
                    P.dma("sp", fm(q_d, i), qkr_st.t[:, 0:2, :], reads=[qkr_st.res[0], qkr_st.res[1]], writes=[R_q[i]])
                    P.dma("sp", fm(k_d, i), qkr_st.t[:, 2:4, :], reads=[qkr_st.res[2], qkr_st.res[3]], writes=[R_k[i]])
                    P.dma("sp", tm(v_d, i), v_st.t[:], reads=[v_st.r], writes=[R_v[i]])
                    P.dma("sp", fm(gb_d, i), gb_st.t[:], reads=[gb_st.r], writes=[R_gb[i]])
                    P.dma("sp", fm(gcv_d, i), gcv_st.t[:], reads=[gcv_st.r], writes=[R_gcv[i]])
                P.barrier()
            st_win.close()
            if dbg == "s1":
                st_dg.close()
                break

            st_wout = contextlib.ExitStack()
            uid[0] += 1
            w_out_sb = Buf(st_wout.enter_context(nc.sbuf_tensor("w_out_sb_u%d" % uid[0], [128, 8, D], BF16,
                                                                side="right")), "w_out_sb", 1)
            for c in range(8):
                P.dma("pool", w_out_sb.t[:, c, :], w_out_d[l, c * 128:(c + 1) * 128, :], writes=[w_out_sb.r])
            st_kv = contextlib.ExitStack()
            kT = sbuf(st_kv, "at_kT", [128, 2, S], BF16)
            Vs = sbuf(st_kv, "at_V", [128, 32, 512], BF16)
            P.dma("sp", kT.t[:], k_d.rearrange("(c p) t -> p c t", p=128), reads=R_k, writes=[kT.r])
            for i8 in range(NT):
                P.dma("sp", Vs.t[:, i8 * 4:(i8 + 1) * 4, :], tm(v_d, i8), reads=[R_v[i8]], writes=[Vs.r])
            with contextlib.ExitStack() as st:
                set_psum(st, 8, 0)
                glu_in = [sbuf(st, "s2_glu%d" % k, [128, 2, 30 + TS], BF16) for k in range(2)]
                pin_in = [sbuf(st, "s2_pin%d" % k, [128, 2, 16 + TS], F32) for k in range(2)]
                gcv_in = [sbuf(st, "s2_gcv%d" % k, [128, 2, 2 + TS], F32) for k in range(2)]
                gb_in = [sbuf(st, "s2_gb%d" % k, [128, 2, TS], F32) for k in range(2)]
                yc = sbuf(st, "s2_y", [128, 2, TS], F32, 2)
                ysq = sbuf(st, "s2_ysq", [128, 2, TS], F32, 2)
                m2 = sbuf(st, "s2_m2", [128, TS], F32)
                var = sbuf(st, "s2_var", [128, TS], F32)
                dd = sbuf(st, "s2_dd", [128, TS], F32)
                sA = sbuf(st, "s2_sA", [128, 16 + TS], F32)
                sB = sbuf(st, "s2_sB", [128, 16 + TS], F32)
                pooled = sbuf(st, "s2_pooled", [128, TS], BF16)
                acc3 = sbuf(st, "s2_acc3", [128, TS], F32)
                mixA = [sbuf(st, "s2_mixA%d" % k, [128, 6, TS], BF16) for k in range(2)]


                def s2_load(i):
                    k = i % 2
                    if i == 0:
                        P.op("dve", lambda e: e.memset(glu_in[k].t[:, :, 0:30], 0.0), writes=[glu_in[k].r])
                        P.op("dve", lambda e: e.memset(pin_in[k].t[:, :, 0:16], 0.0), writes=[pin_in[k].r])
                        P.op("dve", lambda e: e.memset(gcv_in[k].t[:, :, 0:2], 0.0), writes=[gcv_in[k].r])
                        P.dma("sp", glu_in[k].t[:, :, 30:30 + TS], fm(glu_d, 0), reads=[R_glu[0]], writes=[glu_in[k].r])
                        P.dma("sp", pin_in[k].t[:, :, 16:16 + TS], fm(pin_d, 0), reads=[R_pin[0]], writes=[pin_in[k].r])
                        P.dma("sp", gcv_in[k].t[:, :, 2:2 + TS], fm(gcv_d, 0), reads=[R_gcv[0]], writes=[gcv_in[k].r])
                    else:
                        P.dma("sp", glu_in[k].t[:], fm(glu_d, i, -30, TS), reads=[R_glu[i - 1], R_glu[i]],
                              writes=[glu_in[k].r])
                        P.dma("sp", pin_in[k].t[:], fm(pin_d, i, -16, TS), reads=[R_pin[i - 1], R_pin[i]],
                              writes=[pin_in[k].r])
                        P.dma("sp", gcv_in[k].t[:], fm(gcv_d, i, -2, TS), reads=[R_gcv[i - 1], R_gcv[i]],
                              writes=[gcv_in[k].r])
                    P.dma("sp", gb_in[k].t[:], fm(gb_d, i), reads=[R_gb[i]], writes=[gb_in[k].r])

                s2_load(0)
                for i in range(NT):
                    k = i % 2
                    if i + 1 < NT:
                        s2_load(i + 1)
                    mx = mixA[k]
                    for cc in range(2):
                        bank = next_pf()
                        mm_group(bank, (0, TS), [dg.t[:, cc, j, :] for j in range(CK)],
                                 [glu_in[k].t[:, cc, j:j + TS] for j in range(CK)],
                                 [[dg.r, glu_in[k].r]] * CK)
                        P.op("act", lambda e: e.activation(out=yc.t[:, cc, :], in_=bank.t[:], func=AF.Identity,
                                                           bias=prm.t[:, PC_CB + cc:PC_CB + cc + 1]),
                             reads=[bank.r, prm.r], writes=[yc.res[cc]])
                        P.op("act", lambda e: e.activation(out=ysq.t[:, cc, :], in_=bank.t[:], func=AF.Square,
                                                           bias=prm.t[:, PC_CB + cc:PC_CB + cc + 1]),
                             reads=[bank.r, prm.r], writes=[ysq.res[cc]])
                    bm = next_pf()
                    mm_group(bm, (0, TS), [ones256.t[:], ones256.t[:]], [yc.t[:, 0, :], yc.t[:, 1, :]],
                             [[ones256.r, yc.res[0]], [ones256.r, yc.res[1]]])
                    bq = next_pf()
                    mm_group(bq, (0, TS), [ones256.t[:], ones256.t[:]], [ysq.t[:, 0, :], ysq.t[:, 1, :]],
                             [[ones256.r, ysq.res[0]], [ones256.r, ysq.res[1]]])
                    for cc in range(2):
                        u = pin_in[k].t[:, cc, :]
                        W_ = 16 + TS
                        P.op("dve", lambda e: e.memset(sA.t[:, 0:1], 0.0), writes=[sA.r])
                        P.op("dve", lambda e: e.tensor_tensor(out=sA.t[:, 1:W_], in0=pin_in[k].t[:, cc, 1:W_],
                                                              in1=pin_in[k].t[:, cc, 0:W_ - 1], op=ALU.add),
                             reads=[pin_in[k].r], writes=[sA.r])
                        if cc == 0:
                            P.op("dve", lambda e: e.tensor_tensor(out=sB.t[64:128, 3:W_], in0=sA.t[64:128, 3:W_],
                                                                  in1=sA.t[64:128, 1:W_ - 2], op=ALU.add),
                                 reads=[sA.r], writes=[sB.r])
                            P.op("dve", lambda e: e.tensor_copy(out=sB.t[0:64, 3:W_], in_=sA.t[0:64, 3:W_]),
                                 reads=[sA.r, sB.r], writes=[sB.r])
                            fin = sB
                        else:
                            P.op("dve", lambda e: e.tensor_tensor(out=sB.t[:, 3:W_], in0=sA.t[:, 3:W_],
                                                                  in1=sA.t[:, 1:W_ - 2], op=ALU.add),
                                 reads=[sA.r], writes=[sB.r])
                            P.op("dve", lambda e: e.tensor_tensor(out=sA.t[:, 7:W_], in0=sB.t[:, 7:W_],
                                                                  in1=sB.t[:, 3:W_ - 4], op=ALU.add),
                                 reads=[sB.r, sA.r], writes=[sA.r])
                            P.op("dve", lambda e: e.tensor_tensor(out=sB.t[64:128, 15:W_], in0=sA.t[64:128, 15:W_],
                                                                  in1=sA.t[64:128, 7:W_ - 8], op=ALU.add),
                                 reads=[sA.r, sB.r], writes=[sB.r])
                            P.op("dve", lambda e: e.tensor_copy(out=sB.t[0:64, 15:W_], in_=sA.t[0:64, 15:W_]),
                                 reads=[sA.r, sB.r], writes=[sB.r])
                            fin = sB
                        if i == 0:
                            P.op("dve", lambda e: e.tensor_tensor(out=fin.t[:, 16:32], in0=fin.t[:, 16:32],
                                                                  in1=corr.t[:, cc, :], op=ALU.mult),
                                 reads=[fin.r, corr.r], writes=[fin.r])
                        P.op("dve", lambda e: e.scalar_tensor_tensor(
                            out=pooled.t[:], in0=fin.t[:, 16:16 + TS], scalar=prm.t[:, PC_IW + cc:PC_IW + cc + 1],
                            in1=pin_in[k].t[:, cc, 16:16 + TS], op0=ALU.mult, op1=ALU.subtract),
                            reads=[fin.r, prm.r, pin_in[k].r], writes=[pooled.r])
                        bank = next_pf()
                        P.op("pe", lambda e: e.matmul(bank.t[:], pw_sb.t[:, cc, :], pooled.t[:], start=True, stop=True),
                             reads=[pw_sb.r, pooled.r], writes=[bank.r])
                        P.op("act", lambda e: e.activation(out=mx.t[:, 2 + cc, :], in_=bank.t[:], func=AF.Identity,
                                                           scale=prm.t[:, PC_PS + cc:PC_PS + cc + 1],
                                                           bias=pbs.t[:, cc:cc + 1]),
                             reads=[bank.r, prm.r, pbs.r], writes=[mx.r])
                    for cc in range(2):
                        g_ = gcv_in[k]
                        P.op("dve", lambda e: e.tensor_scalar(out=acc3.t[:], in0=g_.t[:, cc, 0:TS],
                                                              scalar1=prm.t[:, PC_SW + cc * 3:PC_SW + cc * 3 + 1],
                                                              scalar2=None, op0=ALU.mult),
                             reads=[g_.r, prm.r], writes=[acc3.r])
                        for j in (1, 2):
                            P.op("dve", lambda e, j=j: e.scalar_tensor_tensor(
                                out=acc3.t[:], in0=g_.t[:, cc, j:j + TS],
                                scalar=prm.t[:, PC_SW + cc * 3 + j:PC_SW + cc * 3 + j + 1], in1=acc3.t[:],
                                op0=ALU.mult, op1=ALU.add), reads=[g_.r, prm.r, acc3.r], writes=[acc3.r])
                        P.op("dve", lambda e: e.tensor_tensor(out=mx.t[:, 4 + cc, :], in0=acc3.t[:],
                                                              in1=gb_in[k].t[:, cc, :], op=ALU.mult),
                             reads=[acc3.r, gb_in[k].r], writes=[mx.r])
                    P.op("act", lambda e: e.activation(out=m2.t[:], in_=bm.t[:], func=AF.Square),
                         reads=[bm.r], writes=[m2.r])
                    P.op("dve", lambda e: e.tensor_tensor(out=var.t[:], in0=bq.t[:], in1=m2.t[:], op=ALU.subtract),
                         reads=[bq.r, m2.r], writes=[var.r])
                    P.op("dve", lambda e: e.tensor_scalar(out=var.t[:], in0=var.t[:], scalar1=0.0, scalar2=EPS,
                                                          op0=ALU.max, op1=ALU.add), reads=[var.r], writes=[var.r])
                    P.op("act", lambda e: e.activation(out=var.t[:], in_=var.t[:], func=AF.Ln),
                         reads=[var.r], writes=[var.r])
                    P.op("act", lambda e: e.activation(out=var.t[:], in_=var.t[:], func=AF.Exp, scale=-0.5),
                         reads=[var.r], writes=[var.r])
                    for cc in range(2):
                        P.op("dve", lambda e: e.tensor_tensor(out=dd.t[:], in0=bm.t[:], in1=yc.t[:, cc, :],
                                                              op=ALU.subtract),
                             reads=[bm.r, yc.res[cc]], writes=[dd.r])
                        P.op("dve", lambda e: e.tensor_tensor(out=dd.t[:], in0=dd.t[:], in1=var.t[:], op=ALU.mult),
                             reads=[dd.r, var.r], writes=[dd.r])
                        P.op("dve", lambda e: e.tensor_scalar(out=dd.t[:], in0=dd.t[:],
                                                              scalar1=prm.t[:, PC_LNG + cc:PC_LNG + cc + 1],
                                                              scalar2=-1.0, op0=ALU.mult, op1=ALU.mult),
                             reads=[dd.r, prm.r], writes=[dd.r])
                        P.op("act", lambda e: e.activation(out=mx.t[:, cc, :], in_=dd.t[:], func=AF.Silu,
                                                           bias=prm.t[:, PC_LNB + cc:PC_LNB + cc + 1]),
                             reads=[dd.r, prm.r], writes=[mx.r])
                    mv = mix_d.rearrange("(c p) t -> p c t", p=128)
                    P.dma("sp", mv[:, 0:4, i * TS:(i + 1) * TS], mx.t[:, 0:4, :], reads=[mx.r], writes=[R_mixA[i]])
                    P.dma("sp", mv[:, 6:8, i * TS:(i + 1) * TS], mx.t[:, 4:6, :], reads=[mx.r], writes=[R_mixA[i]])
                P.barrier()
            if dbg == "s2a":
                st_kv.close()
                st_wout.close()
                st_dg.close()
                break

            with contextlib.ExitStack() as st:
                qTs = [sbuf(st, "at_q%d" % k, [128, 2, TS], BF16) for k in range(2)]
                pts = [sbuf(st, "at_pt%d" % k, [128, TS], BF16) for k in range(4)]
                rz = [sbuf(st, "at_rz%d" % k, [128, TS], F32) for k in range(2)]
                o12 = [sbuf(st, "at_o%d" % k, [128, TS], F32) for k in range(2)]
                od = sbuf(st, "at_od", [128, TS], F32)
                osq = sbuf(st, "at_osq", [128, TS], F32)
                rs = sbuf(st, "at_rs", [128, TS], F32)
                mixB = [sbuf(st, "at_mix%d" % k, [128, 2, TS], BF16) for k in range(2)]
                set_psum(st, 8, 0)
                accb = pf[0:4]
                scb = pf[4:8]
                pts8 = pts + [sbuf(st, "at_ptx%d" % k, [128, TS], BF16) for k in range(4)]
                P.dma("sp", qTs[0].t[:], fm(q_d, 0), reads=[R_q[0]], writes=[qTs[0].r])
                mv = mix_d.rearrange("(c p) t -> p c t", p=128)

                groups = []
                for i in range(NT):
                    nkt = 4 * i + 4
                    for hp in range(2):
                        for kt in range(nkt):
                            groups.append((i, hp, kt, nkt))

                def front(g):
                    i, hp, kt, nkt = groups[g]
                    if hp == 0 and kt == 0 and i + 1 < NT:
                        P.dma("sp", qTs[(i + 1) % 2].t[:], fm(q_d, i + 1), reads=[R_q[i + 1]],
                              writes=[qTs[(i + 1) % 2].r])
                    qT = qTs[i % 2]
                    jd = kt - 4 * i
                    qs = 128 * jd if jd > 0 else 0
                    n = TS - qs
                    for s_ in range(4):
                        po = s_ * 32
                        sc = scb[s_]
                        P.op("pe", lambda e: e.matmul(sc.t[:, 0:n], kT.t[po:po + 32, hp, kt * 128:(kt + 1) * 128],
                                                      qT.t[po:po + 32, hp, qs:TS], start=True, stop=True,
                                                      tile_position=(po, 0)),
                             reads=[kT.r, qT.r], writes=[sc.r])
                    for s_ in range(4):
                        sc = scb[s_]
                        pt = pts8[(g % 2) * 4 + s_]
                        P.op("act", lambda e: e.activation(out=pt.t[:, 0:n], in_=sc.t[:, 0:n], func=AF.Exp, scale=SCALE),
                             reads=[sc.r], writes=[pt.r])
                        if jd >= 0:
                            P.op("dve", lambda e: e.tensor_tensor(out=pt.t[:, 0:128], in0=pt.t[:, 0:128], in1=trib.t[:],
                                                                  op=ALU.mult),
                                 reads=[pt.r, trib.r], writes=[pt.r])

                def back(g):
                    i, hp, kt, nkt = groups[g]
                    jd = kt - 4 * i
                    qs = 128 * jd if jd > 0 else 0
                    n = TS - qs
                    for s_ in range(4):
                        h = 2 * hp + s_ // 2
                        pt = pts8[(g % 2) * 4 + s_]
                        acc = accb[s_]
                        P.op("pe", lambda e: e.matmul(acc.t[:, qs:TS], Vs.t[:, kt, h * 128:(h + 1) * 128], pt.t[:, 0:n],
                                                      start=(kt == 0), stop=(kt == nkt - 1)),
                             reads=[Vs.r, pt.r], writes=[acc.r])
                    if kt == nkt - 1:
                        finalize(i, 2 * hp)
                        finalize(i, 2 * hp + 1)

                def finalize(i, h):
                    ch = h // 2
                    mb = mixB[i % 2]
                    lo, hi = (0, 64) if h % 2 == 0 else (64, 128)
                    zlo, zhi = (64, 128) if h % 2 == 0 else (0, 64)
                    accs = [accb[(h % 2) * 2], accb[(h % 2) * 2 + 1]]
                    for comp in range(2):
                        P.op("act", lambda e: e.activation(out=rz[comp].t[zlo:zhi, :], in_=accs[comp].t[zlo:zhi, :],
                                                           func=AF.Ln),
                             reads=[accs[comp].r], writes=[rz[comp].r])
                        P.op("act", lambda e: e.activation(out=rz[comp].t[zlo:zhi, :], in_=rz[comp].t[zlo:zhi, :],
                                                           func=AF.Exp, scale=-1.0),
                             reads=[rz[comp].r], writes=[rz[comp].r])
                        P.op("dve", lambda e: e.tensor_tensor(out=o12[comp].t[lo:hi, :], in0=accs[comp].t[lo:hi, :],
                                                              in1=rz[comp].t[zlo:zhi, :], op=ALU.mult),
                             reads=[accs[comp].r, rz[comp].r], writes=[o12[comp].r])
                    P.op("dve", lambda e: e.scalar_tensor_tensor(out=od.t[lo:hi, :], in0=o12[1].t[lo:hi, :],
                                                                 scalar=neglam.t[lo:hi, 0:1], in1=o12[0].t[lo:hi, :],
                                                                 op0=ALU.mult, op1=ALU.add),
                         reads=[o12[0].r, o12[1].r, neglam.r], writes=[od.r])
                    if h % 2 == 1:
                        P.op("act", lambda e: e.activation(out=osq.t[:], in_=od.t[:], func=AF.Square),
                             reads=[od.r], writes=[osq.r])
                        bms = scb[3]
                        P.op("pe", lambda e: e.matmul(bms.t[:], blk64.t[:], osq.t[:], start=True, stop=True),
                             reads=[blk64.r, osq.r], writes=[bms.r])
                        P.op("act", lambda e: e.activation(out=rs.t[:], in_=bms.t[:], func=AF.Ln, bias=epsc.t[:, 0:1]),
                             reads=[bms.r, epsc.r], writes=[rs.r])
                        P.op("act", lambda e: e.activation(out=rs.t[:], in_=rs.t[:], func=AF.Exp, scale=-0.5),
                             reads=[rs.r], writes=[rs.r])
                        P.op("dve", lambda e: e.tensor_tensor(out=rs.t[:], in0=rs.t[:], in1=od.t[:], op=ALU.mult),
                             reads=[rs.r, od.r], writes=[rs.r])
                        P.op("dve", lambda e: e.tensor_scalar(out=mb.t[:, ch, :], in0=rs.t[:],
                                                              scalar1=prm.t[:, PC_SUB:PC_SUB + 1],
                                                              scalar2=1.0 - lam_init, op0=ALU.mult, op1=ALU.mult),
                             reads=[rs.r, prm.r], writes=[mb.r])
                    if h == 3:
                        P.dma("sp", mv[:, 4:6, i * TS:(i + 1) * TS], mb.t[:], reads=[mb.r], writes=[R_mixB[i]])

                LA = 1
                for g in range(len(groups) + LA):
                    if g < len(groups):
                        front(g)
                    if g >= LA:
                        back(g - LA)
                P.barrier()
            st_kv.close()
            st_dg.close()
            if dbg == "s2c":
                st_wout.close()
                break

            st_ple = contextlib.ExitStack()
            wpg = sbuf(st_ple, "s5_wpg", [128, 8, D], BF16)
            wpp = sbuf(st_ple, "s5_wpp", [128, 2, D], BF16)
            stm = contextlib.ExitStack()
            wgs = [sbuf(stm, "wgs0", [128, 4, 8, 256], BF16)]
            wus = [sbuf(stm, "wus0", [128, 4, 8, 256], BF16)]
            wds = [sbuf(stm, "wds0", [128, 4, 2, D], BF16)]
            def load_experts(pz, slot):
                for el in range(4):
                    eidx = pz * 4 + el
                    P.dma("pool", wgs[slot].t[:, el, :, :], wg_d[l, eidx].rearrange("(c p) n -> p c n", p=128),
                          writes=[wgs[slot].r])
                    P.dma("pool", wus[slot].t[:, el, :, :], wu_d[l, eidx].rearrange("(c p) n -> p c n", p=128),
                          writes=[wus[slot].r])
                    P.dma("pool", wds[slot].t[:, el, :, :], wd_d[l, eidx].rearrange("(c p) n -> p c n", p=128),
                          writes=[wds[slot].r])

            with contextlib.ExitStack() as st:
                set_psum(st, 6, 2)
                wrg = sbuf(st, "s3_wrg", [128, 8, 20], F32)
                rbias = sbuf(st, "s3_rb", [128, 20], F32)
                hts = [sbuf(st, "s3_ht%d" % k, [128, 4, D], F32) for k in range(2)]
                mxs = [sbuf(st, "s3_mx%d" % k, [128, 8, TS], BF16) for k in range(2)]
                ss2 = [sbuf(st, "s3_ss%d" % k, [128, 4], F32) for k in range(2)]
                rstd2 = [sbuf(st, "s3_rstd%d" % k, [128, 4], F32) for k in range(2)]
                sqj = sbuf(st, "s3_sqj", [128, D], BF16)
                xn2 = [sbuf(st, "s3_xn%d" % k, [128, 4, D], BF16, 4) for k in range(2)]
                xT2 = [sbuf(st, "s3_xT%d" % k, [128, 8, TS], BF16, 8) for k in range(2)]
                hTf = sbuf(st, "s3_hTf", [128, 8, 128], F32, 2)
                lg = sbuf(st, "s3_lg", [128, 20], F32)
                lg4 = sbuf(st, "s3_lg4", [128, 4, 20], F32)
                r4 = sbuf(st, "s3_r4", [128, 8, 4], F32)
                mg4 = sbuf(st, "s3_mg4", [128, 4, 4], F32)
                t4 = sbuf(st, "s3_t4", [128, 4, 4], F32)
                es4 = sbuf(st, "s3_es4", [128, 4, 4], F32)
                eq4 = sbuf(st, "s3_eq4", [128, 4, 4], F32)
                pr4 = sbuf(st, "s3_pr4", [128, 4, 4, 4], F32)
                sm = sbuf(st, "s3_sm", [128, 16], F32)
                mg = sbuf(st, "s3_mg", [128, 4], F32)
                ge = sbuf(st, "s3_ge", [128, 4], F32)
                esel = sbuf(st, "s3_esel", [128, 4], F32)
                eq = sbuf(st, "s3_eq", [128, 4], F32)
                em2 = sbuf(st, "s3_em2", [128, 4], F32)
                ee = sbuf(st, "s3_ee", [128, 4], F32)
                wsel = sbuf(st, "s3_wsel", [128, 4], F32)
                comb = sbuf(st, "s3_comb", [128, 4, 16], F32)
                cmbT = sbuf(st, "s3_cmbT", [16, TS], F32)

                load_experts(0, 0)
                P.dma("sp", wrg.t[:], wr_d[l], writes=[wrg.r])
                P.dma("sp", rbias.t[:], rb_d[l], writes=[rbias.r])
                for c in range(8):
                    P.op("dve", lambda e, c=c: e.tensor_scalar(out=wrg.t[:, c, :], in0=wrg.t[:, c, :],
                                                               scalar1=prm.t[:, PC_FFNG + c:PC_FFNG + c + 1],
                                                               scalar2=None, op0=ALU.mult),
                         reads=[wrg.r, prm.r], writes=[wrg.r])

                def s3_load(i):
                    k = i % 2
                    P.dma("sp", hts[k].t[:], tm(h_src, i), reads=[R_h[i]], writes=[hts[k].r])
                    P.dma("sp", mxs[k].t[:], fm(mix_d, i), reads=[R_mixA[i], R_mixB[i]], writes=[mxs[k].r])

                def s3_A(i):
                    k = i % 2
                    ht = hts[k]
                    mx = mxs[k]
                    ss, rstd, xn, xT = ss2[k], rstd2[k], xn2[k], xT2[k]
                    for j in range(4):
                        for half in range(2):
                            bank = next_pf()
                            mm_group(bank, (0, 512), [mx.t[:, c, j * 128:(j + 1) * 128] for c in range(8)],
                                     [w_out_sb.t[:, c, half * 512:(half + 1) * 512] for c in range(8)],
                                     [[mx.r, w_out_sb.r]] * 8)
                            P.op("dve", lambda e: e.tensor_tensor(out=ht.t[:, j, half * 512:(half + 1) * 512],
                                                                  in0=bank.t[:], in1=ht.t[:, j, half * 512:(half + 1) * 512],
                                                                  op=ALU.add),
                                 reads=[bank.r, ht.r], writes=[ht.r])
                    P.dma("sp", tm(h_d, i), ht.t[:], reads=[ht.r], writes=[R_h[i]])
                    norm_stats(st, ht, 0, ss, rstd, sqj)
                    norm_transpose(ht, 0, rstd, xn, xT, PC_FFNG)
                    P.dma("sp", fm(xT_d, i), xT.t[:], reads=xT.res, writes=[R_xT[i]])

                def s3_B(i):
                    k = i % 2
                    ht = hts[k]
                    ss, rstd, xn, xT = ss2[k], rstd2[k], xn2[k], xT2[k]
                    bct = next_pf()
                    for j in range(4):
                        for c2 in range(2):
                            bank = next_pf()
                            while bank is bct:
                                bank = next_pf()
                            for cq in range(4):
                                c = c2 * 4 + cq
                                P.op("pe", lambda e: e.transpose(bank.t[:, cq * 128:(cq + 1) * 128],
                                                                 ht.t[:, j, c * 128:(c + 1) * 128], identf.t[:]),
                                     reads=[ht.r, identf.r], writes=[bank.r], signal=(cq == 3))
                            if c2 == 0:
                                P.op("act", lambda e: e.copy(out=hTf.t[:, 0:4, :], in_=bank.t[:].rearrange("p (a b) -> p a b", a=4)),
                                     reads=[bank.r], writes=[hTf.res[0]])
                            else:
                                P.op("dve", lambda e: e.tensor_copy(out=hTf.t[:, 4:8, :], in_=bank.t[:].rearrange("p (a b) -> p a b", a=4)),
                                     reads=[bank.r], writes=[hTf.res[1]])
                        bl = next_pf()
                        while bl is bct:
                            bl = next_pf()
                        mm_group(bl, (0, 20), [hTf.t[:, c, :] for c in range(8)], [wrg.t[:, c, :] for c in range(8)],
                                 [[hTf.res[c // 4], wrg.r] for c in range(8)])
                        P.op("dve", lambda e: e.scalar_tensor_tensor(out=lg4.t[:, j, :], in0=bl.t[:, 0:20],
                                                                     scalar=rstd.t[:, j:j + 1], in1=rbias.t[:],
                                                                     op0=ALU.mult, op1=ALU.add),
                             reads=[bl.r, rstd.r, rbias.r], writes=[lg4.r])
                    V = lambda fn, rd, wr: P.op("dve", fn, reads=rd, writes=wr)
                    S3 = [128, 4, 4]
                    bc = lambda ap_: ap_.unsqueeze(2).to_broadcast(S3)
                    glg = lg4.t[:, :, 0:4]
                    V(lambda e: e.reduce_max(out=r4.t[:, 0, :], in_=glg, axis=AX.X), [lg4.r], [r4.r])
                    V(lambda e: e.tensor_tensor(out=mg4.t[:], in0=glg, in1=bc(r4.t[:, 0, :]), op=ALU.is_ge),
                      [lg4.r, r4.r], [mg4.r])
                    V(lambda e: e.tensor_tensor(out=t4.t[:], in0=glg, in1=bc(r4.t[:, 0, :]), op=ALU.subtract),
                      [lg4.r, r4.r], [t4.r])
                    P.op("act", lambda e: e.activation(out=t4.t[:], in_=t4.t[:], func=AF.Exp), reads=[t4.r], writes=[t4.r])
                    V(lambda e: e.reduce_sum(out=r4.t[:, 1, :], in_=t4.t[:], axis=AX.X), [t4.r, r4.r], [r4.r])
                    V(lambda e: e.reciprocal(out=r4.t[:, 2, :], in_=r4.t[:, 1, :]), [r4.r], [r4.r])
                    el4 = lg4.t[:, :, 4:20].rearrange("p j (g i) -> p j g i", g=4)
                    V(lambda e: e.tensor_tensor(out=pr4.t[:], in0=el4,
                                                in1=mg4.t[:].unsqueeze(3).to_broadcast([128, 4, 4, 4]), op=ALU.mult),
                      [lg4.r, mg4.r], [pr4.r])
                    V(lambda e: e.reduce_sum(out=es4.t[:], in_=pr4.t[:].rearrange("p j g i -> p j i g"), axis=AX.X),
                      [pr4.r], [es4.r])
                    V(lambda e: e.reduce_max(out=r4.t[:, 3, :], in_=es4.t[:], axis=AX.X), [es4.r, r4.r], [r4.r])
                    V(lambda e: e.tensor_tensor(out=eq4.t[:], in0=es4.t[:], in1=bc(r4.t[:, 3, :]), op=ALU.is_ge),
                      [es4.r, r4.r], [eq4.r])
                    V(lambda e: e.scalar_tensor_tensor(out=t4.t[:], in0=eq4.t[:], scalar=-1e30, in1=es4.t[:],
                                                       op0=ALU.mult, op1=ALU.add), [eq4.r, es4.r, t4.r], [t4.r])
                    V(lambda e: e.reduce_max(out=r4.t[:, 4, :], in_=t4.t[:], axis=AX.X), [t4.r, r4.r], [r4.r])
                    V(lambda e: e.tensor_tensor(out=eq4.t[:], in0=es4.t[:], in1=bc(r4.t[:, 4, :]), op=ALU.is_ge),
                      [es4.r, r4.r, eq4.r], [eq4.r])
                    V(lambda e: e.tensor_tensor(out=t4.t[:], in0=es4.t[:], in1=bc(r4.t[:, 3, :]), op=ALU.subtract),
                      [es4.r, r4.r, t4.r], [t4.r])
                    P.op("act", lambda e: e.activation(out=t4.t[:], in_=t4.t[:], func=AF.Exp), reads=[t4.r], writes=[t4.r])
                    V(lambda e: e.tensor_tensor(out=t4.t[:], in0=t4.t[:], in1=eq4.t[:], op=ALU.mult),
                      [t4.r, eq4.r], [t4.r])
                    V(lambda e: e.reduce_sum(out=r4.t[:, 5, :], in_=t4.t[:], axis=AX.X), [t4.r, r4.r], [r4.r])
                    V(lambda e: e.reciprocal(out=r4.t[:, 6, :], in_=r4.t[:, 5, :]), [r4.r], [r4.r])
                    V(lambda e: e.tensor_tensor(out=r4.t[:, 6, :], in0=r4.t[:, 6, :], in1=r4.t[:, 2, :], op=ALU.mult),
                      [r4.r], [r4.r])
                    V(lambda e: e.tensor_tensor(out=t4.t[:], in0=t4.t[:], in1=bc(r4.t[:, 6, :]), op=ALU.mult),
                      [t4.r, r4.r], [t4.r])
                    V(lambda e: e.tensor_tensor(out=comb.t[:].rearrange("p j (g i) -> p j g i", g=4),
                                                in0=t4.t[:].unsqueeze(2).to_broadcast([128, 4, 4, 4]),
                                                in1=mg4.t[:].unsqueeze(3).to_broadcast([128, 4, 4, 4]), op=ALU.mult),
                      [t4.r, mg4.r], [comb.r])
                    for j in range(4):
                        P.op("pe", lambda e: e.transpose(bct.t[0:16, j * 128:(j + 1) * 128], comb.t[:, j, :], identf.t[:]),
                             reads=[comb.r, identf.r], writes=[bct.r])
                    P.op("act", lambda e: e.copy(out=cmbT.t[:], in_=bct.t[0:16, :]), reads=[bct.r], writes=[cmbT.r])
                    P.dma("sp", cmb_d[:, i * TS:(i + 1) * TS], cmbT.t[:], reads=[cmbT.r], writes=[R_cmb[i]])

                s3_load(0)
                if NT > 1:
                    s3_load(1)
                s3_A(0)
                for i in range(NT):
                    if i + 1 < NT:
                        s3_A(i + 1)
                    s3_B(i)
                    if i + 2 < NT:
                        s3_load(i + 2)
                P.barrier()
            st_wout.close()
            if dbg == "s3":
                stm.close()
                st_ple.close()
                break

            wgs.append(sbuf(stm, "wgs1", [128, 4, 8, 256], BF16))
            wus.append(sbuf(stm, "wus1", [128, 4, 8, 256], BF16))
            wds.append(sbuf(stm, "wds1", [128, 4, 2, D], BF16))
            with contextlib.ExitStack() as st:
                set_psum(st, 8, 0)
                hts = [sbuf(st, "s4_ht%d" % k, [128, 4, D], F32) for k in range(2)]
                xTs = [sbuf(st, "s4_xT%d" % k, [128, 8, TS], BF16) for k in range(2)]
                cms = [sbuf(st, "s4_cm%d" % k, [16, TS], F32) for k in range(2)]
                cbs = sbuf(st, "s4_cb", [128, TS], F32)
                sg_ = sbuf(st, "s4_sg", [128, TS], F32)
                tt_ = sbuf(st, "s4_tt", [128, TS], F32)
                hdn = sbuf(st, "s4_hdn", [128, 4, 2, TS], BF16)

                def s4_load(i):
                    k = i % 2
                    P.dma("sp", hts[k].t[:], tm(h_d, i), reads=[R_h[i]], writes=[hts[k].r])
                    P.dma("sp", xTs[k].t[:], fm(xT_d, i), reads=[R_xT[i]], writes=[xTs[k].r])
                    P.dma("sp", cms[k].t[:], cmb_d[:, i * TS:(i + 1) * TS], reads=[R_cmb[i]], writes=[cms[k].r])

                s4_load(0)
                for pz in range(4):
                    slot = pz % 2
                    if pz + 1 < 4:
                        load_experts(pz + 1, (pz + 1) % 2)
                    if pz == 0:
                        for c in range(8):
                            P.dma("pool", wpg.t[:, c, :], pgw_d[l, c * 128:(c + 1) * 128, :], writes=[wpg.r])
                        P.dma("pool", wpp.t[:], ppj_d[l].rearrange("(c p) n -> p c n", p=128), writes=[wpp.r])
                    for i in range(NT):
                        k = i % 2
                        if i + 1 < NT:
                            s4_load(i + 1)
                        elif pz + 1 < 4:
                            s4_load(0)
                        ht, xT_, cm = hts[k], xTs[k], cms[k]
                        for el in range(4):
                            eidx = pz * 4 + el
                            bcb = next_pf()
                            P.op("pe", lambda e: e.matmul(bcb.t[:], sel.t[0:16, eidx, :], cm.t[0:16, :],
                                                          start=True, stop=True),
                                 reads=[sel.r, cm.r], writes=[bcb.r])
                            P.op("act", lambda e: e.copy(out=cbs.t[:], in_=bcb.t[:]), reads=[bcb.r], writes=[cbs.r])
                            for hc in range(2):
                                bg_ = next_pf()
                                mm_group(bg_, (0, TS), [wgs[slot].t[:, el, c, hc * 128:(hc + 1) * 128] for c in range(8)],
                                         [xT_.t[:, c, :] for c in range(8)], [[wgs[slot].r, xT_.r]] * 8)
                                bu_ = next_pf()
                                mm_group(bu_, (0, TS), [wus[slot].t[:, el, c, hc * 128:(hc + 1) * 128] for c in range(8)],
                                         [xT_.t[:, c, :] for c in range(8)], [[wus[slot].r, xT_.r]] * 8)
                                P.op("act", lambda e: e.activation(out=sg_.t[:], in_=bg_.t[:], func=AF.Silu),
                                     reads=[bg_.r], writes=[sg_.r])
                                P.op("dve", lambda e: e.tensor_tensor(out=tt_.t[:], in0=bu_.t[:], in1=cbs.t[:],
                                                                      op=ALU.mult),
                                     reads=[bu_.r, cbs.r], writes=[tt_.r])
                                P.op("dve", lambda e: e.tensor_tensor(out=hdn.t[:, el, hc, :], in0=sg_.t[:], in1=tt_.t[:],
                                                                      op=ALU.mult),
                                     reads=[sg_.r, tt_.r], writes=[hdn.r])
                        for j in range(4):
                            for half in range(2):
                                by = next_pf()
                                mm_group(by, (0, 512),
                                         [hdn.t[:, el, hc, j * 128:(j + 1) * 128] for el in range(4) for hc in range(2)],
                                         [wds[slot].t[:, el, hc, half * 512:(half + 1) * 512]
                                          for el in range(4) for hc in range(2)],
                                         [[hdn.r, wds[slot].r]] * 8)
                                P.op("dve", lambda e: e.tensor_tensor(out=ht.t[:, j, half * 512:(half + 1) * 512],
                                                                      in0=by.t[:],
                                                                      in1=ht.t[:, j, half * 512:(half + 1) * 512],
                                                                      op=ALU.add),
                                     reads=[by.r, ht.r], writes=[ht.r])
                        P.dma("sp", tm(h_d, i), ht.t[:], reads=[ht.r], writes=[R_h[i]])
                P.barrier()
            stm.close()
            if dbg == "s4":
                st_ple.close()
                break

            with contextlib.ExitStack() as st:
                set_psum(st, 6, 2)
                gbf = sbuf(st, "s5_gbf", [1, D], F32)
                gbb = sbuf(st, "s5_gbb", [1, D], BF16)
                hts = [sbuf(st, "s5_ht%d" % k, [128, 4, D], F32) for k in range(3)]
                pts_ = [sbuf(st, "s5_p%d" % k, [128, 4, 256], F32) for k in range(3)]
                ss2 = [sbuf(st, "s5_ss%d" % k, [128, 4], F32) for k in range(2)]
                rstd2 = [sbuf(st, "s5_rstd%d" % k, [128, 4], F32) for k in range(2)]
                sqj = sbuf(st, "s5_sqj", [128, D], BF16)
                xn2 = [sbuf(st, "s5_xn%d" % k, [128, 4, D], BF16, 4) for k in range(2)]
                xT2 = [sbuf(st, "s5_xT%d" % k, [128, 8, TS], BF16, 8) for k in range(2)]
                pT2 = [sbuf(st, "s5_pT%d" % k, [128, 2, TS], BF16) for k in range(2)]
                gts = [sbuf(st, "s5_gt%d" % k, [128, 512], F32) for k in range(3)]
                tqs = [sbuf(st, "s5_tq%d" % k, [128, 512], F32) for k in range(3)]
                if l + 1 < DEPTH:
                    st_win, w_in_next = load_w_in(l + 1)
                P.dma("sp", gbf.t[:], pgb_d[l:l + 1, :], writes=[gbf.r])
                P.op("dve", lambda e: e.tensor_copy(out=gbb.t[:], in_=gbf.t[:]), reads=[gbf.r], writes=[gbb.r])

                def s5_load(i):
                    k = i % 3
                    P.dma("sp", hts[k].t[:], tm(h_d, i), reads=[R_h[i]], writes=[hts[k].r])
                    P.dma("sp", pts_[k].t[:], tm(p_d[l], i), writes=[pts_[k].r])

                def s5_P(i):
                    k = i % 2
                    ht, pt_ = hts[i % 3], pts_[i % 3]
                    ss, rstd, xn, xT, pT = ss2[k], rstd2[k], xn2[k], xT2[k], pT2[k]
                    norm_stats(st, ht, 0, ss, rstd, sqj)
                    norm_transpose(ht, 0, rstd, xn, xT, PC_PLEG)
                    for c in range(2):
                        bank = next_pf()
                        for j in range(4):
                            P.op("pe", lambda e: e.transpose(bank.t[:, j * 128:(j + 1) * 128],
                                                             pt_.t[:, j, c * 128:(c + 1) * 128], identf.t[:]),
                                 reads=[pt_.r, identf.r], writes=[bank.r], signal=(j == 3))
                        P.op("act", lambda e: e.copy(out=pT.t[:, c, :], in_=bank.t[:]), reads=[bank.r], writes=[pT.r])

                def s5_M(i):
                    k = i % 2
                    ht, pt_ = hts[i % 3], pts_[i % 3]
                    ss, rstd, xn, xT, pT = ss2[k], rstd2[k], xn2[k], xT2[k], pT2[k]
                    for j in range(4):
                        for half in range(2):
                            hs = slice(half * 512, (half + 1) * 512)
                            gt = gts[(j * 2 + half) % 3]
                            tq = tqs[(j * 2 + half) % 3]
                            bg_ = next_pf()
                            mm_group(bg_, (0, 512),
                                     [xT.t[:, c, j * 128:(j + 1) * 128] for c in range(8)] + [onesrow.t[0:1, :]],
                                     [wpg.t[:, c, hs] for c in range(8)] + [gbb.t[0:1, hs]],
                                     [[xT.res[c], wpg.r] for c in range(8)] + [[onesrow.r, gbb.r]])
                            bp_ = next_pf()
                            mm_group(bp_, (0, 512), [pT.t[:, c, j * 128:(j + 1) * 128] for c in range(2)],
                                     [wpp.t[:, c, hs] for c in range(2)], [[pT.r, wpp.r]] * 2)
                            P.op("act", lambda e: e.activation(out=gt.t[:], in_=bg_.t[:], func=AF.Sigmoid),
                                 reads=[bg_.r], writes=[gt.r])
                            P.op("dve", lambda e: e.tensor_tensor(out=tq.t[:], in0=bp_.t[:], in1=gt.t[:], op=ALU.mult),
                                 reads=[bp_.r, gt.r], writes=[tq.r])
                            P.op("dve", lambda e: e.tensor_tensor(out=ht.t[:, j, hs], in0=ht.t[:, j, hs], in1=tq.t[:],
                                                                  op=ALU.add),
                                 reads=[ht.r, tq.r], writes=[ht.r])
                    if l < DEPTH - 1:
                        P.dma("sp", tm(h_d, i), ht.t[:], reads=[ht.r], writes=[R_h[i]])
                    else:
                        norm_stats(st, ht, 0, ss, rstd, sqj)
                        for j in range(4):
                            P.op("dve", lambda e: e.scalar_tensor_tensor(out=ht.t[:, j, :], in0=ht.t[:, j, :],
                                                                         scalar=rstd.t[:, j:j + 1], in1=gfin.t[:],
                                                                         op0=ALU.mult, op1=ALU.mult),
                                 reads=[ht.r, rstd.r, gfin.r], writes=[ht.r])
                        P.dma("sp", tm(out_d, i), ht.t[:], reads=[ht.r], writes=[R_out[i]])

                s5_load(0)
                if NT > 1:
                    s5_load(1)
                s5_P(0)
                for i in range(NT):
                    if i + 2 < NT:
                        s5_load(i + 2)
                    if i + 1 < NT:
                        s5_P(i + 1)
                    s5_M(i)
                P.barrier()
            st_ple.close()

        P.barrier()
        P.final_wait()
    return P


def _host_consts():
    c = {}
    c["c_identf"] = np.eye(128, dtype=np.float32)
    rp = np.zeros((128, 128), np.float32)
    for b in range(4):
        for d in range(16):
            rp[b * 32 + d + 16, b * 32 + d] = -1.0
            rp[b * 32 + d, b * 32 + d + 16] = 1.0
    c["c_rperm"] = rp
    kk = np.arange(128)[:, None]
    qq = np.arange(128)[None, :]
    c["c_tri"] = (qq >= kk).astype(np.float32)
    b64 = np.zeros((128, 128), np.float32)
    b64[0:64, 0:64] = 1.0 / 64
    b64[64:128, 64:128] = 1.0 / 64
    c["c_blk64"] = b64
    c["c_ones256"] = np.full((128, 128), 1.0 / 256, np.float32)
    sel = np.zeros((16, NE, 128), np.float32)
    for e in range(NE):
        sel[e, e, :] = 1.0
    c["c_sel"] = sel
    pos = np.arange(S, dtype=np.float32)
    inv = (10000.0 ** (-np.arange(0, 32, 2, dtype=np.float32) / np.float32(32))).astype(np.float32)
    ang = (pos[:, None] * inv[None, :]).astype(np.float32)
    ang = np.concatenate([ang, ang], axis=-1)
    cosT = np.cos(ang.astype(np.float64)).astype(np.float32).T
    sinT = np.sin(ang.astype(np.float64)).astype(np.float32).T
    c["c_cos"] = np.ascontiguousarray(np.tile(cosT, (4, 1)))
    c["c_sin"] = np.ascontiguousarray(np.tile(sinT, (4, 1)))
    wins = np.array([2, 4, 8, 16])
    corr = np.zeros((128, 2, 16), np.float32)
    for cc in range(2):
        for p in range(128):
            w = wins[cc * 2 + p // 64]
            for t in range(16):
                corr[p, cc, t] = w / min(t + 1, w)
    c["c_corr"] = corr
    iw = np.zeros((128, 2), np.float32)
    for cc in range(2):
        for p in range(128):
            iw[p, cc] = 1.0 / wins[cc * 2 + p // 64]
    c["_iw"] = iw
    return c


def _fmcols(v, nch):
    return np.ascontiguousarray(np.asarray(v, np.float32).reshape(nch, 128).T)


def _layout_inputs(inp):
    c = _host_consts()
    iw = c.pop("_iw")
    shared = dict(c)
    prm = np.zeros((DEPTH, 128, PC_N), np.float32)
    wr = np.zeros((DEPTH, 128, 8, 20), np.float32)
    rb = np.zeros((DEPTH, 128, 20), np.float32)
    lamv = np.zeros((DEPTH, 128, 4, 32), np.float32)
    pw = np.zeros((DEPTH, 128, 2, 128), np.float32)
    for l in range(DEPTH):
        prm[l, :, PC_MIXG:PC_MIXG + 8] = _fmcols(inp["mix_norm"][l], 8)
        prm[l, :, PC_FFNG:PC_FFNG + 8] = _fmcols(inp["ffn_norm"][l], 8)
        prm[l, :, PC_PLEG:PC_PLEG + 8] = _fmcols(inp["ple_norm"][l], 8)
        prm[l, :, PC_FING:PC_FING + 8] = _fmcols(inp["final_norm"], 8)
        cw = np.asarray(inp["conf_conv_w"][l], np.float32)
        for cc in range(2):
            prm[l, :, PC_CW + cc * CK:PC_CW + (cc + 1) * CK] = cw[:, cc * 128:(cc + 1) * 128].T
        prm[l, :, PC_CB:PC_CB + 2] = _fmcols(inp["conf_conv_b"][l], 2)
        prm[l, :, PC_LNG:PC_LNG + 2] = _fmcols(inp["conf_ln_g"][l], 2)
        prm[l, :, PC_LNB:PC_LNB + 2] = _fmcols(inp["conf_ln_b"][l], 2)
        prm[l, :, PC_PB:PC_PB + 2] = _fmcols(np.asarray(inp["pool_b"][l]).reshape(256), 2)
        prm[l, :, PC_PS:PC_PS + 2] = _fmcols(inp["pool_scale"][l], 2)
        sw = np.asarray(inp["sconv_w"][l], np.float32)
        for cc in range(2):
            prm[l, :, PC_SW + cc * 3:PC_SW + (cc + 1) * 3] = sw[:, cc * 128:(cc + 1) * 128].T
        prm[l, :, PC_SUB] = np.tile(np.asarray(inp["diff_subln_g"][l], np.float32), 2)
        prm[l, :, PC_IW:PC_IW + 2] = iw
        wcat = np.concatenate([np.asarray(inp["router_group_w"][l], np.float32),
                               np.asarray(inp["router_expert_w"][l], np.float32)], axis=1)
        wr[l] = wcat.reshape(8, 128, 20).transpose(1, 0, 2)
        bcat = np.concatenate([np.asarray(inp["router_group_b"][l], np.float32),
                               np.asarray(inp["router_expert_b"][l], np.float32)])
        rb[l] = np.tile(bcat[None, :], (128, 1))
        for n_, key in enumerate(["diff_lam_q1", "diff_lam_k1", "diff_lam_q2", "diff_lam_k2"]):
            lamv[l, :, n_, :] = np.tile(np.asarray(inp[key][l], np.float32)[None, :], (128, 1))
        pwl = np.asarray(inp["pool_w"][l], np.float32)
        for g in range(4):
            cc, hh = g // 2, g % 2
            pw[l, hh * 64:(hh + 1) * 64, cc, hh * 64:(hh + 1) * 64] = pwl[g]
    shared.update({
        "prm": prm, "wr": wr, "rbias": rb, "lamv": lamv, "poolw": pw,
        "gfin": np.ascontiguousarray(np.tile(np.asarray(inp["final_norm"], np.float32)[None, :], (128, 1))),
    })
    for key in ["w_in", "w_out", "expert_w_gate", "expert_w_up", "expert_w_down", "ple_gate_w", "ple_gate_b",
                "ple_proj"]:
        shared[key] = np.ascontiguousarray(np.asarray(inp[key], np.float32))
    return shared


_NC_CACHE = {}


def _get_nc():
    if "nc" not in _NC_CACHE:
        nc = bass.Bass("TRN2", target_bir_lowering=False)
        build(nc)
        _NC_CACHE["nc"] = nc
    return _NC_CACHE["nc"]


def kernel(**inputs):
    shared = _layout_inputs(inputs)
    x = np.asarray(inputs["x"], np.float32)
    p = np.asarray(inputs["p"], np.float32)
    n = x.shape[0]
    in_maps = []
    for b in range(n):
        m = dict(shared)
        m["x"] = np.ascontiguousarray(x[b])
        m["p"] = np.ascontiguousarray(p[:, b])
        in_maps.append(m)
    nc = _get_nc()
    res = run_bass_kernel_spmd(nc, in_maps, core_ids=list(range(n)))
    return np.stack([np.asarray(r["out"], np.float32) for r in res.results], axis=0)
```

```python
import math
import contextlib
import numpy as np
import ml_dtypes
import concourse.bass as bass
import concourse.mybir as mybir
from concourse.bass_utils import run_bass_kernel_spmd

F32 = mybir.dt.float32
BF16 = mybir.dt.bfloat16
ALU = mybir.AluOpType
AF = mybir.ActivationFunctionType
AX = mybir.AxisListType

S = 4096
D = 1024
DEPTH = 2
NT = 8
TS = 512
INC = 2304
NE = 16
EPS = 1e-6
CK = 31
SCALE = 32 ** -0.5
SEM_CHUNK = 30000

PC_MIXG, PC_FFNG, PC_PLEG, PC_FING = 0, 8, 16, 24
PC_CW = 32
PC_CB = PC_CW + 62
PC_LNG = PC_CB + 2
PC_LNB = PC_LNG + 2
PC_PB = PC_LNB + 2
PC_PS = PC_PB + 2
PC_SW = PC_PS + 2
PC_SUB = PC_SW + 6
PC_IW = PC_SUB + 1
PC_N = PC_IW + 2


class Tok:
    __slots__ = ("sem", "val", "eng")

    def __init__(self, eng):
        self.sem = None
        self.val = None
        self.eng = eng


class Res:
    __slots__ = ("name", "w", "r", "excl")

    def __init__(self, name, excl=False):
        self.name = name
        self.w = None
        self.r = []
        self.excl = excl


class Prog:
    def __init__(self, nc, es):
        self.nc = nc
        self.es = es
        self.eobj = {"pe": nc.tensor, "act": nc.scalar, "dve": nc.vector,
                     "pool": nc.gpsimd, "sp": nc.sync}
        self.sems = {e: [] for e in self.eobj}
        self.count = {e: 0 for e in self.eobj}
        self.known = {e: {} for e in self.eobj}
        self.pending = {e: None for e in self.eobj}
        self.last_tok = {e: None for e in self.eobj}
        self.nsem = 0
        self.dma_sems = []
        self.dma_cnt = []
        self.dma_i = 0
        self.dma_ip = 0
        for i in range(24):
            self.dma_sems.append(self._new_sem("dq%d" % i))
            self.dma_cnt.append(0)
        self.dma_toks = []
        self.n_ops = 0
        self.n_waits = 0
        self.limit = None
        self.stores_on_pool = False
        self.n_all = 0
        self.skip = False

    def _skipping(self):
        if self.skip:
            return True
        if self.limit is not None and self.n_all > self.limit and all(v is None for v in self.pending.values()):
            self.skip = True
            return True
        return False

    def _new_sem(self, name):
        self.nsem += 1
        return self.es.enter_context(self.nc.semaphore(name))

    def _eng_tok(self, eng, tok):
        c = self.count[eng]
        idx = c // SEM_CHUNK
        while len(self.sems[eng]) <= idx:
            self.sems[eng].append(self._new_sem("%s%d" % (eng, len(self.sems[eng]))))
        tok.sem = self.sems[eng][idx]
        tok.val = c % SEM_CHUNK + 1
        self.count[eng] = c + 1
        return tok

    def _wait(self, eng, tok):
        if tok is None:
            return
        if tok.sem is None:
            assert tok.eng == eng, "dependency on unsignaled op of %s from %s" % (tok.eng, eng)
            return
        k = self.known[eng]
        sid = id(tok.sem)
        if k.get(sid, 0) >= tok.val:
            return
        k[sid] = tok.val
        self.eobj[eng].wait_ge(tok.sem, tok.val)
        self.n_waits += 1

    def _deps(self, eng, reads, writes, is_dma=False):
        for r in reads:
            if r.w is not None:
                if r.w.eng == eng and not is_dma and eng == "pe":
                    continue
                self._wait(eng, r.w)
        same_ok = (eng == "pe") and not is_dma
        for w in writes:
            if w.w is not None and not (same_ok and w.w.eng == eng):
                self._wait(eng, w.w)
            for t in w.r:
                if not (same_ok and t.eng == eng):
                    self._wait(eng, t)

    def op(self, eng, fn, reads=(), writes=(), signal=True):
        self.n_all += 1
        if self._skipping():
            return None
        if any(r.excl for r in reads):
            writes = list(writes) + [r for r in reads if r.excl and r not in writes]
            reads = [r for r in reads if not r.excl]
        self._deps(eng, reads, writes)
        ins = fn(self.eobj[eng])
        self.n_ops += 1
        tok = self.pending[eng]
        if tok is None:
            tok = Tok(eng)
            self.pending[eng] = tok
        if signal:
            self._eng_tok(eng, tok)
            ins.then_inc(tok.sem, 1)
            self.pending[eng] = None
            self.last_tok[eng] = tok
        for r in reads:
            r.r.append(tok)
        for w in writes:
            w.w = tok
            w.r = []
        return tok

    def dma(self, q, out, in_, reads=(), writes=()):
        if q == "sp" and self.stores_on_pool and str(out.space).endswith("DRAM"):
            q = "pool"
        self.n_all += 1
        if self._skipping():
            return None
        self._deps(q, reads, writes, is_dma=True)
        if q == "pool":
            i = 16 + self.dma_ip % 8
            self.dma_ip += 1
        else:
            i = self.dma_i % 16
            self.dma_i += 1
        sem = self.dma_sems[i]
        if self.dma_cnt[i] > 0:
            prev = Tok("dma")
            prev.sem = sem
            prev.val = self.dma_cnt[i]
            self._wait(q, prev)
        self.eobj[q].dma_start(out=out, in_=in_).then_inc(sem, 16)
        self.dma_cnt[i] += 16
        tok = Tok("dma")
        tok.sem = sem
        tok.val = self.dma_cnt[i]
        self.dma_toks.append(tok)
        for r in reads:
            r.r.append(tok)
        for w in writes:
            w.w = tok
            w.r = []
        return tok

    def barrier(self):
        toks = [t for t in self.last_tok.values() if t is not None]
        for i, s in enumerate(self.dma_sems):
            if self.dma_cnt[i] > 0:
                t = Tok("dma")
                t.sem = s
                t.val = self.dma_cnt[i]
                toks.append(t)
        for e in self.eobj:
            assert self.pending[e] is None
            for t in toks:
                if t.eng == e:
                    continue
                self._wait(e, t)

    def final_wait(self):
        for i, s in enumerate(self.dma_sems):
            if self.dma_cnt[i] > 0:
                t = Tok("dma")
                t.sem = s
                t.val = self.dma_cnt[i]
                self._wait("sp", t)


class Buf:
    def __init__(self, t, name, nslots=1):
        self.t = t
        self.res = [Res("%s.%d" % (name, i)) for i in range(nslots)]

    @property
    def r(self):
        return self.res[0]


def build(nc, dbg=None, limit=None):
    P = None
    with contextlib.ExitStack() as es:
        P = Prog(nc, es)
        P.limit = limit

        def dram_in(name, shape, dt=F32):
            return nc.dram_tensor(name, list(shape), dt, kind="ExternalInput").ap()

        def dram_scr(name, shape, dt, kind="Internal"):
            return nc.dram_tensor(name, list(shape), dt, kind=kind).ap()

        x_d = dram_in("x", [S, D])
        p_d = dram_in("p", [DEPTH, S, 256])
        w_in_d = dram_in("w_in", [DEPTH, D, INC])
        w_out_d = dram_in("w_out", [DEPTH, D, D])
        wg_d = dram_in("expert_w_gate", [DEPTH, NE, D, 256])
        wu_d = dram_in("expert_w_up", [DEPTH, NE, D, 256])
        wd_d = dram_in("expert_w_down", [DEPTH, NE, 256, D])
        pgw_d = dram_in("ple_gate_w", [DEPTH, D, D])
        pgb_d = dram_in("ple_gate_b", [DEPTH, D])
        ppj_d = dram_in("ple_proj", [DEPTH, 256, D])
        prm_d = dram_in("prm", [DEPTH, 128, PC_N])
        wr_d = dram_in("wr", [DEPTH, 128, 8, 20])
        rb_d = dram_in("rbias", [DEPTH, 128, 20])
        lam_d = dram_in("lamv", [DEPTH, 128, 4, 32])
        pw_d = dram_in("poolw", [DEPTH, 128, 2, 128])
        gfin_d = dram_in("gfin", [128, D])
        cidf_d = dram_in("c_identf", [128, 128])
        crp_d = dram_in("c_rperm", [128, 128])
        ctri_d = dram_in("c_tri", [128, 128])
        cb64_d = dram_in("c_blk64", [128, 128])
        cones_d = dram_in("c_ones256", [128, 128])
        csel_d = dram_in("c_sel", [16, NE, 128])
        ccos_d = dram_in("c_cos", [128, S])
        csin_d = dram_in("c_sin", [128, S])
        ccorr_d = dram_in("c_corr", [128, 2, 16])
        out_d = nc.dram_tensor("out", [S, D], F32, kind="ExternalOutput").ap()

        dkind = "ExternalOutput" if dbg else "Internal"
        h_d = dram_scr("h_scr", [S, D], F32, dkind)
        glu_d = dram_scr("glu_scr", [256, S], BF16, dkind)
        pin_d = dram_scr("pin_scr", [256, S], F32, dkind)
        q_d = dram_scr("q_scr", [256, S], BF16, dkind)
        k_d = dram_scr("k_scr", [256, S], BF16, dkind)
        v_d = dram_scr("v_scr", [S, 512], BF16, dkind)
        gb_d = dram_scr("gb_scr", [256, S], F32, dkind)
        gcv_d = dram_scr("gcv_scr", [256, S], F32, dkind)
        mix_d = dram_scr("mix_scr", [D, S], BF16, dkind)
        xT_d = dram_scr("xT_scr", [D, S], BF16, dkind)
        cmb_d = dram_scr("cmb_scr", [16, S], F32, dkind)

        def tiles(name):
            return [Res("%s%d" % (name, i)) for i in range(NT)]
        R_h = tiles("h")
        R_glu, R_pin, R_q, R_k, R_v = tiles("glu"), tiles("pin"), tiles("q"), tiles("k"), tiles("v")
        R_gb, R_gcv, R_mixA, R_mixB, R_xT, R_cmb = (tiles("gb"), tiles("gcv"), tiles("mixA"),
                                                    tiles("mixB"), tiles("xT"), tiles("cmb"))
        R_out = tiles("out")

        def fm(ap, i, lo=0, hi=TS):
            return ap.rearrange("(c p) t -> p c t", p=128)[:, :, i * TS + lo:i * TS + hi]

        def tm(ap, i):
            return ap[i * TS:(i + 1) * TS, :].rearrange("(j p) f -> p j f", p=128)

        uid = [0]
        def sbuf(stack, name, shape, dt, nslots=1):
            uid[0] += 1
            t = stack.enter_context(nc.sbuf_tensor("%s_u%d" % (name, uid[0]), list(shape), dt))
            return Buf(t, name, nslots)

        def psum(stack, name, shape, dt=F32):
            t = stack.enter_context(nc.psum_tensor(name, list(shape), dt))
            b = Buf(t, name, 1)
            b.res[0].excl = True
            return b

        identf = sbuf(es, "identf", [128, 128], F32)
        identb = sbuf(es, "identb", [128, 128], BF16)
        rperm = sbuf(es, "rperm", [128, 128], F32)
        trib = sbuf(es, "trib", [128, 128], BF16)
        blk64 = sbuf(es, "blk64", [128, 128], F32)
        ones256 = sbuf(es, "ones256", [128, 128], F32)
        sel = sbuf(es, "sel", [16, NE, 128], F32)
        prm = sbuf(es, "prm_sb", [128, PC_N], F32)
        corr = sbuf(es, "corr", [128, 2, 16], F32)
        gfin = sbuf(es, "gfin_sb", [128, D], F32)
        neglam = sbuf(es, "neglam", [128, 1], F32)
        pbs = sbuf(es, "pbs", [128, 2], F32)
        onesrow = sbuf(es, "onesrow", [1, 128], BF16)
        epsc = sbuf(es, "epsc", [128, 1], F32)

        pf = []
        pb = []
        pf_i = [0]
        pb_i = [0]

        def set_psum(stack, nf, nb):
            uid[0] += 1
            pf[:] = [psum(stack, "pf%d_%d" % (i, uid[0]), [128, 512], F32) for i in range(nf)]
            pb[:] = [psum(stack, "pb%d_%d" % (i, uid[0]), [128, 1024], BF16) for i in range(nb)]

        def next_pf():
            b = pf[pf_i[0] % len(pf)]
            pf_i[0] += 1
            return b

        def next_pb():
            b = pb[pb_i[0] % len(pb)]
            pb_i[0] += 1
            return b

        P.dma("sp", identf.t[:], cidf_d, writes=[identf.r])
        P.dma("sp", rperm.t[:], crp_d, writes=[rperm.r])
        P.dma("sp", blk64.t[:], cb64_d, writes=[blk64.r])
        P.dma("sp", ones256.t[:], cones_d, writes=[ones256.r])
        P.dma("sp", sel.t[:], csel_d, writes=[sel.r])
        P.dma("sp", corr.t[:], ccorr_d, writes=[corr.r])
        P.dma("sp", gfin.t[:], gfin_d, writes=[gfin.r])
        P.dma("pool", identb.t[:], cidf_d, writes=[identb.r])
        P.dma("pool", trib.t[:], ctri_d, writes=[trib.r])
        P.op("dve", lambda e: e.memset(onesrow.t[:], 1.0), writes=[onesrow.r])
        P.op("dve", lambda e: e.memset(epsc.t[:], EPS), writes=[epsc.r])

        def norm_stats(st, ht, slot, ss, rstd, sqj):
            P.op("dve", lambda e: e.memset(ss.t[:], 0.0), writes=[ss.r])
            for j in range(4):
                P.op("act", lambda e, j=j: e.activation(out=sqj.t[:], in_=ht.t[:, j, :], func=AF.Square,
                                                        accum_out=ss.t[:, j:j + 1]),
                     reads=[ht.res[slot], ss.r], writes=[sqj.r, ss.r])
            P.op("dve", lambda e: e.tensor_scalar(out=rstd.t[:], in0=ss.t[:], scalar1=1.0 / D, scalar2=EPS,
                                                  op0=ALU.mult, op1=ALU.add), reads=[ss.r], writes=[rstd.r])
            P.op("act", lambda e: e.activation(out=rstd.t[:], in_=rstd.t[:], func=AF.Sqrt),
                 reads=[rstd.r], writes=[rstd.r])
            P.op("dve", lambda e: e.reciprocal(out=rstd.t[:], in_=rstd.t[:]), reads=[rstd.r], writes=[rstd.r])

        def norm_transpose(ht, slot, rstd, xn, xT, gcol, part=None):
            for j in range(4 if part in (None, 0) else 0):
                P.op("dve", lambda e, j=j: e.tensor_scalar(out=xn.t[:, j, :], in0=ht.t[:, j, :],
                                                           scalar1=rstd.t[:, j:j + 1], scalar2=None, op0=ALU.mult),
                     reads=[ht.res[slot], rstd.r], writes=[xn.res[j]])
            for c2 in range(4 if part in (None, 1) else 0):
                bank = next_pb()
                for cc in range(2):
                    c = c2 * 2 + cc
                    for j in range(4):
                        last = (cc == 1 and j == 3)
                        P.op("pe", lambda e, c=c, cc=cc, j=j: e.transpose(
                            bank.t[:, cc * 512 + j * 128: cc * 512 + (j + 1) * 128],
                            xn.t[:, j, c * 128:(c + 1) * 128], identb.t[:]),
                            reads=[xn.res[j], identb.r], writes=[bank.r], signal=last)
                for cc in range(2):
                    c = c2 * 2 + cc
                    if cc == 0:
                        P.op("act", lambda e, c=c, cc=cc: e.activation(
                            out=xT.t[:, c, :], in_=bank.t[:, cc * 512:(cc + 1) * 512], func=AF.Identity,
                            scale=prm.t[:, gcol + c:gcol + c + 1]),
                            reads=[bank.r, prm.r], writes=[xT.res[c]])
                    else:
                        P.op("dve", lambda e, c=c, cc=cc: e.tensor_scalar(
                            out=xT.t[:, c, :], in0=bank.t[:, cc * 512:(cc + 1) * 512],
                            scalar1=prm.t[:, gcol + c:gcol + c + 1], scalar2=None, op0=ALU.mult),
                            reads=[bank.r, prm.r], writes=[xT.res[c]])

        def load_h(i, ht, slot, src):
            P.dma("sp", ht.t[:], tm(src, i), reads=[R_h[i]], writes=[ht.res[slot]])

        def mm_group(bank, cols, lhs_list, rhs_list, reads, tile_position=None):
            n = len(lhs_list)
            for k in range(n):
                P.op("pe", lambda e, k=k: e.matmul(bank.t[:, cols[0]:cols[1]], lhs_list[k], rhs_list[k],
                                                   start=(k == 0), stop=(k == n - 1)),
                     reads=reads[k], writes=[bank.r], signal=(k == n - 1))

        def load_w_in(l_):
            stw = contextlib.ExitStack()
            uid[0] += 1
            t_ = stw.enter_context(nc.sbuf_tensor("w_in_sb_u%d" % uid[0], [128, 8, INC], BF16, side="right"))
            b_ = Buf(t_, "w_in_sb", 1)
            for c in range(8):
                P.dma("pool", b_.t[:, c, :], w_in_d[l_, c * 128:(c + 1) * 128, :], writes=[b_.r])
            return stw, b_

        st_win, w_in_next = load_w_in(0)
        for l in range(DEPTH):
            lam_init = 0.8 - 0.6 * math.exp(-0.3 * l)
            h_src = x_d if l == 0 else h_d

            P.barrier()
            P.dma("sp", prm.t[:], prm_d[l], writes=[prm.r])
            with contextlib.ExitStack() as st:
                lamv = sbuf(st, "lamv", [128, 4, 32], F32)
                lt = sbuf(st, "lt", [128, 2, 32], F32)
                ls = sbuf(st, "ls", [128, 2], F32)
                P.dma("sp", lamv.t[:], lam_d[l], writes=[lamv.r])
                P.op("dve", lambda e: e.tensor_tensor(out=lt.t[:, 0, :], in0=lamv.t[:, 0, :], in1=lamv.t[:, 1, :],
                                                      op=ALU.mult), reads=[lamv.r], writes=[lt.r])
                P.op("dve", lambda e: e.tensor_tensor(out=lt.t[:, 1, :], in0=lamv.t[:, 2, :], in1=lamv.t[:, 3, :],
                                                      op=ALU.mult), reads=[lamv.r, lt.r], writes=[lt.r])
                P.op("dve", lambda e: e.reduce_sum(out=ls.t[:], in_=lt.t[:], axis=AX.X), reads=[lt.r], writes=[ls.r])
                P.op("act", lambda e: e.activation(out=ls.t[:], in_=ls.t[:], func=AF.Exp), reads=[ls.r], writes=[ls.r])
                P.op("dve", lambda e: e.scalar_tensor_tensor(out=neglam.t[:], in0=ls.t[:, 1:2], scalar=-lam_init,
                                                             in1=ls.t[:, 0:1], op0=ALU.add, op1=ALU.subtract),
                     reads=[ls.r], writes=[neglam.r])
                P.op("dve", lambda e: e.tensor_tensor(out=pbs.t[:], in0=prm.t[:, PC_PB:PC_PB + 2],
                                                      in1=prm.t[:, PC_PS:PC_PS + 2], op=ALU.mult),
                     reads=[prm.r], writes=[pbs.r])
                P.barrier()

            st_dg = contextlib.ExitStack()
            dg = sbuf(st_dg, "s2_dg", [128, 2, CK, 128], BF16)
            pw_sb = sbuf(st_dg, "s2_pw", [128, 2, 128], BF16)
            with contextlib.ExitStack() as st:
                set_psum(st, 6, 2)
                w_in_sb = w_in_next
                ht = sbuf(st, "s1_ht", [128, 4, D], F32, 2)
                hts = [ht, sbuf(st, "s1_ht2", [128, 4, D], F32, 2)]
                ss2 = [sbuf(st, "s1_ss%d" % k, [128, 4], F32) for k in range(2)]
                rstd2 = [sbuf(st, "s1_rstd%d" % k, [128, 4], F32) for k in range(2)]
                sqj = sbuf(st, "s1_sqj", [128, D], BF16)
                xn2 = [sbuf(st, "s1_xn%d" % k, [128, 4, D], BF16, 4) for k in range(2)]
                nT2 = [sbuf(st, "s1_nT%d" % k, [128, 8, TS], BF16, 8) for k in range(2)]
                sig = sbuf(st, "s1_sig", [128, TS], F32)
                gcs = sbuf(st, "s1_gc", [128, TS], F32)
                glu_st = sbuf(st, "s1_glu", [128, 2, TS], BF16)
                pin_st = sbuf(st, "s1_pin", [128, 2, TS], F32)
                qk_st = sbuf(st, "s1_qk", [128, 4, TS], F32, 4)
                qkr_st = sbuf(st, "s1_qkr", [128, 4, TS], BF16, 4)
                gb_st = sbuf(st, "s1_gb", [128, 2, TS], F32)
                gcv_st = sbuf(st, "s1_gcv", [128, 2, TS], F32)
                v_st = sbuf(st, "s1_v", [128, 4, 512], BF16)
                cos_t = sbuf(st, "s1_cos", [128, TS], F32)
                sin_t = sbuf(st, "s1_sin", [128, TS], F32)
                t1 = sbuf(st, "s1_t1", [128, TS], F32)
                t2 = sbuf(st, "s1_t2", [128, TS], F32)

                P.op("dve", lambda e: e.memset(v_st.t[:], 1.0), writes=[v_st.r])

                def s1_prologue(i_):
                    norm_stats(st, hts[i_ % 2], 0, ss2[i_ % 2], rstd2[i_ % 2], sqj)
                    norm_transpose(hts[i_ % 2], 0, rstd2[i_ % 2], xn2[i_ % 2], nT2[i_ % 2], PC_MIXG)

                P.dma("sp", hts[0].t[:], tm(h_src, 0), reads=[R_h[0]], writes=[hts[0].r])
                P.dma("sp", hts[1].t[:], tm(h_src, 1), reads=[R_h[1]], writes=[hts[1].r])
                s1_prologue(0)
                P.dma("pool", pw_sb.t[:], pw_d[l], writes=[pw_sb.r])
                for cc in range(2):
                    for j in range(CK):
                        col = PC_CW + cc * CK + j
                        P.op("dve", lambda e, cc=cc, j=j, col=col: e.tensor_scalar(
                            out=dg.t[:, cc, j, :], in0=identf.t[:], scalar1=prm.t[:, col:col + 1], scalar2=None,
                            op0=ALU.mult), reads=[identf.r, prm.r], writes=[dg.r])
                for i in range(NT):
                    P.dma("sp", cos_t.t[:], ccos_d[:, i * TS:(i + 1) * TS], writes=[cos_t.r])
                    P.dma("sp", sin_t.t[:], csin_d[:, i * TS:(i + 1) * TS], writes=[sin_t.r])
                    nT = nT2[i % 2]

                    def proj(col0):
                        bank = next_pf()
                        mm_group(bank, (0, TS), [w_in_sb.t[:, c, col0:col0 + 128] for c in range(8)],
                                 [nT.t[:, c, :] for c in range(8)],
                                 [[w_in_sb.r, nT.res[c]] for c in range(8)])
                        return bank

                    for cc in range(2):
                        bg_ = proj(256 + cc * 128)
                        P.op("act", lambda e: e.activation(out=sig.t[:], in_=bg_.t[:], func=AF.Sigmoid),
                             reads=[bg_.r], writes=[sig.r])
                        bv_ = proj(0 + cc * 128)
                        P.op("dve", lambda e: e.tensor_tensor(out=glu_st.t[:, cc, :], in0=bv_.t[:], in1=sig.t[:],
                                                              op=ALU.mult),
                             reads=[bv_.r, sig.r], writes=[glu_st.r])
                    for cc in range(2):
                        bp_ = proj(512 + cc * 128)
                        P.op("act", lambda e: e.copy(out=pin_st.t[:, cc, :], in_=bp_.t[:]),
                             reads=[bp_.r], writes=[pin_st.r])
                    if i + 1 < NT:
                        s1_prologue(i + 1)
                    for m in range(4):
                        bq_ = proj(768 + m * 128)
                        if m % 2 == 0:
                            P.op("act", lambda e: e.copy(out=qk_st.t[:, m, :], in_=bq_.t[:]),
                                 reads=[bq_.r], writes=[qk_st.res[m]])
                        else:
                            P.op("dve", lambda e: e.tensor_copy(out=qk_st.t[:, m, :], in_=bq_.t[:]),
                                 reads=[bq_.r], writes=[qk_st.res[m]])
                    for cc in range(2):
                        bb_ = proj(1536 + cc * 128)
                        P.op("act", lambda e: e.copy(out=gb_st.t[:, cc, :], in_=bb_.t[:]),
                             reads=[bb_.r], writes=[gb_st.r])
                    for cc in range(2):
                        bc_ = proj(1792 + cc * 128)
                        P.op("act", lambda e: e.copy(out=gcs.t[:], in_=bc_.t[:]), reads=[bc_.r], writes=[gcs.r])
                        bs_ = proj(2048 + cc * 128)
                        P.op("dve", lambda e: e.tensor_tensor(out=gcv_st.t[:, cc, :], in0=bs_.t[:], in1=gcs.t[:],
                                                              op=ALU.mult),
                             reads=[bs_.r, gcs.r], writes=[gcv_st.r])
                    for j in range(4):
                        bank = next_pf()
                        mm_group(bank, (0, 256), [nT.t[:, c, j * 128:(j + 1) * 128] for c in range(8)],
                                 [w_in_sb.t[:, c, 1280:1536] for c in range(8)],
                                 [[w_in_sb.r, nT.res[c]] for c in range(8)])
                        for hh in range(4):
                            off = hh * 128 + (0 if hh % 2 == 0 else 64)
                            eng = "act" if hh % 2 == 0 else "dve"
                            if eng == "act":
                                P.op("act", lambda e: e.copy(out=v_st.t[:, j, off:off + 64],
                                                             in_=bank.t[:, hh * 64:(hh + 1) * 64]),
                                     reads=[bank.r], writes=[v_st.r])
                            else:
                                P.op("dve", lambda e: e.tensor_copy(out=v_st.t[:, j, off:off + 64],
                                                                    in_=bank.t[:, hh * 64:(hh + 1) * 64]),
                                     reads=[bank.r], writes=[v_st.r])
                    for m in range(4):
                        bank = next_pf()
                        P.op("pe", lambda e: e.matmul(bank.t[:], rperm.t[:], qk_st.t[:, m, :], start=True, stop=True),
                             reads=[rperm.r, qk_st.res[m]], writes=[bank.r])
                        P.op("dve", lambda e: e.tensor_tensor(out=t1.t[:], in0=qk_st.t[:, m, :], in1=cos_t.t[:],
                                                              op=ALU.mult),
                             reads=[qk_st.res[m], cos_t.r], writes=[t1.r])
                        P.op("dve", lambda e: e.tensor_tensor(out=t2.t[:], in0=bank.t[:], in1=sin_t.t[:],
                                                              op=ALU.mult),
                             reads=[bank.r, sin_t.r], writes=[t2.r])
                        P.op("dve", lambda e: e.tensor_tensor(out=qkr_st.t[:, m, :], in0=t1.t[:], in1=t2.t[:],
                                                              op=ALU.add),
                             reads=[t1.r, t2.r], writes=[qkr_st.res[m]])
                    if i + 2 < NT:
                        P.dma("sp", hts[i % 2].t[:], tm(h_src, i + 2), reads=[R_h[i + 2]], writes=[hts[i % 2].r])
                    P.dma("sp", fm(glu_d, i), glu_st.t[:], reads=[glu_st.r], writes=[R_glu[i]])
                    P.dma("sp", fm(pin_d, i), pin_st.t[:], reads=[pin_st.r], writes=[R_pin[i]])
                    P.dma("sp", fm(q_d, i), qkr_st.t[:, 0:2, :], reads=[qkr_st.res[0], qkr_st.res[1]], writes=[R_q[i]])
                    P.dma("sp", fm(k_d, i), qkr_st.t[:, 2:4, :], reads=[qkr_st.res[2], qkr_st.res[3]], writes=[R_k[i]])
                    P.dma("sp", tm(v_d, i), v_st.t[:], reads=[v_st.r], writes=[R_v[i]])
                    P.dma("sp", fm(gb_d, i), gb_st.t[:], reads=[gb_st.r], writes=[R_gb[i]])
                    P.dma("sp", fm(gcv_d, i), gcv_st.t[:], reads=[gcv_st.r], writes=[R_gcv[i]])
                P.barrier()
            st_win.close()
            if dbg == "s1":
                st_dg.close()
                break

            st_wout = contextlib.ExitStack()
            uid[0] += 1
            w_out_sb = Buf(st_wout.enter_context(nc.sbuf_tensor("w_out_sb_u%d" % uid[0], [128, 8, D], BF16,
                                                                side="right")), "w_out_sb", 1)
            for c in range(8):
                P.dma("pool", w_out_sb.t[:, c, :], w_out_d[l, c * 128:(c + 1) * 128, :], writes=[w_out_sb.r])
            st_kv = contextlib.ExitStack()
            kT = sbuf(st_kv, "at_kT", [128, 2, S], BF16)
            Vs = sbuf(st_kv, "at_V", [128, 32, 512], BF16)
            P.dma("sp", kT.t[:], k_d.rearrange("(c p) t -> p c t", p=128), reads=R_k, writes=[kT.r])
            for i8 in range(NT):
                P.dma("sp", Vs.t[:, i8 * 4:(i8 + 1) * 4, :], tm(v_d, i8), reads=[R_v[i8]], writes=[Vs.r])
            with contextlib.ExitStack() as st:
                set_psum(st, 8, 0)
                glu_in = [sbuf(st, "s2_glu%d" % k, [128, 2, 30 + TS], BF16) for k in range(2)]
                pin_in = [sbuf(st, "s2_pin%d" % k, [128, 2, 16 + TS], F32) for k in range(2)]
                gcv_in = [sbuf(st, "s2_gcv%d" % k, [128, 2, 2 + TS], F32) for k in range(2)]
                gb_in = [sbuf(st, "s2_gb%d" % k, [128, 2, TS], F32) for k in range(2)]
                yc = sbuf(st, "s2_y", [128, 2, TS], F32, 2)
                ysq = sbuf(st, "s2_ysq", [128, 2, TS], F32, 2)
                m2 = sbuf(st, "s2_m2", [128, TS], F32)
                var = sbuf(st, "s2_var", [128, TS], F32)
                dd = sbuf(st, "s2_dd", [128, TS], F32)
                sA = sbuf(st, "s2_sA", [128, 16 + TS], F32)
                sB = sbuf(st, "s2_sB", [128, 16 + TS], F32)
                pooled = sbuf(st, "s2_pooled", [128, TS], BF16)
                acc3 = sbuf(st, "s2_acc3", [128, TS], F32)
                mixA = [sbuf(st, "s2_mixA%d" % k, [128, 6, TS], BF16) for k in range(2)]


                def s2_load(i):
                    k = i % 2
                    if i == 0:
                        P.op("dve", lambda e: e.memset(glu_in[k].t[:, :, 0:30], 0.0), writes=[glu_in[k].r])
                        P.op("dve", lambda e: e.memset(pin_in[k].t[:, :, 0:16], 0.0), writes=[pin_in[k].r])
                        P.op("dve", lambda e: e.memset(gcv_in[k].t[:, :, 0:2], 0.0), writes=[gcv_in[k].r])
                        P.dma("sp", glu_in[k].t[:, :, 30:30 + TS], fm(glu_d, 0), reads=[R_glu[0]], writes=[glu_in[k].r])
                        P.dma("sp", pin_in[k].t[:, :, 16:16 + TS], fm(pin_d, 0), reads=[R_pin[0]], writes=[pin_in[k].r])
                        P.dma("sp", gcv_in[k].t[:, :, 2:2 + TS], fm(gcv_d, 0), reads=[R_gcv[0]], writes=[gcv_in[k].r])
                    else:
                        P.dma("sp", glu_in[k].t[:], fm(glu_d, i, -30, TS), reads=[R_glu[i - 1], R_glu[i]],
                              writes=[glu_in[k].r])
                        P.dma("sp", pin_in[k].t[:], fm(pin_d, i, -16, TS), reads=[R_pin[i - 1], R_pin[i]],
                              writes=[pin_in[k].r])
                        P.dma("sp", gcv_in[k].t[:], fm(gcv_d, i, -2, TS), reads=[R_gcv[i - 1], R_gcv[i]],
                              writes=[gcv_in[k].r])
                    P.dma("sp", gb_in[k].t[:], fm(gb_d, i), reads=[R_gb[i]], writes=[gb_in[k].r])

                s2_load(0)
                for i in range(NT):
                    k = i % 2
                    if i + 1 < NT:
                        s2_load(i + 1)
                    mx = mixA[k]
                    for cc in range(2):
                        bank = next_pf()
                        mm_group(bank, (0, TS), [dg.t[:, cc, j, :] for j in range(CK)],
                                 [glu_in[k].t[:, cc, j:j + TS] for j in range(CK)],
                                 [[dg.r, glu_in[k].r]] * CK)
                        P.op("act", lambda e: e.activation(out=yc.t[:, cc, :], in_=bank.t[:], func=AF.Identity,
                                                           bias=prm.t[:, PC_CB + cc:PC_CB + cc + 1]),
                             reads=[bank.r, prm.r], writes=[yc.res[cc]])
                        P.op("act", lambda e: e.activation(out=ysq.t[:, cc, :], in_=bank.t[:], func=AF.Square,
                                                           bias=prm.t[:, PC_CB + cc:PC_CB + cc + 1]),
                             reads=[bank.r, prm.r], writes=[ysq.res[cc]])
                    bm = next_pf()
                    mm_group(bm, (0, TS), [ones256.t[:], ones256.t[:]], [yc.t[:, 0, :], yc.t[:, 1, :]],
                             [[ones256.r, yc.res[0]], [ones256.r, yc.res[1]]])
                    bq = next_pf()
                    mm_group(bq, (0, TS), [ones256.t[:], ones256.t[:]], [ysq.t[:, 0, :], ysq.t[:, 1, :]],
                             [[ones256.r, ysq.res[0]], [ones256.r, ysq.res[1]]])
                    for cc in range(2):
                        u = pin_in[k].t[:, cc, :]
                        W_ = 16 + TS
                        P.op("dve", lambda e: e.memset(sA.t[:, 0:1], 0.0), writes=[sA.r])
                        P.op("dve", lambda e: e.tensor_tensor(out=sA.t[:, 1:W_], in0=pin_in[k].t[:, cc, 1:W_],
                                                              in1=pin_in[k].t[:, cc, 0:W_ - 1], op=ALU.add),
                             reads=[pin_in[k].r], writes=[sA.r])
                        if cc == 0:
                            P.op("dve", lambda e: e.tensor_tensor(out=sB.t[64:128, 3:W_], in0=sA.t[64:128, 3:W_],
                                                                  in1=sA.t[64:128, 1:W_ - 2], op=ALU.add),
                                 reads=[sA.r], writes=[sB.r])
                            P.op("dve", lambda e: e.tensor_copy(out=sB.t[0:64, 3:W_], in_=sA.t[0:64, 3:W_]),
                                 reads=[sA.r, sB.r], writes=[sB.r])
                            fin = sB
                        else:
                            P.op("dve", lambda e: e.tensor_tensor(out=sB.t[:, 3:W_], in0=sA.t[:, 3:W_],
                                                                  in1=sA.t[:, 1:W_ - 2], op=ALU.add),
                                 reads=[sA.r], writes=[sB.r])
                            P.op("dve", lambda e: e.tensor_tensor(out=sA.t[:, 7:W_], in0=sB.t[:, 7:W_],
                                                                  in1=sB.t[:, 3:W_ - 4], op=ALU.add),
                                 reads=[sB.r, sA.r], writes=[sA.r])
                            P.op("dve", lambda e: e.tensor_tensor(out=sB.t[64:128, 15:W_], in0=sA.t[64:128, 15:W_],
                                                                  in1=sA.t[64:128, 7:W_ - 8], op=ALU.add),
                                 reads=[sA.r, sB.r], writes=[sB.r])
                            P.op("dve", lambda e: e.tensor_copy(out=sB.t[0:64, 15:W_], in_=sA.t[0:64, 15:W_]),
                                 reads=[sA.r, sB.r], writes=[sB.r])
                            fin = sB
                        if i == 0:
                            P.op("dve", lambda e: e.tensor_tensor(out=fin.t[:, 16:32], in0=fin.t[:, 16:32],
                                                                  in1=corr.t[:, cc, :], op=ALU.mult),
                                 reads=[fin.r, corr.r], writes=[fin.r])
                        P.op("dve", lambda e: e.scalar_tensor_tensor(
                            out=pooled.t[:], in0=fin.t[:, 16:16 + TS], scalar=prm.t[:, PC_IW + cc:PC_IW + cc + 1],
                            in1=pin_in[k].t[:, cc, 16:16 + TS], op0=ALU.mult, op1=ALU.subtract),
                            reads=[fin.r, prm.r, pin_in[k].r], writes=[pooled.r])
                        bank = next_pf()
                        P.op("pe", lambda e: e.matmul(bank.t[:], pw_sb.t[:, cc, :], pooled.t[:], start=True, stop=True),
                             reads=[pw_sb.r, pooled.r], writes=[bank.r])
                        P.op("act", lambda e: e.activation(out=mx.t[:, 2 + cc, :], in_=bank.t[:], func=AF.Identity,
                                                           scale=prm.t[:, PC_PS + cc:PC_PS + cc + 1],
                                                           bias=pbs.t[:, cc:cc + 1]),
                             reads=[bank.r, prm.r, pbs.r], writes=[mx.r])
                    for cc in range(2):
                        g_ = gcv_in[k]
                        P.op("dve", lambda e: e.tensor_scalar(out=acc3.t[:], in0=g_.t[:, cc, 0:TS],
                                                              scalar1=prm.t[:, PC_SW + cc * 3:PC_SW + cc * 3 + 1],
                                                              scalar2=None, op0=ALU.mult),
                             reads=[g_.r, prm.r], writes=[acc3.r])
                        for j in (1, 2):
                            P.op("dve", lambda e, j=j: e.scalar_tensor_tensor(
                                out=acc3.t[:], in0=g_.t[:, cc, j:j + TS],
                                scalar=prm.t[:, PC_SW + cc * 3 + j:PC_SW + cc * 3 + j + 1], in1=acc3.t[:],
                                op0=ALU.mult, op1=ALU.add), reads=[g_.r, prm.r, acc3.r], writes=[acc3.r])
                        P.op("dve", lambda e: e.tensor_tensor(out=mx.t[:, 4 + cc, :], in0=acc3.t[:],
                                                              in1=gb_in[k].t[:, cc, :], op=ALU.mult),
                             reads=[acc3.r, gb_in[k].r], writes=[mx.r])
                    P.op("act", lambda e: e.activation(out=m2.t[:], in_=bm.t[:], func=AF.Square),
                         reads=[bm.r], writes=[m2.r])
                    P.op("dve", lambda e: e.tensor_tensor(out=var.t[:], in0=bq.t[:], in1=m2.t[:], op=ALU.subtract),
                         reads=[bq.r, m2.r], writes=[var.r])
                    P.op("dve", lambda e: e.tensor_scalar(out=var.t[:], in0=var.t[:], scalar1=0.0, scalar2=EPS,
                                                          op0=ALU.max, op1=ALU.add), reads=[var.r], writes=[var.r])
                    P.op("act", lambda e: e.activation(out=var.t[:], in_=var.t[:], func=AF.Ln),
                         reads=[var.r], writes=[var.r])
                    P.op("act", lambda e: e.activation(out=var.t[:], in_=var.t[:], func=AF.Exp, scale=-0.5),
                         reads=[var.r], writes=[var.r])
                    for cc in range(2):
                        P.op("dve", lambda e: e.tensor_tensor(out=dd.t[:], in0=bm.t[:], in1=yc.t[:, cc, :],
                                                              op=ALU.subtract),
                             reads=[bm.r, yc.res[cc]], writes=[dd.r])
                        P.op("dve", lambda e: e.tensor_tensor(out=dd.t[:], in0=dd.t[:], in1=var.t[:], op=ALU.mult),
                             reads=[dd.r, var.r], writes=[dd.r])
                        P.op("dve", lambda e: e.tensor_scalar(out=dd.t[:], in0=dd.t[:],
                                                              scalar1=prm.t[:, PC_LNG + cc:PC_LNG + cc + 1],
                                                              scalar2=-1.0, op0=ALU.mult, op1=ALU.mult),
                             reads=[dd.r, prm.r], writes=[dd.r])
                        P.op("act", lambda e: e.activation(out=mx.t[:, cc, :], in_=dd.t[:], func=AF.Silu,
                                                           bias=prm.t[:, PC_LNB + cc:PC_LNB + cc + 1]),
                             reads=[dd.r, prm.r], writes=[mx.r])
                    mv = mix_d.rearrange("(c p) t -> p c t", p=128)
                    P.dma("sp", mv[:, 0:4, i * TS:(i + 1) * TS], mx.t[:, 0:4, :], reads=[mx.r], writes=[R_mixA[i]])
                    P.dma("sp", mv[:, 6:8, i * TS:(i + 1) * TS], mx.t[:, 4:6, :], reads=[mx.r], writes=[R_mixA[i]])
                P.barrier()
            if dbg == "s2a":
                st_kv.close()
                st_wout.close()
                st_dg.close()
                break

            with contextlib.ExitStack() as st:
                qTs = [sbuf(st, "at_q%d" % k, [128, 2, TS], BF16) for k in range(2)]
                pts = [sbuf(st, "at_pt%d" % k, [128, TS], BF16) for k in range(4)]
                rz = [sbuf(st, "at_rz%d" % k, [128, TS], F32) for k in range(2)]
                o12 = [sbuf(st, "at_o%d" % k, [128, TS], F32) for k in range(2)]
                od = sbuf(st, "at_od", [128, TS], F32)
                osq = sbuf(st, "at_osq", [128, TS], F32)
                rs = sbuf(st, "at_rs", [128, TS], F32)
                mixB = [sbuf(st, "at_mix%d" % k, [128, 2, TS], BF16) for k in range(2)]
                set_psum(st, 8, 0)
                accb = pf[0:4]
                scb = pf[4:8]
                pts8 = pts + [sbuf(st, "at_ptx%d" % k, [128, TS], BF16) for k in range(4)]
                P.dma("sp", qTs[0].t[:], fm(q_d, 0), reads=[R_q[0]], writes=[qTs[0].r])
                mv = mix_d.rearrange("(c p) t -> p c t", p=128)

                groups = []
                for i in range(NT):
                    nkt = 4 * i + 4
                    for hp in range(2):
                        for kt in range(nkt):
                            groups.append((i, hp, kt, nkt))

                def front(g):
                    i, hp, kt, nkt = groups[g]
                    if hp == 0 and kt == 0 and i + 1 < NT:
                        P.dma("sp", qTs[(i + 1) % 2].t[:], fm(q_d, i + 1), reads=[R_q[i + 1]],
                              writes=[qTs[(i + 1) % 2].r])
                    qT = qTs[i % 2]
                    jd = kt - 4 * i
                    qs = 128 * jd if jd > 0 else 0
                    n = TS - qs
                    for s_ in range(4):
                        po = s_ * 32
                        sc = scb[s_]
                        P.op("pe", lambda e: e.matmul(sc.t[:, 0:n], kT.t[po:po + 32, hp, kt * 128:(kt + 1) * 128],
                                                      qT.t[po:po + 32, hp, qs:TS], start=True, stop=True,
                                                      tile_position=(po, 0)),
                             reads=[kT.r, qT.r], writes=[sc.r])
                    for s_ in range(4):
                        sc = scb[s_]
                        pt = pts8[(g % 2) * 4 + s_]
                        P.op("act", lambda e: e.activation(out=pt.t[:, 0:n], in_=sc.t[:, 0:n], func=AF.Exp, scale=SCALE),
                             reads=[sc.r], writes=[pt.r])
                        if jd >= 0:
                            P.op("dve", lambda e: e.tensor_tensor(out=pt.t[:, 0:128], in0=pt.t[:, 0:128], in1=trib.t[:],
                                                                  op=ALU.mult),
                                 reads=[pt.r, trib.r], writes=[pt.r])

                def back(g):
                    i, hp, kt, nkt = groups[g]
                    jd = kt - 4 * i
                    qs = 128 * jd if jd > 0 else 0
                    n = TS - qs
                    for s_ in range(4):
                        h = 2 * hp + s_ // 2
                        pt = pts8[(g % 2) * 4 + s_]
                        acc = accb[s_]
                        P.op("pe", lambda e: e.matmul(acc.t[:, qs:TS], Vs.t[:, kt, h * 128:(h + 1) * 128], pt.t[:, 0:n],
                                                      start=(kt == 0), stop=(kt == nkt - 1)),
                             reads=[Vs.r, pt.r], writes=[acc.r])
                    if kt == nkt - 1:
                        finalize(i, 2 * hp)
                        finalize(i, 2 * hp + 1)

                def finalize(i, h):
                    ch = h // 2
                    mb = mixB[i % 2]
                    lo, hi = (0, 64) if h % 2 == 0 else (64, 128)
                    zlo, zhi = (64, 128) if h % 2 == 0 else (0, 64)
                    accs = [accb[(h % 2) * 2], accb[(h % 2) * 2 + 1]]
                    for comp in range(2):
                        P.op("act", lambda e: e.activation(out=rz[comp].t[zlo:zhi, :], in_=accs[comp].t[zlo:zhi, :],
                                                           func=AF.Ln),
                             reads=[accs[comp].r], writes=[rz[comp].r])
                        P.op("act", lambda e: e.activation(out=rz[comp].t[zlo:zhi, :], in_=rz[comp].t[zlo:zhi, :],
                                                           func=AF.Exp, scale=-1.0),
                             reads=[rz[comp].r], writes=[rz[comp].r])
                        P.op("dve", lambda e: e.tensor_tensor(out=o12[comp].t[lo:hi, :], in0=accs[comp].t[lo:hi, :],
                                                              in1=rz[comp].t[zlo:zhi, :], op=ALU.mult),
                             reads=[accs[comp].r, rz[comp].r], writes=[o12[comp].r])
                    P.op("dve", lambda e: e.scalar_tensor_tensor(out=od.t[lo:hi, :], in0=o12[1].t[lo:hi, :],
                                                                 scalar=neglam.t[lo:hi, 0:1], in1=o12[0].t[lo:hi, :],
                                                                 op0=ALU.mult, op1=ALU.add),
                         reads=[o12[0].r, o12[1].r, neglam.r], writes=[od.r])
                    if h % 2 == 1:
                        P.op("act", lambda e: e.activation(out=osq.t[:], in_=od.t[:], func=AF.Square),
                             reads=[od.r], writes=[osq.r])
                        bms = scb[3]
                        P.op("pe", lambda e: e.matmul(bms.t[:], blk64.t[:], osq.t[:], start=True, stop=True),
                             reads=[blk64.r, osq.r], writes=[bms.r])
                        P.op("act", lambda e: e.activation(out=rs.t[:], in_=bms.t[:], func=AF.Ln, bias=epsc.t[:, 0:1]),
                             reads=[bms.r, epsc.r], writes=[rs.r])
                        P.op("act", lambda e: e.activation(out=rs.t[:], in_=rs.t[:], func=AF.Exp, scale=-0.5),
                             reads=[rs.r], writes=[rs.r])
                        P.op("dve", lambda e: e.tensor_tensor(out=rs.t[:], in0=rs.t[:], in1=od.t[:], op=ALU.mult),
                             reads=[rs.r, od.r], writes=[rs.r])
                        P.op("dve", lambda e: e.tensor_scalar(out=mb.t[:, ch, :], in0=rs.t[:],
                                                              scalar1=prm.t[:, PC_SUB:PC_SUB + 1],
                                                              scalar2=1.0 - lam_init, op0=ALU.mult, op1=ALU.mult),
                             reads=[rs.r, prm.r], writes=[mb.r])
                    if h == 3:
                        P.dma("sp", mv[:, 4:6, i * TS:(i + 1) * TS], mb.t[:], reads=[mb.r], writes=[R_mixB[i]])

                LA = 1
                for g in range(len(groups) + LA):
                    if g < len(groups):
                        front(g)
                    if g >= LA:
                        back(g - LA)
                P.barrier()
            st_kv.close()
            st_dg.close()
            if dbg == "s2c":
                st_wout.close()
                break

            st_ple = contextlib.ExitStack()
            wpg = sbuf(st_ple, "s5_wpg", [128, 8, D], BF16)
            wpp = sbuf(st_ple, "s5_wpp", [128, 2, D], BF16)
            stm = contextlib.ExitStack()
            wgs = [sbuf(stm, "wgs0", [128, 4, 8, 256], BF16)]
            wus = [sbuf(stm, "wus0", [128, 4, 8, 256], BF16)]
            wds = [sbuf(stm, "wds0", [128, 4, 2, D], BF16)]
            def load_experts(pz, slot):
                for el in range(4):
                    eidx = pz * 4 + el
                    P.dma("pool", wgs[slot].t[:, el, :, :], wg_d[l, eidx].rearrange("(c p) n -> p c n", p=128),
                          writes=[wgs[slot].r])
                    P.dma("pool", wus[slot].t[:, el, :, :], wu_d[l, eidx].rearrange("(c p) n -> p c n", p=128),
                          writes=[wus[slot].r])
                    P.dma("pool", wds[slot].t[:, el, :, :], wd_d[l, eidx].rearrange("(c p) n -> p c n", p=128),
                          writes=[wds[slot].r])

            with contextlib.ExitStack() as st:
                set_psum(st, 6, 2)
                wrg = sbuf(st, "s3_wrg", [128, 8, 20], F32)
                rbias = sbuf(st, "s3_rb", [128, 20], F32)
                hts = [sbuf(st, "s3_ht%d" % k, [128, 4, D], F32) for k in range(2)]
                mxs = [sbuf(st, "s3_mx%d" % k, [128, 8, TS], BF16) for k in range(2)]
                ss2 = [sbuf(st, "s3_ss%d" % k, [128, 4], F32) for k in range(2)]
                rstd2 = [sbuf(st, "s3_rstd%d" % k, [128, 4], F32) for k in range(2)]
                sqj = sbuf(st, "s3_sqj", [128, D], BF16)
                xn2 = [sbuf(st, "s3_xn%d" % k, [128, 4, D], BF16, 4) for k in range(2)]
                xT2 = [sbuf(st, "s3_xT%d" % k, [128, 8, TS], BF16, 8) for k in range(2)]
                hTf = sbuf(st, "s3_hTf", [128, 8, 128], F32, 2)
                lg = sbuf(st, "s3_lg", [128, 20], F32)
                lg4 = sbuf(st, "s3_lg4", [128, 4, 20], F32)
                r4 = sbuf(st, "s3_r4", [128, 8, 4], F32)
                mg4 = sbuf(st, "s3_mg4", [128, 4, 4], F32)
                t4 = sbuf(st, "s3_t4", [128, 4, 4], F32)
                es4 = sbuf(st, "s3_es4", [128, 4, 4], F32)
                eq4 = sbuf(st, "s3_eq4", [128, 4, 4], F32)
                pr4 = sbuf(st, "s3_pr4", [128, 4, 4, 4], F32)
                sm = sbuf(st, "s3_sm", [128, 16], F32)
                mg = sbuf(st, "s3_mg", [128, 4], F32)
                ge = sbuf(st, "s3_ge", [128, 4], F32)
                esel = sbuf(st, "s3_esel", [128, 4], F32)
                eq = sbuf(st, "s3_eq", [128, 4], F32)
                em2 = sbuf(st, "s3_em2", [128, 4], F32)
                ee = sbuf(st, "s3_ee", [128, 4], F32)
                wsel = sbuf(st, "s3_wsel", [128, 4], F32)
                comb = sbuf(st, "s3_comb", [128, 4, 16], F32)
                cmbT = sbuf(st, "s3_cmbT", [16, TS], F32)

                load_experts(0, 0)
                P.dma("sp", wrg.t[:], wr_d[l], writes=[wrg.r])
                P.dma("sp", rbias.t[:], rb_d[l], writes=[rbias.r])
                for c in range(8):
                    P.op("dve", lambda e, c=c: e.tensor_scalar(out=wrg.t[:, c, :], in0=wrg.t[:, c, :],
                                                               scalar1=prm.t[:, PC_FFNG + c:PC_FFNG + c + 1],
                                                               scalar2=None, op0=ALU.mult),
                         reads=[wrg.r, prm.r], writes=[wrg.r])

                def s3_load(i):
                    k = i % 2
                    P.dma("sp", hts[k].t[:], tm(h_src, i), reads=[R_h[i]], writes=[hts[k].r])
                    P.dma("sp", mxs[k].t[:], fm(mix_d, i), reads=[R_mixA[i], R_mixB[i]], writes=[mxs[k].r])

                def s3_A(i):
                    k = i % 2
                    ht = hts[k]
                    mx = mxs[k]
                    ss, rstd, xn, xT = ss2[k], rstd2[k], xn2[k], xT2[k]
                    for j in range(4):
                        for half in range(2):
                            bank = next_pf()
                            mm_group(bank, (0, 512), [mx.t[:, c, j * 128:(j + 1) * 128] for c in range(8)],
                                     [w_out_sb.t[:, c, half * 512:(half + 1) * 512] for c in range(8)],
                                     [[mx.r, w_out_sb.r]] * 8)
                            P.op("dve", lambda e: e.tensor_tensor(out=ht.t[:, j, half * 512:(half + 1) * 512],
                                                                  in0=bank.t[:], in1=ht.t[:, j, half * 512:(half + 1) * 512],
                                                                  op=ALU.add),
                                 reads=[bank.r, ht.r], writes=[ht.r])
                    P.dma("sp", tm(h_d, i), ht.t[:], reads=[ht.r], writes=[R_h[i]])
                    norm_stats(st, ht, 0, ss, rstd, sqj)
                    norm_transpose(ht, 0, rstd, xn, xT, PC_FFNG, part=0)

                def s3_A2(i):
                    k = i % 2
                    ht = hts[k]
                    ss, rstd, xn, xT = ss2[k], rstd2[k], xn2[k], xT2[k]
                    norm_transpose(ht, 0, rstd, xn, xT, PC_FFNG, part=1)
                    P.dma("sp", fm(xT_d, i), xT.t[:], reads=xT.res, writes=[R_xT[i]])

                def s3_Ba(i):
                    k = i % 2
                    ht = hts[k]
                    ss, rstd, xn, xT = ss2[k], rstd2[k], xn2[k], xT2[k]
                    for j in range(4):
                        for c2 in range(2):
                            bank = next_pf()
                            while bank is bct:
                                bank = next_pf()
                            for cq in range(4):
                                c = c2 * 4 + cq
                                P.op("pe", lambda e: e.transpose(bank.t[:, cq * 128:(cq + 1) * 128],
                                                                 ht.t[:, j, c * 128:(c + 1) * 128], identf.t[:]),
                                     reads=[ht.r, identf.r], writes=[bank.r], signal=(cq == 3))
                            if c2 == 0:
                                P.op("act", lambda e: e.copy(out=hTf.t[:, 0:4, :], in_=bank.t[:].rearrange("p (a b) -> p a b", a=4)),
                                     reads=[bank.r], writes=[hTf.res[0]])
                            else:
                                P.op("dve", lambda e: e.tensor_copy(out=hTf.t[:, 4:8, :], in_=bank.t[:].rearrange("p (a b) -> p a b", a=4)),
                                     reads=[bank.r], writes=[hTf.res[1]])
                        bl = next_pf()
                        while bl is bct:
                            bl = next_pf()
                        mm_group(bl, (0, 20), [hTf.t[:, c, :] for c in range(8)], [wrg.t[:, c, :] for c in range(8)],
                                 [[hTf.res[c // 4], wrg.r] for c in range(8)])
                        P.op("dve", lambda e: e.scalar_tensor_tensor(out=lg4.t[:, j, :], in0=bl.t[:, 0:20],
                                                                     scalar=rstd.t[:, j:j + 1], in1=rbias.t[:],
                                                                     op0=ALU.mult, op1=ALU.add),
                             reads=[bl.r, rstd.r, rbias.r], writes=[lg4.r])

                def s3_Bb(i):
                    V = lambda fn, rd, wr: P.op("dve", fn, reads=rd, writes=wr)
                    S3 = [128, 4, 4]
                    bc = lambda ap_: ap_.unsqueeze(2).to_broadcast(S3)
                    glg = lg4.t[:, :, 0:4]
                    V(lambda e: e.reduce_max(out=r4.t[:, 0, :], in_=glg, axis=AX.X), [lg4.r], [r4.r])
                    V(lambda e: e.tensor_tensor(out=mg4.t[:], in0=glg, in1=bc(r4.t[:, 0, :]), op=ALU.is_ge),
                      [lg4.r, r4.r], [mg4.r])
                    V(lambda e: e.tensor_tensor(out=t4.t[:], in0=glg, in1=bc(r4.t[:, 0, :]), op=ALU.subtract),
                      [lg4.r, r4.r], [t4.r])
                    P.op("act", lambda e: e.activation(out=t4.t[:], in_=t4.t[:], func=AF.Exp), reads=[t4.r], writes=[t4.r])
                    V(lambda e: e.reduce_sum(out=r4.t[:, 1, :], in_=t4.t[:], axis=AX.X), [t4.r, r4.r], [r4.r])
                    V(lambda e: e.reciprocal(out=r4.t[:, 2, :], in_=r4.t[:, 1, :]), [r4.r], [r4.r])
                    el4 = lg4.t[:, :, 4:20].rearrange("p j (g i) -> p j g i", g=4)
                    V(lambda e: e.tensor_tensor(out=pr4.t[:], in0=el4,
                                                in1=mg4.t[:].unsqueeze(3).to_broadcast([128, 4, 4, 4]), op=ALU.mult),
                      [lg4.r, mg4.r], [pr4.r])
                    V(lambda e: e.reduce_sum(out=es4.t[:], in_=pr4.t[:].rearrange("p j g i -> p j i g"), axis=AX.X),
                      [pr4.r], [es4.r])
                    V(lambda e: e.reduce_max(out=r4.t[:, 3, :], in_=es4.t[:], axis=AX.X), [es4.r, r4.r], [r4.r])
                    V(lambda e: e.tensor_tensor(out=eq4.t[:], in0=es4.t[:], in1=bc(r4.t[:, 3, :]), op=ALU.is_ge),
                      [es4.r, r4.r], [eq4.r])
                    V(lambda e: e.scalar_tensor_tensor(out=t4.t[:], in0=eq4.t[:], scalar=-1e30, in1=es4.t[:],
                                                       op0=ALU.mult, op1=ALU.add), [eq4.r, es4.r, t4.r], [t4.r])
                    V(lambda e: e.reduce_max(out=r4.t[:, 4, :], in_=t4.t[:], axis=AX.X), [t4.r, r4.r], [r4.r])
                    V(lambda e: e.tensor_tensor(out=eq4.t[:], in0=es4.t[:], in1=bc(r4.t[:, 4, :]), op=ALU.is_ge),
                      [es4.r, r4.r, eq4.r], [eq4.r])
                    V(lambda e: e.tensor_tensor(out=t4.t[:], in0=es4.t[:], in1=bc(r4.t[:, 3, :]), op=ALU.subtract),
                      [es4.r, r4.r, t4.r], [t4.r])
                    P.op("act", lambda e: e.activation(out=t4.t[:], in_=t4.t[:], func=AF.Exp), reads=[t4.r], writes=[t4.r])
                    V(lambda e: e.tensor_tensor(out=t4.t[:], in0=t4.t[:], in1=eq4.t[:], op=ALU.mult),
                      [t4.r, eq4.r], [t4.r])
                    V(lambda e: e.reduce_sum(out=r4.t[:, 5, :], in_=t4.t[:], axis=AX.X), [t4.r, r4.r], [r4.r])
                    V(lambda e: e.reciprocal(out=r4.t[:, 6, :], in_=r4.t[:, 5, :]), [r4.r], [r4.r])
                    V(lambda e: e.tensor_tensor(out=r4.t[:, 6, :], in0=r4.t[:, 6, :], in1=r4.t[:, 2, :], op=ALU.mult),
                      [r4.r], [r4.r])
                    V(lambda e: e.tensor_tensor(out=t4.t[:], in0=t4.t[:], in1=bc(r4.t[:, 6, :]), op=ALU.mult),
                      [t4.r, r4.r], [t4.r])
                    V(lambda e: e.tensor_tensor(out=comb.t[:].rearrange("p j (g i) -> p j g i", g=4),
                                                in0=t4.t[:].unsqueeze(2).to_broadcast([128, 4, 4, 4]),
                                                in1=mg4.t[:].unsqueeze(3).to_broadcast([128, 4, 4, 4]), op=ALU.mult),
                      [t4.r, mg4.r], [comb.r])

                def s3_B2(i):
                    for j in range(4):
                        P.op("pe", lambda e: e.transpose(bct.t[0:16, j * 128:(j + 1) * 128], comb.t[:, j, :], identf.t[:]),
                             reads=[comb.r, identf.r], writes=[bct.r])
                    P.op("act", lambda e: e.copy(out=cmbT.t[:], in_=bct.t[0:16, :]), reads=[bct.r], writes=[cmbT.r])
                    P.dma("sp", cmb_d[:, i * TS:(i + 1) * TS], cmbT.t[:], reads=[cmbT.r], writes=[R_cmb[i]])

                bct = pf.pop()
                s3_load(0)
                if NT > 1:
                    s3_load(1)
                s3_A(0)
                s3_A2(0)
                for i in range(NT):
                    if i + 1 < NT:
                        s3_A(i + 1)
                    if i >= 1:
                        s3_B2(i - 1)
                    s3_Ba(i)
                    if i + 2 < NT:
                        s3_load(i + 2)
                    if i + 1 < NT:
                        s3_A2(i + 1)
                    s3_Bb(i)
                s3_B2(NT - 1)
                P.barrier()
            st_wout.close()
            if dbg == "s3":
                stm.close()
                st_ple.close()
                break

            wgs.append(sbuf(stm, "wgs1", [128, 4, 8, 256], BF16))
            wus.append(sbuf(stm, "wus1", [128, 4, 8, 256], BF16))
            wds.append(sbuf(stm, "wds1", [128, 4, 2, D], BF16))
            with contextlib.ExitStack() as st:
                set_psum(st, 8, 0)
                hts = [sbuf(st, "s4_ht%d" % k, [128, 4, D], F32) for k in range(2)]
                xTs = [sbuf(st, "s4_xT%d" % k, [128, 8, TS], BF16) for k in range(2)]
                cms = [sbuf(st, "s4_cm%d" % k, [16, TS], F32) for k in range(2)]
                cbs = sbuf(st, "s4_cb", [128, TS], F32)
                sg_ = sbuf(st, "s4_sg", [128, TS], F32)
                tt_ = sbuf(st, "s4_tt", [128, TS], F32)
                hdn = sbuf(st, "s4_hdn", [128, 4, 2, TS], BF16)

                def s4_load(i):
                    k = i % 2
                    P.dma("sp", hts[k].t[:], tm(h_d, i), reads=[R_h[i]], writes=[hts[k].r])
                    P.dma("sp", xTs[k].t[:], fm(xT_d, i), reads=[R_xT[i]], writes=[xTs[k].r])
                    P.dma("sp", cms[k].t[:], cmb_d[:, i * TS:(i + 1) * TS], reads=[R_cmb[i]], writes=[cms[k].r])

                s4_load(0)
                for pz in range(4):
                    slot = pz % 2
                    if pz + 1 < 4:
                        load_experts(pz + 1, (pz + 1) % 2)
                    if pz == 0:
                        for c in range(8):
                            P.dma("pool", wpg.t[:, c, :], pgw_d[l, c * 128:(c + 1) * 128, :], writes=[wpg.r])
                        P.dma("pool", wpp.t[:], ppj_d[l].rearrange("(c p) n -> p c n", p=128), writes=[wpp.r])
                    for i in range(NT):
                        k = i % 2
                        if i + 1 < NT:
                            s4_load(i + 1)
                        elif pz + 1 < 4:
                            s4_load(0)
                        ht, xT_, cm = hts[k], xTs[k], cms[k]
                        for el in range(4):
                            eidx = pz * 4 + el
                            bcb = next_pf()
                            P.op("pe", lambda e: e.matmul(bcb.t[:], sel.t[0:16, eidx, :], cm.t[0:16, :],
                                                          start=True, stop=True),
                                 reads=[sel.r, cm.r], writes=[bcb.r])
                            P.op("act", lambda e: e.copy(out=cbs.t[:], in_=bcb.t[:]), reads=[bcb.r], writes=[cbs.r])
                            for hc in range(2):
                                bg_ = next_pf()
                                mm_group(bg_, (0, TS), [wgs[slot].t[:, el, c, hc * 128:(hc + 1) * 128] for c in range(8)],
                                         [xT_.t[:, c, :] for c in range(8)], [[wgs[slot].r, xT_.r]] * 8)
                                bu_ = next_pf()
                                mm_group(bu_, (0, TS), [wus[slot].t[:, el, c, hc * 128:(hc + 1) * 128] for c in range(8)],
                                         [xT_.t[:, c, :] for c in range(8)], [[wus[slot].r, xT_.r]] * 8)
                                P.op("act", lambda e: e.activation(out=sg_.t[:], in_=bg_.t[:], func=AF.Silu),
                                     reads=[bg_.r], writes=[sg_.r])
                                P.op("dve", lambda e: e.tensor_tensor(out=tt_.t[:], in0=bu_.t[:], in1=cbs.t[:],
                                                                      op=ALU.mult),
                                     reads=[bu_.r, cbs.r], writes=[tt_.r])
                                P.op("dve", lambda e: e.tensor_tensor(out=hdn.t[:, el, hc, :], in0=sg_.t[:], in1=tt_.t[:],
                                                                      op=ALU.mult),
                                     reads=[sg_.r, tt_.r], writes=[hdn.r])
                        for j in range(4):
                            for half in range(2):
                                by = next_pf()
                                mm_group(by, (0, 512),
                                         [hdn.t[:, el, hc, j * 128:(j + 1) * 128] for el in range(4) for hc in range(2)],
                                         [wds[slot].t[:, el, hc, half * 512:(half + 1) * 512]
                                          for el in range(4) for hc in range(2)],
                                         [[hdn.r, wds[slot].r]] * 8)
                                P.op("dve", lambda e: e.tensor_tensor(out=ht.t[:, j, half * 512:(half + 1) * 512],
                                                                      in0=by.t[:],
                                                                      in1=ht.t[:, j, half * 512:(half + 1) * 512],
                                                                      op=ALU.add),
                                     reads=[by.r, ht.r], writes=[ht.r])
                        P.dma("sp", tm(h_d, i), ht.t[:], reads=[ht.r], writes=[R_h[i]])
                P.barrier()
            stm.close()
            if dbg == "s4":
                st_ple.close()
                break

            with contextlib.ExitStack() as st:
                set_psum(st, 6, 2)
                gbf = sbuf(st, "s5_gbf", [1, D], F32)
                gbb = sbuf(st, "s5_gbb", [1, D], BF16)
                hts = [sbuf(st, "s5_ht%d" % k, [128, 4, D], F32) for k in range(3)]
                pts_ = [sbuf(st, "s5_p%d" % k, [128, 4, 256], F32) for k in range(3)]
                ss2 = [sbuf(st, "s5_ss%d" % k, [128, 4], F32) for k in range(2)]
                rstd2 = [sbuf(st, "s5_rstd%d" % k, [128, 4], F32) for k in range(2)]
                sqj = sbuf(st, "s5_sqj", [128, D], BF16)
                xn2 = [sbuf(st, "s5_xn%d" % k, [128, 4, D], BF16, 4) for k in range(2)]
                xT2 = [sbuf(st, "s5_xT%d" % k, [128, 8, TS], BF16, 8) for k in range(2)]
                pT2 = [sbuf(st, "s5_pT%d" % k, [128, 2, TS], BF16) for k in range(2)]
                gts = [sbuf(st, "s5_gt%d" % k, [128, 512], F32) for k in range(3)]
                tqs = [sbuf(st, "s5_tq%d" % k, [128, 512], F32) for k in range(3)]
                if l + 1 < DEPTH:
                    st_win, w_in_next = load_w_in(l + 1)
                P.dma("sp", gbf.t[:], pgb_d[l:l + 1, :], writes=[gbf.r])
                P.op("dve", lambda e: e.tensor_copy(out=gbb.t[:], in_=gbf.t[:]), reads=[gbf.r], writes=[gbb.r])

                def s5_load(i):
                    k = i % 3
                    P.dma("sp", hts[k].t[:], tm(h_d, i), reads=[R_h[i]], writes=[hts[k].r])
                    P.dma("sp", pts_[k].t[:], tm(p_d[l], i), writes=[pts_[k].r])

                def s5_P(i):
                    k = i % 2
                    ht, pt_ = hts[i % 3], pts_[i % 3]
                    ss, rstd, xn, xT, pT = ss2[k], rstd2[k], xn2[k], xT2[k], pT2[k]
                    norm_stats(st, ht, 0, ss, rstd, sqj)
                    norm_transpose(ht, 0, rstd, xn, xT, PC_PLEG)
                    for c in range(2):
                        bank = next_pf()
                        for j in range(4):
                            P.op("pe", lambda e: e.transpose(bank.t[:, j * 128:(j + 1) * 128],
                                                             pt_.t[:, j, c * 128:(c + 1) * 128], identf.t[:]),
                                 reads=[pt_.r, identf.r], writes=[bank.r], signal=(j == 3))
                        P.op("act", lambda e: e.copy(out=pT.t[:, c, :], in_=bank.t[:]), reads=[bank.r], writes=[pT.r])

                def s5_M(i):
                    k = i % 2
                    ht, pt_ = hts[i % 3], pts_[i % 3]
                    ss, rstd, xn, xT, pT = ss2[k], rstd2[k], xn2[k], xT2[k], pT2[k]
                    for j in range(4):
                        for half in range(2):
                            hs = slice(half * 512, (half + 1) * 512)
                            gt = gts[(j * 2 + half) % 3]
                            tq = tqs[(j * 2 + half) % 3]
                            bg_ = next_pf()
                            mm_group(bg_, (0, 512),
                                     [xT.t[:, c, j * 128:(j + 1) * 128] for c in range(8)] + [onesrow.t[0:1, :]],
                                     [wpg.t[:, c, hs] for c in range(8)] + [gbb.t[0:1, hs]],
                                     [[xT.res[c], wpg.r] for c in range(8)] + [[onesrow.r, gbb.r]])
                            bp_ = next_pf()
                            mm_group(bp_, (0, 512), [pT.t[:, c, j * 128:(j + 1) * 128] for c in range(2)],
                                     [wpp.t[:, c, hs] for c in range(2)], [[pT.r, wpp.r]] * 2)
                            P.op("act", lambda e: e.activation(out=gt.t[:], in_=bg_.t[:], func=AF.Sigmoid),
                                 reads=[bg_.r], writes=[gt.r])
                            P.op("dve", lambda e: e.tensor_tensor(out=tq.t[:], in0=bp_.t[:], in1=gt.t[:], op=ALU.mult),
                                 reads=[bp_.r, gt.r], writes=[tq.r])
                            P.op("dve", lambda e: e.tensor_tensor(out=ht.t[:, j, hs], in0=ht.t[:, j, hs], in1=tq.t[:],
                                                                  op=ALU.add),
                                 reads=[ht.r, tq.r], writes=[ht.r])
                    if l < DEPTH - 1:
                        P.dma("sp", tm(h_d, i), ht.t[:], reads=[ht.r], writes=[R_h[i]])
                    else:
                        norm_stats(st, ht, 0, ss, rstd, sqj)
                        for j in range(4):
                            P.op("dve", lambda e: e.scalar_tensor_tensor(out=ht.t[:, j, :], in0=ht.t[:, j, :],
                                                                         scalar=rstd.t[:, j:j + 1], in1=gfin.t[:],
                                                                         op0=ALU.mult, op1=ALU.mult),
                                 reads=[ht.r, rstd.r, gfin.r], writes=[ht.r])
                        P.dma("sp", tm(out_d, i), ht.t[:], reads=[ht.r], writes=[R_out[i]])

                s5_load(0)
                if NT > 1:
                    s5_load(1)
                s5_P(0)
                for i in range(NT):
                    if i + 2 < NT:
                        s5_load(i + 2)
                    if i + 1 < NT:
                        s5_P(i + 1)
                    s5_M(i)
                P.barrier()
            st_ple.close()

        P.barrier()
        P.final_wait()
    return P


def _host_consts():
    c = {}
    c["c_identf"] = np.eye(128, dtype=np.float32)
    rp = np.zeros((128, 128), np.float32)
    for b in range(4):
        for d in range(16):
            rp[b * 32 + d + 16, b * 32 + d] = -1.0
            rp[b * 32 + d, b * 32 + d + 16] = 1.0
    c["c_rperm"] = rp
    kk = np.arange(128)[:, None]
    qq = np.arange(128)[None, :]
    c["c_tri"] = (qq >= kk).astype(np.float32)
    b64 = np.zeros((128, 128), np.float32)
    b64[0:64, 0:64] = 1.0 / 64
    b64[64:128, 64:128] = 1.0 / 64
    c["c_blk64"] = b64
    c["c_ones256"] = np.full((128, 128), 1.0 / 256, np.float32)
    sel = np.zeros((16, NE, 128), np.float32)
    for e in range(NE):
        sel[e, e, :] = 1.0
    c["c_sel"] = sel
    pos = np.arange(S, dtype=np.float32)
    inv = (10000.0 ** (-np.arange(0, 32, 2, dtype=np.float32) / np.float32(32))).astype(np.float32)
    ang = (pos[:, None] * inv[None, :]).astype(np.float32)
    ang = np.concatenate([ang, ang], axis=-1)
    cosT = np.cos(ang.astype(np.float64)).astype(np.float32).T
    sinT = np.sin(ang.astype(np.float64)).astype(np.float32).T
    c["c_cos"] = np.ascontiguousarray(np.tile(cosT, (4, 1)))
    c["c_sin"] = np.ascontiguousarray(np.tile(sinT, (4, 1)))
    wins = np.array([2, 4, 8, 16])
    corr = np.zeros((128, 2, 16), np.float32)
    for cc in range(2):
        for p in range(128):
            w = wins[cc * 2 + p // 64]
            for t in range(16):
                corr[p, cc, t] = w / min(t + 1, w)
    c["c_corr"] = corr
    iw = np.zeros((128, 2), np.float32)
    for cc in range(2):
        for p in range(128):
            iw[p, cc] = 1.0 / wins[cc * 2 + p // 64]
    c["_iw"] = iw
    return c


def _fmcols(v, nch):
    return np.ascontiguousarray(np.asarray(v, np.float32).reshape(nch, 128).T)


def _layout_inputs(inp):
    c = _host_consts()
    iw = c.pop("_iw")
    shared = dict(c)
    prm = np.zeros((DEPTH, 128, PC_N), np.float32)
    wr = np.zeros((DEPTH, 128, 8, 20), np.float32)
    rb = np.zeros((DEPTH, 128, 20), np.float32)
    lamv = np.zeros((DEPTH, 128, 4, 32), np.float32)
    pw = np.zeros((DEPTH, 128, 2, 128), np.float32)
    for l in range(DEPTH):
        prm[l, :, PC_MIXG:PC_MIXG + 8] = _fmcols(inp["mix_norm"][l], 8)
        prm[l, :, PC_FFNG:PC_FFNG + 8] = _fmcols(inp["ffn_norm"][l], 8)
        prm[l, :, PC_PLEG:PC_PLEG + 8] = _fmcols(inp["ple_norm"][l], 8)
        prm[l, :, PC_FING:PC_FING + 8] = _fmcols(inp["final_norm"], 8)
        cw = np.asarray(inp["conf_conv_w"][l], np.float32)
        for cc in range(2):
            prm[l, :, PC_CW + cc * CK:PC_CW + (cc + 1) * CK] = cw[:, cc * 128:(cc + 1) * 128].T
        prm[l, :, PC_CB:PC_CB + 2] = _fmcols(inp["conf_conv_b"][l], 2)
        prm[l, :, PC_LNG:PC_LNG + 2] = _fmcols(inp["conf_ln_g"][l], 2)
        prm[l, :, PC_LNB:PC_LNB + 2] = _fmcols(inp["conf_ln_b"][l], 2)
        prm[l, :, PC_PB:PC_PB + 2] = _fmcols(np.asarray(inp["pool_b"][l]).reshape(256), 2)
        prm[l, :, PC_PS:PC_PS + 2] = _fmcols(inp["pool_scale"][l], 2)
        sw = np.asarray(inp["sconv_w"][l], np.float32)
        for cc in range(2):
            prm[l, :, PC_SW + cc * 3:PC_SW + (cc + 1) * 3] = sw[:, cc * 128:(cc + 1) * 128].T
        prm[l, :, PC_SUB] = np.tile(np.asarray(inp["diff_subln_g"][l], np.float32), 2)
        prm[l, :, PC_IW:PC_IW + 2] = iw
        wcat = np.concatenate([np.asarray(inp["router_group_w"][l], np.float32),
                               np.asarray(inp["router_expert_w"][l], np.float32)], axis=1)
        wr[l] = wcat.reshape(8, 128, 20).transpose(1, 0, 2)
        bcat = np.concatenate([np.asarray(inp["router_group_b"][l], np.float32),
                               np.asarray(inp["router_expert_b"][l], np.float32)])
        rb[l] = np.tile(bcat[None, :], (128, 1))
        for n_, key in enumerate(["diff_lam_q1", "diff_lam_k1", "diff_lam_q2", "diff_lam_k2"]):
            lamv[l, :, n_, :] = np.tile(np.asarray(inp[key][l], np.float32)[None, :], (128, 1))
        pwl = np.asarray(inp["pool_w"][l], np.float32)
        for g in range(4):
            cc, hh = g // 2, g % 2
            pw[l, hh * 64:(hh + 1) * 64, cc, hh * 64:(hh + 1) * 64] = pwl[g]
    shared.update({
        "prm": prm, "wr": wr, "rbias": rb, "lamv": lamv, "poolw": pw,
        "gfin": np.ascontiguousarray(np.tile(np.asarray(inp["final_norm"], np.float32)[None, :], (128, 1))),
    })
    for key in ["w_in", "w_out", "expert_w_gate", "expert_w_up", "expert_w_down", "ple_gate_w", "ple_gate_b",
                "ple_proj"]:
        shared[key] = np.ascontiguousarray(np.asarray(inp[key], np.float32))
    return shared


_NC_CACHE = {}


def _get_nc():
    if "nc" not in _NC_CACHE:
        nc = bass.Bass("TRN2", target_bir_lowering=False)
        build(nc)
        _NC_CACHE["nc"] = nc
    return _NC_CACHE["nc"]


def kernel(**inputs):
    shared = _layout_inputs(inputs)
    x = np.asarray(inputs["x"], np.float32)
    p = np.asarray(inputs["p"], np.float32)
    n = x.shape[0]
    in_maps = []
    for b in range(n):
        m = dict(shared)
        m["x"] = np.ascontiguousarray(x[b])
        m["p"] = np.ascontiguousarray(p[:, b])
        in_maps.append(m)
    nc = _get_nc()
    res = run_bass_kernel_spmd(nc, in_maps, core_ids=list(range(n)))
    return np.stack([np.asarray(r["out"], np.float32) for r in res.results], axis=0)
```

```python
import math
import contextlib
import numpy as np
import ml_dtypes
import concourse.bass as bass
import concourse.mybir as mybir
from concourse.bass_utils import run_bass_kernel_spmd

F32 = mybir.dt.float32
BF16 = mybir.dt.bfloat16
ALU = mybir.AluOpType
AF = mybir.ActivationFunctionType
AX = mybir.AxisListType

S = 4096
D = 1024
DEPTH = 2
NT = 8
TS = 512
INC = 2304
NE = 16
EPS = 1e-6
CK = 31
SCALE = 32 ** -0.5
SEM_CHUNK = 30000

PC_MIXG, PC_FFNG, PC_PLEG, PC_FING = 0, 8, 16, 24
PC_CW = 32
PC_CB = PC_CW + 62
PC_LNG = PC_CB + 2
PC_LNB = PC_LNG + 2
PC_PB = PC_LNB + 2
PC_PS = PC_PB + 2
PC_SW = PC_PS + 2
PC_SUB = PC_SW + 6
PC_IW = PC_SUB + 1
PC_N = PC_IW + 2


class Tok:
    __slots__ = ("sem", "val", "eng")

    def __init__(self, eng):
        self.sem = None
        self.val = None
        self.eng = eng


class Res:
    __slots__ = ("name", "w", "r", "excl")

    def __init__(self, name, excl=False):
        self.name = name
        self.w = None
        self.r = []
        self.excl = excl


class Prog:
    def __init__(self, nc, es):
        self.nc = nc
        self.es = es
        self.eobj = {"pe": nc.tensor, "act": nc.scalar, "dve": nc.vector,
                     "pool": nc.gpsimd, "sp": nc.sync}
        self.sems = {e: [] for e in self.eobj}
        self.count = {e: 0 for e in self.eobj}
        self.known = {e: {} for e in self.eobj}
        self.pending = {e: None for e in self.eobj}
        self.last_tok = {e: None for e in self.eobj}
        self.nsem = 0
        self.dma_sems = []
        self.dma_cnt = []
        self.dma_i = 0
        self.dma_ip = 0
        for i in range(24):
            self.dma_sems.append(self._new_sem("dq%d" % i))
            self.dma_cnt.append(0)
        self.dma_toks = []
        self.n_ops = 0
        self.n_waits = 0
        self.limit = None
        self.stores_on_pool = False
        self.n_all = 0
        self.skip = False

    def _skipping(self):
        if self.skip:
            return True
        if self.limit is not None and self.n_all > self.limit and all(v is None for v in self.pending.values()):
            self.skip = True
            return True
        return False

    def _new_sem(self, name):
        self.nsem += 1
        return self.es.enter_context(self.nc.semaphore(name))

    def _eng_tok(self, eng, tok):
        c = self.count[eng]
        idx = c // SEM_CHUNK
        while len(self.sems[eng]) <= idx:
            self.sems[eng].append(self._new_sem("%s%d" % (eng, len(self.sems[eng]))))
        tok.sem = self.sems[eng][idx]
        tok.val = c % SEM_CHUNK + 1
        self.count[eng] = c + 1
        return tok

    def _wait(self, eng, tok):
        if tok is None:
            return
        if tok.sem is None:
            assert tok.eng == eng, "dependency on unsignaled op of %s from %s" % (tok.eng, eng)
            return
        k = self.known[eng]
        sid = id(tok.sem)
        if k.get(sid, 0) >= tok.val:
            return
        k[sid] = tok.val
        self.eobj[eng].wait_ge(tok.sem, tok.val)
        self.n_waits += 1

    def _deps(self, eng, reads, writes, is_dma=False):
        for r in reads:
            if r.w is not None:
                if r.w.eng == eng and not is_dma and eng == "pe":
                    continue
                self._wait(eng, r.w)
        same_ok = (eng == "pe") and not is_dma
        for w in writes:
            if w.w is not None and not (same_ok and w.w.eng == eng):
                self._wait(eng, w.w)
            for t in w.r:
                if not (same_ok and t.eng == eng):
                    self._wait(eng, t)

    def op(self, eng, fn, reads=(), writes=(), signal=True):
        self.n_all += 1
        if self._skipping():
            return None
        if any(r.excl for r in reads):
            writes = list(writes) + [r for r in reads if r.excl and r not in writes]
            reads = [r for r in reads if not r.excl]
        self._deps(eng, reads, writes)
        ins = fn(self.eobj[eng])
        self.n_ops += 1
        tok = self.pending[eng]
        if tok is None:
            tok = Tok(eng)
            self.pending[eng] = tok
        if signal:
            self._eng_tok(eng, tok)
            ins.then_inc(tok.sem, 1)
            self.pending[eng] = None
            self.last_tok[eng] = tok
        for r in reads:
            r.r.append(tok)
        for w in writes:
            w.w = tok
            w.r = []
        return tok

    def dma(self, q, out, in_, reads=(), writes=()):
        if q == "sp" and self.stores_on_pool and str(out.space).endswith("DRAM"):
            q = "pool"
        self.n_all += 1
        if self._skipping():
            return None
        self._deps(q, reads, writes, is_dma=True)
        if q == "pool":
            i = 16 + self.dma_ip % 8
            self.dma_ip += 1
        else:
            i = self.dma_i % 16
            self.dma_i += 1
        sem = self.dma_sems[i]
        if self.dma_cnt[i] > 0:
            prev = Tok("dma")
            prev.sem = sem
            prev.val = self.dma_cnt[i]
            self._wait(q, prev)
        self.eobj[q].dma_start(out=out, in_=in_).then_inc(sem, 16)
        self.dma_cnt[i] += 16
        tok = Tok("dma")
        tok.sem = sem
        tok.val = self.dma_cnt[i]
        self.dma_toks.append(tok)
        for r in reads:
            r.r.append(tok)
        for w in writes:
            w.w = tok
            w.r = []
        return tok

    def barrier(self):
        toks = [t for t in self.last_tok.values() if t is not None]
        for i, s in enumerate(self.dma_sems):
            if self.dma_cnt[i] > 0:
                t = Tok("dma")
                t.sem = s
                t.val = self.dma_cnt[i]
                toks.append(t)
        for e in self.eobj:
            assert self.pending[e] is None
            for t in toks:
                if t.eng == e:
                    continue
                self._wait(e, t)

    def final_wait(self):
        for i, s in enumerate(self.dma_sems):
            if self.dma_cnt[i] > 0:
                t = Tok("dma")
                t.sem = s
                t.val = self.dma_cnt[i]
                self._wait("sp", t)


class Buf:
    def __init__(self, t, name, nslots=1):
        self.t = t
        self.res = [Res("%s.%d" % (name, i)) for i in range(nslots)]

    @property
    def r(self):
        return self.res[0]


def build(nc, dbg=None, limit=None):
    P = None
    with contextlib.ExitStack() as es:
        P = Prog(nc, es)
        P.limit = limit

        def dram_in(name, shape, dt=F32):
            return nc.dram_tensor(name, list(shape), dt, kind="ExternalInput").ap()

        def dram_scr(name, shape, dt, kind="Internal"):
            return nc.dram_tensor(name, list(shape), dt, kind=kind).ap()

        x_d = dram_in("x", [S, D])
        p_d = dram_in("p", [DEPTH, S, 256])
        w_in_d = dram_in("w_in", [DEPTH, D, INC])
        w_out_d = dram_in("w_out", [DEPTH, D, D])
        wg_d = dram_in("expert_w_gate", [DEPTH, NE, D, 256])
        wu_d = dram_in("expert_w_up", [DEPTH, NE, D, 256])
        wd_d = dram_in("expert_w_down", [DEPTH, NE, 256, D])
        pgw_d = dram_in("ple_gate_w", [DEPTH, D, D])
        pgb_d = dram_in("ple_gate_b", [DEPTH, D])
        ppj_d = dram_in("ple_proj", [DEPTH, 256, D])
        prm_d = dram_in("prm", [DEPTH, 128, PC_N])
        wr_d = dram_in("wr", [DEPTH, 128, 8, 20])
        rb_d = dram_in("rbias", [DEPTH, 128, 20])
        lam_d = dram_in("lamv", [DEPTH, 128, 4, 32])
        pw_d = dram_in("poolw", [DEPTH, 128, 2, 128])
        gfin_d = dram_in("gfin", [128, D])
        cidf_d = dram_in("c_identf", [128, 128])
        crp_d = dram_in("c_rperm", [128, 128])
        ctri_d = dram_in("c_tri", [128, 128])
        cb64_d = dram_in("c_blk64", [128, 128])
        cones_d = dram_in("c_ones256", [128, 128])
        csel_d = dram_in("c_sel", [16, NE, 128])
        ccos_d = dram_in("c_cos", [128, S])
        csin_d = dram_in("c_sin", [128, S])
        ccorr_d = dram_in("c_corr", [128, 2, 16])
        out_d = nc.dram_tensor("out", [S, D], F32, kind="ExternalOutput").ap()

        dkind = "ExternalOutput" if dbg else "Internal"
        h_d = dram_scr("h_scr", [S, D], F32, dkind)
        glu_d = dram_scr("glu_scr", [256, S], BF16, dkind)
        pin_d = dram_scr("pin_scr", [256, S], F32, dkind)
        q_d = dram_scr("q_scr", [256, S], BF16, dkind)
        k_d = dram_scr("k_scr", [256, S], BF16, dkind)
        v_d = dram_scr("v_scr", [S, 512], BF16, dkind)
        gb_d = dram_scr("gb_scr", [256, S], F32, dkind)
        gcv_d = dram_scr("gcv_scr", [256, S], F32, dkind)
        mix_d = dram_scr("mix_scr", [D, S], BF16, dkind)
        xT_d = dram_scr("xT_scr", [D, S], BF16, dkind)
        cmb_d = dram_scr("cmb_scr", [16, S], F32, dkind)

        def tiles(name):
            return [Res("%s%d" % (name, i)) for i in range(NT)]
        R_h = tiles("h")
        R_glu, R_pin, R_q, R_k, R_v = tiles("glu"), tiles("pin"), tiles("q"), tiles("k"), tiles("v")
        R_gb, R_gcv, R_mixA, R_mixB, R_xT, R_cmb = (tiles("gb"), tiles("gcv"), tiles("mixA"),
                                                    tiles("mixB"), tiles("xT"), tiles("cmb"))
        R_out = tiles("out")

        def fm(ap, i, lo=0, hi=TS):
            return ap.rearrange("(c p) t -> p c t", p=128)[:, :, i * TS + lo:i * TS + hi]

        def tm(ap, i):
            return ap[i * TS:(i + 1) * TS, :].rearrange("(j p) f -> p j f", p=128)

        uid = [0]
        def sbuf(stack, name, shape, dt, nslots=1):
            uid[0] += 1
            t = stack.enter_context(nc.sbuf_tensor("%s_u%d" % (name, uid[0]), list(shape), dt))
            return Buf(t, name, nslots)

        def psum(stack, name, shape, dt=F32):
            t = stack.enter_context(nc.psum_tensor(name, list(shape), dt))
            b = Buf(t, name, 1)
            b.res[0].excl = True
            return b

        identf = sbuf(es, "identf", [128, 128], F32)
        identb = sbuf(es, "identb", [128, 128], BF16)
        rperm = sbuf(es, "rperm", [128, 128], F32)
        trib = sbuf(es, "trib", [128, 128], BF16)
        blk64 = sbuf(es, "blk64", [128, 128], F32)
        ones256 = sbuf(es, "ones256", [128, 128], F32)
        sel = sbuf(es, "sel", [16, NE, 128], F32)
        prm = sbuf(es, "prm_sb", [128, PC_N], F32)
        corr = sbuf(es, "corr", [128, 2, 16], F32)
        gfin = sbuf(es, "gfin_sb", [128, D], F32)
        neglam = sbuf(es, "neglam", [128, 1], F32)
        pbs = sbuf(es, "pbs", [128, 2], F32)
        onesrow = sbuf(es, "onesrow", [1, 128], BF16)
        epsc = sbuf(es, "epsc", [128, 1], F32)

        pf = []
        pb = []
        pf_i = [0]
        pb_i = [0]

        def set_psum(stack, nf, nb):
            uid[0] += 1
            pf[:] = [psum(stack, "pf%d_%d" % (i, uid[0]), [128, 512], F32) for i in range(nf)]
            pb[:] = [psum(stack, "pb%d_%d" % (i, uid[0]), [128, 1024], BF16) for i in range(nb)]

        def next_pf():
            b = pf[pf_i[0] % len(pf)]
            pf_i[0] += 1
            return b

        def next_pb():
            b = pb[pb_i[0] % len(pb)]
            pb_i[0] += 1
            return b

        P.dma("sp", identf.t[:], cidf_d, writes=[identf.r])
        P.dma("sp", rperm.t[:], crp_d, writes=[rperm.r])
        P.dma("sp", blk64.t[:], cb64_d, writes=[blk64.r])
        P.dma("sp", ones256.t[:], cones_d, writes=[ones256.r])
        P.dma("sp", sel.t[:], csel_d, writes=[sel.r])
        P.dma("sp", corr.t[:], ccorr_d, writes=[corr.r])
        P.dma("sp", gfin.t[:], gfin_d, writes=[gfin.r])
        P.dma("pool", identb.t[:], cidf_d, writes=[identb.r])
        P.dma("pool", trib.t[:], ctri_d, writes=[trib.r])
        P.op("dve", lambda e: e.memset(onesrow.t[:], 1.0), writes=[onesrow.r])
        P.op("dve", lambda e: e.memset(epsc.t[:], EPS), writes=[epsc.r])

        def norm_stats(st, ht, slot, ss, rstd, sqj):
            P.op("dve", lambda e: e.memset(ss.t[:], 0.0), writes=[ss.r])
            for j in range(4):
                P.op("act", lambda e, j=j: e.activation(out=sqj.t[:], in_=ht.t[:, j, :], func=AF.Square,
                                                        accum_out=ss.t[:, j:j + 1]),
                     reads=[ht.res[slot], ss.r], writes=[sqj.r, ss.r])
            P.op("dve", lambda e: e.tensor_scalar(out=rstd.t[:], in0=ss.t[:], scalar1=1.0 / D, scalar2=EPS,
                                                  op0=ALU.mult, op1=ALU.add), reads=[ss.r], writes=[rstd.r])
            P.op("act", lambda e: e.activation(out=rstd.t[:], in_=rstd.t[:], func=AF.Sqrt),
                 reads=[rstd.r], writes=[rstd.r])
            P.op("dve", lambda e: e.reciprocal(out=rstd.t[:], in_=rstd.t[:]), reads=[rstd.r], writes=[rstd.r])

        def norm_transpose(ht, slot, rstd, xn, xT, gcol, part=None):
            for j in range(4 if part in (None, 0) else 0):
                P.op("dve", lambda e, j=j: e.tensor_scalar(out=xn.t[:, j, :], in0=ht.t[:, j, :],
                                                           scalar1=rstd.t[:, j:j + 1], scalar2=None, op0=ALU.mult),
                     reads=[ht.res[slot], rstd.r], writes=[xn.res[j]])
            for c2 in range(4 if part in (None, 1) else 0):
                bank = next_pb()
                for cc in range(2):
                    c = c2 * 2 + cc
                    for j in range(4):
                        last = (cc == 1 and j == 3)
                        P.op("pe", lambda e, c=c, cc=cc, j=j: e.transpose(
                            bank.t[:, cc * 512 + j * 128: cc * 512 + (j + 1) * 128],
                            xn.t[:, j, c * 128:(c + 1) * 128], identb.t[:]),
                            reads=[xn.res[j], identb.r], writes=[bank.r], signal=last)
                for cc in range(2):
                    c = c2 * 2 + cc
                    if cc == 0:
                        P.op("act", lambda e, c=c, cc=cc: e.activation(
                            out=xT.t[:, c, :], in_=bank.t[:, cc * 512:(cc + 1) * 512], func=AF.Identity,
                            scale=prm.t[:, gcol + c:gcol + c + 1]),
                            reads=[bank.r, prm.r], writes=[xT.res[c]])
                    else:
                        P.op("dve", lambda e, c=c, cc=cc: e.tensor_scalar(
                            out=xT.t[:, c, :], in0=bank.t[:, cc * 512:(cc + 1) * 512],
                            scalar1=prm.t[:, gcol + c:gcol + c + 1], scalar2=None, op0=ALU.mult),
                            reads=[bank.r, prm.r], writes=[xT.res[c]])

        def load_h(i, ht, slot, src):
            P.dma("sp", ht.t[:], tm(src, i), reads=[R_h[i]], writes=[ht.res[slot]])

        def mm_group(bank, cols, lhs_list, rhs_list, reads, tile_position=None):
            n = len(lhs_list)
            for k in range(n):
                P.op("pe", lambda e, k=k: e.matmul(bank.t[:, cols[0]:cols[1]], lhs_list[k], rhs_list[k],
                                                   start=(k == 0), stop=(k == n - 1)),
                     reads=reads[k], writes=[bank.r], signal=(k == n - 1))

        def load_w_in(l_):
            stw = contextlib.ExitStack()
            uid[0] += 1
            t_ = stw.enter_context(nc.sbuf_tensor("w_in_sb_u%d" % uid[0], [128, 8, INC], BF16, side="right"))
            b_ = Buf(t_, "w_in_sb", 1)
            for c in range(8):
                P.dma("pool", b_.t[:, c, :], w_in_d[l_, c * 128:(c + 1) * 128, :], writes=[b_.r])
            return stw, b_

        st_win, w_in_next = load_w_in(0)
        for l in range(DEPTH):
            lam_init = 0.8 - 0.6 * math.exp(-0.3 * l)
            h_src = x_d if l == 0 else h_d

            P.barrier()
            P.dma("sp", prm.t[:], prm_d[l], writes=[prm.r])
            with contextlib.ExitStack() as st:
                lamv = sbuf(st, "lamv", [128, 4, 32], F32)
                lt = sbuf(st, "lt", [128, 2, 32], F32)
                ls = sbuf(st, "ls", [128, 2], F32)
                P.dma("sp", lamv.t[:], lam_d[l], writes=[lamv.r])
                P.op("dve", lambda e: e.tensor_tensor(out=lt.t[:, 0, :], in0=lamv.t[:, 0, :], in1=lamv.t[:, 1, :],
                                                      op=ALU.mult), reads=[lamv.r], writes=[lt.r])
                P.op("dve", lambda e: e.tensor_tensor(out=lt.t[:, 1, :], in0=lamv.t[:, 2, :], in1=lamv.t[:, 3, :],
                                                      op=ALU.mult), reads=[lamv.r, lt.r], writes=[lt.r])
                P.op("dve", lambda e: e.reduce_sum(out=ls.t[:], in_=lt.t[:], axis=AX.X), reads=[lt.r], writes=[ls.r])
                P.op("act", lambda e: e.activation(out=ls.t[:], in_=ls.t[:], func=AF.Exp), reads=[ls.r], writes=[ls.r])
                P.op("dve", lambda e: e.scalar_tensor_tensor(out=neglam.t[:], in0=ls.t[:, 1:2], scalar=-lam_init,
                                                             in1=ls.t[:, 0:1], op0=ALU.add, op1=ALU.subtract),
                     reads=[ls.r], writes=[neglam.r])
                P.op("dve", lambda e: e.tensor_tensor(out=pbs.t[:], in0=prm.t[:, PC_PB:PC_PB + 2],
                                                      in1=prm.t[:, PC_PS:PC_PS + 2], op=ALU.mult),
                     reads=[prm.r], writes=[pbs.r])
                P.barrier()

            st_dg = contextlib.ExitStack()
            dg = sbuf(st_dg, "s2_dg", [128, 2, CK, 128], BF16)
            pw_sb = sbuf(st_dg, "s2_pw", [128, 2, 128], BF16)
            with contextlib.ExitStack() as st:
                set_psum(st, 6, 2)
                w_in_sb = w_in_next
                ht = sbuf(st, "s1_ht", [128, 4, D], F32, 2)
                hts = [ht, sbuf(st, "s1_ht2", [128, 4, D], F32, 2)]
                ss2 = [sbuf(st, "s1_ss%d" % k, [128, 4], F32) for k in range(2)]
                rstd2 = [sbuf(st, "s1_rstd%d" % k, [128, 4], F32) for k in range(2)]
                sqj = sbuf(st, "s1_sqj", [128, D], BF16)
                xn2 = [sbuf(st, "s1_xn%d" % k, [128, 4, D], BF16, 4) for k in range(2)]
                nT2 = [sbuf(st, "s1_nT%d" % k, [128, 8, TS], BF16, 8) for k in range(2)]
                sig = sbuf(st, "s1_sig", [128, TS], F32)
                gcs = sbuf(st, "s1_gc", [128, TS], F32)
                glu_st = sbuf(st, "s1_glu", [128, 2, TS], BF16)
                pin_st = sbuf(st, "s1_pin", [128, 2, TS], F32)
                qk_st = sbuf(st, "s1_qk", [128, 4, TS], F32, 4)
                qkr_st = sbuf(st, "s1_qkr", [128, 4, TS], BF16, 4)
                gb_st = sbuf(st, "s1_gb", [128, 2, TS], F32)
                gcv_st = sbuf(st, "s1_gcv", [128, 2, TS], F32)
                v_st = sbuf(st, "s1_v", [128, 4, 512], BF16)
                cos_t = sbuf(st, "s1_cos", [128, TS], F32)
                sin_t = sbuf(st, "s1_sin", [128, TS], F32)
                t1 = sbuf(st, "s1_t1", [128, TS], F32)
                t2 = sbuf(st, "s1_t2", [128, TS], F32)

                P.op("dve", lambda e: e.memset(v_st.t[:], 1.0), writes=[v_st.r])

                def s1_prologue(i_):
                    norm_stats(st, hts[i_ % 2], 0, ss2[i_ % 2], rstd2[i_ % 2], sqj)
                    norm_transpose(hts[i_ % 2], 0, rstd2[i_ % 2], xn2[i_ % 2], nT2[i_ % 2], PC_MIXG)

                P.dma("sp", hts[0].t[:], tm(h_src, 0), reads=[R_h[0]], writes=[hts[0].r])
                P.dma("sp", hts[1].t[:], tm(h_src, 1), reads=[R_h[1]], writes=[hts[1].r])
                s1_prologue(0)
                P.dma("pool", pw_sb.t[:], pw_d[l], writes=[pw_sb.r])
                for cc in range(2):
                    for j in range(CK):
                        col = PC_CW + cc * CK + j
                        P.op("dve", lambda e, cc=cc, j=j, col=col: e.tensor_scalar(
                            out=dg.t[:, cc, j, :], in0=identf.t[:], scalar1=prm.t[:, col:col + 1], scalar2=None,
                            op0=ALU.mult), reads=[identf.r, prm.r], writes=[dg.r])
                for i in range(NT):
                    P.dma("sp", cos_t.t[:], ccos_d[:, i * TS:(i + 1) * TS], writes=[cos_t.r])
                    P.dma("sp", sin_t.t[:], csin_d[:, i * TS:(i + 1) * TS], writes=[sin_t.r])
                    nT = nT2[i % 2]

                    def proj(col0):
                        bank = next_pf()
                        mm_group(bank, (0, TS), [w_in_sb.t[:, c, col0:col0 + 128] for c in range(8)],
                                 [nT.t[:, c, :] for c in range(8)],
                                 [[w_in_sb.r, nT.res[c]] for c in range(8)])
                        return bank

                    for cc in range(2):
                        bg_ = proj(256 + cc * 128)
                        P.op("act", lambda e: e.activation(out=sig.t[:], in_=bg_.t[:], func=AF.Sigmoid),
                             reads=[bg_.r], writes=[sig.r])
                        bv_ = proj(0 + cc * 128)
                        P.op("dve", lambda e: e.tensor_tensor(out=glu_st.t[:, cc, :], in0=bv_.t[:], in1=sig.t[:],
                                                              op=ALU.mult),
                             reads=[bv_.r, sig.r], writes=[glu_st.r])
                    for cc in range(2):
                        bp_ = proj(512 + cc * 128)
                        P.op("act", lambda e: e.copy(out=pin_st.t[:, cc, :], in_=bp_.t[:]),
                             reads=[bp_.r], writes=[pin_st.r])
                    if i + 1 < NT:
                        s1_prologue(i + 1)
                    for m in range(4):
                        bq_ = proj(768 + m * 128)
                        if m % 2 == 0:
                            P.op("act", lambda e: e.copy(out=qk_st.t[:, m, :], in_=bq_.t[:]),
                                 reads=[bq_.r], writes=[qk_st.res[m]])
                        else:
                            P.op("dve", lambda e: e.tensor_copy(out=qk_st.t[:, m, :], in_=bq_.t[:]),
                                 reads=[bq_.r], writes=[qk_st.res[m]])
                    for cc in range(2):
                        bb_ = proj(1536 + cc * 128)
                        P.op("act", lambda e: e.copy(out=gb_st.t[:, cc, :], in_=bb_.t[:]),
                             reads=[bb_.r], writes=[gb_st.r])
                    for cc in range(2):
                        bc_ = proj(1792 + cc * 128)
                        P.op("act", lambda e: e.copy(out=gcs.t[:], in_=bc_.t[:]), reads=[bc_.r], writes=[gcs.r])
                        bs_ = proj(2048 + cc * 128)
                        P.op("dve", lambda e: e.tensor_tensor(out=gcv_st.t[:, cc, :], in0=bs_.t[:], in1=gcs.t[:],
                                                              op=ALU.mult),
                             reads=[bs_.r, gcs.r], writes=[gcv_st.r])
                    for j in range(4):
                        bank = next_pf()
                        mm_group(bank, (0, 256), [nT.t[:, c, j * 128:(j + 1) * 128] for c in range(8)],
                                 [w_in_sb.t[:, c, 1280:1536] for c in range(8)],
                                 [[w_in_sb.r, nT.res[c]] for c in range(8)])
                        for hh in range(4):
                            off = hh * 128 + (0 if hh % 2 == 0 else 64)
                            eng = "act" if hh % 2 == 0 else "dve"
                            if eng == "act":
                                P.op("act", lambda e: e.copy(out=v_st.t[:, j, off:off + 64],
                                                             in_=bank.t[:, hh * 64:(hh + 1) * 64]),
                                     reads=[bank.r], writes=[v_st.r])
                            else:
                                P.op("dve", lambda e: e.tensor_copy(out=v_st.t[:, j, off:off + 64],
                                                                    in_=bank.t[:, hh * 64:(hh + 1) * 64]),
                                     reads=[bank.r], writes=[v_st.r])
                    for m in range(4):
                        bank = next_pf()
                        P.op("pe", lambda e: e.matmul(bank.t[:], rperm.t[:], qk_st.t[:, m, :], start=True, stop=True),
                             reads=[rperm.r, qk_st.res[m]], writes=[bank.r])
                        P.op("dve", lambda e: e.tensor_tensor(out=t1.t[:], in0=qk_st.t[:, m, :], in1=cos_t.t[:],
                                                              op=ALU.mult),
                             reads=[qk_st.res[m], cos_t.r], writes=[t1.r])
                        P.op("dve", lambda e: e.tensor_tensor(out=t2.t[:], in0=bank.t[:], in1=sin_t.t[:],
                                                              op=ALU.mult),
                             reads=[bank.r, sin_t.r], writes=[t2.r])
                        P.op("dve", lambda e: e.tensor_tensor(out=qkr_st.t[:, m, :], in0=t1.t[:], in1=t2.t[:],
                                                              op=ALU.add),
                             reads=[t1.r, t2.r], writes=[qkr_st.res[m]])
                    if i + 2 < NT:
                        P.dma("sp", hts[i % 2].t[:], tm(h_src, i + 2), reads=[R_h[i + 2]], writes=[hts[i % 2].r])
                    P.dma("sp", fm(glu_d, i), glu_st.t[:], reads=[glu_st.r], writes=[R_glu[i]])
                    P.dma("sp", fm(pin_d, i), pin_st.t[:], reads=[pin_st.r], writes=[R_pin[i]])
                    P.dma("sp", fm(q_d, i), qkr_st.t[:, 0:2, :], reads=[qkr_st.res[0], qkr_st.res[1]], writes=[R_q[i]])
                    P.dma("sp", fm(k_d, i), qkr_st.t[:, 2:4, :], reads=[qkr_st.res[2], qkr_st.res[3]], writes=[R_k[i]])
                    P.dma("sp", tm(v_d, i), v_st.t[:], reads=[v_st.r], writes=[R_v[i]])
                    P.dma("sp", fm(gb_d, i), gb_st.t[:], reads=[gb_st.r], writes=[R_gb[i]])
                    P.dma("sp", fm(gcv_d, i), gcv_st.t[:], reads=[gcv_st.r], writes=[R_gcv[i]])
                P.barrier()
            st_win.close()
            if dbg == "s1":
                st_dg.close()
                break

            st_wout = contextlib.ExitStack()
            uid[0] += 1
            w_out_sb = Buf(st_wout.enter_context(nc.sbuf_tensor("w_out_sb_u%d" % uid[0], [128, 8, D], BF16,
                                                                side="right")), "w_out_sb", 1)
            for c in range(8):
                P.dma("pool", w_out_sb.t[:, c, :], w_out_d[l, c * 128:(c + 1) * 128, :], writes=[w_out_sb.r])
            st_kv = contextlib.ExitStack()
            kT = sbuf(st_kv, "at_kT", [128, 2, S], BF16)
            Vs = sbuf(st_kv, "at_V", [128, 32, 512], BF16)
            P.dma("sp", kT.t[:], k_d.rearrange("(c p) t -> p c t", p=128), reads=R_k, writes=[kT.r])
            for i8 in range(NT):
                P.dma("sp", Vs.t[:, i8 * 4:(i8 + 1) * 4, :], tm(v_d, i8), reads=[R_v[i8]], writes=[Vs.r])
            with contextlib.ExitStack() as st:
                set_psum(st, 8, 0)
                glu_in = [sbuf(st, "s2_glu%d" % k, [128, 2, 30 + TS], BF16) for k in range(2)]
                pin_in = [sbuf(st, "s2_pin%d" % k, [128, 2, 16 + TS], F32) for k in range(2)]
                gcv_in = [sbuf(st, "s2_gcv%d" % k, [128, 2, 2 + TS], F32) for k in range(2)]
                gb_in = [sbuf(st, "s2_gb%d" % k, [128, 2, TS], F32) for k in range(2)]
                yc = sbuf(st, "s2_y", [128, 2, TS], F32, 2)
                ysq = sbuf(st, "s2_ysq", [128, 2, TS], F32, 2)
                m2 = sbuf(st, "s2_m2", [128, TS], F32)
                var = sbuf(st, "s2_var", [128, TS], F32)
                dd = sbuf(st, "s2_dd", [128, TS], F32)
                sA = sbuf(st, "s2_sA", [128, 16 + TS], F32)
                sB = sbuf(st, "s2_sB", [128, 16 + TS], F32)
                pooled = sbuf(st, "s2_pooled", [128, TS], BF16)
                acc3 = sbuf(st, "s2_acc3", [128, TS], F32)
                mixA = [sbuf(st, "s2_mixA%d" % k, [128, 6, TS], BF16) for k in range(2)]


                def s2_load(i):
                    k = i % 2
                    if i == 0:
                        P.op("dve", lambda e: e.memset(glu_in[k].t[:, :, 0:30], 0.0), writes=[glu_in[k].r])
                        P.op("dve", lambda e: e.memset(pin_in[k].t[:, :, 0:16], 0.0), writes=[pin_in[k].r])
                        P.op("dve", lambda e: e.memset(gcv_in[k].t[:, :, 0:2], 0.0), writes=[gcv_in[k].r])
                        P.dma("sp", glu_in[k].t[:, :, 30:30 + TS], fm(glu_d, 0), reads=[R_glu[0]], writes=[glu_in[k].r])
                        P.dma("sp", pin_in[k].t[:, :, 16:16 + TS], fm(pin_d, 0), reads=[R_pin[0]], writes=[pin_in[k].r])
                        P.dma("sp", gcv_in[k].t[:, :, 2:2 + TS], fm(gcv_d, 0), reads=[R_gcv[0]], writes=[gcv_in[k].r])
                    else:
                        P.dma("sp", glu_in[k].t[:], fm(glu_d, i, -30, TS), reads=[R_glu[i - 1], R_glu[i]],
                              writes=[glu_in[k].r])
                        P.dma("sp", pin_in[k].t[:], fm(pin_d, i, -16, TS), reads=[R_pin[i - 1], R_pin[i]],
                              writes=[pin_in[k].r])
                        P.dma("sp", gcv_in[k].t[:], fm(gcv_d, i, -2, TS), reads=[R_gcv[i - 1], R_gcv[i]],
                              writes=[gcv_in[k].r])
                    P.dma("sp", gb_in[k].t[:], fm(gb_d, i), reads=[R_gb[i]], writes=[gb_in[k].r])

                s2_load(0)
                for i in range(NT):
                    k = i % 2
                    if i + 1 < NT:
                        s2_load(i + 1)
                    mx = mixA[k]
                    for cc in range(2):
                        bank = next_pf()
                        mm_group(bank, (0, TS), [dg.t[:, cc, j, :] for j in range(CK)],
                                 [glu_in[k].t[:, cc, j:j + TS] for j in range(CK)],
                                 [[dg.r, glu_in[k].r]] * CK)
                        P.op("act", lambda e: e.activation(out=yc.t[:, cc, :], in_=bank.t[:], func=AF.Identity,
                                                           bias=prm.t[:, PC_CB + cc:PC_CB + cc + 1]),
                             reads=[bank.r, prm.r], writes=[yc.res[cc]])
                        P.op("act", lambda e: e.activation(out=ysq.t[:, cc, :], in_=bank.t[:], func=AF.Square,
                                                           bias=prm.t[:, PC_CB + cc:PC_CB + cc + 1]),
                             reads=[bank.r, prm.r], writes=[ysq.res[cc]])
                    bm = next_pf()
                    mm_group(bm, (0, TS), [ones256.t[:], ones256.t[:]], [yc.t[:, 0, :], yc.t[:, 1, :]],
                             [[ones256.r, yc.res[0]], [ones256.r, yc.res[1]]])
                    bq = next_pf()
                    mm_group(bq, (0, TS), [ones256.t[:], ones256.t[:]], [ysq.t[:, 0, :], ysq.t[:, 1, :]],
                             [[ones256.r, ysq.res[0]], [ones256.r, ysq.res[1]]])
                    for cc in range(2):
                        u = pin_in[k].t[:, cc, :]
                        W_ = 16 + TS
                        P.op("dve", lambda e: e.memset(sA.t[:, 0:1], 0.0), writes=[sA.r])
                        P.op("dve", lambda e: e.tensor_tensor(out=sA.t[:, 1:W_], in0=pin_in[k].t[:, cc, 1:W_],
                                                              in1=pin_in[k].t[:, cc, 0:W_ - 1], op=ALU.add),
                             reads=[pin_in[k].r], writes=[sA.r])
                        if cc == 0:
                            P.op("dve", lambda e: e.tensor_tensor(out=sB.t[64:128, 3:W_], in0=sA.t[64:128, 3:W_],
                                                                  in1=sA.t[64:128, 1:W_ - 2], op=ALU.add),
                                 reads=[sA.r], writes=[sB.r])
                            P.op("dve", lambda e: e.tensor_copy(out=sB.t[0:64, 3:W_], in_=sA.t[0:64, 3:W_]),
                                 reads=[sA.r, sB.r], writes=[sB.r])
                            fin = sB
                        else:
                            P.op("dve", lambda e: e.tensor_tensor(out=sB.t[:, 3:W_], in0=sA.t[:, 3:W_],
                                                                  in1=sA.t[:, 1:W_ - 2], op=ALU.add),
                                 reads=[sA.r], writes=[sB.r])
                            P.op("dve", lambda e: e.tensor_tensor(out=sA.t[:, 7:W_], in0=sB.t[:, 7:W_],
                                                                  in1=sB.t[:, 3:W_ - 4], op=ALU.add),
                                 reads=[sB.r, sA.r], writes=[sA.r])
                            P.op("dve", lambda e: e.tensor_tensor(out=sB.t[64:128, 15:W_], in0=sA.t[64:128, 15:W_],
                                                                  in1=sA.t[64:128, 7:W_ - 8], op=ALU.add),
                                 reads=[sA.r, sB.r], writes=[sB.r])
                            P.op("dve", lambda e: e.tensor_copy(out=sB.t[0:64, 15:W_], in_=sA.t[0:64, 15:W_]),
                                 reads=[sA.r, sB.r], writes=[sB.r])
                            fin = sB
                        if i == 0:
                            P.op("dve", lambda e: e.tensor_tensor(out=fin.t[:, 16:32], in0=fin.t[:, 16:32],
                                                                  in1=corr.t[:, cc, :], op=ALU.mult),
                                 reads=[fin.r, corr.r], writes=[fin.r])
                        P.op("dve", lambda e: e.scalar_tensor_tensor(
                            out=pooled.t[:], in0=fin.t[:, 16:16 + TS], scalar=prm.t[:, PC_IW + cc:PC_IW + cc + 1],
                            in1=pin_in[k].t[:, cc, 16:16 + TS], op0=ALU.mult, op1=ALU.subtract),
                            reads=[fin.r, prm.r, pin_in[k].r], writes=[pooled.r])
                        bank = next_pf()
                        P.op("pe", lambda e: e.matmul(bank.t[:], pw_sb.t[:, cc, :], pooled.t[:], start=True, stop=True),
                             reads=[pw_sb.r, pooled.r], writes=[bank.r])
                        P.op("act", lambda e: e.activation(out=mx.t[:, 2 + cc, :], in_=bank.t[:], func=AF.Identity,
                                                           scale=prm.t[:, PC_PS + cc:PC_PS + cc + 1],
                                                           bias=pbs.t[:, cc:cc + 1]),
                             reads=[bank.r, prm.r, pbs.r], writes=[mx.r])
                    for cc in range(2):
                        g_ = gcv_in[k]
                        P.op("dve", lambda e: e.tensor_scalar(out=acc3.t[:], in0=g_.t[:, cc, 0:TS],
                                                              scalar1=prm.t[:, PC_SW + cc * 3:PC_SW + cc * 3 + 1],
                                                              scalar2=None, op0=ALU.mult),
                             reads=[g_.r, prm.r], writes=[acc3.r])
                        for j in (1, 2):
                            P.op("dve", lambda e, j=j: e.scalar_tensor_tensor(
                                out=acc3.t[:], in0=g_.t[:, cc, j:j + TS],
                                scalar=prm.t[:, PC_SW + cc * 3 + j:PC_SW + cc * 3 + j + 1], in1=acc3.t[:],
                                op0=ALU.mult, op1=ALU.add), reads=[g_.r, prm.r, acc3.r], writes=[acc3.r])
                        P.op("dve", lambda e: e.tensor_tensor(out=mx.t[:, 4 + cc, :], in0=acc3.t[:],
                                                              in1=gb_in[k].t[:, cc, :], op=ALU.mult),
                             reads=[acc3.r, gb_in[k].r], writes=[mx.r])
                    P.op("act", lambda e: e.activation(out=m2.t[:], in_=bm.t[:], func=AF.Square),
                         reads=[bm.r], writes=[m2.r])
                    P.op("dve", lambda e: e.tensor_tensor(out=var.t[:], in0=bq.t[:], in1=m2.t[:], op=ALU.subtract),
                         reads=[bq.r, m2.r], writes=[var.r])
                    P.op("dve", lambda e: e.tensor_scalar(out=var.t[:], in0=var.t[:], scalar1=0.0, scalar2=EPS,
                                                          op0=ALU.max, op1=ALU.add), reads=[var.r], writes=[var.r])
                    P.op("act", lambda e: e.activation(out=var.t[:], in_=var.t[:], func=AF.Ln),
                         reads=[var.r], writes=[var.r])
                    P.op("act", lambda e: e.activation(out=var.t[:], in_=var.t[:], func=AF.Exp, scale=-0.5),
                         reads=[var.r], writes=[var.r])
                    for cc in range(2):
                        P.op("dve", lambda e: e.tensor_tensor(out=dd.t[:], in0=bm.t[:], in1=yc.t[:, cc, :],
                                                              op=ALU.subtract),
                             reads=[bm.r, yc.res[cc]], writes=[dd.r])
                        P.op("dve", lambda e: e.tensor_tensor(out=dd.t[:], in0=dd.t[:], in1=var.t[:], op=ALU.mult),
                             reads=[dd.r, var.r], writes=[dd.r])
                        P.op("dve", lambda e: e.tensor_scalar(out=dd.t[:], in0=dd.t[:],
                                                              scalar1=prm.t[:, PC_LNG + cc:PC_LNG + cc + 1],
                                                              scalar2=-1.0, op0=ALU.mult, op1=ALU.mult),
                             reads=[dd.r, prm.r], writes=[dd.r])
                        P.op("act", lambda e: e.activation(out=mx.t[:, cc, :], in_=dd.t[:], func=AF.Silu,
                                                           bias=prm.t[:, PC_LNB + cc:PC_LNB + cc + 1]),
                             reads=[dd.r, prm.r], writes=[mx.r])
                    mv = mix_d.rearrange("(c p) t -> p c t", p=128)
                    P.dma("sp", mv[:, 0:4, i * TS:(i + 1) * TS], mx.t[:, 0:4, :], reads=[mx.r], writes=[R_mixA[i]])
                    P.dma("sp", mv[:, 6:8, i * TS:(i + 1) * TS], mx.t[:, 4:6, :], reads=[mx.r], writes=[R_mixA[i]])
                P.barrier()
            if dbg == "s2a":
                st_kv.close()
                st_wout.close()
                st_dg.close()
                break

            with contextlib.ExitStack() as st:
                qTs = [sbuf(st, "at_q%d" % k, [128, 2, TS], BF16) for k in range(2)]
                pts = [sbuf(st, "at_pt%d" % k, [128, TS], BF16) for k in range(4)]
                rz = [sbuf(st, "at_rz%d" % k, [128, TS], F32) for k in range(2)]
                o12 = [sbuf(st, "at_o%d" % k, [128, TS], F32) for k in range(2)]
                od = sbuf(st, "at_od", [128, TS], F32)
                osq = sbuf(st, "at_osq", [128, TS], F32)
                rs = sbuf(st, "at_rs", [128, TS], F32)
                mixB = [sbuf(st, "at_mix%d" % k, [128, 2, TS], BF16) for k in range(2)]
                set_psum(st, 4, 0)
                accb = pf[0:4]
                sc2 = [psum(st, "sc2_%d_%d" % (k, uid[0]), [128, 2 * TS], F32) for k in range(2)]
                pt2 = [sbuf(st, "at_pt2_%d" % k, [128, 2 * TS], BF16) for k in range(4)]
                P.dma("sp", qTs[0].t[:], fm(q_d, 0), reads=[R_q[0]], writes=[qTs[0].r])
                mv = mix_d.rearrange("(c p) t -> p c t", p=128)

                groups = []
                for i in range(NT):
                    nkt = 4 * i + 4
                    for hp in range(2):
                        for kt in range(nkt):
                            groups.append((i, hp, kt, nkt))

                def front(g):
                    i, hp, kt, nkt = groups[g]
                    if hp == 0 and kt == 0 and i + 1 < NT:
                        P.dma("sp", qTs[(i + 1) % 2].t[:], fm(q_d, i + 1), reads=[R_q[i + 1]],
                              writes=[qTs[(i + 1) % 2].r])
                    qT = qTs[i % 2]
                    jd = kt - 4 * i
                    qs = 128 * jd if jd > 0 else 0
                    n = TS - qs
                    for s_ in range(4):
                        po = s_ * 32
                        sc = sc2[s_ // 2]
                        o_ = (s_ % 2) * TS
                        P.op("pe", lambda e: e.matmul(sc.t[:, o_:o_ + n], kT.t[po:po + 32, hp, kt * 128:(kt + 1) * 128],
                                                      qT.t[po:po + 32, hp, qs:TS], start=True, stop=True,
                                                      tile_position=(po, 0)),
                             reads=[kT.r, qT.r], writes=[sc.r])
                    for pr in range(2):
                        sc = sc2[pr]
                        pt = pt2[(g % 2) * 2 + pr]
                        P.op("act", lambda e: e.activation(
                            out=pt.t[:].rearrange("p (a b) -> p a b", a=2)[:, :, 0:n],
                            in_=sc.t[:].rearrange("p (a b) -> p a b", a=2)[:, :, 0:n], func=AF.Exp, scale=SCALE),
                            reads=[sc.r], writes=[pt.r])
                        if jd >= 0:
                            P.op("dve", lambda e: e.tensor_tensor(
                                out=pt.t[:].rearrange("p (a b) -> p a b", a=2)[:, :, 0:128],
                                in0=pt.t[:].rearrange("p (a b) -> p a b", a=2)[:, :, 0:128],
                                in1=trib.t[:].unsqueeze(1).to_broadcast([128, 2, 128]), op=ALU.mult),
                                reads=[pt.r, trib.r], writes=[pt.r])

                def back(g):
                    i, hp, kt, nkt = groups[g]
                    jd = kt - 4 * i
                    qs = 128 * jd if jd > 0 else 0
                    n = TS - qs
                    for s_ in range(4):
                        h = 2 * hp + s_ // 2
                        pt = pt2[(g % 2) * 2 + s_ // 2]
                        o_ = (s_ % 2) * TS
                        acc = accb[s_]
                        P.op("pe", lambda e: e.matmul(acc.t[:, qs:TS], Vs.t[:, kt, h * 128:(h + 1) * 128],
                                                      pt.t[:, o_:o_ + n], start=(kt == 0), stop=(kt == nkt - 1)),
                             reads=[Vs.r, pt.r], writes=[acc.r])
                    if kt == nkt - 1:
                        finalize(i, 2 * hp)
                        finalize(i, 2 * hp + 1)

                def finalize(i, h):
                    ch = h // 2
                    mb = mixB[i % 2]
                    lo, hi = (0, 64) if h % 2 == 0 else (64, 128)
                    zlo, zhi = (64, 128) if h % 2 == 0 else (0, 64)
                    accs = [accb[(h % 2) * 2], accb[(h % 2) * 2 + 1]]
                    for comp in range(2):
                        P.op("act", lambda e: e.activation(out=rz[comp].t[zlo:zhi, :], in_=accs[comp].t[zlo:zhi, :],
                                                           func=AF.Ln),
                             reads=[accs[comp].r], writes=[rz[comp].r])
                        P.op("act", lambda e: e.activation(out=rz[comp].t[zlo:zhi, :], in_=rz[comp].t[zlo:zhi, :],
                                                           func=AF.Exp, scale=-1.0),
                             reads=[rz[comp].r], writes=[rz[comp].r])
                        P.op("dve", lambda e: e.tensor_tensor(out=o12[comp].t[lo:hi, :], in0=accs[comp].t[lo:hi, :],
                                                              in1=rz[comp].t[zlo:zhi, :], op=ALU.mult),
                             reads=[accs[comp].r, rz[comp].r], writes=[o12[comp].r])
                    P.op("dve", lambda e: e.scalar_tensor_tensor(out=od.t[lo:hi, :], in0=o12[1].t[lo:hi, :],
                                                                 scalar=neglam.t[lo:hi, 0:1], in1=o12[0].t[lo:hi, :],
                                                                 op0=ALU.mult, op1=ALU.add),
                         reads=[o12[0].r, o12[1].r, neglam.r], writes=[od.r])
                    if h % 2 == 1:
                        P.op("act", lambda e: e.activation(out=osq.t[:], in_=od.t[:], func=AF.Square),
                             reads=[od.r], writes=[osq.r])
                        bms = sc2[1]
                        P.op("pe", lambda e: e.matmul(bms.t[:, TS:2 * TS], blk64.t[:], osq.t[:], start=True, stop=True),
                             reads=[blk64.r, osq.r], writes=[bms.r])
                        P.op("act", lambda e: e.activation(out=rs.t[:], in_=bms.t[:, TS:2 * TS], func=AF.Ln,
                                                           bias=epsc.t[:, 0:1]),
                             reads=[bms.r, epsc.r], writes=[rs.r])
                        P.op("act", lambda e: e.activation(out=rs.t[:], in_=rs.t[:], func=AF.Exp, scale=-0.5),
                             reads=[rs.r], writes=[rs.r])
                        P.op("dve", lambda e: e.tensor_tensor(out=rs.t[:], in0=rs.t[:], in1=od.t[:], op=ALU.mult),
                             reads=[rs.r, od.r], writes=[rs.r])
                        P.op("dve", lambda e: e.tensor_scalar(out=mb.t[:, ch, :], in0=rs.t[:],
                                                              scalar1=prm.t[:, PC_SUB:PC_SUB + 1],
                                                              scalar2=1.0 - lam_init, op0=ALU.mult, op1=ALU.mult),
                             reads=[rs.r, prm.r], writes=[mb.r])
                    if h == 3:
                        P.dma("sp", mv[:, 4:6, i * TS:(i + 1) * TS], mb.t[:], reads=[mb.r], writes=[R_mixB[i]])

                LA = 1
                for g in range(len(groups) + LA):
                    if g < len(groups):
                        front(g)
                    if g >= LA:
                        back(g - LA)
                P.barrier()
            st_kv.close()
            st_dg.close()
            if dbg == "s2c":
                st_wout.close()
                break

            st_ple = contextlib.ExitStack()
            wpg = sbuf(st_ple, "s5_wpg", [128, 8, D], BF16)
            wpp = sbuf(st_ple, "s5_wpp", [128, 2, D], BF16)
            stm = contextlib.ExitStack()
            wgs = [sbuf(stm, "wgs0", [128, 4, 8, 256], BF16)]
            wus = [sbuf(stm, "wus0", [128, 4, 8, 256], BF16)]
            wds = [sbuf(stm, "wds0", [128, 4, 2, D], BF16)]
            def load_experts(pz, slot):
                for el in range(4):
                    eidx = pz * 4 + el
                    P.dma("pool", wgs[slot].t[:, el, :, :], wg_d[l, eidx].rearrange("(c p) n -> p c n", p=128),
                          writes=[wgs[slot].r])
                    P.dma("pool", wus[slot].t[:, el, :, :], wu_d[l, eidx].rearrange("(c p) n -> p c n", p=128),
                          writes=[wus[slot].r])
                    P.dma("pool", wds[slot].t[:, el, :, :], wd_d[l, eidx].rearrange("(c p) n -> p c n", p=128),
                          writes=[wds[slot].r])

            with contextlib.ExitStack() as st:
                set_psum(st, 6, 2)
                wrg = sbuf(st, "s3_wrg", [128, 8, 20], F32)
                rbias = sbuf(st, "s3_rb", [128, 20], F32)
                hts = [sbuf(st, "s3_ht%d" % k, [128, 4, D], F32) for k in range(2)]
                mxs = [sbuf(st, "s3_mx%d" % k, [128, 8, TS], BF16) for k in range(2)]
                ss2 = [sbuf(st, "s3_ss%d" % k, [128, 4], F32) for k in range(2)]
                rstd2 = [sbuf(st, "s3_rstd%d" % k, [128, 4], F32) for k in range(2)]
                sqj = sbuf(st, "s3_sqj", [128, D], BF16)
                xn2 = [sbuf(st, "s3_xn%d" % k, [128, 4, D], BF16, 4) for k in range(2)]
                xT2 = [sbuf(st, "s3_xT%d" % k, [128, 8, TS], BF16, 8) for k in range(2)]
                hTf = sbuf(st, "s3_hTf", [128, 8, 128], F32, 2)
                lg = sbuf(st, "s3_lg", [128, 20], F32)
                lg4 = sbuf(st, "s3_lg4", [128, 4, 20], F32)
                r4 = sbuf(st, "s3_r4", [128, 8, 4], F32)
                mg4 = sbuf(st, "s3_mg4", [128, 4, 4], F32)
                t4 = sbuf(st, "s3_t4", [128, 4, 4], F32)
                es4 = sbuf(st, "s3_es4", [128, 4, 4], F32)
                eq4 = sbuf(st, "s3_eq4", [128, 4, 4], F32)
                pr4 = sbuf(st, "s3_pr4", [128, 4, 4, 4], F32)
                sm = sbuf(st, "s3_sm", [128, 16], F32)
                mg = sbuf(st, "s3_mg", [128, 4], F32)
                ge = sbuf(st, "s3_ge", [128, 4], F32)
                esel = sbuf(st, "s3_esel", [128, 4], F32)
                eq = sbuf(st, "s3_eq", [128, 4], F32)
                em2 = sbuf(st, "s3_em2", [128, 4], F32)
                ee = sbuf(st, "s3_ee", [128, 4], F32)
                wsel = sbuf(st, "s3_wsel", [128, 4], F32)
                comb = sbuf(st, "s3_comb", [128, 4, 16], F32)
                cmbT = sbuf(st, "s3_cmbT", [16, TS], F32)

                load_experts(0, 0)
                P.dma("sp", wrg.t[:], wr_d[l], writes=[wrg.r])
                P.dma("sp", rbias.t[:], rb_d[l], writes=[rbias.r])
                for c in range(8):
                    P.op("dve", lambda e, c=c: e.tensor_scalar(out=wrg.t[:, c, :], in0=wrg.t[:, c, :],
                                                               scalar1=prm.t[:, PC_FFNG + c:PC_FFNG + c + 1],
                                                               scalar2=None, op0=ALU.mult),
                         reads=[wrg.r, prm.r], writes=[wrg.r])

                def s3_load(i):
                    k = i % 2
                    P.dma("sp", hts[k].t[:], tm(h_src, i), reads=[R_h[i]], writes=[hts[k].r])
                    P.dma("sp", mxs[k].t[:], fm(mix_d, i), reads=[R_mixA[i], R_mixB[i]], writes=[mxs[k].r])

                def s3_A(i):
                    k = i % 2
                    ht = hts[k]
                    mx = mxs[k]
                    ss, rstd, xn, xT = ss2[k], rstd2[k], xn2[k], xT2[k]
                    for j in range(4):
                        for half in range(2):
                            bank = next_pf()
                            mm_group(bank, (0, 512), [mx.t[:, c, j * 128:(j + 1) * 128] for c in range(8)],
                                     [w_out_sb.t[:, c, half * 512:(half + 1) * 512] for c in range(8)],
                                     [[mx.r, w_out_sb.r]] * 8)
                            P.op("dve", lambda e: e.tensor_tensor(out=ht.t[:, j, half * 512:(half + 1) * 512],
                                                                  in0=bank.t[:], in1=ht.t[:, j, half * 512:(half + 1) * 512],
                                                                  op=ALU.add),
                                 reads=[bank.r, ht.r], writes=[ht.r])
                    P.dma("sp", tm(h_d, i), ht.t[:], reads=[ht.r], writes=[R_h[i]])
                    norm_stats(st, ht, 0, ss, rstd, sqj)
                    norm_transpose(ht, 0, rstd, xn, xT, PC_FFNG, part=0)

                def s3_A2(i):
                    k = i % 2
                    ht = hts[k]
                    ss, rstd, xn, xT = ss2[k], rstd2[k], xn2[k], xT2[k]
                    norm_transpose(ht, 0, rstd, xn, xT, PC_FFNG, part=1)
                    P.dma("sp", fm(xT_d, i), xT.t[:], reads=xT.res, writes=[R_xT[i]])

                def s3_Ba(i):
                    k = i % 2
                    ht = hts[k]
                    ss, rstd, xn, xT = ss2[k], rstd2[k], xn2[k], xT2[k]
                    for j in range(4):
                        for c2 in range(2):
                            bank = next_pf()
                            while bank is bct:
                                bank = next_pf()
                            for cq in range(4):
                                c = c2 * 4 + cq
                                P.op("pe", lambda e: e.transpose(bank.t[:, cq * 128:(cq + 1) * 128],
                                                                 ht.t[:, j, c * 128:(c + 1) * 128], identf.t[:]),
                                     reads=[ht.r, identf.r], writes=[bank.r], signal=(cq == 3))
                            if c2 == 0:
                                P.op("act", lambda e: e.copy(out=hTf.t[:, 0:4, :], in_=bank.t[:].rearrange("p (a b) -> p a b", a=4)),
                                     reads=[bank.r], writes=[hTf.res[0]])
                            else:
                                P.op("dve", lambda e: e.tensor_copy(out=hTf.t[:, 4:8, :], in_=bank.t[:].rearrange("p (a b) -> p a b", a=4)),
                                     reads=[bank.r], writes=[hTf.res[1]])
                        bl = next_pf()
                        while bl is bct:
                            bl = next_pf()
                        mm_group(bl, (0, 20), [hTf.t[:, c, :] for c in range(8)], [wrg.t[:, c, :] for c in range(8)],
                                 [[hTf.res[c // 4], wrg.r] for c in range(8)])
                        P.op("dve", lambda e: e.scalar_tensor_tensor(out=lg4.t[:, j, :], in0=bl.t[:, 0:20],
                                                                     scalar=rstd.t[:, j:j + 1], in1=rbias.t[:],
                                                                     op0=ALU.mult, op1=ALU.add),
                             reads=[bl.r, rstd.r, rbias.r], writes=[lg4.r])

                def s3_Bb(i):
                    V = lambda fn, rd, wr: P.op("dve", fn, reads=rd, writes=wr)
                    S3 = [128, 4, 4]
                    bc = lambda ap_: ap_.unsqueeze(2).to_broadcast(S3)
                    glg = lg4.t[:, :, 0:4]
                    V(lambda e: e.reduce_max(out=r4.t[:, 0, :], in_=glg, axis=AX.X), [lg4.r], [r4.r])
                    V(lambda e: e.tensor_tensor(out=mg4.t[:], in0=glg, in1=bc(r4.t[:, 0, :]), op=ALU.is_ge),
                      [lg4.r, r4.r], [mg4.r])
                    V(lambda e: e.tensor_tensor(out=t4.t[:], in0=glg, in1=bc(r4.t[:, 0, :]), op=ALU.subtract),
                      [lg4.r, r4.r], [t4.r])
                    P.op("act", lambda e: e.activation(out=t4.t[:], in_=t4.t[:], func=AF.Exp), reads=[t4.r], writes=[t4.r])
                    V(lambda e: e.reduce_sum(out=r4.t[:, 1, :], in_=t4.t[:], axis=AX.X), [t4.r, r4.r], [r4.r])
                    V(lambda e: e.reciprocal(out=r4.t[:, 2, :], in_=r4.t[:, 1, :]), [r4.r], [r4.r])
                    el4 = lg4.t[:, :, 4:20].rearrange("p j (g i) -> p j g i", g=4)
                    V(lambda e: e.tensor_tensor(out=pr4.t[:], in0=el4,
                                                in1=mg4.t[:].unsqueeze(3).to_broadcast([128, 4, 4, 4]), op=ALU.mult),
                      [lg4.r, mg4.r], [pr4.r])
                    V(lambda e: e.reduce_sum(out=es4.t[:], in_=pr4.t[:].rearrange("p j g i -> p j i g"), axis=AX.X),
                      [pr4.r], [es4.r])
                    V(lambda e: e.reduce_max(out=r4.t[:, 3, :], in_=es4.t[:], axis=AX.X), [es4.r, r4.r], [r4.r])
                    V(lambda e: e.tensor_tensor(out=eq4.t[:], in0=es4.t[:], in1=bc(r4.t[:, 3, :]), op=ALU.is_ge),
                      [es4.r, r4.r], [eq4.r])
                    V(lambda e: e.scalar_tensor_tensor(out=t4.t[:], in0=eq4.t[:], scalar=-1e30, in1=es4.t[:],
                                                       op0=ALU.mult, op1=ALU.add), [eq4.r, es4.r, t4.r], [t4.r])
                    V(lambda e: e.reduce_max(out=r4.t[:, 4, :], in_=t4.t[:], axis=AX.X), [t4.r, r4.r], [r4.r])
                    V(lambda e: e.tensor_tensor(out=eq4.t[:], in0=es4.t[:], in1=bc(r4.t[:, 4, :]), op=ALU.is_ge),
                      [es4.r, r4.r, eq4.r], [eq4.r])
                    V(lambda e: e.tensor_tensor(out=t4.t[:], in0=es4.t[:], in1=bc(r4.t[:, 3, :]), op=ALU.subtract),
                      [es4.r, r4.r, t4.r], [t4.r])
                    P.op("act", lambda e: e.activation(out=t4.t[:], in_=t4.t[:], func=AF.Exp), reads=[t4.r], writes=[t4.r])
                    V(lambda e: e.tensor_tensor(out=t4.t[:], in0=t4.t[:], in1=eq4.t[:], op=ALU.mult),
                      [t4.r, eq4.r], [t4.r])
                    V(lambda e: e.reduce_sum(out=r4.t[:, 5, :], in_=t4.t[:], axis=AX.X), [t4.r, r4.r], [r4.r])
                    V(lambda e: e.reciprocal(out=r4.t[:, 6, :], in_=r4.t[:, 5, :]), [r4.r], [r4.r])
                    V(lambda e: e.tensor_tensor(out=r4.t[:, 6, :], in0=r4.t[:, 6, :], in1=r4.t[:, 2, :], op=ALU.mult),
                      [r4.r], [r4.r])
                    V(lambda e: e.tensor_tensor(out=t4.t[:], in0=t4.t[:], in1=bc(r4.t[:, 6, :]), op=ALU.mult),
                      [t4.r, r4.r], [t4.r])
                    V(lambda e: e.tensor_tensor(out=comb.t[:].rearrange("p j (g i) -> p j g i", g=4),
                                                in0=t4.t[:].unsqueeze(2).to_broadcast([128, 4, 4, 4]),
                                                in1=mg4.t[:].unsqueeze(3).to_broadcast([128, 4, 4, 4]), op=ALU.mult),
                      [t4.r, mg4.r], [comb.r])

                def s3_B2(i):
                    for j in range(4):
                        P.op("pe", lambda e: e.transpose(bct.t[0:16, j * 128:(j + 1) * 128], comb.t[:, j, :], identf.t[:]),
                             reads=[comb.r, identf.r], writes=[bct.r])
                    P.op("act", lambda e: e.copy(out=cmbT.t[:], in_=bct.t[0:16, :]), reads=[bct.r], writes=[cmbT.r])
                    P.dma("sp", cmb_d[:, i * TS:(i + 1) * TS], cmbT.t[:], reads=[cmbT.r], writes=[R_cmb[i]])

                bct = pf.pop()
                s3_load(0)
                if NT > 1:
                    s3_load(1)
                s3_A(0)
                s3_A2(0)
                for i in range(NT):
                    if i + 1 < NT:
                        s3_A(i + 1)
                    if i >= 1:
                        s3_B2(i - 1)
                    s3_Ba(i)
                    if i + 2 < NT:
                        s3_load(i + 2)
                    if i + 1 < NT:
                        s3_A2(i + 1)
                    s3_Bb(i)
                s3_B2(NT - 1)
                P.barrier()
            st_wout.close()
            if dbg == "s3":
                stm.close()
                st_ple.close()
                break

            wgs.append(sbuf(stm, "wgs1", [128, 4, 8, 256], BF16))
            wus.append(sbuf(stm, "wus1", [128, 4, 8, 256], BF16))
            wds.append(sbuf(stm, "wds1", [128, 4, 2, D], BF16))
            with contextlib.ExitStack() as st:
                set_psum(st, 8, 0)
                hts = [sbuf(st, "s4_ht%d" % k, [128, 4, D], F32) for k in range(2)]
                xTs = [sbuf(st, "s4_xT%d" % k, [128, 8, TS], BF16) for k in range(2)]
                cms = [sbuf(st, "s4_cm%d" % k, [16, TS], F32) for k in range(2)]
                cbs = sbuf(st, "s4_cb", [128, TS], F32)
                sg_ = sbuf(st, "s4_sg", [128, TS], F32)
                tt_ = sbuf(st, "s4_tt", [128, TS], F32)
                hdn = sbuf(st, "s4_hdn", [128, 4, 2, TS], BF16)

                def s4_load(i):
                    k = i % 2
                    P.dma("sp", hts[k].t[:], tm(h_d, i), reads=[R_h[i]], writes=[hts[k].r])
                    P.dma("sp", xTs[k].t[:], fm(xT_d, i), reads=[R_xT[i]], writes=[xTs[k].r])
                    P.dma("sp", cms[k].t[:], cmb_d[:, i * TS:(i + 1) * TS], reads=[R_cmb[i]], writes=[cms[k].r])

                s4_load(0)
                for pz in range(4):
                    slot = pz % 2
                    if pz + 1 < 4:
                        load_experts(pz + 1, (pz + 1) % 2)
                    if pz == 0:
                        for c in range(8):
                            P.dma("pool", wpg.t[:, c, :], pgw_d[l, c * 128:(c + 1) * 128, :], writes=[wpg.r])
                        P.dma("pool", wpp.t[:], ppj_d[l].rearrange("(c p) n -> p c n", p=128), writes=[wpp.r])
                    for i in range(NT):
                        k = i % 2
                        if i + 1 < NT:
                            s4_load(i + 1)
                        elif pz + 1 < 4:
                            s4_load(0)
                        ht, xT_, cm = hts[k], xTs[k], cms[k]
                        for el in range(4):
                            eidx = pz * 4 + el
                            bcb = next_pf()
                            P.op("pe", lambda e: e.matmul(bcb.t[:], sel.t[0:16, eidx, :], cm.t[0:16, :],
                                                          start=True, stop=True),
                                 reads=[sel.r, cm.r], writes=[bcb.r])
                            P.op("act", lambda e: e.copy(out=cbs.t[:], in_=bcb.t[:]), reads=[bcb.r], writes=[cbs.r])
                            for hc in range(2):
                                bg_ = next_pf()
                                mm_group(bg_, (0, TS), [wgs[slot].t[:, el, c, hc * 128:(hc + 1) * 128] for c in range(8)],
                                         [xT_.t[:, c, :] for c in range(8)], [[wgs[slot].r, xT_.r]] * 8)
                                bu_ = next_pf()
                                mm_group(bu_, (0, TS), [wus[slot].t[:, el, c, hc * 128:(hc + 1) * 128] for c in range(8)],
                                         [xT_.t[:, c, :] for c in range(8)], [[wus[slot].r, xT_.r]] * 8)
                                P.op("act", lambda e: e.activation(out=sg_.t[:], in_=bg_.t[:], func=AF.Silu),
                                     reads=[bg_.r], writes=[sg_.r])
                                P.op("dve", lambda e: e.tensor_tensor(out=tt_.t[:], in0=bu_.t[:], in1=cbs.t[:],
                                                                      op=ALU.mult),
                                     reads=[bu_.r, cbs.r], writes=[tt_.r])
                                P.op("dve", lambda e: e.tensor_tensor(out=hdn.t[:, el, hc, :], in0=sg_.t[:], in1=tt_.t[:],
                                                                      op=ALU.mult),
                                     reads=[sg_.r, tt_.r], writes=[hdn.r])
                        for j in range(4):
                            for half in range(2):
                                by = next_pf()
                                mm_group(by, (0, 512),
                                         [hdn.t[:, el, hc, j * 128:(j + 1) * 128] for el in range(4) for hc in range(2)],
                                         [wds[slot].t[:, el, hc, half * 512:(half + 1) * 512]
                                          for el in range(4) for hc in range(2)],
                                         [[hdn.r, wds[slot].r]] * 8)
                                P.op("dve", lambda e: e.tensor_tensor(out=ht.t[:, j, half * 512:(half + 1) * 512],
                                                                      in0=by.t[:],
                                                                      in1=ht.t[:, j, half * 512:(half + 1) * 512],
                                                                      op=ALU.add),
                                     reads=[by.r, ht.r], writes=[ht.r])
                        P.dma("sp", tm(h_d, i), ht.t[:], reads=[ht.r], writes=[R_h[i]])
                P.barrier()
            stm.close()
            if dbg == "s4":
                st_ple.close()
                break

            with contextlib.ExitStack() as st:
                set_psum(st, 6, 2)
                gbf = sbuf(st, "s5_gbf", [1, D], F32)
                gbb = sbuf(st, "s5_gbb", [1, D], BF16)
                hts = [sbuf(st, "s5_ht%d" % k, [128, 4, D], F32) for k in range(3)]
                pts_ = [sbuf(st, "s5_p%d" % k, [128, 4, 256], F32) for k in range(3)]
                ss2 = [sbuf(st, "s5_ss%d" % k, [128, 4], F32) for k in range(2)]
                rstd2 = [sbuf(st, "s5_rstd%d" % k, [128, 4], F32) for k in range(2)]
                sqj = sbuf(st, "s5_sqj", [128, D], BF16)
                xn2 = [sbuf(st, "s5_xn%d" % k, [128, 4, D], BF16, 4) for k in range(2)]
                xT2 = [sbuf(st, "s5_xT%d" % k, [128, 8, TS], BF16, 8) for k in range(2)]
                pT2 = [sbuf(st, "s5_pT%d" % k, [128, 2, TS], BF16) for k in range(2)]
                gts = [sbuf(st, "s5_gt%d" % k, [128, 512], F32) for k in range(3)]
                tqs = [sbuf(st, "s5_tq%d" % k, [128, 512], F32) for k in range(3)]
                if l + 1 < DEPTH:
                    st_win, w_in_next = load_w_in(l + 1)
                P.dma("sp", gbf.t[:], pgb_d[l:l + 1, :], writes=[gbf.r])
                P.op("dve", lambda e: e.tensor_copy(out=gbb.t[:], in_=gbf.t[:]), reads=[gbf.r], writes=[gbb.r])

                def s5_load(i):
                    k = i % 3
                    P.dma("sp", hts[k].t[:], tm(h_d, i), reads=[R_h[i]], writes=[hts[k].r])
                    P.dma("sp", pts_[k].t[:], tm(p_d[l], i), writes=[pts_[k].r])

                def s5_P(i):
                    k = i % 2
                    ht, pt_ = hts[i % 3], pts_[i % 3]
                    ss, rstd, xn, xT, pT = ss2[k], rstd2[k], xn2[k], xT2[k], pT2[k]
                    norm_stats(st, ht, 0, ss, rstd, sqj)
                    norm_transpose(ht, 0, rstd, xn, xT, PC_PLEG)
                    for c in range(2):
                        bank = next_pf()
                        for j in range(4):
                            P.op("pe", lambda e: e.transpose(bank.t[:, j * 128:(j + 1) * 128],
                                                             pt_.t[:, j, c * 128:(c + 1) * 128], identf.t[:]),
                                 reads=[pt_.r, identf.r], writes=[bank.r], signal=(j == 3))
                        P.op("act", lambda e: e.copy(out=pT.t[:, c, :], in_=bank.t[:]), reads=[bank.r], writes=[pT.r])

                def s5_M(i):
                    k = i % 2
                    ht, pt_ = hts[i % 3], pts_[i % 3]
                    ss, rstd, xn, xT, pT = ss2[k], rstd2[k], xn2[k], xT2[k], pT2[k]
                    for j in range(4):
                        for half in range(2):
                            hs = slice(half * 512, (half + 1) * 512)
                            gt = gts[(j * 2 + half) % 3]
                            tq = tqs[(j * 2 + half) % 3]
                            bg_ = next_pf()
                            mm_group(bg_, (0, 512),
                                     [xT.t[:, c, j * 128:(j + 1) * 128] for c in range(8)] + [onesrow.t[0:1, :]],
                                     [wpg.t[:, c, hs] for c in range(8)] + [gbb.t[0:1, hs]],
                                     [[xT.res[c], wpg.r] for c in range(8)] + [[onesrow.r, gbb.r]])
                            bp_ = next_pf()
                            mm_group(bp_, (0, 512), [pT.t[:, c, j * 128:(j + 1) * 128] for c in range(2)],
                                     [wpp.t[:, c, hs] for c in range(2)], [[pT.r, wpp.r]] * 2)
                            P.op("act", lambda e: e.activation(out=gt.t[:], in_=bg_.t[:], func=AF.Sigmoid),
                                 reads=[bg_.r], writes=[gt.r])
                            P.op("dve", lambda e: e.tensor_tensor(out=tq.t[:], in0=bp_.t[:], in1=gt.t[:], op=ALU.mult),
                                 reads=[bp_.r, gt.r], writes=[tq.r])
                            P.op("dve", lambda e: e.tensor_tensor(out=ht.t[:, j, hs], in0=ht.t[:, j, hs], in1=tq.t[:],
                                                                  op=ALU.add),
                                 reads=[ht.r, tq.r], writes=[ht.r])
                    if l < DEPTH - 1:
                        P.dma("sp", tm(h_d, i), ht.t[:], reads=[ht.r], writes=[R_h[i]])
                    else:
                        norm_stats(st, ht, 0, ss, rstd, sqj)
                        for j in range(4):
                            P.op("dve", lambda e: e.scalar_tensor_tensor(out=ht.t[:, j, :], in0=ht.t[:, j, :],
                                                                         scalar=rstd.t[:, j:j + 1], in1=gfin.t[:],
                                                                         op0=ALU.mult, op1=ALU.mult),
                                 reads=[ht.r, rstd.r, gfin.r], writes=[ht.r])
                        P.dma("sp", tm(out_d, i), ht.t[:], reads=[ht.r], writes=[R_out[i]])

                s5_load(0)
                if NT > 1:
                    s5_load(1)
                s5_P(0)
                for i in range(NT):
                    if i + 2 < NT:
                        s5_load(i + 2)
                    if i + 1 < NT:
                        s5_P(i + 1)
                    s5_M(i)
                P.barrier()
            st_ple.close()

        P.barrier()
        P.final_wait()
    return P


def _host_consts():
    c = {}
    c["c_identf"] = np.eye(128, dtype=np.float32)
    rp = np.zeros((128, 128), np.float32)
    for b in range(4):
        for d in range(16):
            rp[b * 32 + d + 16, b * 32 + d] = -1.0
            rp[b * 32 + d, b * 32 + d + 16] = 1.0
    c["c_rperm"] = rp
    kk = np.arange(128)[:, None]
    qq = np.arange(128)[None, :]
    c["c_tri"] = (qq >= kk).astype(np.float32)
    b64 = np.zeros((128, 128), np.float32)
    b64[0:64, 0:64] = 1.0 / 64
    b64[64:128, 64:128] = 1.0 / 64
    c["c_blk64"] = b64
    c["c_ones256"] = np.full((128, 128), 1.0 / 256, np.float32)
    sel = np.zeros((16, NE, 128), np.float32)
    for e in range(NE):
        sel[e, e, :] = 1.0
    c["c_sel"] = sel
    pos = np.arange(S, dtype=np.float32)
    inv = (10000.0 ** (-np.arange(0, 32, 2, dtype=np.float32) / np.float32(32))).astype(np.float32)
    ang = (pos[:, None] * inv[None, :]).astype(np.float32)
    ang = np.concatenate([ang, ang], axis=-1)
    cosT = np.cos(ang.astype(np.float64)).astype(np.float32).T
    sinT = np.sin(ang.astype(np.float64)).astype(np.float32).T
    c["c_cos"] = np.ascontiguousarray(np.tile(cosT, (4, 1)))
    c["c_sin"] = np.ascontiguousarray(np.tile(sinT, (4, 1)))
    wins = np.array([2, 4, 8, 16])
    corr = np.zeros((128, 2, 16), np.float32)
    for cc in range(2):
        for p in range(128):
            w = wins[cc * 2 + p // 64]
            for t in range(16):
                corr[p, cc, t] = w / min(t + 1, w)
    c["c_corr"] = corr
    iw = np.zeros((128, 2), np.float32)
    for cc in range(2):
        for p in range(128):
            iw[p, cc] = 1.0 / wins[cc * 2 + p // 64]
    c["_iw"] = iw
    return c


def _fmcols(v, nch):
    return np.ascontiguousarray(np.asarray(v, np.float32).reshape(nch, 128).T)


def _layout_inputs(inp):
    c = _host_consts()
    iw = c.pop("_iw")
    shared = dict(c)
    prm = np.zeros((DEPTH, 128, PC_N), np.float32)
    wr = np.zeros((DEPTH, 128, 8, 20), np.float32)
    rb = np.zeros((DEPTH, 128, 20), np.float32)
    lamv = np.zeros((DEPTH, 128, 4, 32), np.float32)
    pw = np.zeros((DEPTH, 128, 2, 128), np.float32)
    for l in range(DEPTH):
        prm[l, :, PC_MIXG:PC_MIXG + 8] = _fmcols(inp["mix_norm"][l], 8)
        prm[l, :, PC_FFNG:PC_FFNG + 8] = _fmcols(inp["ffn_norm"][l], 8)
        prm[l, :, PC_PLEG:PC_PLEG + 8] = _fmcols(inp["ple_norm"][l], 8)
        prm[l, :, PC_FING:PC_FING + 8] = _fmcols(inp["final_norm"], 8)
        cw = np.asarray(inp["conf_conv_w"][l], np.float32)
        for cc in range(2):
            prm[l, :, PC_CW + cc * CK:PC_CW + (cc + 1) * CK] = cw[:, cc * 128:(cc + 1) * 128].T
        prm[l, :, PC_CB:PC_CB + 2] = _fmcols(inp["conf_conv_b"][l], 2)
        prm[l, :, PC_LNG:PC_LNG + 2] = _fmcols(inp["conf_ln_g"][l], 2)
        prm[l, :, PC_LNB:PC_LNB + 2] = _fmcols(inp["conf_ln_b"][l], 2)
        prm[l, :, PC_PB:PC_PB + 2] = _fmcols(np.asarray(inp["pool_b"][l]).reshape(256), 2)
        prm[l, :, PC_PS:PC_PS + 2] = _fmcols(inp["pool_scale"][l], 2)
        sw = np.asarray(inp["sconv_w"][l], np.float32)
        for cc in range(2):
            prm[l, :, PC_SW + cc * 3:PC_SW + (cc + 1) * 3] = sw[:, cc * 128:(cc + 1) * 128].T
        prm[l, :, PC_SUB] = np.tile(np.asarray(inp["diff_subln_g"][l], np.float32), 2)
        prm[l, :, PC_IW:PC_IW + 2] = iw
        wcat = np.concatenate([np.asarray(inp["router_group_w"][l], np.float32),
                               np.asarray(inp["router_expert_w"][l], np.float32)], axis=1)
        wr[l] = wcat.reshape(8, 128, 20).transpose(1, 0, 2)
        bcat = np.concatenate([np.asarray(inp["router_group_b"][l], np.float32),
                               np.asarray(inp["router_expert_b"][l], np.float32)])
        rb[l] = np.tile(bcat[None, :], (128, 1))
        for n_, key in enumerate(["diff_lam_q1", "diff_lam_k1", "diff_lam_q2", "diff_lam_k2"]):
            lamv[l, :, n_, :] = np.tile(np.asarray(inp[key][l], np.float32)[None, :], (128, 1))
        pwl = np.asarray(inp["pool_w"][l], np.float32)
        for g in range(4):
            cc, hh = g // 2, g % 2
            pw[l, hh * 64:(hh + 1) * 64, cc, hh * 64:(hh + 1) * 64] = pwl[g]
    shared.update({
        "prm": prm, "wr": wr, "rbias": rb, "lamv": lamv, "poolw": pw,
        "gfin": np.ascontiguousarray(np.tile(np.asarray(inp["final_norm"], np.float32)[None, :], (128, 1))),
    })
    for key in ["w_in", "w_out", "expert_w_gate", "expert_w_up", "expert_w_down", "ple_gate_w", "ple_gate_b",
                "ple_proj"]:
        shared[key] = np.ascontiguousarray(np.asarray(inp[key], np.float32))
    return shared


_NC_CACHE = {}


def _get_nc():
    if "nc" not in _NC_CACHE:
        nc = bass.Bass("TRN2", target_bir_lowering=False)
        build(nc)
        _NC_CACHE["nc"] = nc
    return _NC_CACHE["nc"]


def kernel(**inputs):
    shared = _layout_inputs(inputs)
    x = np.asarray(inputs["x"], np.float32)
    p = np.asarray(inputs["p"], np.float32)
    n = x.shape[0]
    in_maps = []
    for b in range(n):
        m = dict(shared)
        m["x"] = np.ascontiguousarray(x[b])
        m["p"] = np.ascontiguousarray(p[:, b])
        in_maps.append(m)
    nc = _get_nc()
    res = run_bass_kernel_spmd(nc, in_maps, core_ids=list(range(n)))
    return np.stack([np.asarray(r["out"], np.float32) for r in res.results], axis=0)
```

```python
import math
import contextlib
import numpy as np
import ml_dtypes
import concourse.bass as bass
import concourse.mybir as mybir
from concourse.bass_utils import run_bass_kernel_spmd

F32 = mybir.dt.float32
BF16 = mybir.dt.bfloat16
ALU = mybir.AluOpType
AF = mybir.ActivationFunctionType
AX = mybir.AxisListType

S = 4096
D = 1024
DEPTH = 2
NT = 8
TS = 512
INC = 2304
NE = 16
EPS = 1e-6
CK = 31
SCALE = 32 ** -0.5
SEM_CHUNK = 30000

PC_MIXG, PC_FFNG, PC_PLEG, PC_FING = 0, 8, 16, 24
PC_CW = 32
PC_CB = PC_CW + 62
PC_LNG = PC_CB + 2
PC_LNB = PC_LNG + 2
PC_PB = PC_LNB + 2
PC_PS = PC_PB + 2
PC_SW = PC_PS + 2
PC_SUB = PC_SW + 6
PC_IW = PC_SUB + 1
PC_N = PC_IW + 2


class Tok:
    __slots__ = ("sem", "val", "eng")

    def __init__(self, eng):
        self.sem = None
        self.val = None
        self.eng = eng


class Res:
    __slots__ = ("name", "w", "r", "excl")

    def __init__(self, name, excl=False):
        self.name = name
        self.w = None
        self.r = []
        self.excl = excl


class Prog:
    def __init__(self, nc, es):
        self.nc = nc
        self.es = es
        self.eobj = {"pe": nc.tensor, "act": nc.scalar, "dve": nc.vector,
                     "pool": nc.gpsimd, "sp": nc.sync}
        self.sems = {e: [] for e in self.eobj}
        self.count = {e: 0 for e in self.eobj}
        self.known = {e: {} for e in self.eobj}
        self.pending = {e: None for e in self.eobj}
        self.last_tok = {e: None for e in self.eobj}
        self.nsem = 0
        self.dma_sems = []
        self.dma_cnt = []
        self.dma_i = 0
        self.dma_ip = 0
        for i in range(24):
            self.dma_sems.append(self._new_sem("dq%d" % i))
            self.dma_cnt.append(0)
        self.dma_toks = []
        self.n_ops = 0
        self.n_waits = 0
        self.limit = None
        self.stores_on_pool = False
        self.n_all = 0
        self.skip = False

    def _skipping(self):
        if self.skip:
            return True
        if self.limit is not None and self.n_all > self.limit and all(v is None for v in self.pending.values()):
            self.skip = True
            return True
        return False

    def _new_sem(self, name):
        self.nsem += 1
        return self.es.enter_context(self.nc.semaphore(name))

    def _eng_tok(self, eng, tok):
        c = self.count[eng]
        idx = c // SEM_CHUNK
        while len(self.sems[eng]) <= idx:
            self.sems[eng].append(self._new_sem("%s%d" % (eng, len(self.sems[eng]))))
        tok.sem = self.sems[eng][idx]
        tok.val = c % SEM_CHUNK + 1
        self.count[eng] = c + 1
        return tok

    def _wait(self, eng, tok):
        if tok is None:
            return
        if tok.sem is None:
            assert tok.eng == eng, "dependency on unsignaled op of %s from %s" % (tok.eng, eng)
            return
        k = self.known[eng]
        sid = id(tok.sem)
        if k.get(sid, 0) >= tok.val:
            return
        k[sid] = tok.val
        self.eobj[eng].wait_ge(tok.sem, tok.val)
        self.n_waits += 1

    def _deps(self, eng, reads, writes, is_dma=False):
        for r in reads:
            if r.w is not None:
                if r.w.eng == eng and not is_dma and eng == "pe":
                    continue
                self._wait(eng, r.w)
        same_ok = (eng == "pe") and not is_dma
        for w in writes:
            if w.w is not None and not (same_ok and w.w.eng == eng):
                self._wait(eng, w.w)
            for t in w.r:
                if not (same_ok and t.eng == eng):
                    self._wait(eng, t)

    def op(self, eng, fn, reads=(), writes=(), signal=True):
        self.n_all += 1
        if self._skipping():
            return None
        if any(r.excl for r in reads):
            writes = list(writes) + [r for r in reads if r.excl and r not in writes]
            reads = [r for r in reads if not r.excl]
        self._deps(eng, reads, writes)
        ins = fn(self.eobj[eng])
        self.n_ops += 1
        tok = self.pending[eng]
        if tok is None:
            tok = Tok(eng)
            self.pending[eng] = tok
        if signal:
            self._eng_tok(eng, tok)
            ins.then_inc(tok.sem, 1)
            self.pending[eng] = None
            self.last_tok[eng] = tok
        for r in reads:
            r.r.append(tok)
        for w in writes:
            w.w = tok
            w.r = []
        return tok

    def dma(self, q, out, in_, reads=(), writes=()):
        if q == "sp" and self.stores_on_pool and str(out.space).endswith("DRAM"):
            q = "pool"
        self.n_all += 1
        if self._skipping():
            return None
        self._deps(q, reads, writes, is_dma=True)
        if q == "pool":
            i = 16 + self.dma_ip % 8
            self.dma_ip += 1
        else:
            i = self.dma_i % 16
            self.dma_i += 1
        sem = self.dma_sems[i]
        if self.dma_cnt[i] > 0:
            prev = Tok("dma")
            prev.sem = sem
            prev.val = self.dma_cnt[i]
            self._wait(q, prev)
        self.eobj[q].dma_start(out=out, in_=in_).then_inc(sem, 16)
        self.dma_cnt[i] += 16
        tok = Tok("dma")
        tok.sem = sem
        tok.val = self.dma_cnt[i]
        self.dma_toks.append(tok)
        for r in reads:
            r.r.append(tok)
        for w in writes:
            w.w = tok
            w.r = []
        return tok

    def barrier(self):
        toks = [t for t in self.last_tok.values() if t is not None]
        for i, s in enumerate(self.dma_sems):
            if self.dma_cnt[i] > 0:
                t = Tok("dma")
                t.sem = s
                t.val = self.dma_cnt[i]
                toks.append(t)
        for e in self.eobj:
            assert self.pending[e] is None
            for t in toks:
                if t.eng == e:
                    continue
                self._wait(e, t)

    def final_wait(self):
        for i, s in enumerate(self.dma_sems):
            if self.dma_cnt[i] > 0:
                t = Tok("dma")
                t.sem = s
                t.val = self.dma_cnt[i]
                self._wait("sp", t)


class Buf:
    def __init__(self, t, name, nslots=1):
        self.t = t
        self.res = [Res("%s.%d" % (name, i)) for i in range(nslots)]

    @property
    def r(self):
        return self.res[0]


def build(nc, dbg=None, limit=None):
    P = None
    with contextlib.ExitStack() as es:
        P = Prog(nc, es)
        P.limit = limit

        def dram_in(name, shape, dt=F32):
            return nc.dram_tensor(name, list(shape), dt, kind="ExternalInput").ap()

        def dram_scr(name, shape, dt, kind="Internal"):
            return nc.dram_tensor(name, list(shape), dt, kind=kind).ap()

        x_d = dram_in("x", [S, D])
        p_d = dram_in("p", [DEPTH, S, 256])
        w_in_d = dram_in("w_in", [DEPTH, D, INC])
        w_out_d = dram_in("w_out", [DEPTH, D, D])
        wg_d = dram_in("expert_w_gate", [DEPTH, NE, D, 256])
        wu_d = dram_in("expert_w_up", [DEPTH, NE, D, 256])
        wd_d = dram_in("expert_w_down", [DEPTH, NE, 256, D])
        pgw_d = dram_in("ple_gate_w", [DEPTH, D, D])
        pgb_d = dram_in("ple_gate_b", [DEPTH, D])
        ppj_d = dram_in("ple_proj", [DEPTH, 256, D])
        prm_d = dram_in("prm", [DEPTH, 128, PC_N])
        wr_d = dram_in("wr", [DEPTH, 128, 8, 20])
        rb_d = dram_in("rbias", [DEPTH, 128, 20])
        lam_d = dram_in("lamv", [DEPTH, 128, 4, 32])
        pw_d = dram_in("poolw", [DEPTH, 128, 2, 128])
        gfin_d = dram_in("gfin", [128, D])
        cidf_d = dram_in("c_identf", [128, 128])
        crp_d = dram_in("c_rperm", [128, 128])
        ctri_d = dram_in("c_tri", [128, 128])
        cb64_d = dram_in("c_blk64", [128, 128])
        cones_d = dram_in("c_ones256", [128, 128])
        csel_d = dram_in("c_sel", [16, NE, 128])
        ccos_d = dram_in("c_cos", [128, S])
        csin_d = dram_in("c_sin", [128, S])
        ccorr_d = dram_in("c_corr", [128, 2, 16])
        out_d = nc.dram_tensor("out", [S, D], F32, kind="ExternalOutput").ap()

        dkind = "ExternalOutput" if dbg else "Internal"
        h_d = dram_scr("h_scr", [S, D], F32, dkind)
        glu_d = dram_scr("glu_scr", [256, S], BF16, dkind)
        pin_d = dram_scr("pin_scr", [256, S], F32, dkind)
        q_d = dram_scr("q_scr", [256, S], BF16, dkind)
        k_d = dram_scr("k_scr", [256, S], BF16, dkind)
        v_d = dram_scr("v_scr", [S, 512], BF16, dkind)
        gb_d = dram_scr("gb_scr", [256, S], F32, dkind)
        gcv_d = dram_scr("gcv_scr", [256, S], F32, dkind)
        mix_d = dram_scr("mix_scr", [D, S], BF16, dkind)
        xT_d = dram_scr("xT_scr", [D, S], BF16, dkind)
        cmb_d = dram_scr("cmb_scr", [16, S], F32, dkind)

        def tiles(name):
            return [Res("%s%d" % (name, i)) for i in range(NT)]
        R_h = tiles("h")
        R_glu, R_pin, R_q, R_k, R_v = tiles("glu"), tiles("pin"), tiles("q"), tiles("k"), tiles("v")
        R_gb, R_gcv, R_mixA, R_mixB, R_xT, R_cmb = (tiles("gb"), tiles("gcv"), tiles("mixA"),
                                                    tiles("mixB"), tiles("xT"), tiles("cmb"))
        R_out = tiles("out")

        def fm(ap, i, lo=0, hi=TS):
            return ap.rearrange("(c p) t -> p c t", p=128)[:, :, i * TS + lo:i * TS + hi]

        def tm(ap, i):
            return ap[i * TS:(i + 1) * TS, :].rearrange("(j p) f -> p j f", p=128)

        uid = [0]
        def sbuf(stack, name, shape, dt, nslots=1):
            uid[0] += 1
            t = stack.enter_context(nc.sbuf_tensor("%s_u%d" % (name, uid[0]), list(shape), dt))
            return Buf(t, name, nslots)

        def psum(stack, name, shape, dt=F32):
            t = stack.enter_context(nc.psum_tensor(name, list(shape), dt))
            b = Buf(t, name, 1)
            b.res[0].excl = True
            return b

        identf = sbuf(es, "identf", [128, 128], F32)
        identb = sbuf(es, "identb", [128, 128], BF16)
        rperm = sbuf(es, "rperm", [128, 128], F32)
        trib = sbuf(es, "trib", [128, 128], BF16)
        blk64 = sbuf(es, "blk64", [128, 128], F32)
        ones256 = sbuf(es, "ones256", [128, 128], F32)
        sel = sbuf(es, "sel", [16, NE, 128], F32)
        prm_l = [sbuf(es, "prm_sb%d" % l_, [128, PC_N], F32) for l_ in range(DEPTH)]
        corr = sbuf(es, "corr", [128, 2, 16], F32)
        gfin = sbuf(es, "gfin_sb", [128, D], F32)
        neglam_l = [sbuf(es, "neglam%d" % l_, [128, 1], F32) for l_ in range(DEPTH)]
        pbs_l = [sbuf(es, "pbs%d" % l_, [128, 2], F32) for l_ in range(DEPTH)]
        prm, neglam, pbs = prm_l[0], neglam_l[0], pbs_l[0]
        onesrow = sbuf(es, "onesrow", [1, 128], BF16)
        epsc = sbuf(es, "epsc", [128, 1], F32)

        pf = []
        pb = []
        pf_i = [0]
        pb_i = [0]

        def set_psum(stack, nf, nb):
            uid[0] += 1
            pf[:] = [psum(stack, "pf%d_%d" % (i, uid[0]), [128, 512], F32) for i in range(nf)]
            pb[:] = [psum(stack, "pb%d_%d" % (i, uid[0]), [128, 1024], BF16) for i in range(nb)]

        def next_pf():
            b = pf[pf_i[0] % len(pf)]
            pf_i[0] += 1
            return b

        def next_pb():
            b = pb[pb_i[0] % len(pb)]
            pb_i[0] += 1
            return b

        P.dma("sp", identf.t[:], cidf_d, writes=[identf.r])
        P.dma("sp", rperm.t[:], crp_d, writes=[rperm.r])
        P.dma("sp", blk64.t[:], cb64_d, writes=[blk64.r])
        P.dma("sp", ones256.t[:], cones_d, writes=[ones256.r])
        P.dma("sp", sel.t[:], csel_d, writes=[sel.r])
        P.dma("sp", corr.t[:], ccorr_d, writes=[corr.r])
        P.dma("sp", gfin.t[:], gfin_d, writes=[gfin.r])
        P.dma("pool", identb.t[:], cidf_d, writes=[identb.r])
        P.dma("pool", trib.t[:], ctri_d, writes=[trib.r])
        P.op("dve", lambda e: e.memset(onesrow.t[:], 1.0), writes=[onesrow.r])
        P.op("dve", lambda e: e.memset(epsc.t[:], EPS), writes=[epsc.r])

        def norm_stats(st, ht, slot, ss, rstd, sqj):
            P.op("dve", lambda e: e.memset(ss.t[:], 0.0), writes=[ss.r])
            for j in range(4):
                P.op("act", lambda e, j=j: e.activation(out=sqj.t[:], in_=ht.t[:, j, :], func=AF.Square,
                                                        accum_out=ss.t[:, j:j + 1]),
                     reads=[ht.res[slot], ss.r], writes=[sqj.r, ss.r])
            P.op("dve", lambda e: e.tensor_scalar(out=rstd.t[:], in0=ss.t[:], scalar1=1.0 / D, scalar2=EPS,
                                                  op0=ALU.mult, op1=ALU.add), reads=[ss.r], writes=[rstd.r])
            P.op("act", lambda e: e.activation(out=rstd.t[:], in_=rstd.t[:], func=AF.Sqrt),
                 reads=[rstd.r], writes=[rstd.r])
            P.op("dve", lambda e: e.reciprocal(out=rstd.t[:], in_=rstd.t[:]), reads=[rstd.r], writes=[rstd.r])

        def norm_transpose(ht, slot, rstd, xn, xT, gcol, part=None):
            for j in range(4 if part in (None, 0) else 0):
                P.op("dve", lambda e, j=j: e.tensor_scalar(out=xn.t[:, j, :], in0=ht.t[:, j, :],
                                                           scalar1=rstd.t[:, j:j + 1], scalar2=None, op0=ALU.mult),
                     reads=[ht.res[slot], rstd.r], writes=[xn.res[j]])
            for c2 in range(4 if part in (None, 1) else 0):
                bank = next_pb()
                for cc in range(2):
                    c = c2 * 2 + cc
                    for j in range(4):
                        last = (cc == 1 and j == 3)
                        P.op("pe", lambda e, c=c, cc=cc, j=j: e.transpose(
                            bank.t[:, cc * 512 + j * 128: cc * 512 + (j + 1) * 128],
                            xn.t[:, j, c * 128:(c + 1) * 128], identb.t[:]),
                            reads=[xn.res[j], identb.r], writes=[bank.r], signal=last)
                for cc in range(2):
                    c = c2 * 2 + cc
                    if cc == 0:
                        P.op("act", lambda e, c=c, cc=cc: e.activation(
                            out=xT.t[:, c, :], in_=bank.t[:, cc * 512:(cc + 1) * 512], func=AF.Identity,
                            scale=prm.t[:, gcol + c:gcol + c + 1]),
                            reads=[bank.r, prm.r], writes=[xT.res[c]])
                    else:
                        P.op("dve", lambda e, c=c, cc=cc: e.tensor_scalar(
                            out=xT.t[:, c, :], in0=bank.t[:, cc * 512:(cc + 1) * 512],
                            scalar1=prm.t[:, gcol + c:gcol + c + 1], scalar2=None, op0=ALU.mult),
                            reads=[bank.r, prm.r], writes=[xT.res[c]])

        def load_h(i, ht, slot, src):
            P.dma("sp", ht.t[:], tm(src, i), reads=[R_h[i]], writes=[ht.res[slot]])

        def mm_group(bank, cols, lhs_list, rhs_list, reads, tile_position=None):
            n = len(lhs_list)
            for k in range(n):
                P.op("pe", lambda e, k=k: e.matmul(bank.t[:, cols[0]:cols[1]], lhs_list[k], rhs_list[k],
                                                   start=(k == 0), stop=(k == n - 1)),
                     reads=reads[k], writes=[bank.r], signal=(k == n - 1))

        def load_w_in(l_):
            stw = contextlib.ExitStack()
            uid[0] += 1
            t_ = stw.enter_context(nc.sbuf_tensor("w_in_sb_u%d" % uid[0], [128, 8, INC], BF16, side="right"))
            b_ = Buf(t_, "w_in_sb", 1)
            for c in range(8):
                P.dma("pool", b_.t[:, c, :], w_in_d[l_, c * 128:(c + 1) * 128, :], writes=[b_.r])
            return stw, b_

        for l in range(DEPTH):
            lam_init = 0.8 - 0.6 * math.exp(-0.3 * l)
            prm, neglam, pbs = prm_l[l], neglam_l[l], pbs_l[l]
            P.dma("sp", prm.t[:], prm_d[l], writes=[prm.r])
            with contextlib.ExitStack() as st:
                lamv = sbuf(st, "lamv", [128, 4, 32], F32)
                lt = sbuf(st, "lt", [128, 2, 32], F32)
                ls = sbuf(st, "ls", [128, 2], F32)
                P.dma("sp", lamv.t[:], lam_d[l], writes=[lamv.r])
                P.op("dve", lambda e: e.tensor_tensor(out=lt.t[:, 0, :], in0=lamv.t[:, 0, :], in1=lamv.t[:, 1, :],
                                                      op=ALU.mult), reads=[lamv.r], writes=[lt.r])
                P.op("dve", lambda e: e.tensor_tensor(out=lt.t[:, 1, :], in0=lamv.t[:, 2, :], in1=lamv.t[:, 3, :],
                                                      op=ALU.mult), reads=[lamv.r, lt.r], writes=[lt.r])
                P.op("dve", lambda e: e.reduce_sum(out=ls.t[:], in_=lt.t[:], axis=AX.X), reads=[lt.r], writes=[ls.r])
                P.op("act", lambda e: e.activation(out=ls.t[:], in_=ls.t[:], func=AF.Exp), reads=[ls.r], writes=[ls.r])
                P.op("dve", lambda e: e.scalar_tensor_tensor(out=neglam.t[:], in0=ls.t[:, 1:2], scalar=-lam_init,
                                                             in1=ls.t[:, 0:1], op0=ALU.add, op1=ALU.subtract),
                     reads=[ls.r], writes=[neglam.r])
                P.op("dve", lambda e: e.tensor_tensor(out=pbs.t[:], in0=prm.t[:, PC_PB:PC_PB + 2],
                                                      in1=prm.t[:, PC_PS:PC_PS + 2], op=ALU.mult),
                     reads=[prm.r], writes=[pbs.r])

            P.barrier()
        st_win, w_in_next = load_w_in(0)
        for l in range(DEPTH):
            lam_init = 0.8 - 0.6 * math.exp(-0.3 * l)
            h_src = x_d if l == 0 else h_d

            prm, neglam, pbs = prm_l[l], neglam_l[l], pbs_l[l]
            st_dg = contextlib.ExitStack()
            dg = sbuf(st_dg, "s2_dg", [128, 2, CK, 128], BF16)
            pw_sb = sbuf(st_dg, "s2_pw", [128, 2, 128], BF16)
            with contextlib.ExitStack() as st:
                set_psum(st, 6, 2)
                w_in_sb = w_in_next
                ht = sbuf(st, "s1_ht", [128, 4, D], F32, 2)
                hts = [ht, sbuf(st, "s1_ht2", [128, 4, D], F32, 2)]
                ss2 = [sbuf(st, "s1_ss%d" % k, [128, 4], F32) for k in range(2)]
                rstd2 = [sbuf(st, "s1_rstd%d" % k, [128, 4], F32) for k in range(2)]
                sqj = sbuf(st, "s1_sqj", [128, D], BF16)
                xn2 = [sbuf(st, "s1_xn%d" % k, [128, 4, D], BF16, 4) for k in range(2)]
                nT2 = [sbuf(st, "s1_nT%d" % k, [128, 8, TS], BF16, 8) for k in range(2)]
                sig = sbuf(st, "s1_sig", [128, TS], F32)
                gcs = sbuf(st, "s1_gc", [128, TS], F32)
                glu_st = sbuf(st, "s1_glu", [128, 2, TS], BF16)
                pin_st = sbuf(st, "s1_pin", [128, 2, TS], F32)
                qk_st = sbuf(st, "s1_qk", [128, 4, TS], F32, 4)
                qkr_st = sbuf(st, "s1_qkr", [128, 4, TS], BF16, 4)
                gb_st = sbuf(st, "s1_gb", [128, 2, TS], F32)
                gcv_st = sbuf(st, "s1_gcv", [128, 2, TS], F32)
                v_st = sbuf(st, "s1_v", [128, 4, 512], BF16)
                cos_t = sbuf(st, "s1_cos", [128, TS], F32)
                sin_t = sbuf(st, "s1_sin", [128, TS], F32)
                t1 = sbuf(st, "s1_t1", [128, TS], F32)
                t2 = sbuf(st, "s1_t2", [128, TS], F32)

                P.op("dve", lambda e: e.memset(v_st.t[:], 1.0), writes=[v_st.r])

                def s1_prologue(i_):
                    norm_stats(st, hts[i_ % 2], 0, ss2[i_ % 2], rstd2[i_ % 2], sqj)
                    norm_transpose(hts[i_ % 2], 0, rstd2[i_ % 2], xn2[i_ % 2], nT2[i_ % 2], PC_MIXG)

                P.dma("sp", hts[0].t[:], tm(h_src, 0), reads=[R_h[0]], writes=[hts[0].r])
                P.dma("sp", hts[1].t[:], tm(h_src, 1), reads=[R_h[1]], writes=[hts[1].r])
                s1_prologue(0)
                P.dma("pool", pw_sb.t[:], pw_d[l], writes=[pw_sb.r])
                for cc in range(2):
                    for j in range(CK):
                        col = PC_CW + cc * CK + j
                        P.op("dve", lambda e, cc=cc, j=j, col=col: e.tensor_scalar(
                            out=dg.t[:, cc, j, :], in0=identf.t[:], scalar1=prm.t[:, col:col + 1], scalar2=None,
                            op0=ALU.mult), reads=[identf.r, prm.r], writes=[dg.r])
                for i in range(NT):
                    P.dma("sp", cos_t.t[:], ccos_d[:, i * TS:(i + 1) * TS], writes=[cos_t.r])
                    P.dma("sp", sin_t.t[:], csin_d[:, i * TS:(i + 1) * TS], writes=[sin_t.r])
                    nT = nT2[i % 2]

                    def proj(col0):
                        bank = next_pf()
                        mm_group(bank, (0, TS), [w_in_sb.t[:, c, col0:col0 + 128] for c in range(8)],
                                 [nT.t[:, c, :] for c in range(8)],
                                 [[w_in_sb.r, nT.res[c]] for c in range(8)])
                        return bank

                    for cc in range(2):
                        bg_ = proj(256 + cc * 128)
                        P.op("act", lambda e: e.activation(out=sig.t[:], in_=bg_.t[:], func=AF.Sigmoid),
                             reads=[bg_.r], writes=[sig.r])
                        bv_ = proj(0 + cc * 128)
                        P.op("dve", lambda e: e.tensor_tensor(out=glu_st.t[:, cc, :], in0=bv_.t[:], in1=sig.t[:],
                                                              op=ALU.mult),
                             reads=[bv_.r, sig.r], writes=[glu_st.r])
                    for cc in range(2):
                        bp_ = proj(512 + cc * 128)
                        P.op("act", lambda e: e.copy(out=pin_st.t[:, cc, :], in_=bp_.t[:]),
                             reads=[bp_.r], writes=[pin_st.r])
                    if i + 1 < NT:
                        s1_prologue(i + 1)
                    for m in range(4):
                        bq_ = proj(768 + m * 128)
                        if m % 2 == 0:
                            P.op("act", lambda e: e.copy(out=qk_st.t[:, m, :], in_=bq_.t[:]),
                                 reads=[bq_.r], writes=[qk_st.res[m]])
                        else:
                            P.op("dve", lambda e: e.tensor_copy(out=qk_st.t[:, m, :], in_=bq_.t[:]),
                                 reads=[bq_.r], writes=[qk_st.res[m]])
                    for cc in range(2):
                        bb_ = proj(1536 + cc * 128)
                        P.op("act", lambda e: e.copy(out=gb_st.t[:, cc, :], in_=bb_.t[:]),
                             reads=[bb_.r], writes=[gb_st.r])
                    for cc in range(2):
                        bc_ = proj(1792 + cc * 128)
                        P.op("act", lambda e: e.copy(out=gcs.t[:], in_=bc_.t[:]), reads=[bc_.r], writes=[gcs.r])
                        bs_ = proj(2048 + cc * 128)
                        P.op("dve", lambda e: e.tensor_tensor(out=gcv_st.t[:, cc, :], in0=bs_.t[:], in1=gcs.t[:],
                                                              op=ALU.mult),
                             reads=[bs_.r, gcs.r], writes=[gcv_st.r])
                    for j in range(4):
                        bank = next_pf()
                        mm_group(bank, (0, 256), [nT.t[:, c, j * 128:(j + 1) * 128] for c in range(8)],
                                 [w_in_sb.t[:, c, 1280:1536] for c in range(8)],
                                 [[w_in_sb.r, nT.res[c]] for c in range(8)])
                        for hh in range(4):
                            off = hh * 128 + (0 if hh % 2 == 0 else 64)
                            eng = "act" if hh % 2 == 0 else "dve"
                            if eng == "act":
                                P.op("act", lambda e: e.copy(out=v_st.t[:, j, off:off + 64],
                                                             in_=bank.t[:, hh * 64:(hh + 1) * 64]),
                                     reads=[bank.r], writes=[v_st.r])
                            else:
                                P.op("dve", lambda e: e.tensor_copy(out=v_st.t[:, j, off:off + 64],
                                                                    in_=bank.t[:, hh * 64:(hh + 1) * 64]),
                                     reads=[bank.r], writes=[v_st.r])
                    for m in range(4):
                        bank = next_pf()
                        P.op("pe", lambda e: e.matmul(bank.t[:], rperm.t[:], qk_st.t[:, m, :], start=True, stop=True),
                             reads=[rperm.r, qk_st.res[m]], writes=[bank.r])
                        P.op("dve", lambda e: e.tensor_tensor(out=t1.t[:], in0=qk_st.t[:, m, :], in1=cos_t.t[:],
                                                              op=ALU.mult),
                             reads=[qk_st.res[m], cos_t.r], writes=[t1.r])
                        P.op("dve", lambda e: e.tensor_tensor(out=t2.t[:], in0=bank.t[:], in1=sin_t.t[:],
                                                              op=ALU.mult),
                             reads=[bank.r, sin_t.r], writes=[t2.r])
                        P.op("dve", lambda e: e.tensor_tensor(out=qkr_st.t[:, m, :], in0=t1.t[:], in1=t2.t[:],
                                                              op=ALU.add),
                             reads=[t1.r, t2.r], writes=[qkr_st.res[m]])
                    if i + 2 < NT:
                        P.dma("sp", hts[i % 2].t[:], tm(h_src, i + 2), reads=[R_h[i + 2]], writes=[hts[i % 2].r])
                    P.dma("sp", fm(glu_d, i), glu_st.t[:], reads=[glu_st.r], writes=[R_glu[i]])
                    P.dma("sp", fm(pin_d, i), pin_st.t[:], reads=[pin_st.r], writes=[R_pin[i]])
                    P.dma("sp", fm(q_d, i), qkr_st.t[:, 0:2, :], reads=[qkr_st.res[0], qkr_st.res[1]], writes=[R_q[i]])
                    P.dma("sp", fm(k_d, i), qkr_st.t[:, 2:4, :], reads=[qkr_st.res[2], qkr_st.res[3]], writes=[R_k[i]])
                    P.dma("sp", tm(v_d, i), v_st.t[:], reads=[v_st.r], writes=[R_v[i]])
                    P.dma("sp", fm(gb_d, i), gb_st.t[:], reads=[gb_st.r], writes=[R_gb[i]])
                    P.dma("sp", fm(gcv_d, i), gcv_st.t[:], reads=[gcv_st.r], writes=[R_gcv[i]])
                P.barrier()
            st_win.close()
            if dbg == "s1":
                st_dg.close()
                break

            st_wout = contextlib.ExitStack()
            uid[0] += 1
            w_out_sb = Buf(st_wout.enter_context(nc.sbuf_tensor("w_out_sb_u%d" % uid[0], [128, 8, D], BF16,
                                                                side="right")), "w_out_sb", 1)
            st_kv = contextlib.ExitStack()
            kT = sbuf(st_kv, "at_kT", [128, 2, S], BF16)
            Vs = sbuf(st_kv, "at_V", [128, 32, 512], BF16)
            with contextlib.ExitStack() as st:
                set_psum(st, 8, 0)
                glu_in = [sbuf(st, "s2_glu%d" % k, [128, 2, 30 + TS], BF16) for k in range(2)]
                pin_in = [sbuf(st, "s2_pin%d" % k, [128, 2, 16 + TS], F32) for k in range(2)]
                gcv_in = [sbuf(st, "s2_gcv%d" % k, [128, 2, 2 + TS], F32) for k in range(2)]
                gb_in = [sbuf(st, "s2_gb%d" % k, [128, 2, TS], F32) for k in range(2)]
                yc = sbuf(st, "s2_y", [128, 2, TS], F32, 2)
                ysq = sbuf(st, "s2_ysq", [128, 2, TS], F32, 2)
                m2 = sbuf(st, "s2_m2", [128, TS], F32)
                var = sbuf(st, "s2_var", [128, TS], F32)
                dd = sbuf(st, "s2_dd", [128, TS], F32)
                sA = sbuf(st, "s2_sA", [128, 16 + TS], F32)
                sB = sbuf(st, "s2_sB", [128, 16 + TS], F32)
                pooled = sbuf(st, "s2_pooled", [128, TS], BF16)
                acc3 = sbuf(st, "s2_acc3", [128, TS], F32)
                mixA = [sbuf(st, "s2_mixA%d" % k, [128, 6, TS], BF16) for k in range(2)]


                def s2_load(i):
                    k = i % 2
                    if i == 0:
                        P.op("dve", lambda e: e.memset(glu_in[k].t[:, :, 0:30], 0.0), writes=[glu_in[k].r])
                        P.op("dve", lambda e: e.memset(pin_in[k].t[:, :, 0:16], 0.0), writes=[pin_in[k].r])
                        P.op("dve", lambda e: e.memset(gcv_in[k].t[:, :, 0:2], 0.0), writes=[gcv_in[k].r])
                        P.dma("sp", glu_in[k].t[:, :, 30:30 + TS], fm(glu_d, 0), reads=[R_glu[0]], writes=[glu_in[k].r])
                        P.dma("sp", pin_in[k].t[:, :, 16:16 + TS], fm(pin_d, 0), reads=[R_pin[0]], writes=[pin_in[k].r])
                        P.dma("sp", gcv_in[k].t[:, :, 2:2 + TS], fm(gcv_d, 0), reads=[R_gcv[0]], writes=[gcv_in[k].r])
                    else:
                        P.dma("sp", glu_in[k].t[:], fm(glu_d, i, -30, TS), reads=[R_glu[i - 1], R_glu[i]],
                              writes=[glu_in[k].r])
                        P.dma("sp", pin_in[k].t[:], fm(pin_d, i, -16, TS), reads=[R_pin[i - 1], R_pin[i]],
                              writes=[pin_in[k].r])
                        P.dma("sp", gcv_in[k].t[:], fm(gcv_d, i, -2, TS), reads=[R_gcv[i - 1], R_gcv[i]],
                              writes=[gcv_in[k].r])
                    P.dma("sp", gb_in[k].t[:], fm(gb_d, i), reads=[R_gb[i]], writes=[gb_in[k].r])

                s2_load(0)
                s2_load(1)
                for c in range(8):
                    P.dma("pool", w_out_sb.t[:, c, :], w_out_d[l, c * 128:(c + 1) * 128, :], writes=[w_out_sb.r])
                P.dma("sp", kT.t[:], k_d.rearrange("(c p) t -> p c t", p=128), reads=R_k, writes=[kT.r])
                for i8 in range(NT):
                    P.dma("sp", Vs.t[:, i8 * 4:(i8 + 1) * 4, :], tm(v_d, i8), reads=[R_v[i8]], writes=[Vs.r])
                for i in range(NT):
                    k = i % 2
                    if 1 <= i and i + 1 < NT:
                        s2_load(i + 1)
                    mx = mixA[k]
                    for cc in range(2):
                        bank = next_pf()
                        mm_group(bank, (0, TS), [dg.t[:, cc, j, :] for j in range(CK)],
                                 [glu_in[k].t[:, cc, j:j + TS] for j in range(CK)],
                                 [[dg.r, glu_in[k].r]] * CK)
                        P.op("act", lambda e: e.activation(out=yc.t[:, cc, :], in_=bank.t[:], func=AF.Identity,
                                                           bias=prm.t[:, PC_CB + cc:PC_CB + cc + 1]),
                             reads=[bank.r, prm.r], writes=[yc.res[cc]])
                        P.op("act", lambda e: e.activation(out=ysq.t[:, cc, :], in_=bank.t[:], func=AF.Square,
                                                           bias=prm.t[:, PC_CB + cc:PC_CB + cc + 1]),
                             reads=[bank.r, prm.r], writes=[ysq.res[cc]])
                    bm = next_pf()
                    mm_group(bm, (0, TS), [ones256.t[:], ones256.t[:]], [yc.t[:, 0, :], yc.t[:, 1, :]],
                             [[ones256.r, yc.res[0]], [ones256.r, yc.res[1]]])
                    bq = next_pf()
                    mm_group(bq, (0, TS), [ones256.t[:], ones256.t[:]], [ysq.t[:, 0, :], ysq.t[:, 1, :]],
                             [[ones256.r, ysq.res[0]], [ones256.r, ysq.res[1]]])
                    for cc in range(2):
                        u = pin_in[k].t[:, cc, :]
                        W_ = 16 + TS
                        P.op("dve", lambda e: e.memset(sA.t[:, 0:1], 0.0), writes=[sA.r])
                        P.op("dve", lambda e: e.tensor_tensor(out=sA.t[:, 1:W_], in0=pin_in[k].t[:, cc, 1:W_],
                                                              in1=pin_in[k].t[:, cc, 0:W_ - 1], op=ALU.add),
                             reads=[pin_in[k].r], writes=[sA.r])
                        if cc == 0:
                            P.op("dve", lambda e: e.tensor_tensor(out=sB.t[64:128, 3:W_], in0=sA.t[64:128, 3:W_],
                                                                  in1=sA.t[64:128, 1:W_ - 2], op=ALU.add),
                                 reads=[sA.r], writes=[sB.r])
                            P.op("dve", lambda e: e.tensor_copy(out=sB.t[0:64, 3:W_], in_=sA.t[0:64, 3:W_]),
                                 reads=[sA.r, sB.r], writes=[sB.r])
                            fin = sB
                        else:
                            P.op("dve", lambda e: e.tensor_tensor(out=sB.t[:, 3:W_], in0=sA.t[:, 3:W_],
                                                                  in1=sA.t[:, 1:W_ - 2], op=ALU.add),
                                 reads=[sA.r], writes=[sB.r])
                            P.op("dve", lambda e: e.tensor_tensor(out=sA.t[:, 7:W_], in0=sB.t[:, 7:W_],
                                                                  in1=sB.t[:, 3:W_ - 4], op=ALU.add),
                                 reads=[sB.r, sA.r], writes=[sA.r])
                            P.op("dve", lambda e: e.tensor_tensor(out=sB.t[64:128, 15:W_], in0=sA.t[64:128, 15:W_],
                                                                  in1=sA.t[64:128, 7:W_ - 8], op=ALU.add),
                                 reads=[sA.r, sB.r], writes=[sB.r])
                            P.op("dve", lambda e: e.tensor_copy(out=sB.t[0:64, 15:W_], in_=sA.t[0:64, 15:W_]),
                                 reads=[sA.r, sB.r], writes=[sB.r])
                            fin = sB
                        if i == 0:
                            P.op("dve", lambda e: e.tensor_tensor(out=fin.t[:, 16:32], in0=fin.t[:, 16:32],
                                                                  in1=corr.t[:, cc, :], op=ALU.mult),
                                 reads=[fin.r, corr.r], writes=[fin.r])
                        P.op("dve", lambda e: e.scalar_tensor_tensor(
                            out=pooled.t[:], in0=fin.t[:, 16:16 + TS], scalar=prm.t[:, PC_IW + cc:PC_IW + cc + 1],
                            in1=pin_in[k].t[:, cc, 16:16 + TS], op0=ALU.mult, op1=ALU.subtract),
                            reads=[fin.r, prm.r, pin_in[k].r], writes=[pooled.r])
                        bank = next_pf()
                        P.op("pe", lambda e: e.matmul(bank.t[:], pw_sb.t[:, cc, :], pooled.t[:], start=True, stop=True),
                             reads=[pw_sb.r, pooled.r], writes=[bank.r])
                        P.op("act", lambda e: e.activation(out=mx.t[:, 2 + cc, :], in_=bank.t[:], func=AF.Identity,
                                                           scale=prm.t[:, PC_PS + cc:PC_PS + cc + 1],
                                                           bias=pbs.t[:, cc:cc + 1]),
                             reads=[bank.r, prm.r, pbs.r], writes=[mx.r])
                    for cc in range(2):
                        g_ = gcv_in[k]
                        P.op("dve", lambda e: e.tensor_scalar(out=acc3.t[:], in0=g_.t[:, cc, 0:TS],
                                                              scalar1=prm.t[:, PC_SW + cc * 3:PC_SW + cc * 3 + 1],
                                                              scalar2=None, op0=ALU.mult),
                             reads=[g_.r, prm.r], writes=[acc3.r])
                        for j in (1, 2):
                            P.op("dve", lambda e, j=j: e.scalar_tensor_tensor(
                                out=acc3.t[:], in0=g_.t[:, cc, j:j + TS],
                                scalar=prm.t[:, PC_SW + cc * 3 + j:PC_SW + cc * 3 + j + 1], in1=acc3.t[:],
                                op0=ALU.mult, op1=ALU.add), reads=[g_.r, prm.r, acc3.r], writes=[acc3.r])
                        P.op("dve", lambda e: e.tensor_tensor(out=mx.t[:, 4 + cc, :], in0=acc3.t[:],
                                                              in1=gb_in[k].t[:, cc, :], op=ALU.mult),
                             reads=[acc3.r, gb_in[k].r], writes=[mx.r])
                    P.op("act", lambda e: e.activation(out=m2.t[:], in_=bm.t[:], func=AF.Square),
                         reads=[bm.r], writes=[m2.r])
                    P.op("dve", lambda e: e.tensor_tensor(out=var.t[:], in0=bq.t[:], in1=m2.t[:], op=ALU.subtract),
                         reads=[bq.r, m2.r], writes=[var.r])
                    P.op("dve", lambda e: e.tensor_scalar(out=var.t[:], in0=var.t[:], scalar1=0.0, scalar2=EPS,
                                                          op0=ALU.max, op1=ALU.add), reads=[var.r], writes=[var.r])
                    P.op("act", lambda e: e.activation(out=var.t[:], in_=var.t[:], func=AF.Ln),
                         reads=[var.r], writes=[var.r])
                    P.op("act", lambda e: e.activation(out=var.t[:], in_=var.t[:], func=AF.Exp, scale=-0.5),
                         reads=[var.r], writes=[var.r])
                    for cc in range(2):
                        P.op("dve", lambda e: e.tensor_tensor(out=dd.t[:], in0=bm.t[:], in1=yc.t[:, cc, :],
                                                              op=ALU.subtract),
                             reads=[bm.r, yc.res[cc]], writes=[dd.r])
                        P.op("dve", lambda e: e.tensor_tensor(out=dd.t[:], in0=dd.t[:], in1=var.t[:], op=ALU.mult),
                             reads=[dd.r, var.r], writes=[dd.r])
                        P.op("dve", lambda e: e.tensor_scalar(out=dd.t[:], in0=dd.t[:],
                                                              scalar1=prm.t[:, PC_LNG + cc:PC_LNG + cc + 1],
                                                              scalar2=-1.0, op0=ALU.mult, op1=ALU.mult),
                             reads=[dd.r, prm.r], writes=[dd.r])
                        P.op("act", lambda e: e.activation(out=mx.t[:, cc, :], in_=dd.t[:], func=AF.Silu,
                                                           bias=prm.t[:, PC_LNB + cc:PC_LNB + cc + 1]),
                             reads=[dd.r, prm.r], writes=[mx.r])
                    mv = mix_d.rearrange("(c p) t -> p c t", p=128)
                    P.dma("sp", mv[:, 0:4, i * TS:(i + 1) * TS], mx.t[:, 0:4, :], reads=[mx.r], writes=[R_mixA[i]])
                    P.dma("sp", mv[:, 6:8, i * TS:(i + 1) * TS], mx.t[:, 4:6, :], reads=[mx.r], writes=[R_mixA[i]])
                P.barrier()
            if dbg == "s2a":
                st_kv.close()
                st_wout.close()
                st_dg.close()
                break

            with contextlib.ExitStack() as st:
                qTs = [sbuf(st, "at_q%d" % k, [128, 2, TS], BF16) for k in range(2)]
                pts = [sbuf(st, "at_pt%d" % k, [128, TS], BF16) for k in range(4)]
                rz = [sbuf(st, "at_rz%d" % k, [128, TS], F32) for k in range(2)]
                o12 = [sbuf(st, "at_o%d" % k, [128, TS], F32) for k in range(2)]
                od = sbuf(st, "at_od", [128, TS], F32)
                osq = sbuf(st, "at_osq", [128, TS], F32)
                rs = sbuf(st, "at_rs", [128, TS], F32)
                mixB = [sbuf(st, "at_mix%d" % k, [128, 2, TS], BF16) for k in range(2)]
                set_psum(st, 4, 0)
                accb = pf[0:4]
                sc2 = [psum(st, "sc2_%d_%d" % (k, uid[0]), [128, 2 * TS], F32) for k in range(2)]
                pt2 = [sbuf(st, "at_pt2_%d" % k, [128, 2 * TS], BF16) for k in range(4)]
                P.dma("sp", qTs[0].t[:], fm(q_d, 0), reads=[R_q[0]], writes=[qTs[0].r])
                mv = mix_d.rearrange("(c p) t -> p c t", p=128)

                groups = []
                for i in range(NT):
                    nkt = 4 * i + 4
                    for hp in range(2):
                        for kt in range(nkt):
                            groups.append((i, hp, kt, nkt))

                def front(g):
                    i, hp, kt, nkt = groups[g]
                    if hp == 0 and kt == 0 and i + 1 < NT:
                        P.dma("sp", qTs[(i + 1) % 2].t[:], fm(q_d, i + 1), reads=[R_q[i + 1]],
                              writes=[qTs[(i + 1) % 2].r])
                    qT = qTs[i % 2]
                    jd = kt - 4 * i
                    qs = 128 * jd if jd > 0 else 0
                    n = TS - qs
                    for s_ in range(4):
                        po = s_ * 32
                        sc = sc2[s_ // 2]
                        o_ = (s_ % 2) * TS
                        P.op("pe", lambda e: e.matmul(sc.t[:, o_:o_ + n], kT.t[po:po + 32, hp, kt * 128:(kt + 1) * 128],
                                                      qT.t[po:po + 32, hp, qs:TS], start=True, stop=True,
                                                      tile_position=(po, 0)),
                             reads=[kT.r, qT.r], writes=[sc.r])
                    for pr in range(2):
                        sc = sc2[pr]
                        pt = pt2[(g % 2) * 2 + pr]
                        P.op("act", lambda e: e.activation(
                            out=pt.t[:].rearrange("p (a b) -> p a b", a=2)[:, :, 0:n],
                            in_=sc.t[:].rearrange("p (a b) -> p a b", a=2)[:, :, 0:n], func=AF.Exp, scale=SCALE),
                            reads=[sc.r], writes=[pt.r])
                        if jd >= 0:
                            P.op("dve", lambda e: e.tensor_tensor(
                                out=pt.t[:].rearrange("p (a b) -> p a b", a=2)[:, :, 0:128],
                                in0=pt.t[:].rearrange("p (a b) -> p a b", a=2)[:, :, 0:128],
                                in1=trib.t[:].unsqueeze(1).to_broadcast([128, 2, 128]), op=ALU.mult),
                                reads=[pt.r, trib.r], writes=[pt.r])

                def back(g):
                    i, hp, kt, nkt = groups[g]
                    jd = kt - 4 * i
                    qs = 128 * jd if jd > 0 else 0
                    n = TS - qs
                    for s_ in range(4):
                        h = 2 * hp + s_ // 2
                        pt = pt2[(g % 2) * 2 + s_ // 2]
                        o_ = (s_ % 2) * TS
                        acc = accb[s_]
                        P.op("pe", lambda e: e.matmul(acc.t[:, qs:TS], Vs.t[:, kt, h * 128:(h + 1) * 128],
                                                      pt.t[:, o_:o_ + n], start=(kt == 0), stop=(kt == nkt - 1)),
                             reads=[Vs.r, pt.r], writes=[acc.r])
                    if kt == nkt - 1:
                        finalize(i, 2 * hp)
                        finalize(i, 2 * hp + 1)

                def finalize(i, h):
                    ch = h // 2
                    mb = mixB[i % 2]
                    lo, hi = (0, 64) if h % 2 == 0 else (64, 128)
                    zlo, zhi = (64, 128) if h % 2 == 0 else (0, 64)
                    accs = [accb[(h % 2) * 2], accb[(h % 2) * 2 + 1]]
                    for comp in range(2):
                        P.op("act", lambda e: e.activation(out=rz[comp].t[zlo:zhi, :], in_=accs[comp].t[zlo:zhi, :],
                                                           func=AF.Ln),
                             reads=[accs[comp].r], writes=[rz[comp].r])
                        P.op("act", lambda e: e.activation(out=rz[comp].t[zlo:zhi, :], in_=rz[comp].t[zlo:zhi, :],
                                                           func=AF.Exp, scale=-1.0),
                             reads=[rz[comp].r], writes=[rz[comp].r])
                        P.op("dve", lambda e: e.tensor_tensor(out=o12[comp].t[lo:hi, :], in0=accs[comp].t[lo:hi, :],
                                                              in1=rz[comp].t[zlo:zhi, :], op=ALU.mult),
                             reads=[accs[comp].r, rz[comp].r], writes=[o12[comp].r])
                    P.op("dve", lambda e: e.scalar_tensor_tensor(out=od.t[lo:hi, :], in0=o12[1].t[lo:hi, :],
                                                                 scalar=neglam.t[lo:hi, 0:1], in1=o12[0].t[lo:hi, :],
                                                                 op0=ALU.mult, op1=ALU.add),
                         reads=[o12[0].r, o12[1].r, neglam.r], writes=[od.r])
                    if h % 2 == 1:
                        P.op("act", lambda e: e.activation(out=osq.t[:], in_=od.t[:], func=AF.Square),
                             reads=[od.r], writes=[osq.r])
                        bms = sc2[1]
                        P.op("pe", lambda e: e.matmul(bms.t[:, TS:2 * TS], blk64.t[:], osq.t[:], start=True, stop=True),
                             reads=[blk64.r, osq.r], writes=[bms.r])
                        P.op("act", lambda e: e.activation(out=rs.t[:], in_=bms.t[:, TS:2 * TS], func=AF.Ln,
                                                           bias=epsc.t[:, 0:1]),
                             reads=[bms.r, epsc.r], writes=[rs.r])
                        P.op("act", lambda e: e.activation(out=rs.t[:], in_=rs.t[:], func=AF.Exp, scale=-0.5),
                             reads=[rs.r], writes=[rs.r])
                        P.op("dve", lambda e: e.tensor_tensor(out=rs.t[:], in0=rs.t[:], in1=od.t[:], op=ALU.mult),
                             reads=[rs.r, od.r], writes=[rs.r])
                        P.op("dve", lambda e: e.tensor_scalar(out=mb.t[:, ch, :], in0=rs.t[:],
                                                              scalar1=prm.t[:, PC_SUB:PC_SUB + 1],
                                                              scalar2=1.0 - lam_init, op0=ALU.mult, op1=ALU.mult),
                             reads=[rs.r, prm.r], writes=[mb.r])
                    if h == 3:
                        P.dma("sp", mv[:, 4:6, i * TS:(i + 1) * TS], mb.t[:], reads=[mb.r], writes=[R_mixB[i]])

                LA = 1
                for g in range(len(groups) + LA):
                    if g < len(groups):
                        front(g)
                    if g >= LA:
                        back(g - LA)
                P.barrier()
            st_kv.close()
            st_dg.close()
            if dbg == "s2c":
                st_wout.close()
                break

            st_ple = contextlib.ExitStack()
            wpg = sbuf(st_ple, "s5_wpg", [128, 8, D], BF16)
            wpp = sbuf(st_ple, "s5_wpp", [128, 2, D], BF16)
            stm = contextlib.ExitStack()
            wgs = [sbuf(stm, "wgs0", [128, 4, 8, 256], BF16)]
            wus = [sbuf(stm, "wus0", [128, 4, 8, 256], BF16)]
            wds = [sbuf(stm, "wds0", [128, 4, 2, D], BF16)]
            def load_experts(pz, slot):
                for el in range(4):
                    eidx = pz * 4 + el
                    P.dma("pool", wgs[slot].t[:, el, :, :], wg_d[l, eidx].rearrange("(c p) n -> p c n", p=128),
                          writes=[wgs[slot].r])
                    P.dma("pool", wus[slot].t[:, el, :, :], wu_d[l, eidx].rearrange("(c p) n -> p c n", p=128),
                          writes=[wus[slot].r])
                    P.dma("pool", wds[slot].t[:, el, :, :], wd_d[l, eidx].rearrange("(c p) n -> p c n", p=128),
                          writes=[wds[slot].r])

            with contextlib.ExitStack() as st:
                set_psum(st, 6, 2)
                wrg = sbuf(st, "s3_wrg", [128, 8, 20], F32)
                rbias = sbuf(st, "s3_rb", [128, 20], F32)
                hts = [sbuf(st, "s3_ht%d" % k, [128, 4, D], F32) for k in range(2)]
                mxs = [sbuf(st, "s3_mx%d" % k, [128, 8, TS], BF16) for k in range(2)]
                ss2 = [sbuf(st, "s3_ss%d" % k, [128, 4], F32) for k in range(2)]
                rstd2 = [sbuf(st, "s3_rstd%d" % k, [128, 4], F32) for k in range(2)]
                sqj = sbuf(st, "s3_sqj", [128, D], BF16)
                xn2 = [sbuf(st, "s3_xn%d" % k, [128, 4, D], BF16, 4) for k in range(2)]
                xT2 = [sbuf(st, "s3_xT%d" % k, [128, 8, TS], BF16, 8) for k in range(2)]
                hTf = sbuf(st, "s3_hTf", [128, 8, 128], F32, 2)
                lg = sbuf(st, "s3_lg", [128, 20], F32)
                lg4 = sbuf(st, "s3_lg4", [128, 4, 20], F32)
                r4 = sbuf(st, "s3_r4", [128, 8, 4], F32)
                mg4 = sbuf(st, "s3_mg4", [128, 4, 4], F32)
                t4 = sbuf(st, "s3_t4", [128, 4, 4], F32)
                es4 = sbuf(st, "s3_es4", [128, 4, 4], F32)
                eq4 = sbuf(st, "s3_eq4", [128, 4, 4], F32)
                pr4 = sbuf(st, "s3_pr4", [128, 4, 4, 4], F32)
                sm = sbuf(st, "s3_sm", [128, 16], F32)
                mg = sbuf(st, "s3_mg", [128, 4], F32)
                ge = sbuf(st, "s3_ge", [128, 4], F32)
                esel = sbuf(st, "s3_esel", [128, 4], F32)
                eq = sbuf(st, "s3_eq", [128, 4], F32)
                em2 = sbuf(st, "s3_em2", [128, 4], F32)
                ee = sbuf(st, "s3_ee", [128, 4], F32)
                wsel = sbuf(st, "s3_wsel", [128, 4], F32)
                comb = sbuf(st, "s3_comb", [128, 4, 16], F32)
                cmbT = sbuf(st, "s3_cmbT", [16, TS], F32)

                P.dma("sp", wrg.t[:], wr_d[l], writes=[wrg.r])
                P.dma("sp", rbias.t[:], rb_d[l], writes=[rbias.r])
                for c in range(8):
                    P.op("dve", lambda e, c=c: e.tensor_scalar(out=wrg.t[:, c, :], in0=wrg.t[:, c, :],
                                                               scalar1=prm.t[:, PC_FFNG + c:PC_FFNG + c + 1],
                                                               scalar2=None, op0=ALU.mult),
                         reads=[wrg.r, prm.r], writes=[wrg.r])

                def s3_load(i):
                    k = i % 2
                    P.dma("sp", hts[k].t[:], tm(h_src, i), reads=[R_h[i]], writes=[hts[k].r])
                    P.dma("sp", mxs[k].t[:], fm(mix_d, i), reads=[R_mixA[i], R_mixB[i]], writes=[mxs[k].r])

                def s3_A(i):
                    k = i % 2
                    ht = hts[k]
                    mx = mxs[k]
                    ss, rstd, xn, xT = ss2[k], rstd2[k], xn2[k], xT2[k]
                    for j in range(4):
                        for half in range(2):
                            bank = next_pf()
                            mm_group(bank, (0, 512), [mx.t[:, c, j * 128:(j + 1) * 128] for c in range(8)],
                                     [w_out_sb.t[:, c, half * 512:(half + 1) * 512] for c in range(8)],
                                     [[mx.r, w_out_sb.r]] * 8)
                            P.op("dve", lambda e: e.tensor_tensor(out=ht.t[:, j, half * 512:(half + 1) * 512],
                                                                  in0=bank.t[:], in1=ht.t[:, j, half * 512:(half + 1) * 512],
                                                                  op=ALU.add),
                                 reads=[bank.r, ht.r], writes=[ht.r])
                    P.dma("sp", tm(h_d, i), ht.t[:], reads=[ht.r], writes=[R_h[i]])
                    norm_stats(st, ht, 0, ss, rstd, sqj)
                    norm_transpose(ht, 0, rstd, xn, xT, PC_FFNG, part=0)

                def s3_A2(i):
                    k = i % 2
                    ht = hts[k]
                    ss, rstd, xn, xT = ss2[k], rstd2[k], xn2[k], xT2[k]
                    norm_transpose(ht, 0, rstd, xn, xT, PC_FFNG, part=1)
                    P.dma("sp", fm(xT_d, i), xT.t[:], reads=xT.res, writes=[R_xT[i]])

                def s3_Ba(i):
                    k = i % 2
                    ht = hts[k]
                    ss, rstd, xn, xT = ss2[k], rstd2[k], xn2[k], xT2[k]
                    for j in range(4):
                        for c2 in range(2):
                            bank = next_pf()
                            while bank is bct:
                                bank = next_pf()
                            for cq in range(4):
                                c = c2 * 4 + cq
                                P.op("pe", lambda e: e.transpose(bank.t[:, cq * 128:(cq + 1) * 128],
                                                                 ht.t[:, j, c * 128:(c + 1) * 128], identf.t[:]),
                                     reads=[ht.r, identf.r], writes=[bank.r], signal=(cq == 3))
                            if c2 == 0:
                                P.op("act", lambda e: e.copy(out=hTf.t[:, 0:4, :], in_=bank.t[:].rearrange("p (a b) -> p a b", a=4)),
                                     reads=[bank.r], writes=[hTf.res[0]])
                            else:
                                P.op("dve", lambda e: e.tensor_copy(out=hTf.t[:, 4:8, :], in_=bank.t[:].rearrange("p (a b) -> p a b", a=4)),
                                     reads=[bank.r], writes=[hTf.res[1]])
                        bl = next_pf()
                        while bl is bct:
                            bl = next_pf()
                        mm_group(bl, (0, 20), [hTf.t[:, c, :] for c in range(8)], [wrg.t[:, c, :] for c in range(8)],
                                 [[hTf.res[c // 4], wrg.r] for c in range(8)])
                        P.op("dve", lambda e: e.scalar_tensor_tensor(out=lg4.t[:, j, :], in0=bl.t[:, 0:20],
                                                                     scalar=rstd.t[:, j:j + 1], in1=rbias.t[:],
                                                                     op0=ALU.mult, op1=ALU.add),
                             reads=[bl.r, rstd.r, rbias.r], writes=[lg4.r])

                def s3_Bb(i):
                    V = lambda fn, rd, wr: P.op("dve", fn, reads=rd, writes=wr)
                    S3 = [128, 4, 4]
                    bc = lambda ap_: ap_.unsqueeze(2).to_broadcast(S3)
                    glg = lg4.t[:, :, 0:4]
                    V(lambda e: e.reduce_max(out=r4.t[:, 0, :], in_=glg, axis=AX.X), [lg4.r], [r4.r])
                    V(lambda e: e.tensor_tensor(out=mg4.t[:], in0=glg, in1=bc(r4.t[:, 0, :]), op=ALU.is_ge),
                      [lg4.r, r4.r], [mg4.r])
                    V(lambda e: e.tensor_tensor(out=t4.t[:], in0=glg, in1=bc(r4.t[:, 0, :]), op=ALU.subtract),
                      [lg4.r, r4.r], [t4.r])
                    P.op("act", lambda e: e.activation(out=t4.t[:], in_=t4.t[:], func=AF.Exp), reads=[t4.r], writes=[t4.r])
                    V(lambda e: e.reduce_sum(out=r4.t[:, 1, :], in_=t4.t[:], axis=AX.X), [t4.r, r4.r], [r4.r])
                    V(lambda e: e.reciprocal(out=r4.t[:, 2, :], in_=r4.t[:, 1, :]), [r4.r], [r4.r])
                    el4 = lg4.t[:, :, 4:20].rearrange("p j (g i) -> p j g i", g=4)
                    V(lambda e: e.tensor_tensor(out=pr4.t[:], in0=el4,
                                                in1=mg4.t[:].unsqueeze(3).to_broadcast([128, 4, 4, 4]), op=ALU.mult),
                      [lg4.r, mg4.r], [pr4.r])
                    V(lambda e: e.reduce_sum(out=es4.t[:], in_=pr4.t[:].rearrange("p j g i -> p j i g"), axis=AX.X),
                      [pr4.r], [es4.r])
                    V(lambda e: e.reduce_max(out=r4.t[:, 3, :], in_=es4.t[:], axis=AX.X), [es4.r, r4.r], [r4.r])
                    V(lambda e: e.tensor_tensor(out=eq4.t[:], in0=es4.t[:], in1=bc(r4.t[:, 3, :]), op=ALU.is_ge),
                      [es4.r, r4.r], [eq4.r])
                    V(lambda e: e.scalar_tensor_tensor(out=t4.t[:], in0=eq4.t[:], scalar=-1e30, in1=es4.t[:],
                                                       op0=ALU.mult, op1=ALU.add), [eq4.r, es4.r, t4.r], [t4.r])
                    V(lambda e: e.reduce_max(out=r4.t[:, 4, :], in_=t4.t[:], axis=AX.X), [t4.r, r4.r], [r4.r])
                    V(lambda e: e.tensor_tensor(out=eq4.t[:], in0=es4.t[:], in1=bc(r4.t[:, 4, :]), op=ALU.is_ge),
                      [es4.r, r4.r, eq4.r], [eq4.r])
                    V(lambda e: e.tensor_tensor(out=t4.t[:], in0=es4.t[:], in1=bc(r4.t[:, 3, :]), op=ALU.subtract),
                      [es4.r, r4.r, t4.r], [t4.r])
                    P.op("act", lambda e: e.activation(out=t4.t[:], in_=t4.t[:], func=AF.Exp), reads=[t4.r], writes=[t4.r])
                    V(lambda e: e.tensor_tensor(out=t4.t[:], in0=t4.t[:], in1=eq4.t[:], op=ALU.mult),
                      [t4.r, eq4.r], [t4.r])
                    V(lambda e: e.reduce_sum(out=r4.t[:, 5, :], in_=t4.t[:], axis=AX.X), [t4.r, r4.r], [r4.r])
                    V(lambda e: e.reciprocal(out=r4.t[:, 6, :], in_=r4.t[:, 5, :]), [r4.r], [r4.r])
                    V(lambda e: e.tensor_tensor(out=r4.t[:, 6, :], in0=r4.t[:, 6, :], in1=r4.t[:, 2, :], op=ALU.mult),
                      [r4.r], [r4.r])
                    V(lambda e: e.tensor_tensor(out=t4.t[:], in0=t4.t[:], in1=bc(r4.t[:, 6, :]), op=ALU.mult),
                      [t4.r, r4.r], [t4.r])
                    V(lambda e: e.tensor_tensor(out=comb.t[:].rearrange("p j (g i) -> p j g i", g=4),
                                                in0=t4.t[:].unsqueeze(2).to_broadcast([128, 4, 4, 4]),
                                                in1=mg4.t[:].unsqueeze(3).to_broadcast([128, 4, 4, 4]), op=ALU.mult),
                      [t4.r, mg4.r], [comb.r])

                def s3_B2(i):
                    for j in range(4):
                        P.op("pe", lambda e: e.transpose(bct.t[0:16, j * 128:(j + 1) * 128], comb.t[:, j, :], identf.t[:]),
                             reads=[comb.r, identf.r], writes=[bct.r])
                    P.op("act", lambda e: e.copy(out=cmbT.t[:], in_=bct.t[0:16, :]), reads=[bct.r], writes=[cmbT.r])
                    P.dma("sp", cmb_d[:, i * TS:(i + 1) * TS], cmbT.t[:], reads=[cmbT.r], writes=[R_cmb[i]])

                bct = pf.pop()
                s3_load(0)
                if NT > 1:
                    s3_load(1)
                s3_A(0)
                s3_A2(0)
                load_experts(0, 0)
                for i in range(NT):
                    if i + 1 < NT:
                        s3_A(i + 1)
                    if i >= 1:
                        s3_B2(i - 1)
                    s3_Ba(i)
                    if i + 2 < NT:
                        s3_load(i + 2)
                    if i + 1 < NT:
                        s3_A2(i + 1)
                    s3_Bb(i)
                s3_B2(NT - 1)
                P.barrier()
            st_wout.close()
            if dbg == "s3":
                stm.close()
                st_ple.close()
                break

            wgs.append(sbuf(stm, "wgs1", [128, 4, 8, 256], BF16))
            wus.append(sbuf(stm, "wus1", [128, 4, 8, 256], BF16))
            wds.append(sbuf(stm, "wds1", [128, 4, 2, D], BF16))
            with contextlib.ExitStack() as st:
                set_psum(st, 8, 0)
                hts = [sbuf(st, "s4_ht%d" % k, [128, 4, D], F32) for k in range(2)]
                xTs = [sbuf(st, "s4_xT%d" % k, [128, 8, TS], BF16) for k in range(2)]
                cms = [sbuf(st, "s4_cm%d" % k, [16, TS], F32) for k in range(2)]
                cbs = sbuf(st, "s4_cb", [128, TS], F32)
                sg_ = sbuf(st, "s4_sg", [128, TS], F32)
                tt_ = sbuf(st, "s4_tt", [128, TS], F32)
                hdn = sbuf(st, "s4_hdn", [128, 4, 2, TS], BF16)

                def s4_load(i):
                    k = i % 2
                    P.dma("sp", hts[k].t[:], tm(h_d, i), reads=[R_h[i]], writes=[hts[k].r])
                    P.dma("sp", xTs[k].t[:], fm(xT_d, i), reads=[R_xT[i]], writes=[xTs[k].r])
                    P.dma("sp", cms[k].t[:], cmb_d[:, i * TS:(i + 1) * TS], reads=[R_cmb[i]], writes=[cms[k].r])

                s4_load(0)
                for pz in range(4):
                    slot = pz % 2
                    if pz + 1 < 4:
                        load_experts(pz + 1, (pz + 1) % 2)
                    if pz == 0:
                        for c in range(8):
                            P.dma("pool", wpg.t[:, c, :], pgw_d[l, c * 128:(c + 1) * 128, :], writes=[wpg.r])
                        P.dma("pool", wpp.t[:], ppj_d[l].rearrange("(c p) n -> p c n", p=128), writes=[wpp.r])
                    for i in range(NT):
                        k = i % 2
                        if i + 1 < NT:
                            s4_load(i + 1)
                        elif pz + 1 < 4:
                            s4_load(0)
                        ht, xT_, cm = hts[k], xTs[k], cms[k]
                        for el in range(4):
                            eidx = pz * 4 + el
                            bcb = next_pf()
                            P.op("pe", lambda e: e.matmul(bcb.t[:], sel.t[0:16, eidx, :], cm.t[0:16, :],
                                                          start=True, stop=True),
                                 reads=[sel.r, cm.r], writes=[bcb.r])
                            P.op("act", lambda e: e.copy(out=cbs.t[:], in_=bcb.t[:]), reads=[bcb.r], writes=[cbs.r])
                            for hc in range(2):
                                bg_ = next_pf()
                                mm_group(bg_, (0, TS), [wgs[slot].t[:, el, c, hc * 128:(hc + 1) * 128] for c in range(8)],
                                         [xT_.t[:, c, :] for c in range(8)], [[wgs[slot].r, xT_.r]] * 8)
                                bu_ = next_pf()
                                mm_group(bu_, (0, TS), [wus[slot].t[:, el, c, hc * 128:(hc + 1) * 128] for c in range(8)],
                                         [xT_.t[:, c, :] for c in range(8)], [[wus[slot].r, xT_.r]] * 8)
                                P.op("act", lambda e: e.activation(out=sg_.t[:], in_=bg_.t[:], func=AF.Silu),
                                     reads=[bg_.r], writes=[sg_.r])
                                P.op("dve", lambda e: e.tensor_tensor(out=tt_.t[:], in0=bu_.t[:], in1=cbs.t[:],
                                                                      op=ALU.mult),
                                     reads=[bu_.r, cbs.r], writes=[tt_.r])
                                P.op("dve", lambda e: e.tensor_tensor(out=hdn.t[:, el, hc, :], in0=sg_.t[:], in1=tt_.t[:],
                                                                      op=ALU.mult),
                                     reads=[sg_.r, tt_.r], writes=[hdn.r])
                        for j in range(4):
                            for half in range(2):
                                by = next_pf()
                                mm_group(by, (0, 512),
                                         [hdn.t[:, el, hc, j * 128:(j + 1) * 128] for el in range(4) for hc in range(2)],
                                         [wds[slot].t[:, el, hc, half * 512:(half + 1) * 512]
                                          for el in range(4) for hc in range(2)],
                                         [[hdn.r, wds[slot].r]] * 8)
                                P.op("dve", lambda e: e.tensor_tensor(out=ht.t[:, j, half * 512:(half + 1) * 512],
                                                                      in0=by.t[:],
                                                                      in1=ht.t[:, j, half * 512:(half + 1) * 512],
                                                                      op=ALU.add),
                                     reads=[by.r, ht.r], writes=[ht.r])
                        P.dma("sp", tm(h_d, i), ht.t[:], reads=[ht.r], writes=[R_h[i]])
                P.barrier()
            stm.close()
            if dbg == "s4":
                st_ple.close()
                break

            with contextlib.ExitStack() as st:
                set_psum(st, 6, 2)
                gbf = sbuf(st, "s5_gbf", [1, D], F32)
                gbb = sbuf(st, "s5_gbb", [1, D], BF16)
                hts = [sbuf(st, "s5_ht%d" % k, [128, 4, D], F32) for k in range(3)]
                pts_ = [sbuf(st, "s5_p%d" % k, [128, 4, 256], F32) for k in range(3)]
                ss2 = [sbuf(st, "s5_ss%d" % k, [128, 4], F32) for k in range(2)]
                rstd2 = [sbuf(st, "s5_rstd%d" % k, [128, 4], F32) for k in range(2)]
                sqj = sbuf(st, "s5_sqj", [128, D], BF16)
                xn2 = [sbuf(st, "s5_xn%d" % k, [128, 4, D], BF16, 4) for k in range(2)]
                xT2 = [sbuf(st, "s5_xT%d" % k, [128, 8, TS], BF16, 8) for k in range(2)]
                pT2 = [sbuf(st, "s5_pT%d" % k, [128, 2, TS], BF16) for k in range(2)]
                gts = [sbuf(st, "s5_gt%d" % k, [128, 512], F32) for k in range(3)]
                tqs = [sbuf(st, "s5_tq%d" % k, [128, 512], F32) for k in range(3)]
                if l + 1 < DEPTH:
                    st_win, w_in_next = load_w_in(l + 1)
                P.dma("sp", gbf.t[:], pgb_d[l:l + 1, :], writes=[gbf.r])
                P.op("dve", lambda e: e.tensor_copy(out=gbb.t[:], in_=gbf.t[:]), reads=[gbf.r], writes=[gbb.r])

                def s5_load(i):
                    k = i % 3
                    P.dma("sp", hts[k].t[:], tm(h_d, i), reads=[R_h[i]], writes=[hts[k].r])
                    P.dma("sp", pts_[k].t[:], tm(p_d[l], i), writes=[pts_[k].r])

                def s5_P(i):
                    k = i % 2
                    ht, pt_ = hts[i % 3], pts_[i % 3]
                    ss, rstd, xn, xT, pT = ss2[k], rstd2[k], xn2[k], xT2[k], pT2[k]
                    norm_stats(st, ht, 0, ss, rstd, sqj)
                    norm_transpose(ht, 0, rstd, xn, xT, PC_PLEG)
                    for c in range(2):
                        bank = next_pf()
                        for j in range(4):
                            P.op("pe", lambda e: e.transpose(bank.t[:, j * 128:(j + 1) * 128],
                                                             pt_.t[:, j, c * 128:(c + 1) * 128], identf.t[:]),
                                 reads=[pt_.r, identf.r], writes=[bank.r], signal=(j == 3))
                        P.op("act", lambda e: e.copy(out=pT.t[:, c, :], in_=bank.t[:]), reads=[bank.r], writes=[pT.r])

                def s5_M(i):
                    k = i % 2
                    ht, pt_ = hts[i % 3], pts_[i % 3]
                    ss, rstd, xn, xT, pT = ss2[k], rstd2[k], xn2[k], xT2[k], pT2[k]
                    for j in range(4):
                        for half in range(2):
                            hs = slice(half * 512, (half + 1) * 512)
                            gt = gts[(j * 2 + half) % 3]
                            tq = tqs[(j * 2 + half) % 3]
                            bg_ = next_pf()
                            mm_group(bg_, (0, 512),
                                     [xT.t[:, c, j * 128:(j + 1) * 128] for c in range(8)] + [onesrow.t[0:1, :]],
                                     [wpg.t[:, c, hs] for c in range(8)] + [gbb.t[0:1, hs]],
                                     [[xT.res[c], wpg.r] for c in range(8)] + [[onesrow.r, gbb.r]])
                            bp_ = next_pf()
                            mm_group(bp_, (0, 512), [pT.t[:, c, j * 128:(j + 1) * 128] for c in range(2)],
                                     [wpp.t[:, c, hs] for c in range(2)], [[pT.r, wpp.r]] * 2)
                            P.op("act", lambda e: e.activation(out=gt.t[:], in_=bg_.t[:], func=AF.Sigmoid),
                                 reads=[bg_.r], writes=[gt.r])
                            P.op("dve", lambda e: e.tensor_tensor(out=tq.t[:], in0=bp_.t[:], in1=gt.t[:], op=ALU.mult),
                                 reads=[bp_.r, gt.r], writes=[tq.r])
                            P.op("dve", lambda e: e.tensor_tensor(out=ht.t[:, j, hs], in0=ht.t[:, j, hs], in1=tq.t[:],
                                                                  op=ALU.add),
                                 reads=[ht.r, tq.r], writes=[ht.r])
                    if l < DEPTH - 1:
                        P.dma("sp", tm(h_d, i), ht.t[:], reads=[ht.r], writes=[R_h[i]])
                    else:
                        norm_stats(st, ht, 0, ss, rstd, sqj)
                        for j in range(4):
                            P.op("dve", lambda e: e.scalar_tensor_tensor(out=ht.t[:, j, :], in0=ht.t[:, j, :],
                                                                         scalar=rstd.t[:, j:j + 1], in1=gfin.t[:],
                                                                         op0=ALU.mult, op1=ALU.mult),
                                 reads=[ht.r, rstd.r, gfin.r], writes=[ht.r])
                        P.dma("sp", tm(out_d, i), ht.t[:], reads=[ht.r], writes=[R_out[i]])

                s5_load(0)
                if NT > 1:
                    s5_load(1)
                s5_P(0)
                for i in range(NT):
                    if i + 2 < NT:
                        s5_load(i + 2)
                    if i + 1 < NT:
                        s5_P(i + 1)
                    s5_M(i)
                P.barrier()
            st_ple.close()

        P.barrier()
        P.final_wait()
    return P


def _host_consts():
    c = {}
    c["c_identf"] = np.eye(128, dtype=np.float32)
    rp = np.zeros((128, 128), np.float32)
    for b in range(4):
        for d in range(16):
            rp[b * 32 + d + 16, b * 32 + d] = -1.0
            rp[b * 32 + d, b * 32 + d + 16] = 1.0
    c["c_rperm"] = rp
    kk = np.arange(128)[:, None]
    qq = np.arange(128)[None, :]
    c["c_tri"] = (qq >= kk).astype(np.float32)
    b64 = np.zeros((128, 128), np.float32)
    b64[0:64, 0:64] = 1.0 / 64
    b64[64:128, 64:128] = 1.0 / 64
    c["c_blk64"] = b64
    c["c_ones256"] = np.full((128, 128), 1.0 / 256, np.float32)
    sel = np.zeros((16, NE, 128), np.float32)
    for e in range(NE):
        sel[e, e, :] = 1.0
    c["c_sel"] = sel
    pos = np.arange(S, dtype=np.float32)
    inv = (10000.0 ** (-np.arange(0, 32, 2, dtype=np.float32) / np.float32(32))).astype(np.float32)
    ang = (pos[:, None] * inv[None, :]).astype(np.float32)
    ang = np.concatenate([ang, ang], axis=-1)
    cosT = np.cos(ang.astype(np.float64)).astype(np.float32).T
    sinT = np.sin(ang.astype(np.float64)).astype(np.float32).T
    c["c_cos"] = np.ascontiguousarray(np.tile(cosT, (4, 1)))
    c["c_sin"] = np.ascontiguousarray(np.tile(sinT, (4, 1)))
    wins = np.array([2, 4, 8, 16])
    corr = np.zeros((128, 2, 16), np.float32)
    for cc in range(2):
        for p in range(128):
            w = wins[cc * 2 + p // 64]
            for t in range(16):
                corr[p, cc, t] = w / min(t + 1, w)
    c["c_corr"] = corr
    iw = np.zeros((128, 2), np.float32)
    for cc in range(2):
        for p in range(128):
            iw[p, cc] = 1.0 / wins[cc * 2 + p // 64]
    c["_iw"] = iw
    return c


def _fmcols(v, nch):
    return np.ascontiguousarray(np.asarray(v, np.float32).reshape(nch, 128).T)


def _layout_inputs(inp):
    c = _host_consts()
    iw = c.pop("_iw")
    shared = dict(c)
    prm = np.zeros((DEPTH, 128, PC_N), np.float32)
    wr = np.zeros((DEPTH, 128, 8, 20), np.float32)
    rb = np.zeros((DEPTH, 128, 20), np.float32)
    lamv = np.zeros((DEPTH, 128, 4, 32), np.float32)
    pw = np.zeros((DEPTH, 128, 2, 128), np.float32)
    for l in range(DEPTH):
        prm[l, :, PC_MIXG:PC_MIXG + 8] = _fmcols(inp["mix_norm"][l], 8)
        prm[l, :, PC_FFNG:PC_FFNG + 8] = _fmcols(inp["ffn_norm"][l], 8)
        prm[l, :, PC_PLEG:PC_PLEG + 8] = _fmcols(inp["ple_norm"][l], 8)
        prm[l, :, PC_FING:PC_FING + 8] = _fmcols(inp["final_norm"], 8)
        cw = np.asarray(inp["conf_conv_w"][l], np.float32)
        for cc in range(2):
            prm[l, :, PC_CW + cc * CK:PC_CW + (cc + 1) * CK] = cw[:, cc * 128:(cc + 1) * 128].T
        prm[l, :, PC_CB:PC_CB + 2] = _fmcols(inp["conf_conv_b"][l], 2)
        prm[l, :, PC_LNG:PC_LNG + 2] = _fmcols(inp["conf_ln_g"][l], 2)
        prm[l, :, PC_LNB:PC_LNB + 2] = _fmcols(inp["conf_ln_b"][l], 2)
        prm[l, :, PC_PB:PC_PB + 2] = _fmcols(np.asarray(inp["pool_b"][l]).reshape(256), 2)
        prm[l, :, PC_PS:PC_PS + 2] = _fmcols(inp["pool_scale"][l], 2)
        sw = np.asarray(inp["sconv_w"][l], np.float32)
        for cc in range(2):
            prm[l, :, PC_SW + cc * 3:PC_SW + (cc + 1) * 3] = sw[:, cc * 128:(cc + 1) * 128].T
        prm[l, :, PC_SUB] = np.tile(np.asarray(inp["diff_subln_g"][l], np.float32), 2)
        prm[l, :, PC_IW:PC_IW + 2] = iw
        wcat = np.concatenate([np.asarray(inp["router_group_w"][l], np.float32),
                               np.asarray(inp["router_expert_w"][l], np.float32)], axis=1)
        wr[l] = wcat.reshape(8, 128, 20).transpose(1, 0, 2)
        bcat = np.concatenate([np.asarray(inp["router_group_b"][l], np.float32),
                               np.asarray(inp["router_expert_b"][l], np.float32)])
        rb[l] = np.tile(bcat[None, :], (128, 1))
        for n_, key in enumerate(["diff_lam_q1", "diff_lam_k1", "diff_lam_q2", "diff_lam_k2"]):
            lamv[l, :, n_, :] = np.tile(np.asarray(inp[key][l], np.float32)[None, :], (128, 1))
        pwl = np.asarray(inp["pool_w"][l], np.float32)
        for g in range(4):
            cc, hh = g // 2, g % 2
            pw[l, hh * 64:(hh + 1) * 64, cc, hh * 64:(hh + 1) * 64] = pwl[g]
    shared.update({
        "prm": prm, "wr": wr, "rbias": rb, "lamv": lamv, "poolw": pw,
        "gfin": np.ascontiguousarray(np.tile(np.asarray(inp["final_norm"], np.float32)[None, :], (128, 1))),
    })
    for key in ["w_in", "w_out", "expert_w_gate", "expert_w_up", "expert_w_down", "ple_gate_w", "ple_gate_b",
                "ple_proj"]:
        shared[key] = np.ascontiguousarray(np.asarray(inp[key], np.float32))
    return shared


_NC_CACHE = {}


def _get_nc():
    if "nc" not in _NC_CACHE:
        nc = bass.Bass("TRN2", target_bir_lowering=False)
        build(nc)
        _NC_CACHE["nc"] = nc
    return _NC_CACHE["nc"]


def kernel(**inputs):
    shared = _layout_inputs(inputs)
    x = np.asarray(inputs["x"], np.float32)
    p = np.asarray(inputs["p"], np.float32)
    n = x.shape[0]
    in_maps = []
    for b in range(n):
        m = dict(shared)
        m["x"] = np.ascontiguousarray(x[b])
        m["p"] = np.ascontiguousarray(p[:, b])
        in_maps.append(m)
    nc = _get_nc()
    res = run_bass_kernel_spmd(nc, in_maps, core_ids=list(range(n)))
    return np.stack([np.asarray(r["out"], np.float32) for r in res.results], axis=0)
```

```python
import math
import contextlib
import numpy as np
import ml_dtypes
import concourse.bass as bass
import concourse.mybir as mybir
from concourse.bass_utils import run_bass_kernel_spmd

F32 = mybir.dt.float32
BF16 = mybir.dt.bfloat16
ALU = mybir.AluOpType
AF = mybir.ActivationFunctionType
AX = mybir.AxisListType

S = 4096
D = 1024
DEPTH = 2
NT = 8
TS = 512
INC = 2304
NE = 16
EPS = 1e-6
CK = 31
SCALE = 32 ** -0.5
SEM_CHUNK = 30000

PC_MIXG, PC_FFNG, PC_PLEG, PC_FING = 0, 8, 16, 24
PC_CW = 32
PC_CB = PC_CW + 62
PC_LNG = PC_CB + 2
PC_LNB = PC_LNG + 2
PC_PB = PC_LNB + 2
PC_PS = PC_PB + 2
PC_SW = PC_PS + 2
PC_SUB = PC_SW + 6
PC_IW = PC_SUB + 1
PC_N = PC_IW + 2


class Tok:
    __slots__ = ("sem", "val", "eng")

    def __init__(self, eng):
        self.sem = None
        self.val = None
        self.eng = eng


class Res:
    __slots__ = ("name", "w", "r", "excl")

    def __init__(self, name, excl=False):
        self.name = name
        self.w = None
        self.r = []
        self.excl = excl


class Prog:
    def __init__(self, nc, es):
        self.nc = nc
        self.es = es
        self.eobj = {"pe": nc.tensor, "act": nc.scalar, "dve": nc.vector,
                     "pool": nc.gpsimd, "sp": nc.sync}
        self.sems = {e: [] for e in self.eobj}
        self.count = {e: 0 for e in self.eobj}
        self.known = {e: {} for e in self.eobj}
        self.pending = {e: None for e in self.eobj}
        self.last_tok = {e: None for e in self.eobj}
        self.nsem = 0
        self.dma_sems = []
        self.dma_cnt = []
        self.dma_i = 0
        self.dma_ip = 0
        for i in range(24):
            self.dma_sems.append(self._new_sem("dq%d" % i))
            self.dma_cnt.append(0)
        self.dma_toks = []
        self.n_ops = 0
        self.n_waits = 0
        self.limit = None
        self.stores_on_pool = False
        self.n_all = 0
        self.skip = False

    def _skipping(self):
        if self.skip:
            return True
        if self.limit is not None and self.n_all > self.limit and all(v is None for v in self.pending.values()):
            self.skip = True
            return True
        return False

    def _new_sem(self, name):
        self.nsem += 1
        return self.es.enter_context(self.nc.semaphore(name))

    def _eng_tok(self, eng, tok):
        c = self.count[eng]
        idx = c // SEM_CHUNK
        while len(self.sems[eng]) <= idx:
            self.sems[eng].append(self._new_sem("%s%d" % (eng, len(self.sems[eng]))))
        tok.sem = self.sems[eng][idx]
        tok.val = c % SEM_CHUNK + 1
        self.count[eng] = c + 1
        return tok

    def _wait(self, eng, tok):
        if tok is None:
            return
        if tok.sem is None:
            assert tok.eng == eng, "dependency on unsignaled op of %s from %s" % (tok.eng, eng)
            return
        k = self.known[eng]
        sid = id(tok.sem)
        if k.get(sid, 0) >= tok.val:
            return
        k[sid] = tok.val
        self.eobj[eng].wait_ge(tok.sem, tok.val)
        self.n_waits += 1

    def _deps(self, eng, reads, writes, is_dma=False):
        for r in reads:
            if r.w is not None:
                if r.w.eng == eng and not is_dma and eng == "pe":
                    continue
                self._wait(eng, r.w)
        same_ok = (eng == "pe") and not is_dma
        for w in writes:
            if w.w is not None and not (same_ok and w.w.eng == eng):
                self._wait(eng, w.w)
            for t in w.r:
                if not (same_ok and t.eng == eng):
                    self._wait(eng, t)

    def op(self, eng, fn, reads=(), writes=(), signal=True):
        self.n_all += 1
        if self._skipping():
            return None
        if any(r.excl for r in reads):
            writes = list(writes) + [r for r in reads if r.excl and r not in writes]
            reads = [r for r in reads if not r.excl]
        self._deps(eng, reads, writes)
        ins = fn(self.eobj[eng])
        self.n_ops += 1
        tok = self.pending[eng]
        if tok is None:
            tok = Tok(eng)
            self.pending[eng] = tok
        if signal:
            self._eng_tok(eng, tok)
            ins.then_inc(tok.sem, 1)
            self.pending[eng] = None
            self.last_tok[eng] = tok
        for r in reads:
            r.r.append(tok)
        for w in writes:
            w.w = tok
            w.r = []
        return tok

    def dma(self, q, out, in_, reads=(), writes=()):
        if q == "sp" and self.stores_on_pool and str(out.space).endswith("DRAM"):
            q = "pool"
        self.n_all += 1
        if self._skipping():
            return None
        self._deps(q, reads, writes, is_dma=True)
        if q == "pool":
            i = 16 + self.dma_ip % 8
            self.dma_ip += 1
        else:
            i = self.dma_i % 16
            self.dma_i += 1
        sem = self.dma_sems[i]
        if self.dma_cnt[i] > 0:
            prev = Tok("dma")
            prev.sem = sem
            prev.val = self.dma_cnt[i]
            self._wait(q, prev)
        self.eobj[q].dma_start(out=out, in_=in_).then_inc(sem, 16)
        self.dma_cnt[i] += 16
        tok = Tok("dma")
        tok.sem = sem
        tok.val = self.dma_cnt[i]
        self.dma_toks.append(tok)
        for r in reads:
            r.r.append(tok)
        for w in writes:
            w.w = tok
            w.r = []
        return tok

    def barrier(self):
        toks = [t for t in self.last_tok.values() if t is not None]
        for i, s in enumerate(self.dma_sems):
            if self.dma_cnt[i] > 0:
                t = Tok("dma")
                t.sem = s
                t.val = self.dma_cnt[i]
                toks.append(t)
        for e in self.eobj:
            assert self.pending[e] is None
            for t in toks:
                if t.eng == e:
                    continue
                self._wait(e, t)

    def final_wait(self):
        for i, s in enumerate(self.dma_sems):
            if self.dma_cnt[i] > 0:
                t = Tok("dma")
                t.sem = s
                t.val = self.dma_cnt[i]
                self._wait("sp", t)


class Buf:
    def __init__(self, t, name, nslots=1):
        self.t = t
        self.res = [Res("%s.%d" % (name, i)) for i in range(nslots)]

    @property
    def r(self):
        return self.res[0]


def build(nc, dbg=None, limit=None):
    P = None
    with contextlib.ExitStack() as es:
        P = Prog(nc, es)
        P.limit = limit

        def dram_in(name, shape, dt=F32):
            return nc.dram_tensor(name, list(shape), dt, kind="ExternalInput").ap()

        def dram_scr(name, shape, dt, kind="Internal"):
            return nc.dram_tensor(name, list(shape), dt, kind=kind).ap()

        x_d = dram_in("x", [S, D])
        p_d = dram_in("p", [DEPTH, S, 256])
        w_in_d = dram_in("w_in", [DEPTH, D, INC])
        w_out_d = dram_in("w_out", [DEPTH, D, D])
        wg_d = dram_in("expert_w_gate", [DEPTH, NE, D, 256])
        wu_d = dram_in("expert_w_up", [DEPTH, NE, D, 256])
        wd_d = dram_in("expert_w_down", [DEPTH, NE, 256, D])
        pgw_d = dram_in("ple_gate_w", [DEPTH, D, D])
        pgb_d = dram_in("ple_gate_b", [DEPTH, D])
        ppj_d = dram_in("ple_proj", [DEPTH, 256, D])
        prm_d = dram_in("prm", [DEPTH, 128, PC_N])
        wr_d = dram_in("wr", [DEPTH, 128, 8, 20])
        rb_d = dram_in("rbias", [DEPTH, 128, 20])
        lam_d = dram_in("lamv", [DEPTH, 128, 4, 32])
        pw_d = dram_in("poolw", [DEPTH, 128, 2, 128])
        gfin_d = dram_in("gfin", [128, D])
        cidf_d = dram_in("c_identf", [128, 128])
        crp_d = dram_in("c_rperm", [128, 128])
        ctri_d = dram_in("c_tri", [128, 128])
        cb64_d = dram_in("c_blk64", [128, 128])
        cones_d = dram_in("c_ones256", [128, 128])
        csel_d = dram_in("c_sel", [16, NE, 128])
        ccos_d = dram_in("c_cos", [128, S])
        csin_d = dram_in("c_sin", [128, S])
        ccorr_d = dram_in("c_corr", [128, 2, 16])
        out_d = nc.dram_tensor("out", [S, D], F32, kind="ExternalOutput").ap()

        dkind = "ExternalOutput" if dbg else "Internal"
        h_d = dram_scr("h_scr", [S, D], F32, dkind)
        glu_d = dram_scr("glu_scr", [256, S], BF16, dkind)
        pin_d = dram_scr("pin_scr", [256, S], F32, dkind)
        q_d = dram_scr("q_scr", [256, S], BF16, dkind)
        k_d = dram_scr("k_scr", [256, S], BF16, dkind)
        v_d = dram_scr("v_scr", [S, 512], BF16, dkind)
        gb_d = dram_scr("gb_scr", [256, S], F32, dkind)
        gcv_d = dram_scr("gcv_scr", [256, S], F32, dkind)
        mix_d = dram_scr("mix_scr", [D, S], BF16, dkind)
        xT_d = dram_scr("xT_scr", [D, S], BF16, dkind)
        cmb_d = dram_scr("cmb_scr", [16, S], BF16, dkind)

        def tiles(name):
            return [Res("%s%d" % (name, i)) for i in range(NT)]
        R_h = tiles("h")
        R_glu, R_pin, R_q, R_k, R_v = tiles("glu"), tiles("pin"), tiles("q"), tiles("k"), tiles("v")
        R_gb, R_gcv, R_mixA, R_mixB, R_xT, R_cmb = (tiles("gb"), tiles("gcv"), tiles("mixA"),
                                                    tiles("mixB"), tiles("xT"), tiles("cmb"))
        R_out = tiles("out")

        def fm(ap, i, lo=0, hi=TS):
            return ap.rearrange("(c p) t -> p c t", p=128)[:, :, i * TS + lo:i * TS + hi]

        def tm(ap, i):
            return ap[i * TS:(i + 1) * TS, :].rearrange("(j p) f -> p j f", p=128)

        uid = [0]
        def sbuf(stack, name, shape, dt, nslots=1):
            uid[0] += 1
            t = stack.enter_context(nc.sbuf_tensor("%s_u%d" % (name, uid[0]), list(shape), dt))
            return Buf(t, name, nslots)

        def psum(stack, name, shape, dt=F32):
            t = stack.enter_context(nc.psum_tensor(name, list(shape), dt))
            b = Buf(t, name, 1)
            b.res[0].excl = True
            return b

        identf = sbuf(es, "identf", [128, 128], F32)
        identb = sbuf(es, "identb", [128, 128], BF16)
        rperm = sbuf(es, "rperm", [128, 128], F32)
        trib = sbuf(es, "trib", [128, 128], BF16)
        blk64 = sbuf(es, "blk64", [128, 128], F32)
        ones256 = sbuf(es, "ones256", [128, 128], F32)
        sel = sbuf(es, "sel", [16, NE, 128], BF16)
        prm_l = [sbuf(es, "prm_sb%d" % l_, [128, PC_N], F32) for l_ in range(DEPTH)]
        corr = sbuf(es, "corr", [128, 2, 16], F32)
        gfin = sbuf(es, "gfin_sb", [128, D], F32)
        neglam_l = [sbuf(es, "neglam%d" % l_, [128, 1], F32) for l_ in range(DEPTH)]
        pbs_l = [sbuf(es, "pbs%d" % l_, [128, 2], F32) for l_ in range(DEPTH)]
        prm, neglam, pbs = prm_l[0], neglam_l[0], pbs_l[0]
        onesrow = sbuf(es, "onesrow", [1, 128], BF16)
        epsc = sbuf(es, "epsc", [128, 1], F32)

        pf = []
        pb = []
        pf_i = [0]
        pb_i = [0]

        def set_psum(stack, nf, nb):
            uid[0] += 1
            pf[:] = [psum(stack, "pf%d_%d" % (i, uid[0]), [128, 512], F32) for i in range(nf)]
            pb[:] = [psum(stack, "pb%d_%d" % (i, uid[0]), [128, 1024], BF16) for i in range(nb)]

        def next_pf():
            b = pf[pf_i[0] % len(pf)]
            pf_i[0] += 1
            return b

        def next_pb():
            b = pb[pb_i[0] % len(pb)]
            pb_i[0] += 1
            return b

        P.dma("sp", identf.t[:], cidf_d, writes=[identf.r])
        P.dma("sp", rperm.t[:], crp_d, writes=[rperm.r])
        P.dma("sp", blk64.t[:], cb64_d, writes=[blk64.r])
        P.dma("sp", ones256.t[:], cones_d, writes=[ones256.r])
        P.dma("pool", sel.t[:], csel_d, writes=[sel.r])
        P.dma("sp", corr.t[:], ccorr_d, writes=[corr.r])
        P.dma("sp", gfin.t[:], gfin_d, writes=[gfin.r])
        P.dma("pool", identb.t[:], cidf_d, writes=[identb.r])
        P.dma("pool", trib.t[:], ctri_d, writes=[trib.r])
        P.op("dve", lambda e: e.memset(onesrow.t[:], 1.0), writes=[onesrow.r])
        P.op("dve", lambda e: e.memset(epsc.t[:], EPS), writes=[epsc.r])

        def norm_stats(st, ht, slot, ss, rstd, sqj):
            P.op("dve", lambda e: e.memset(ss.t[:], 0.0), writes=[ss.r])
            for j in range(4):
                P.op("act", lambda e, j=j: e.activation(out=sqj.t[:], in_=ht.t[:, j, :], func=AF.Square,
                                                        accum_out=ss.t[:, j:j + 1]),
                     reads=[ht.res[slot], ss.r], writes=[sqj.r, ss.r])
            P.op("dve", lambda e: e.tensor_scalar(out=rstd.t[:], in0=ss.t[:], scalar1=1.0 / D, scalar2=EPS,
                                                  op0=ALU.mult, op1=ALU.add), reads=[ss.r], writes=[rstd.r])
            P.op("act", lambda e: e.activation(out=rstd.t[:], in_=rstd.t[:], func=AF.Sqrt),
                 reads=[rstd.r], writes=[rstd.r])
            P.op("dve", lambda e: e.reciprocal(out=rstd.t[:], in_=rstd.t[:]), reads=[rstd.r], writes=[rstd.r])

        def norm_transpose(ht, slot, rstd, xn, xT, gcol, part=None):
            for j in range(4 if part in (None, 0) else 0):
                P.op("dve", lambda e, j=j: e.tensor_scalar(out=xn.t[:, j, :], in0=ht.t[:, j, :],
                                                           scalar1=rstd.t[:, j:j + 1], scalar2=None, op0=ALU.mult),
                     reads=[ht.res[slot], rstd.r], writes=[xn.res[j]])
            for c2 in range(4 if part in (None, 1) else 0):
                bank = next_pb()
                for cc in range(2):
                    c = c2 * 2 + cc
                    for j in range(4):
                        last = (cc == 1 and j == 3)
                        P.op("pe", lambda e, c=c, cc=cc, j=j: e.transpose(
                            bank.t[:, cc * 512 + j * 128: cc * 512 + (j + 1) * 128],
                            xn.t[:, j, c * 128:(c + 1) * 128], identb.t[:]),
                            reads=[xn.res[j], identb.r], writes=[bank.r], signal=last)
                for cc in range(2):
                    c = c2 * 2 + cc
                    if cc == 0:
                        P.op("act", lambda e, c=c, cc=cc: e.activation(
                            out=xT.t[:, c, :], in_=bank.t[:, cc * 512:(cc + 1) * 512], func=AF.Identity,
                            scale=prm.t[:, gcol + c:gcol + c + 1]),
                            reads=[bank.r, prm.r], writes=[xT.res[c]])
                    else:
                        P.op("dve", lambda e, c=c, cc=cc: e.tensor_scalar(
                            out=xT.t[:, c, :], in0=bank.t[:, cc * 512:(cc + 1) * 512],
                            scalar1=prm.t[:, gcol + c:gcol + c + 1], scalar2=None, op0=ALU.mult),
                            reads=[bank.r, prm.r], writes=[xT.res[c]])

        def load_h(i, ht, slot, src):
            P.dma("sp", ht.t[:], tm(src, i), reads=[R_h[i]], writes=[ht.res[slot]])

        def mm_group(bank, cols, lhs_list, rhs_list, reads, tile_position=None):
            n = len(lhs_list)
            for k in range(n):
                P.op("pe", lambda e, k=k: e.matmul(bank.t[:, cols[0]:cols[1]], lhs_list[k], rhs_list[k],
                                                   start=(k == 0), stop=(k == n - 1)),
                     reads=reads[k], writes=[bank.r], signal=(k == n - 1))

        def load_w_in(l_):
            stw = contextlib.ExitStack()
            uid[0] += 1
            t_ = stw.enter_context(nc.sbuf_tensor("w_in_sb_u%d" % uid[0], [128, 8, INC], BF16, side="right"))
            b_ = Buf(t_, "w_in_sb", 1)
            for c in range(8):
                P.dma("pool", b_.t[:, c, :], w_in_d[l_, c * 128:(c + 1) * 128, :], writes=[b_.r])
            return stw, b_

        for l in range(DEPTH):
            lam_init = 0.8 - 0.6 * math.exp(-0.3 * l)
            prm, neglam, pbs = prm_l[l], neglam_l[l], pbs_l[l]
            P.dma("sp", prm.t[:], prm_d[l], writes=[prm.r])
            with contextlib.ExitStack() as st:
                lamv = sbuf(st, "lamv", [128, 4, 32], F32)
                lt = sbuf(st, "lt", [128, 2, 32], F32)
                ls = sbuf(st, "ls", [128, 2], F32)
                P.dma("sp", lamv.t[:], lam_d[l], writes=[lamv.r])
                P.op("dve", lambda e: e.tensor_tensor(out=lt.t[:, 0, :], in0=lamv.t[:, 0, :], in1=lamv.t[:, 1, :],
                                                      op=ALU.mult), reads=[lamv.r], writes=[lt.r])
                P.op("dve", lambda e: e.tensor_tensor(out=lt.t[:, 1, :], in0=lamv.t[:, 2, :], in1=lamv.t[:, 3, :],
                                                      op=ALU.mult), reads=[lamv.r, lt.r], writes=[lt.r])
                P.op("dve", lambda e: e.reduce_sum(out=ls.t[:], in_=lt.t[:], axis=AX.X), reads=[lt.r], writes=[ls.r])
                P.op("act", lambda e: e.activation(out=ls.t[:], in_=ls.t[:], func=AF.Exp), reads=[ls.r], writes=[ls.r])
                P.op("dve", lambda e: e.scalar_tensor_tensor(out=neglam.t[:], in0=ls.t[:, 1:2], scalar=-lam_init,
                                                             in1=ls.t[:, 0:1], op0=ALU.add, op1=ALU.subtract),
                     reads=[ls.r], writes=[neglam.r])
                P.op("dve", lambda e: e.tensor_tensor(out=pbs.t[:], in0=prm.t[:, PC_PB:PC_PB + 2],
                                                      in1=prm.t[:, PC_PS:PC_PS + 2], op=ALU.mult),
                     reads=[prm.r], writes=[pbs.r])

            P.barrier()
        st_win, w_in_next = load_w_in(0)
        for l in range(DEPTH):
            lam_init = 0.8 - 0.6 * math.exp(-0.3 * l)
            h_src = x_d if l == 0 else h_d

            prm, neglam, pbs = prm_l[l], neglam_l[l], pbs_l[l]
            st_dg = contextlib.ExitStack()
            dg = sbuf(st_dg, "s2_dg", [128, 2, CK, 128], BF16)
            pw_sb = sbuf(st_dg, "s2_pw", [128, 2, 128], BF16)
            with contextlib.ExitStack() as st:
                set_psum(st, 6, 2)
                w_in_sb = w_in_next
                ht = sbuf(st, "s1_ht", [128, 4, D], F32, 2)
                hts = [ht, sbuf(st, "s1_ht2", [128, 4, D], F32, 2)]
                ss2 = [sbuf(st, "s1_ss%d" % k, [128, 4], F32) for k in range(2)]
                rstd2 = [sbuf(st, "s1_rstd%d" % k, [128, 4], F32) for k in range(2)]
                sqj = sbuf(st, "s1_sqj", [128, D], BF16)
                xn2 = [sbuf(st, "s1_xn%d" % k, [128, 4, D], BF16, 4) for k in range(2)]
                nT2 = [sbuf(st, "s1_nT%d" % k, [128, 8, TS], BF16, 8) for k in range(2)]
                sig = sbuf(st, "s1_sig", [128, TS], F32)
                gcs = sbuf(st, "s1_gc", [128, TS], F32)
                glu_st = sbuf(st, "s1_glu", [128, 2, TS], BF16)
                pin_st = sbuf(st, "s1_pin", [128, 2, TS], F32)
                qk_st = sbuf(st, "s1_qk", [128, 4, TS], F32, 4)
                qkr_st = sbuf(st, "s1_qkr", [128, 4, TS], BF16, 4)
                gb_st = sbuf(st, "s1_gb", [128, 2, TS], F32)
                gcv_st = sbuf(st, "s1_gcv", [128, 2, TS], F32)
                v_st = sbuf(st, "s1_v", [128, 4, 512], BF16)
                cos_t = sbuf(st, "s1_cos", [128, TS], F32)
                sin_t = sbuf(st, "s1_sin", [128, TS], F32)
                t1 = sbuf(st, "s1_t1", [128, TS], F32)
                t2 = sbuf(st, "s1_t2", [128, TS], F32)

                P.op("dve", lambda e: e.memset(v_st.t[:], 1.0), writes=[v_st.r])

                def s1_prologue(i_):
                    norm_stats(st, hts[i_ % 2], 0, ss2[i_ % 2], rstd2[i_ % 2], sqj)
                    norm_transpose(hts[i_ % 2], 0, rstd2[i_ % 2], xn2[i_ % 2], nT2[i_ % 2], PC_MIXG)

                P.dma("sp", hts[0].t[:], tm(h_src, 0), reads=[R_h[0]], writes=[hts[0].r])
                P.dma("sp", hts[1].t[:], tm(h_src, 1), reads=[R_h[1]], writes=[hts[1].r])
                s1_prologue(0)
                P.dma("pool", pw_sb.t[:], pw_d[l], writes=[pw_sb.r])
                for cc in range(2):
                    for j in range(CK):
                        col = PC_CW + cc * CK + j
                        P.op("dve", lambda e, cc=cc, j=j, col=col: e.tensor_scalar(
                            out=dg.t[:, cc, j, :], in0=identf.t[:], scalar1=prm.t[:, col:col + 1], scalar2=None,
                            op0=ALU.mult), reads=[identf.r, prm.r], writes=[dg.r])
                for i in range(NT):
                    P.dma("sp", cos_t.t[:], ccos_d[:, i * TS:(i + 1) * TS], writes=[cos_t.r])
                    P.dma("sp", sin_t.t[:], csin_d[:, i * TS:(i + 1) * TS], writes=[sin_t.r])
                    nT = nT2[i % 2]

                    def proj(col0):
                        bank = next_pf()
                        mm_group(bank, (0, TS), [w_in_sb.t[:, c, col0:col0 + 128] for c in range(8)],
                                 [nT.t[:, c, :] for c in range(8)],
                                 [[w_in_sb.r, nT.res[c]] for c in range(8)])
                        return bank

                    for cc in range(2):
                        bg_ = proj(256 + cc * 128)
                        P.op("act", lambda e: e.activation(out=sig.t[:], in_=bg_.t[:], func=AF.Sigmoid),
                             reads=[bg_.r], writes=[sig.r])
                        bv_ = proj(0 + cc * 128)
                        P.op("dve", lambda e: e.tensor_tensor(out=glu_st.t[:, cc, :], in0=bv_.t[:], in1=sig.t[:],
                                                              op=ALU.mult),
                             reads=[bv_.r, sig.r], writes=[glu_st.r])
                    for cc in range(2):
                        bp_ = proj(512 + cc * 128)
                        P.op("act", lambda e: e.copy(out=pin_st.t[:, cc, :], in_=bp_.t[:]),
                             reads=[bp_.r], writes=[pin_st.r])
                    if i + 1 < NT:
                        s1_prologue(i + 1)
                    for m in range(4):
                        bq_ = proj(768 + m * 128)
                        if m % 2 == 0:
                            P.op("act", lambda e: e.copy(out=qk_st.t[:, m, :], in_=bq_.t[:]),
                                 reads=[bq_.r], writes=[qk_st.res[m]])
                        else:
                            P.op("dve", lambda e: e.tensor_copy(out=qk_st.t[:, m, :], in_=bq_.t[:]),
                                 reads=[bq_.r], writes=[qk_st.res[m]])
                    for cc in range(2):
                        bb_ = proj(1536 + cc * 128)
                        P.op("act", lambda e: e.copy(out=gb_st.t[:, cc, :], in_=bb_.t[:]),
                             reads=[bb_.r], writes=[gb_st.r])
                    for cc in range(2):
                        bc_ = proj(1792 + cc * 128)
                        P.op("act", lambda e: e.copy(out=gcs.t[:], in_=bc_.t[:]), reads=[bc_.r], writes=[gcs.r])
                        bs_ = proj(2048 + cc * 128)
                        P.op("dve", lambda e: e.tensor_tensor(out=gcv_st.t[:, cc, :], in0=bs_.t[:], in1=gcs.t[:],
                                                              op=ALU.mult),
                             reads=[bs_.r, gcs.r], writes=[gcv_st.r])
                    for j in range(4):
                        bank = next_pf()
                        mm_group(bank, (0, 256), [nT.t[:, c, j * 128:(j + 1) * 128] for c in range(8)],
                                 [w_in_sb.t[:, c, 1280:1536] for c in range(8)],
                                 [[w_in_sb.r, nT.res[c]] for c in range(8)])
                        for hh in range(4):
                            off = hh * 128 + (0 if hh % 2 == 0 else 64)
                            eng = "act" if hh % 2 == 0 else "dve"
                            if eng == "act":
                                P.op("act", lambda e: e.copy(out=v_st.t[:, j, off:off + 64],
                                                             in_=bank.t[:, hh * 64:(hh + 1) * 64]),
                                     reads=[bank.r], writes=[v_st.r])
                            else:
                                P.op("dve", lambda e: e.tensor_copy(out=v_st.t[:, j, off:off + 64],
                                                                    in_=bank.t[:, hh * 64:(hh + 1) * 64]),
                                     reads=[bank.r], writes=[v_st.r])
                    for m in range(4):
                        bank = next_pf()
                        P.op("pe", lambda e: e.matmul(bank.t[:], rperm.t[:], qk_st.t[:, m, :], start=True, stop=True),
                             reads=[rperm.r, qk_st.res[m]], writes=[bank.r])
                        P.op("dve", lambda e: e.tensor_tensor(out=t1.t[:], in0=qk_st.t[:, m, :], in1=cos_t.t[:],
                                                              op=ALU.mult),
                             reads=[qk_st.res[m], cos_t.r], writes=[t1.r])
                        P.op("dve", lambda e: e.tensor_tensor(out=t2.t[:], in0=bank.t[:], in1=sin_t.t[:],
                                                              op=ALU.mult),
                             reads=[bank.r, sin_t.r], writes=[t2.r])
                        P.op("dve", lambda e: e.tensor_tensor(out=qkr_st.t[:, m, :], in0=t1.t[:], in1=t2.t[:],
                                                              op=ALU.add),
                             reads=[t1.r, t2.r], writes=[qkr_st.res[m]])
                    if i + 2 < NT:
                        P.dma("sp", hts[i % 2].t[:], tm(h_src, i + 2), reads=[R_h[i + 2]], writes=[hts[i % 2].r])
                    P.dma("sp", fm(glu_d, i), glu_st.t[:], reads=[glu_st.r], writes=[R_glu[i]])
                    P.dma("sp", fm(pin_d, i), pin_st.t[:], reads=[pin_st.r], writes=[R_pin[i]])
                    P.dma("sp", fm(q_d, i), qkr_st.t[:, 0:2, :], reads=[qkr_st.res[0], qkr_st.res[1]], writes=[R_q[i]])
                    P.dma("sp", fm(k_d, i), qkr_st.t[:, 2:4, :], reads=[qkr_st.res[2], qkr_st.res[3]], writes=[R_k[i]])
                    P.dma("sp", tm(v_d, i), v_st.t[:], reads=[v_st.r], writes=[R_v[i]])
                    P.dma("sp", fm(gb_d, i), gb_st.t[:], reads=[gb_st.r], writes=[R_gb[i]])
                    P.dma("sp", fm(gcv_d, i), gcv_st.t[:], reads=[gcv_st.r], writes=[R_gcv[i]])
                P.barrier()
            st_win.close()
            if dbg == "s1":
                st_dg.close()
                break

            st_wout = contextlib.ExitStack()
            uid[0] += 1
            w_out_sb = Buf(st_wout.enter_context(nc.sbuf_tensor("w_out_sb_u%d" % uid[0], [128, 8, D], BF16,
                                                                side="right")), "w_out_sb", 1)
            st_kv = contextlib.ExitStack()
            kT = sbuf(st_kv, "at_kT", [128, 2, S], BF16)
            Vs = sbuf(st_kv, "at_V", [128, 32, 512], BF16)
            with contextlib.ExitStack() as st:
                set_psum(st, 8, 0)
                glu_in = [sbuf(st, "s2_glu%d" % k, [128, 2, 30 + TS], BF16) for k in range(2)]
                pin_in = [sbuf(st, "s2_pin%d" % k, [128, 2, 16 + TS], F32) for k in range(2)]
                gcv_in = [sbuf(st, "s2_gcv%d" % k, [128, 2, 2 + TS], F32) for k in range(2)]
                gb_in = [sbuf(st, "s2_gb%d" % k, [128, 2, TS], F32) for k in range(2)]
                yc = sbuf(st, "s2_y", [128, 2, TS], F32, 2)
                ysq = sbuf(st, "s2_ysq", [128, 2, TS], F32, 2)
                m2 = sbuf(st, "s2_m2", [128, TS], F32)
                var = sbuf(st, "s2_var", [128, TS], F32)
                dd = sbuf(st, "s2_dd", [128, TS], F32)
                sA = sbuf(st, "s2_sA", [128, 16 + TS], F32)
                sB = sbuf(st, "s2_sB", [128, 16 + TS], F32)
                pooled = sbuf(st, "s2_pooled", [128, TS], BF16)
                acc3 = sbuf(st, "s2_acc3", [128, TS], F32)
                mixA = [sbuf(st, "s2_mixA%d" % k, [128, 6, TS], BF16) for k in range(2)]


                def s2_load(i):
                    k = i % 2
                    if i == 0:
                        P.op("dve", lambda e: e.memset(glu_in[k].t[:, :, 0:30], 0.0), writes=[glu_in[k].r])
                        P.op("dve", lambda e: e.memset(pin_in[k].t[:, :, 0:16], 0.0), writes=[pin_in[k].r])
                        P.op("dve", lambda e: e.memset(gcv_in[k].t[:, :, 0:2], 0.0), writes=[gcv_in[k].r])
                        P.dma("sp", glu_in[k].t[:, :, 30:30 + TS], fm(glu_d, 0), reads=[R_glu[0]], writes=[glu_in[k].r])
                        P.dma("sp", pin_in[k].t[:, :, 16:16 + TS], fm(pin_d, 0), reads=[R_pin[0]], writes=[pin_in[k].r])
                        P.dma("sp", gcv_in[k].t[:, :, 2:2 + TS], fm(gcv_d, 0), reads=[R_gcv[0]], writes=[gcv_in[k].r])
                    else:
                        P.dma("sp", glu_in[k].t[:], fm(glu_d, i, -30, TS), reads=[R_glu[i - 1], R_glu[i]],
                              writes=[glu_in[k].r])
                        P.dma("sp", pin_in[k].t[:], fm(pin_d, i, -16, TS), reads=[R_pin[i - 1], R_pin[i]],
                              writes=[pin_in[k].r])
                        P.dma("sp", gcv_in[k].t[:], fm(gcv_d, i, -2, TS), reads=[R_gcv[i - 1], R_gcv[i]],
                              writes=[gcv_in[k].r])
                    P.dma("sp", gb_in[k].t[:], fm(gb_d, i), reads=[R_gb[i]], writes=[gb_in[k].r])

                s2_load(0)
                s2_load(1)
                for c in range(8):
                    P.dma("pool", w_out_sb.t[:, c, :], w_out_d[l, c * 128:(c + 1) * 128, :], writes=[w_out_sb.r])
                P.dma("sp", kT.t[:], k_d.rearrange("(c p) t -> p c t", p=128), reads=R_k, writes=[kT.r])
                for i8 in range(NT):
                    P.dma("sp", Vs.t[:, i8 * 4:(i8 + 1) * 4, :], tm(v_d, i8), reads=[R_v[i8]], writes=[Vs.r])
                for i in range(NT):
                    k = i % 2
                    if 1 <= i and i + 1 < NT:
                        s2_load(i + 1)
                    mx = mixA[k]
                    for cc in range(2):
                        bank = next_pf()
                        mm_group(bank, (0, TS), [dg.t[:, cc, j, :] for j in range(CK)],
                                 [glu_in[k].t[:, cc, j:j + TS] for j in range(CK)],
                                 [[dg.r, glu_in[k].r]] * CK)
                        P.op("act", lambda e: e.activation(out=yc.t[:, cc, :], in_=bank.t[:], func=AF.Identity,
                                                           bias=prm.t[:, PC_CB + cc:PC_CB + cc + 1]),
                             reads=[bank.r, prm.r], writes=[yc.res[cc]])
                        P.op("act", lambda e: e.activation(out=ysq.t[:, cc, :], in_=bank.t[:], func=AF.Square,
                                                           bias=prm.t[:, PC_CB + cc:PC_CB + cc + 1]),
                             reads=[bank.r, prm.r], writes=[ysq.res[cc]])
                    bm = next_pf()
                    mm_group(bm, (0, TS), [ones256.t[:], ones256.t[:]], [yc.t[:, 0, :], yc.t[:, 1, :]],
                             [[ones256.r, yc.res[0]], [ones256.r, yc.res[1]]])
                    bq = next_pf()
                    mm_group(bq, (0, TS), [ones256.t[:], ones256.t[:]], [ysq.t[:, 0, :], ysq.t[:, 1, :]],
                             [[ones256.r, ysq.res[0]], [ones256.r, ysq.res[1]]])
                    for cc in range(2):
                        u = pin_in[k].t[:, cc, :]
                        W_ = 16 + TS
                        P.op("dve", lambda e: e.memset(sA.t[:, 0:1], 0.0), writes=[sA.r])
                        P.op("dve", lambda e: e.tensor_tensor(out=sA.t[:, 1:W_], in0=pin_in[k].t[:, cc, 1:W_],
                                                              in1=pin_in[k].t[:, cc, 0:W_ - 1], op=ALU.add),
                             reads=[pin_in[k].r], writes=[sA.r])
                        if cc == 0:
                            P.op("dve", lambda e: e.tensor_tensor(out=sB.t[64:128, 3:W_], in0=sA.t[64:128, 3:W_],
                                                                  in1=sA.t[64:128, 1:W_ - 2], op=ALU.add),
                                 reads=[sA.r], writes=[sB.r])
                            P.op("dve", lambda e: e.tensor_copy(out=sB.t[0:64, 3:W_], in_=sA.t[0:64, 3:W_]),
                                 reads=[sA.r, sB.r], writes=[sB.r])
                            fin = sB
                        else:
                            P.op("dve", lambda e: e.tensor_tensor(out=sB.t[:, 3:W_], in0=sA.t[:, 3:W_],
                                                                  in1=sA.t[:, 1:W_ - 2], op=ALU.add),
                                 reads=[sA.r], writes=[sB.r])
                            P.op("dve", lambda e: e.tensor_tensor(out=sA.t[:, 7:W_], in0=sB.t[:, 7:W_],
                                                                  in1=sB.t[:, 3:W_ - 4], op=ALU.add),
                                 reads=[sB.r, sA.r], writes=[sA.r])
                            P.op("dve", lambda e: e.tensor_tensor(out=sB.t[64:128, 15:W_], in0=sA.t[64:128, 15:W_],
                                                                  in1=sA.t[64:128, 7:W_ - 8], op=ALU.add),
                                 reads=[sA.r, sB.r], writes=[sB.r])
                            P.op("dve", lambda e: e.tensor_copy(out=sB.t[0:64, 15:W_], in_=sA.t[0:64, 15:W_]),
                                 reads=[sA.r, sB.r], writes=[sB.r])
                            fin = sB
                        if i == 0:
                            P.op("dve", lambda e: e.tensor_tensor(out=fin.t[:, 16:32], in0=fin.t[:, 16:32],
                                                                  in1=corr.t[:, cc, :], op=ALU.mult),
                                 reads=[fin.r, corr.r], writes=[fin.r])
                        P.op("dve", lambda e: e.scalar_tensor_tensor(
                            out=pooled.t[:], in0=fin.t[:, 16:16 + TS], scalar=prm.t[:, PC_IW + cc:PC_IW + cc + 1],
                            in1=pin_in[k].t[:, cc, 16:16 + TS], op0=ALU.mult, op1=ALU.subtract),
                            reads=[fin.r, prm.r, pin_in[k].r], writes=[pooled.r])
                        bank = next_pf()
                        P.op("pe", lambda e: e.matmul(bank.t[:], pw_sb.t[:, cc, :], pooled.t[:], start=True, stop=True),
                             reads=[pw_sb.r, pooled.r], writes=[bank.r])
                        P.op("act", lambda e: e.activation(out=mx.t[:, 2 + cc, :], in_=bank.t[:], func=AF.Identity,
                                                           scale=prm.t[:, PC_PS + cc:PC_PS + cc + 1],
                                                           bias=pbs.t[:, cc:cc + 1]),
                             reads=[bank.r, prm.r, pbs.r], writes=[mx.r])
                    for cc in range(2):
                        g_ = gcv_in[k]
                        P.op("dve", lambda e: e.tensor_scalar(out=acc3.t[:], in0=g_.t[:, cc, 0:TS],
                                                              scalar1=prm.t[:, PC_SW + cc * 3:PC_SW + cc * 3 + 1],
                                                              scalar2=None, op0=ALU.mult),
                             reads=[g_.r, prm.r], writes=[acc3.r])
                        for j in (1, 2):
                            P.op("dve", lambda e, j=j: e.scalar_tensor_tensor(
                                out=acc3.t[:], in0=g_.t[:, cc, j:j + TS],
                                scalar=prm.t[:, PC_SW + cc * 3 + j:PC_SW + cc * 3 + j + 1], in1=acc3.t[:],
                                op0=ALU.mult, op1=ALU.add), reads=[g_.r, prm.r, acc3.r], writes=[acc3.r])
                        P.op("dve", lambda e: e.tensor_tensor(out=mx.t[:, 4 + cc, :], in0=acc3.t[:],
                                                              in1=gb_in[k].t[:, cc, :], op=ALU.mult),
                             reads=[acc3.r, gb_in[k].r], writes=[mx.r])
                    P.op("act", lambda e: e.activation(out=m2.t[:], in_=bm.t[:], func=AF.Square),
                         reads=[bm.r], writes=[m2.r])
                    P.op("dve", lambda e: e.tensor_tensor(out=var.t[:], in0=bq.t[:], in1=m2.t[:], op=ALU.subtract),
                         reads=[bq.r, m2.r], writes=[var.r])
                    P.op("dve", lambda e: e.tensor_scalar(out=var.t[:], in0=var.t[:], scalar1=0.0, scalar2=EPS,
                                                          op0=ALU.max, op1=ALU.add), reads=[var.r], writes=[var.r])
                    P.op("act", lambda e: e.activation(out=var.t[:], in_=var.t[:], func=AF.Ln),
                         reads=[var.r], writes=[var.r])
                    P.op("act", lambda e: e.activation(out=var.t[:], in_=var.t[:], func=AF.Exp, scale=-0.5),
                         reads=[var.r], writes=[var.r])
                    for cc in range(2):
                        P.op("dve", lambda e: e.tensor_tensor(out=dd.t[:], in0=bm.t[:], in1=yc.t[:, cc, :],
                                                              op=ALU.subtract),
                             reads=[bm.r, yc.res[cc]], writes=[dd.r])
                        P.op("dve", lambda e: e.tensor_tensor(out=dd.t[:], in0=dd.t[:], in1=var.t[:], op=ALU.mult),
                             reads=[dd.r, var.r], writes=[dd.r])
                        P.op("dve", lambda e: e.tensor_scalar(out=dd.t[:], in0=dd.t[:],
                                                              scalar1=prm.t[:, PC_LNG + cc:PC_LNG + cc + 1],
                                                              scalar2=-1.0, op0=ALU.mult, op1=ALU.mult),
                             reads=[dd.r, prm.r], writes=[dd.r])
                        P.op("act", lambda e: e.activation(out=mx.t[:, cc, :], in_=dd.t[:], func=AF.Silu,
                                                           bias=prm.t[:, PC_LNB + cc:PC_LNB + cc + 1]),
                             reads=[dd.r, prm.r], writes=[mx.r])
                    mv = mix_d.rearrange("(c p) t -> p c t", p=128)
                    P.dma("sp", mv[:, 0:4, i * TS:(i + 1) * TS], mx.t[:, 0:4, :], reads=[mx.r], writes=[R_mixA[i]])
                    P.dma("sp", mv[:, 6:8, i * TS:(i + 1) * TS], mx.t[:, 4:6, :], reads=[mx.r], writes=[R_mixA[i]])
                P.barrier()
            if dbg == "s2a":
                st_kv.close()
                st_wout.close()
                st_dg.close()
                break

            with contextlib.ExitStack() as st:
                qTs = [sbuf(st, "at_q%d" % k, [128, 2, TS], BF16) for k in range(2)]
                pts = [sbuf(st, "at_pt%d" % k, [128, TS], BF16) for k in range(4)]
                rz = [sbuf(st, "at_rz%d" % k, [128, TS], F32) for k in range(2)]
                o12 = [sbuf(st, "at_o%d" % k, [128, TS], F32) for k in range(2)]
                od = sbuf(st, "at_od", [128, TS], F32)
                osq = sbuf(st, "at_osq", [128, TS], F32)
                rs = sbuf(st, "at_rs", [128, TS], F32)
                mixB = [sbuf(st, "at_mix%d" % k, [128, 2, TS], BF16) for k in range(2)]
                set_psum(st, 4, 0)
                accb = pf[0:4]
                sc2 = [psum(st, "sc2_%d_%d" % (k, uid[0]), [128, 2 * TS], F32) for k in range(2)]
                pt2 = [sbuf(st, "at_pt2_%d" % k, [128, 2 * TS], BF16) for k in range(4)]
                P.dma("sp", qTs[0].t[:], fm(q_d, 0), reads=[R_q[0]], writes=[qTs[0].r])
                mv = mix_d.rearrange("(c p) t -> p c t", p=128)

                groups = []
                for i in range(NT):
                    nkt = 4 * i + 4
                    for hp in range(2):
                        for kt in range(nkt):
                            groups.append((i, hp, kt, nkt))

                def front(g):
                    i, hp, kt, nkt = groups[g]
                    if hp == 0 and kt == 0 and i + 1 < NT:
                        P.dma("sp", qTs[(i + 1) % 2].t[:], fm(q_d, i + 1), reads=[R_q[i + 1]],
                              writes=[qTs[(i + 1) % 2].r])
                    qT = qTs[i % 2]
                    jd = kt - 4 * i
                    qs = 128 * jd if jd > 0 else 0
                    n = TS - qs
                    for s_ in range(4):
                        po = s_ * 32
                        sc = sc2[s_ // 2]
                        o_ = (s_ % 2) * TS
                        P.op("pe", lambda e: e.matmul(sc.t[:, o_:o_ + n], kT.t[po:po + 32, hp, kt * 128:(kt + 1) * 128],
                                                      qT.t[po:po + 32, hp, qs:TS], start=True, stop=True,
                                                      tile_position=(po, 0)),
                             reads=[kT.r, qT.r], writes=[sc.r])
                    for pr in range(2):
                        sc = sc2[pr]
                        pt = pt2[(g % 2) * 2 + pr]
                        P.op("act", lambda e: e.activation(
                            out=pt.t[:].rearrange("p (a b) -> p a b", a=2)[:, :, 0:n],
                            in_=sc.t[:].rearrange("p (a b) -> p a b", a=2)[:, :, 0:n], func=AF.Exp, scale=SCALE),
                            reads=[sc.r], writes=[pt.r])
                        if jd >= 0:
                            P.op("dve", lambda e: e.tensor_tensor(
                                out=pt.t[:].rearrange("p (a b) -> p a b", a=2)[:, :, 0:128],
                                in0=pt.t[:].rearrange("p (a b) -> p a b", a=2)[:, :, 0:128],
                                in1=trib.t[:].unsqueeze(1).to_broadcast([128, 2, 128]), op=ALU.mult),
                                reads=[pt.r, trib.r], writes=[pt.r])

                def back(g):
                    i, hp, kt, nkt = groups[g]
                    jd = kt - 4 * i
                    qs = 128 * jd if jd > 0 else 0
                    n = TS - qs
                    for s_ in range(4):
                        h = 2 * hp + s_ // 2
                        pt = pt2[(g % 2) * 2 + s_ // 2]
                        o_ = (s_ % 2) * TS
                        acc = accb[s_]
                        P.op("pe", lambda e: e.matmul(acc.t[:, qs:TS], Vs.t[:, kt, h * 128:(h + 1) * 128],
                                                      pt.t[:, o_:o_ + n], start=(kt == 0), stop=(kt == nkt - 1)),
                             reads=[Vs.r, pt.r], writes=[acc.r])
                    if kt == nkt - 1:
                        finalize(i, 2 * hp)
                        finalize(i, 2 * hp + 1)

                def finalize(i, h):
                    ch = h // 2
                    mb = mixB[i % 2]
                    lo, hi = (0, 64) if h % 2 == 0 else (64, 128)
                    zlo, zhi = (64, 128) if h % 2 == 0 else (0, 64)
                    accs = [accb[(h % 2) * 2], accb[(h % 2) * 2 + 1]]
                    for comp in range(2):
                        P.op("act", lambda e: e.activation(out=rz[comp].t[zlo:zhi, :], in_=accs[comp].t[zlo:zhi, :],
                                                           func=AF.Ln),
                             reads=[accs[comp].r], writes=[rz[comp].r])
                        P.op("act", lambda e: e.activation(out=rz[comp].t[zlo:zhi, :], in_=rz[comp].t[zlo:zhi, :],
                                                           func=AF.Exp, scale=-1.0),
                             reads=[rz[comp].r], writes=[rz[comp].r])
                        P.op("dve", lambda e: e.tensor_tensor(out=o12[comp].t[lo:hi, :], in0=accs[comp].t[lo:hi, :],
                                                              in1=rz[comp].t[zlo:zhi, :], op=ALU.mult),
                             reads=[accs[comp].r, rz[comp].r], writes=[o12[comp].r])
                    P.op("dve", lambda e: e.scalar_tensor_tensor(out=od.t[lo:hi, :], in0=o12[1].t[lo:hi, :],
                                                                 scalar=neglam.t[lo:hi, 0:1], in1=o12[0].t[lo:hi, :],
                                                                 op0=ALU.mult, op1=ALU.add),
                         reads=[o12[0].r, o12[1].r, neglam.r], writes=[od.r])
                    if h % 2 == 1:
                        P.op("act", lambda e: e.activation(out=osq.t[:], in_=od.t[:], func=AF.Square),
                             reads=[od.r], writes=[osq.r])
                        bms = sc2[1]
                        P.op("pe", lambda e: e.matmul(bms.t[:, TS:2 * TS], blk64.t[:], osq.t[:], start=True, stop=True),
                             reads=[blk64.r, osq.r], writes=[bms.r])
                        P.op("act", lambda e: e.activation(out=rs.t[:], in_=bms.t[:, TS:2 * TS], func=AF.Ln,
                                                           bias=epsc.t[:, 0:1]),
                             reads=[bms.r, epsc.r], writes=[rs.r])
                        P.op("act", lambda e: e.activation(out=rs.t[:], in_=rs.t[:], func=AF.Exp, scale=-0.5),
                             reads=[rs.r], writes=[rs.r])
                        P.op("dve", lambda e: e.tensor_tensor(out=rs.t[:], in0=rs.t[:], in1=od.t[:], op=ALU.mult),
                             reads=[rs.r, od.r], writes=[rs.r])
                        P.op("dve", lambda e: e.tensor_scalar(out=mb.t[:, ch, :], in0=rs.t[:],
                                                              scalar1=prm.t[:, PC_SUB:PC_SUB + 1],
                                                              scalar2=1.0 - lam_init, op0=ALU.mult, op1=ALU.mult),
                             reads=[rs.r, prm.r], writes=[mb.r])
                    if h == 3:
                        P.dma("sp", mv[:, 4:6, i * TS:(i + 1) * TS], mb.t[:], reads=[mb.r], writes=[R_mixB[i]])

                LA = 1
                for g in range(len(groups) + LA):
                    if g < len(groups):
                        front(g)
                    if g >= LA:
                        back(g - LA)
                P.barrier()
            st_kv.close()
            st_dg.close()
            if dbg == "s2c":
                st_wout.close()
                break

            st_ple = contextlib.ExitStack()
            wpg = sbuf(st_ple, "s5_wpg", [128, 8, D], BF16)
            wpp = sbuf(st_ple, "s5_wpp", [128, 2, D], BF16)
            stm = contextlib.ExitStack()
            wgs = [sbuf(stm, "wgs0", [128, 4, 8, 256], BF16)]
            wus = [sbuf(stm, "wus0", [128, 4, 8, 256], BF16)]
            wds = [sbuf(stm, "wds0", [128, 4, 2, D], BF16)]
            def load_experts(pz, slot):
                for el in range(4):
                    eidx = pz * 4 + el
                    P.dma("pool", wgs[slot].t[:, el, :, :], wg_d[l, eidx].rearrange("(c p) n -> p c n", p=128),
                          writes=[wgs[slot].r])
                    P.dma("pool", wus[slot].t[:, el, :, :], wu_d[l, eidx].rearrange("(c p) n -> p c n", p=128),
                          writes=[wus[slot].r])
                    P.dma("pool", wds[slot].t[:, el, :, :], wd_d[l, eidx].rearrange("(c p) n -> p c n", p=128),
                          writes=[wds[slot].r])

            with contextlib.ExitStack() as st:
                set_psum(st, 6, 2)
                wrg = sbuf(st, "s3_wrg", [128, 8, 20], F32)
                rbias = sbuf(st, "s3_rb", [128, 20], F32)
                hts = [sbuf(st, "s3_ht%d" % k, [128, 4, D], F32) for k in range(2)]
                mxs = [sbuf(st, "s3_mx%d" % k, [128, 8, TS], BF16) for k in range(2)]
                ss2 = [sbuf(st, "s3_ss%d" % k, [128, 4], F32) for k in range(2)]
                rstd2 = [sbuf(st, "s3_rstd%d" % k, [128, 4], F32) for k in range(2)]
                sqj = sbuf(st, "s3_sqj", [128, D], BF16)
                xn2 = [sbuf(st, "s3_xn%d" % k, [128, 4, D], BF16, 4) for k in range(2)]
                xT2 = [sbuf(st, "s3_xT%d" % k, [128, 8, TS], BF16, 8) for k in range(2)]
                hTf = sbuf(st, "s3_hTf", [128, 8, 128], F32, 2)
                lg = sbuf(st, "s3_lg", [128, 20], F32)
                lg4 = sbuf(st, "s3_lg4", [128, 4, 20], F32)
                r4 = sbuf(st, "s3_r4", [128, 8, 4], F32)
                mg4 = sbuf(st, "s3_mg4", [128, 4, 4], F32)
                t4 = sbuf(st, "s3_t4", [128, 4, 4], F32)
                es4 = sbuf(st, "s3_es4", [128, 4, 4], F32)
                eq4 = sbuf(st, "s3_eq4", [128, 4, 4], F32)
                pr4 = sbuf(st, "s3_pr4", [128, 4, 4, 4], F32)
                sm = sbuf(st, "s3_sm", [128, 16], F32)
                mg = sbuf(st, "s3_mg", [128, 4], F32)
                ge = sbuf(st, "s3_ge", [128, 4], F32)
                esel = sbuf(st, "s3_esel", [128, 4], F32)
                eq = sbuf(st, "s3_eq", [128, 4], F32)
                em2 = sbuf(st, "s3_em2", [128, 4], F32)
                ee = sbuf(st, "s3_ee", [128, 4], F32)
                wsel = sbuf(st, "s3_wsel", [128, 4], F32)
                comb = sbuf(st, "s3_comb", [128, 4, 16], F32)
                cmbT = sbuf(st, "s3_cmbT", [16, TS], BF16)

                P.dma("sp", wrg.t[:], wr_d[l], writes=[wrg.r])
                P.dma("sp", rbias.t[:], rb_d[l], writes=[rbias.r])
                for c in range(8):
                    P.op("dve", lambda e, c=c: e.tensor_scalar(out=wrg.t[:, c, :], in0=wrg.t[:, c, :],
                                                               scalar1=prm.t[:, PC_FFNG + c:PC_FFNG + c + 1],
                                                               scalar2=None, op0=ALU.mult),
                         reads=[wrg.r, prm.r], writes=[wrg.r])

                def s3_load(i):
                    k = i % 2
                    P.dma("sp", hts[k].t[:], tm(h_src, i), reads=[R_h[i]], writes=[hts[k].r])
                    P.dma("sp", mxs[k].t[:], fm(mix_d, i), reads=[R_mixA[i], R_mixB[i]], writes=[mxs[k].r])

                def s3_A(i):
                    k = i % 2
                    ht = hts[k]
                    mx = mxs[k]
                    ss, rstd, xn, xT = ss2[k], rstd2[k], xn2[k], xT2[k]
                    for j in range(4):
                        for half in range(2):
                            bank = next_pf()
                            mm_group(bank, (0, 512), [mx.t[:, c, j * 128:(j + 1) * 128] for c in range(8)],
                                     [w_out_sb.t[:, c, half * 512:(half + 1) * 512] for c in range(8)],
                                     [[mx.r, w_out_sb.r]] * 8)
                            P.op("dve", lambda e: e.tensor_tensor(out=ht.t[:, j, half * 512:(half + 1) * 512],
                                                                  in0=bank.t[:], in1=ht.t[:, j, half * 512:(half + 1) * 512],
                                                                  op=ALU.add),
                                 reads=[bank.r, ht.r], writes=[ht.r])
                    P.dma("sp", tm(h_d, i), ht.t[:], reads=[ht.r], writes=[R_h[i]])
                    norm_stats(st, ht, 0, ss, rstd, sqj)
                    norm_transpose(ht, 0, rstd, xn, xT, PC_FFNG, part=0)

                def s3_A2(i):
                    k = i % 2
                    ht = hts[k]
                    ss, rstd, xn, xT = ss2[k], rstd2[k], xn2[k], xT2[k]
                    norm_transpose(ht, 0, rstd, xn, xT, PC_FFNG, part=1)
                    P.dma("sp", fm(xT_d, i), xT.t[:], reads=xT.res, writes=[R_xT[i]])

                def s3_Ba(i):
                    k = i % 2
                    ht = hts[k]
                    ss, rstd, xn, xT = ss2[k], rstd2[k], xn2[k], xT2[k]
                    for j in range(4):
                        for c2 in range(2):
                            bank = next_pf()
                            while bank is bct:
                                bank = next_pf()
                            for cq in range(4):
                                c = c2 * 4 + cq
                                P.op("pe", lambda e: e.transpose(bank.t[:, cq * 128:(cq + 1) * 128],
                                                                 ht.t[:, j, c * 128:(c + 1) * 128], identf.t[:]),
                                     reads=[ht.r, identf.r], writes=[bank.r], signal=(cq == 3))
                            if c2 == 0:
                                P.op("act", lambda e: e.copy(out=hTf.t[:, 0:4, :], in_=bank.t[:].rearrange("p (a b) -> p a b", a=4)),
                                     reads=[bank.r], writes=[hTf.res[0]])
                            else:
                                P.op("dve", lambda e: e.tensor_copy(out=hTf.t[:, 4:8, :], in_=bank.t[:].rearrange("p (a b) -> p a b", a=4)),
                                     reads=[bank.r], writes=[hTf.res[1]])
                        bl = next_pf()
                        while bl is bct:
                            bl = next_pf()
                        mm_group(bl, (0, 20), [hTf.t[:, c, :] for c in range(8)], [wrg.t[:, c, :] for c in range(8)],
                                 [[hTf.res[c // 4], wrg.r] for c in range(8)])
                        P.op("dve", lambda e: e.scalar_tensor_tensor(out=lg4.t[:, j, :], in0=bl.t[:, 0:20],
                                                                     scalar=rstd.t[:, j:j + 1], in1=rbias.t[:],
                                                                     op0=ALU.mult, op1=ALU.add),
                             reads=[bl.r, rstd.r, rbias.r], writes=[lg4.r])

                def s3_Bb(i):
                    V = lambda fn, rd, wr: P.op("dve", fn, reads=rd, writes=wr)
                    S3 = [128, 4, 4]
                    bc = lambda ap_: ap_.unsqueeze(2).to_broadcast(S3)
                    glg = lg4.t[:, :, 0:4]
                    V(lambda e: e.reduce_max(out=r4.t[:, 0, :], in_=glg, axis=AX.X), [lg4.r], [r4.r])
                    V(lambda e: e.tensor_tensor(out=mg4.t[:], in0=glg, in1=bc(r4.t[:, 0, :]), op=ALU.is_ge),
                      [lg4.r, r4.r], [mg4.r])
                    V(lambda e: e.tensor_tensor(out=t4.t[:], in0=glg, in1=bc(r4.t[:, 0, :]), op=ALU.subtract),
                      [lg4.r, r4.r], [t4.r])
                    P.op("act", lambda e: e.activation(out=t4.t[:], in_=t4.t[:], func=AF.Exp), reads=[t4.r], writes=[t4.r])
                    V(lambda e: e.reduce_sum(out=r4.t[:, 1, :], in_=t4.t[:], axis=AX.X), [t4.r, r4.r], [r4.r])
                    V(lambda e: e.reciprocal(out=r4.t[:, 2, :], in_=r4.t[:, 1, :]), [r4.r], [r4.r])
                    el4 = lg4.t[:, :, 4:20].rearrange("p j (g i) -> p j g i", g=4)
                    V(lambda e: e.tensor_tensor(out=pr4.t[:], in0=el4,
                                                in1=mg4.t[:].unsqueeze(3).to_broadcast([128, 4, 4, 4]), op=ALU.mult),
                      [lg4.r, mg4.r], [pr4.r])
                    V(lambda e: e.reduce_sum(out=es4.t[:], in_=pr4.t[:].rearrange("p j g i -> p j i g"), axis=AX.X),
                      [pr4.r], [es4.r])
                    V(lambda e: e.reduce_max(out=r4.t[:, 3, :], in_=es4.t[:], axis=AX.X), [es4.r, r4.r], [r4.r])
                    V(lambda e: e.tensor_tensor(out=eq4.t[:], in0=es4.t[:], in1=bc(r4.t[:, 3, :]), op=ALU.is_ge),
                      [es4.r, r4.r], [eq4.r])
                    V(lambda e: e.scalar_tensor_tensor(out=t4.t[:], in0=eq4.t[:], scalar=-1e30, in1=es4.t[:],
                                                       op0=ALU.mult, op1=ALU.add), [eq4.r, es4.r, t4.r], [t4.r])
                    V(lambda e: e.reduce_max(out=r4.t[:, 4, :], in_=t4.t[:], axis=AX.X), [t4.r, r4.r], [r4.r])
                    V(lambda e: e.tensor_tensor(out=eq4.t[:], in0=es4.t[:], in1=bc(r4.t[:, 4, :]), op=ALU.is_ge),
                      [es4.r, r4.r, eq4.r], [eq4.r])
                    V(lambda e: e.tensor_tensor(out=t4.t[:], in0=es4.t[:], in1=bc(r4.t[:, 3, :]), op=ALU.subtract),
                      [es4.r, r4.r, t4.r], [t4.r])
                    P.op("act", lambda e: e.activation(out=t4.t[:], in_=t4.t[:], func=AF.Exp), reads=[t4.r], writes=[t4.r])
                    V(lambda e: e.tensor_tensor(out=t4.t[:], in0=t4.t[:], in1=eq4.t[:], op=ALU.mult),
                      [t4.r, eq4.r], [t4.r])
                    V(lambda e: e.reduce_sum(out=r4.t[:, 5, :], in_=t4.t[:], axis=AX.X), [t4.r, r4.r], [r4.r])
                    V(lambda e: e.reciprocal(out=r4.t[:, 6, :], in_=r4.t[:, 5, :]), [r4.r], [r4.r])
                    V(lambda e: e.tensor_tensor(out=r4.t[:, 6, :], in0=r4.t[:, 6, :], in1=r4.t[:, 2, :], op=ALU.mult),
                      [r4.r], [r4.r])
                    V(lambda e: e.tensor_tensor(out=t4.t[:], in0=t4.t[:], in1=bc(r4.t[:, 6, :]), op=ALU.mult),
                      [t4.r, r4.r], [t4.r])
                    V(lambda e: e.tensor_tensor(out=comb.t[:].rearrange("p j (g i) -> p j g i", g=4),
                                                in0=t4.t[:].unsqueeze(2).to_broadcast([128, 4, 4, 4]),
                                                in1=mg4.t[:].unsqueeze(3).to_broadcast([128, 4, 4, 4]), op=ALU.mult),
                      [t4.r, mg4.r], [comb.r])

                def s3_B2(i):
                    for j in range(4):
                        P.op("pe", lambda e: e.transpose(bct.t[0:16, j * 128:(j + 1) * 128], comb.t[:, j, :], identf.t[:]),
                             reads=[comb.r, identf.r], writes=[bct.r])
                    P.op("act", lambda e: e.copy(out=cmbT.t[:], in_=bct.t[0:16, :]), reads=[bct.r], writes=[cmbT.r])
                    P.dma("sp", cmb_d[:, i * TS:(i + 1) * TS], cmbT.t[:], reads=[cmbT.r], writes=[R_cmb[i]])

                bct = pf.pop()
                s3_load(0)
                if NT > 1:
                    s3_load(1)
                s3_A(0)
                s3_A2(0)
                load_experts(0, 0)
                for i in range(NT):
                    if i + 1 < NT:
                        s3_A(i + 1)
                    if i >= 1:
                        s3_B2(i - 1)
                    s3_Ba(i)
                    if i + 2 < NT:
                        s3_load(i + 2)
                    if i + 1 < NT:
                        s3_A2(i + 1)
                    s3_Bb(i)
                s3_B2(NT - 1)
                P.barrier()
            st_wout.close()
            if dbg == "s3":
                stm.close()
                st_ple.close()
                break

            wgs.append(sbuf(stm, "wgs1", [128, 4, 8, 256], BF16))
            wus.append(sbuf(stm, "wus1", [128, 4, 8, 256], BF16))
            wds.append(sbuf(stm, "wds1", [128, 4, 2, D], BF16))
            with contextlib.ExitStack() as st:
                set_psum(st, 8, 0)
                hts = [sbuf(st, "s4_ht%d" % k, [128, 4, D], F32) for k in range(2)]
                xTs = [sbuf(st, "s4_xT%d" % k, [128, 8, TS], BF16) for k in range(2)]
                cms = [sbuf(st, "s4_cm%d" % k, [16, TS], BF16) for k in range(2)]
                cbs = sbuf(st, "s4_cb", [128, TS], F32)
                sg_ = sbuf(st, "s4_sg", [128, TS], F32)
                tt_ = sbuf(st, "s4_tt", [128, TS], F32)
                hdn = sbuf(st, "s4_hdn", [128, 4, 2, TS], BF16)

                def s4_load(i):
                    k = i % 2
                    P.dma("sp", hts[k].t[:], tm(h_d, i), reads=[R_h[i]], writes=[hts[k].r])
                    P.dma("sp", xTs[k].t[:], fm(xT_d, i), reads=[R_xT[i]], writes=[xTs[k].r])
                    P.dma("sp", cms[k].t[:], cmb_d[:, i * TS:(i + 1) * TS], reads=[R_cmb[i]], writes=[cms[k].r])

                s4_load(0)
                for pz in range(4):
                    slot = pz % 2
                    if pz + 1 < 4:
                        load_experts(pz + 1, (pz + 1) % 2)
                    if pz == 0:
                        for c in range(8):
                            P.dma("pool", wpg.t[:, c, :], pgw_d[l, c * 128:(c + 1) * 128, :], writes=[wpg.r])
                        P.dma("pool", wpp.t[:], ppj_d[l].rearrange("(c p) n -> p c n", p=128), writes=[wpp.r])
                    for i in range(NT):
                        k = i % 2
                        if i + 1 < NT:
                            s4_load(i + 1)
                        elif pz + 1 < 4:
                            s4_load(0)
                        ht, xT_, cm = hts[k], xTs[k], cms[k]
                        for el in range(4):
                            eidx = pz * 4 + el
                            bcb = next_pf()
                            P.op("pe", lambda e: e.matmul(bcb.t[:], sel.t[0:16, eidx, :], cm.t[0:16, :],
                                                          start=True, stop=True),
                                 reads=[sel.r, cm.r], writes=[bcb.r])
                            P.op("act", lambda e: e.copy(out=cbs.t[:], in_=bcb.t[:]), reads=[bcb.r], writes=[cbs.r])
                            for hc in range(2):
                                bg_ = next_pf()
                                mm_group(bg_, (0, TS), [wgs[slot].t[:, el, c, hc * 128:(hc + 1) * 128] for c in range(8)],
                                         [xT_.t[:, c, :] for c in range(8)], [[wgs[slot].r, xT_.r]] * 8)
                                bu_ = next_pf()
                                mm_group(bu_, (0, TS), [wus[slot].t[:, el, c, hc * 128:(hc + 1) * 128] for c in range(8)],
                                         [xT_.t[:, c, :] for c in range(8)], [[wus[slot].r, xT_.r]] * 8)
                                P.op("act", lambda e: e.activation(out=sg_.t[:], in_=bg_.t[:], func=AF.Silu),
                                     reads=[bg_.r], writes=[sg_.r])
                                P.op("dve", lambda e: e.tensor_tensor(out=tt_.t[:], in0=bu_.t[:], in1=cbs.t[:],
                                                                      op=ALU.mult),
                                     reads=[bu_.r, cbs.r], writes=[tt_.r])
                                P.op("dve", lambda e: e.tensor_tensor(out=hdn.t[:, el, hc, :], in0=sg_.t[:], in1=tt_.t[:],
                                                                      op=ALU.mult),
                                     reads=[sg_.r, tt_.r], writes=[hdn.r])
                        for j in range(4):
                            for half in range(2):
                                by = next_pf()
                                mm_group(by, (0, 512),
                                         [hdn.t[:, el, hc, j * 128:(j + 1) * 128] for el in range(4) for hc in range(2)],
                                         [wds[slot].t[:, el, hc, half * 512:(half + 1) * 512]
                                          for el in range(4) for hc in range(2)],
                                         [[hdn.r, wds[slot].r]] * 8)
                                P.op("dve", lambda e: e.tensor_tensor(out=ht.t[:, j, half * 512:(half + 1) * 512],
                                                                      in0=by.t[:],
                                                                      in1=ht.t[:, j, half * 512:(half + 1) * 512],
                                                                      op=ALU.add),
                                     reads=[by.r, ht.r], writes=[ht.r])
                        P.dma("sp", tm(h_d, i), ht.t[:], reads=[ht.r], writes=[R_h[i]])
                P.barrier()
            stm.close()
            if dbg == "s4":
                st_ple.close()
                break

            with contextlib.ExitStack() as st:
                set_psum(st, 6, 2)
                gbf = sbuf(st, "s5_gbf", [1, D], F32)
                gbb = sbuf(st, "s5_gbb", [1, D], BF16)
                hts = [sbuf(st, "s5_ht%d" % k, [128, 4, D], F32) for k in range(3)]
                pts_ = [sbuf(st, "s5_p%d" % k, [128, 4, 256], F32) for k in range(3)]
                ss2 = [sbuf(st, "s5_ss%d" % k, [128, 4], F32) for k in range(2)]
                rstd2 = [sbuf(st, "s5_rstd%d" % k, [128, 4], F32) for k in range(2)]
                sqj = sbuf(st, "s5_sqj", [128, D], BF16)
                xn2 = [sbuf(st, "s5_xn%d" % k, [128, 4, D], BF16, 4) for k in range(2)]
                xT2 = [sbuf(st, "s5_xT%d" % k, [128, 8, TS], BF16, 8) for k in range(2)]
                pT2 = [sbuf(st, "s5_pT%d" % k, [128, 2, TS], BF16) for k in range(2)]
                gts = [sbuf(st, "s5_gt%d" % k, [128, 512], F32) for k in range(3)]
                tqs = [sbuf(st, "s5_tq%d" % k, [128, 512], F32) for k in range(3)]
                if l + 1 < DEPTH:
                    st_win, w_in_next = load_w_in(l + 1)
                P.dma("sp", gbf.t[:], pgb_d[l:l + 1, :], writes=[gbf.r])
                P.op("dve", lambda e: e.tensor_copy(out=gbb.t[:], in_=gbf.t[:]), reads=[gbf.r], writes=[gbb.r])

                def s5_load(i):
                    k = i % 3
                    P.dma("sp", hts[k].t[:], tm(h_d, i), reads=[R_h[i]], writes=[hts[k].r])
                    P.dma("sp", pts_[k].t[:], tm(p_d[l], i), writes=[pts_[k].r])

                def s5_P(i):
                    k = i % 2
                    ht, pt_ = hts[i % 3], pts_[i % 3]
                    ss, rstd, xn, xT, pT = ss2[k], rstd2[k], xn2[k], xT2[k], pT2[k]
                    norm_stats(st, ht, 0, ss, rstd, sqj)
                    norm_transpose(ht, 0, rstd, xn, xT, PC_PLEG)
                    for c in range(2):
                        bank = next_pf()
                        for j in range(4):
                            P.op("pe", lambda e: e.transpose(bank.t[:, j * 128:(j + 1) * 128],
                                                             pt_.t[:, j, c * 128:(c + 1) * 128], identf.t[:]),
                                 reads=[pt_.r, identf.r], writes=[bank.r], signal=(j == 3))
                        P.op("act", lambda e: e.copy(out=pT.t[:, c, :], in_=bank.t[:]), reads=[bank.r], writes=[pT.r])

                def s5_M(i):
                    k = i % 2
                    ht, pt_ = hts[i % 3], pts_[i % 3]
                    ss, rstd, xn, xT, pT = ss2[k], rstd2[k], xn2[k], xT2[k], pT2[k]
                    for j in range(4):
                        for half in range(2):
                            hs = slice(half * 512, (half + 1) * 512)
                            gt = gts[(j * 2 + half) % 3]
                            tq = tqs[(j * 2 + half) % 3]
                            bg_ = next_pf()
                            mm_group(bg_, (0, 512),
                                     [xT.t[:, c, j * 128:(j + 1) * 128] for c in range(8)] + [onesrow.t[0:1, :]],
                                     [wpg.t[:, c, hs] for c in range(8)] + [gbb.t[0:1, hs]],
                                     [[xT.res[c], wpg.r] for c in range(8)] + [[onesrow.r, gbb.r]])
                            bp_ = next_pf()
                            mm_group(bp_, (0, 512), [pT.t[:, c, j * 128:(j + 1) * 128] for c in range(2)],
                                     [wpp.t[:, c, hs] for c in range(2)], [[pT.r, wpp.r]] * 2)
                            P.op("act", lambda e: e.activation(out=gt.t[:], in_=bg_.t[:], func=AF.Sigmoid),
                                 reads=[bg_.r], writes=[gt.r])
                            P.op("dve", lambda e: e.tensor_tensor(out=tq.t[:], in0=bp_.t[:], in1=gt.t[:], op=ALU.mult),
                                 reads=[bp_.r, gt.r], writes=[tq.r])
                            P.op("dve", lambda e: e.tensor_tensor(out=ht.t[:, j, hs], in0=ht.t[:, j, hs], in1=tq.t[:],
                                                                  op=ALU.add),
                                 reads=[ht.r, tq.r], writes=[ht.r])
                    if l < DEPTH - 1:
                        P.dma("sp", tm(h_d, i), ht.t[:], reads=[ht.r], writes=[R_h[i]])
                    else:
                        norm_stats(st, ht, 0, ss, rstd, sqj)
                        for j in range(4):
                            P.op("dve", lambda e: e.scalar_tensor_tensor(out=ht.t[:, j, :], in0=ht.t[:, j, :],
                                                                         scalar=rstd.t[:, j:j + 1], in1=gfin.t[:],
                                                                         op0=ALU.mult, op1=ALU.mult),
                                 reads=[ht.r, rstd.r, gfin.r], writes=[ht.r])
                        P.dma("sp", tm(out_d, i), ht.t[:], reads=[ht.r], writes=[R_out[i]])

                s5_load(0)
                if NT > 1:
                    s5_load(1)
                s5_P(0)
                for i in range(NT):
                    if i + 2 < NT:
                        s5_load(i + 2)
                    if i + 1 < NT:
                        s5_P(i + 1)
                    s5_M(i)
                P.barrier()
            st_ple.close()

        P.barrier()
        P.final_wait()
    return P


def _host_consts():
    c = {}
    c["c_identf"] = np.eye(128, dtype=np.float32)
    rp = np.zeros((128, 128), np.float32)
    for b in range(4):
        for d in range(16):
            rp[b * 32 + d + 16, b * 32 + d] = -1.0
            rp[b * 32 + d, b * 32 + d + 16] = 1.0
    c["c_rperm"] = rp
    kk = np.arange(128)[:, None]
    qq = np.arange(128)[None, :]
    c["c_tri"] = (qq >= kk).astype(np.float32)
    b64 = np.zeros((128, 128), np.float32)
    b64[0:64, 0:64] = 1.0 / 64
    b64[64:128, 64:128] = 1.0 / 64
    c["c_blk64"] = b64
    c["c_ones256"] = np.full((128, 128), 1.0 / 256, np.float32)
    sel = np.zeros((16, NE, 128), np.float32)
    for e in range(NE):
        sel[e, e, :] = 1.0
    c["c_sel"] = sel
    pos = np.arange(S, dtype=np.float32)
    inv = (10000.0 ** (-np.arange(0, 32, 2, dtype=np.float32) / np.float32(32))).astype(np.float32)
    ang = (pos[:, None] * inv[None, :]).astype(np.float32)
    ang = np.concatenate([ang, ang], axis=-1)
    cosT = np.cos(ang.astype(np.float64)).astype(np.float32).T
    sinT = np.sin(ang.astype(np.float64)).astype(np.float32).T
    c["c_cos"] = np.ascontiguousarray(np.tile(cosT, (4, 1)))
    c["c_sin"] = np.ascontiguousarray(np.tile(sinT, (4, 1)))
    wins = np.array([2, 4, 8, 16])
    corr = np.zeros((128, 2, 16), np.float32)
    for cc in range(2):
        for p in range(128):
            w = wins[cc * 2 + p // 64]
            for t in range(16):
                corr[p, cc, t] = w / min(t + 1, w)
    c["c_corr"] = corr
    iw = np.zeros((128, 2), np.float32)
    for cc in range(2):
        for p in range(128):
            iw[p, cc] = 1.0 / wins[cc * 2 + p // 64]
    c["_iw"] = iw
    return c


def _fmcols(v, nch):
    return np.ascontiguousarray(np.asarray(v, np.float32).reshape(nch, 128).T)


def _layout_inputs(inp):
    c = _host_consts()
    iw = c.pop("_iw")
    shared = dict(c)
    prm = np.zeros((DEPTH, 128, PC_N), np.float32)
    wr = np.zeros((DEPTH, 128, 8, 20), np.float32)
    rb = np.zeros((DEPTH, 128, 20), np.float32)
    lamv = np.zeros((DEPTH, 128, 4, 32), np.float32)
    pw = np.zeros((DEPTH, 128, 2, 128), np.float32)
    for l in range(DEPTH):
        prm[l, :, PC_MIXG:PC_MIXG + 8] = _fmcols(inp["mix_norm"][l], 8)
        prm[l, :, PC_FFNG:PC_FFNG + 8] = _fmcols(inp["ffn_norm"][l], 8)
        prm[l, :, PC_PLEG:PC_PLEG + 8] = _fmcols(inp["ple_norm"][l], 8)
        prm[l, :, PC_FING:PC_FING + 8] = _fmcols(inp["final_norm"], 8)
        cw = np.asarray(inp["conf_conv_w"][l], np.float32)
        for cc in range(2):
            prm[l, :, PC_CW + cc * CK:PC_CW + (cc + 1) * CK] = cw[:, cc * 128:(cc + 1) * 128].T
        prm[l, :, PC_CB:PC_CB + 2] = _fmcols(inp["conf_conv_b"][l], 2)
        prm[l, :, PC_LNG:PC_LNG + 2] = _fmcols(inp["conf_ln_g"][l], 2)
        prm[l, :, PC_LNB:PC_LNB + 2] = _fmcols(inp["conf_ln_b"][l], 2)
        prm[l, :, PC_PB:PC_PB + 2] = _fmcols(np.asarray(inp["pool_b"][l]).reshape(256), 2)
        prm[l, :, PC_PS:PC_PS + 2] = _fmcols(inp["pool_scale"][l], 2)
        sw = np.asarray(inp["sconv_w"][l], np.float32)
        for cc in range(2):
            prm[l, :, PC_SW + cc * 3:PC_SW + (cc + 1) * 3] = sw[:, cc * 128:(cc + 1) * 128].T
        prm[l, :, PC_SUB] = np.tile(np.asarray(inp["diff_subln_g"][l], np.float32), 2)
        prm[l, :, PC_IW:PC_IW + 2] = iw
        wcat = np.concatenate([np.asarray(inp["router_group_w"][l], np.float32),
                               np.asarray(inp["router_expert_w"][l], np.float32)], axis=1)
        wr[l] = wcat.reshape(8, 128, 20).transpose(1, 0, 2)
        bcat = np.concatenate([np.asarray(inp["router_group_b"][l], np.float32),
                               np.asarray(inp["router_expert_b"][l], np.float32)])
        rb[l] = np.tile(bcat[None, :], (128, 1))
        for n_, key in enumerate(["diff_lam_q1", "diff_lam_k1", "diff_lam_q2", "diff_lam_k2"]):
            lamv[l, :, n_, :] = np.tile(np.asarray(inp[key][l], np.float32)[None, :], (128, 1))
        pwl = np.asarray(inp["pool_w"][l], np.float32)
        for g in range(4):
            cc, hh = g // 2, g % 2
            pw[l, hh * 64:(hh + 1) * 64, cc, hh * 64:(hh + 1) * 64] = pwl[g]
    shared.update({
        "prm": prm, "wr": wr, "rbias": rb, "lamv": lamv, "poolw": pw,
        "gfin": np.ascontiguousarray(np.tile(np.asarray(inp["final_norm"], np.float32)[None, :], (128, 1))),
    })
    for key in ["w_in", "w_out", "expert_w_gate", "expert_w_up", "expert_w_down", "ple_gate_w", "ple_gate_b",
                "ple_proj"]:
        shared[key] = np.ascontiguousarray(np.asarray(inp[key], np.float32))
    return shared


_NC_CACHE = {}


def _get_nc():
    if "nc" not in _NC_CACHE:
        nc = bass.Bass("TRN2", target_bir_lowering=False)
        build(nc)
        _NC_CACHE["nc"] = nc
    return _NC_CACHE["nc"]


def kernel(**inputs):
    shared = _layout_inputs(inputs)
    x = np.asarray(inputs["x"], np.float32)
    p = np.asarray(inputs["p"], np.float32)
    n = x.shape[0]
    in_maps = []
    for b in range(n):
        m = dict(shared)
        m["x"] = np.ascontiguousarray(x[b])
        m["p"] = np.ascontiguousarray(p[:, b])
        in_maps.append(m)
    nc = _get_nc()
    res = run_bass_kernel_spmd(nc, in_maps, core_ids=list(range(n)))
    return np.stack([np.asarray(r["out"], np.float32) for r in res.results], axis=0)
```

```python
import math
import contextlib
import numpy as np
import ml_dtypes
import concourse.bass as bass
import concourse.mybir as mybir
from concourse.bass_utils import run_bass_kernel_spmd

F32 = mybir.dt.float32
BF16 = mybir.dt.bfloat16
ALU = mybir.AluOpType
AF = mybir.ActivationFunctionType
AX = mybir.AxisListType

S = 4096
D = 1024
DEPTH = 2
NT = 8
TS = 512
INC = 2304
NE = 16
EPS = 1e-6
CK = 31
SCALE = 32 ** -0.5
SEM_CHUNK = 30000

PC_MIXG, PC_FFNG, PC_PLEG, PC_FING = 0, 8, 16, 24
PC_CW = 32
PC_CB = PC_CW + 62
PC_LNG = PC_CB + 2
PC_LNB = PC_LNG + 2
PC_PB = PC_LNB + 2
PC_PS = PC_PB + 2
PC_SW = PC_PS + 2
PC_SUB = PC_SW + 6
PC_IW = PC_SUB + 1
PC_N = PC_IW + 2


class Tok:
    __slots__ = ("sem", "val", "eng")

    def __init__(self, eng):
        self.sem = None
        self.val = None
        self.eng = eng


class Res:
    __slots__ = ("name", "w", "r", "excl")

    def __init__(self, name, excl=False):
        self.name = name
        self.w = None
        self.r = []
        self.excl = excl


class Prog:
    def __init__(self, nc, es):
        self.nc = nc
        self.es = es
        self.eobj = {"pe": nc.tensor, "act": nc.scalar, "dve": nc.vector,
                     "pool": nc.gpsimd, "sp": nc.sync}
        self.sems = {e: [] for e in self.eobj}
        self.count = {e: 0 for e in self.eobj}
        self.known = {e: {} for e in self.eobj}
        self.pending = {e: None for e in self.eobj}
        self.last_tok = {e: None for e in self.eobj}
        self.nsem = 0
        self.dma_sems = []
        self.dma_cnt = []
        self.dma_i = 0
        self.dma_ip = 0
        for i in range(24):
            self.dma_sems.append(self._new_sem("dq%d" % i))
            self.dma_cnt.append(0)
        self.dma_toks = []
        self.n_ops = 0
        self.n_waits = 0
        self.limit = None
        self.stores_on_pool = False
        self.n_all = 0
        self.skip = False

    def _skipping(self):
        if self.skip:
            return True
        if self.limit is not None and self.n_all > self.limit and all(v is None for v in self.pending.values()):
            self.skip = True
            return True
        return False

    def _new_sem(self, name):
        self.nsem += 1
        return self.es.enter_context(self.nc.semaphore(name))

    def _eng_tok(self, eng, tok):
        c = self.count[eng]
        idx = c // SEM_CHUNK
        while len(self.sems[eng]) <= idx:
            self.sems[eng].append(self._new_sem("%s%d" % (eng, len(self.sems[eng]))))
        tok.sem = self.sems[eng][idx]
        tok.val = c % SEM_CHUNK + 1
        self.count[eng] = c + 1
        return tok

    def _wait(self, eng, tok):
        if tok is None:
            return
        if tok.sem is None:
            assert tok.eng == eng, "dependency on unsignaled op of %s from %s" % (tok.eng, eng)
            return
        k = self.known[eng]
        sid = id(tok.sem)
        if k.get(sid, 0) >= tok.val:
            return
        k[sid] = tok.val
        self.eobj[eng].wait_ge(tok.sem, tok.val)
        self.n_waits += 1

    def _deps(self, eng, reads, writes, is_dma=False):
        for r in reads:
            if r.w is not None:
                if r.w.eng == eng and not is_dma and eng == "pe":
                    continue
                self._wait(eng, r.w)
        same_ok = (eng == "pe") and not is_dma
        for w in writes:
            if w.w is not None and not (same_ok and w.w.eng == eng):
                self._wait(eng, w.w)
            for t in w.r:
                if not (same_ok and t.eng == eng):
                    self._wait(eng, t)

    def op(self, eng, fn, reads=(), writes=(), signal=True):
        self.n_all += 1
        if self._skipping():
            return None
        if any(r.excl for r in reads):
            writes = list(writes) + [r for r in reads if r.excl and r not in writes]
            reads = [r for r in reads if not r.excl]
        self._deps(eng, reads, writes)
        ins = fn(self.eobj[eng])
        self.n_ops += 1
        tok = self.pending[eng]
        if tok is None:
            tok = Tok(eng)
            self.pending[eng] = tok
        if signal:
            self._eng_tok(eng, tok)
            ins.then_inc(tok.sem, 1)
            self.pending[eng] = None
            self.last_tok[eng] = tok
        for r in reads:
            r.r.append(tok)
        for w in writes:
            w.w = tok
            w.r = []
        return tok

    def dma(self, q, out, in_, reads=(), writes=()):
        if q == "sp" and self.stores_on_pool and str(out.space).endswith("DRAM"):
            q = "pool"
        self.n_all += 1
        if self._skipping():
            return None
        self._deps(q, reads, writes, is_dma=True)
        if q == "pool":
            i = 16 + self.dma_ip % 8
            self.dma_ip += 1
        else:
            i = self.dma_i % 16
            self.dma_i += 1
        sem = self.dma_sems[i]
        if self.dma_cnt[i] > 0:
            prev = Tok("dma")
            prev.sem = sem
            prev.val = self.dma_cnt[i]
            self._wait(q, prev)
        self.eobj[q].dma_start(out=out, in_=in_).then_inc(sem, 16)
        self.dma_cnt[i] += 16
        tok = Tok("dma")
        tok.sem = sem
        tok.val = self.dma_cnt[i]
        self.dma_toks.append(tok)
        for r in reads:
            r.r.append(tok)
        for w in writes:
            w.w = tok
            w.r = []
        return tok

    def barrier(self):
        toks = [t for t in self.last_tok.values() if t is not None]
        for i, s in enumerate(self.dma_sems):
            if self.dma_cnt[i] > 0:
                t = Tok("dma")
                t.sem = s
                t.val = self.dma_cnt[i]
                toks.append(t)
        for e in self.eobj:
            assert self.pending[e] is None
            for t in toks:
                if t.eng == e:
                    continue
                self._wait(e, t)

    def final_wait(self):
        for i, s in enumerate(self.dma_sems):
            if self.dma_cnt[i] > 0:
                t = Tok("dma")
                t.sem = s
                t.val = self.dma_cnt[i]
                self._wait("sp", t)


class Buf:
    def __init__(self, t, name, nslots=1):
        self.t = t
        self.res = [Res("%s.%d" % (name, i)) for i in range(nslots)]

    @property
    def r(self):
        return self.res[0]


def build(nc, dbg=None, limit=None):
    P = None
    with contextlib.ExitStack() as es:
        P = Prog(nc, es)
        P.limit = limit

        def dram_in(name, shape, dt=F32):
            return nc.dram_tensor(name, list(shape), dt, kind="ExternalInput").ap()

        def dram_scr(name, shape, dt, kind="Internal"):
            return nc.dram_tensor(name, list(shape), dt, kind=kind).ap()

        x_d = dram_in("x", [S, D])
        p_d = dram_in("p", [DEPTH, S, 256])
        w_in_d = dram_in("w_in", [DEPTH, D, INC])
        w_out_d = dram_in("w_out", [DEPTH, D, D])
        wg_d = dram_in("expert_w_gate", [DEPTH, NE, D, 256])
        wu_d = dram_in("expert_w_up", [DEPTH, NE, D, 256])
        wd_d = dram_in("expert_w_down", [DEPTH, NE, 256, D])
        pgw_d = dram_in("ple_gate_w", [DEPTH, D, D])
        pgb_d = dram_in("ple_gate_b", [DEPTH, D])
        ppj_d = dram_in("ple_proj", [DEPTH, 256, D])
        prm_d = dram_in("prm", [DEPTH, 128, PC_N])
        wr_d = dram_in("wr", [DEPTH, 128, 8, 20])
        rb_d = dram_in("rbias", [DEPTH, 128, 20])
        lam_d = dram_in("lamv", [DEPTH, 128, 4, 32])
        pw_d = dram_in("poolw", [DEPTH, 128, 2, 128])
        gfin_d = dram_in("gfin", [128, D])
        cidf_d = dram_in("c_identf", [128, 128])
        crp_d = dram_in("c_rperm", [128, 128])
        ctri_d = dram_in("c_tri", [128, 128])
        cb64_d = dram_in("c_blk64", [128, 128])
        cones_d = dram_in("c_ones256", [128, 128])
        csel_d = dram_in("c_sel", [16, NE, 128])
        ccos_d = dram_in("c_cos", [128, S])
        csin_d = dram_in("c_sin", [128, S])
        ccorr_d = dram_in("c_corr", [128, 2, 16])
        out_d = nc.dram_tensor("out", [S, D], F32, kind="ExternalOutput").ap()

        dkind = "ExternalOutput" if dbg else "Internal"
        h_d = dram_scr("h_scr", [S, D], F32, dkind)
        glu_d = dram_scr("glu_scr", [256, S], BF16, dkind)
        pin_d = dram_scr("pin_scr", [256, S], F32, dkind)
        q_d = dram_scr("q_scr", [256, S], BF16, dkind)
        k_d = dram_scr("k_scr", [256, S], BF16, dkind)
        v_d = dram_scr("v_scr", [S, 512], BF16, dkind)
        gb_d = dram_scr("gb_scr", [256, S], F32, dkind)
        gcv_d = dram_scr("gcv_scr", [256, S], F32, dkind)
        mix_d = dram_scr("mix_scr", [D, S], BF16, dkind)
        xT_d = dram_scr("xT_scr", [D, S], BF16, dkind)
        cmb_d = dram_scr("cmb_scr", [16, S], BF16, dkind)

        def tiles(name):
            return [Res("%s%d" % (name, i)) for i in range(NT)]
        R_h = tiles("h")
        R_glu, R_pin, R_q, R_k, R_v = tiles("glu"), tiles("pin"), tiles("q"), tiles("k"), tiles("v")
        R_gb, R_gcv, R_mixA, R_mixB, R_xT, R_cmb = (tiles("gb"), tiles("gcv"), tiles("mixA"),
                                                    tiles("mixB"), tiles("xT"), tiles("cmb"))
        R_out = tiles("out")

        def fm(ap, i, lo=0, hi=TS):
            return ap.rearrange("(c p) t -> p c t", p=128)[:, :, i * TS + lo:i * TS + hi]

        def tm(ap, i):
            return ap[i * TS:(i + 1) * TS, :].rearrange("(j p) f -> p j f", p=128)

        uid = [0]
        def sbuf(stack, name, shape, dt, nslots=1):
            uid[0] += 1
            t = stack.enter_context(nc.sbuf_tensor("%s_u%d" % (name, uid[0]), list(shape), dt))
            return Buf(t, name, nslots)

        def psum(stack, name, shape, dt=F32):
            t = stack.enter_context(nc.psum_tensor(name, list(shape), dt))
            b = Buf(t, name, 1)
            b.res[0].excl = True
            return b

        identf = sbuf(es, "identf", [128, 128], F32)
        identb = sbuf(es, "identb", [128, 128], BF16)
        rperm = sbuf(es, "rperm", [128, 128], F32)
        trib = sbuf(es, "trib", [128, 128], BF16)
        blk64 = sbuf(es, "blk64", [128, 128], F32)
        ones256 = sbuf(es, "ones256", [128, 128], F32)
        sel = sbuf(es, "sel", [16, NE, 128], BF16)
        prm_l = [sbuf(es, "prm_sb%d" % l_, [128, PC_N], F32) for l_ in range(DEPTH)]
        corr = sbuf(es, "corr", [128, 2, 16], F32)
        gfin = sbuf(es, "gfin_sb", [128, D], F32)
        neglam_l = [sbuf(es, "neglam%d" % l_, [128, 1], F32) for l_ in range(DEPTH)]
        pbs_l = [sbuf(es, "pbs%d" % l_, [128, 2], F32) for l_ in range(DEPTH)]
        prm, neglam, pbs = prm_l[0], neglam_l[0], pbs_l[0]
        onesrow = sbuf(es, "onesrow", [1, 128], BF16)
        epsc = sbuf(es, "epsc", [128, 1], F32)

        pf = []
        pb = []
        pf_i = [0]
        pb_i = [0]

        def set_psum(stack, nf, nb):
            uid[0] += 1
            pf[:] = [psum(stack, "pf%d_%d" % (i, uid[0]), [128, 512], F32) for i in range(nf)]
            pb[:] = [psum(stack, "pb%d_%d" % (i, uid[0]), [128, 1024], BF16) for i in range(nb)]

        def next_pf():
            b = pf[pf_i[0] % len(pf)]
            pf_i[0] += 1
            return b

        def next_pb():
            b = pb[pb_i[0] % len(pb)]
            pb_i[0] += 1
            return b

        P.dma("sp", identf.t[:], cidf_d, writes=[identf.r])
        P.dma("sp", rperm.t[:], crp_d, writes=[rperm.r])
        P.dma("sp", blk64.t[:], cb64_d, writes=[blk64.r])
        P.dma("sp", ones256.t[:], cones_d, writes=[ones256.r])
        P.dma("pool", sel.t[:], csel_d, writes=[sel.r])
        P.dma("sp", corr.t[:], ccorr_d, writes=[corr.r])
        P.dma("sp", gfin.t[:], gfin_d, writes=[gfin.r])
        P.dma("pool", identb.t[:], cidf_d, writes=[identb.r])
        P.dma("pool", trib.t[:], ctri_d, writes=[trib.r])
        P.op("dve", lambda e: e.memset(onesrow.t[:], 1.0), writes=[onesrow.r])
        P.op("dve", lambda e: e.memset(epsc.t[:], EPS), writes=[epsc.r])

        def norm_stats(st, ht, slot, ss, rstd, sqj):
            P.op("dve", lambda e: e.memset(ss.t[:], 0.0), writes=[ss.r])
            for j in range(4):
                P.op("act", lambda e, j=j: e.activation(out=sqj.t[:], in_=ht.t[:, j, :], func=AF.Square,
                                                        accum_out=ss.t[:, j:j + 1]),
                     reads=[ht.res[slot], ss.r], writes=[sqj.r, ss.r])
            P.op("dve", lambda e: e.tensor_scalar(out=rstd.t[:], in0=ss.t[:], scalar1=1.0 / D, scalar2=EPS,
                                                  op0=ALU.mult, op1=ALU.add), reads=[ss.r], writes=[rstd.r])
            P.op("act", lambda e: e.activation(out=rstd.t[:], in_=rstd.t[:], func=AF.Sqrt),
                 reads=[rstd.r], writes=[rstd.r])
            P.op("dve", lambda e: e.reciprocal(out=rstd.t[:], in_=rstd.t[:]), reads=[rstd.r], writes=[rstd.r])

        def norm_transpose(ht, slot, rstd, xn, xT, gcol, part=None):
            for j in range(4 if part in (None, 0) else 0):
                P.op("dve", lambda e, j=j: e.tensor_scalar(out=xn.t[:, j, :], in0=ht.t[:, j, :],
                                                           scalar1=rstd.t[:, j:j + 1], scalar2=None, op0=ALU.mult),
                     reads=[ht.res[slot], rstd.r], writes=[xn.res[j]])
            for c2 in range(4 if part in (None, 1) else 0):
                bank = next_pb()
                for cc in range(2):
                    c = c2 * 2 + cc
                    for j in range(4):
                        last = (cc == 1 and j == 3)
                        P.op("pe", lambda e, c=c, cc=cc, j=j: e.transpose(
                            bank.t[:, cc * 512 + j * 128: cc * 512 + (j + 1) * 128],
                            xn.t[:, j, c * 128:(c + 1) * 128], identb.t[:]),
                            reads=[xn.res[j], identb.r], writes=[bank.r], signal=last)
                for cc in range(2):
                    c = c2 * 2 + cc
                    if cc == 0:
                        P.op("act", lambda e, c=c, cc=cc: e.activation(
                            out=xT.t[:, c, :], in_=bank.t[:, cc * 512:(cc + 1) * 512], func=AF.Identity,
                            scale=prm.t[:, gcol + c:gcol + c + 1]),
                            reads=[bank.r, prm.r], writes=[xT.res[c]])
                    else:
                        P.op("dve", lambda e, c=c, cc=cc: e.tensor_scalar(
                            out=xT.t[:, c, :], in0=bank.t[:, cc * 512:(cc + 1) * 512],
                            scalar1=prm.t[:, gcol + c:gcol + c + 1], scalar2=None, op0=ALU.mult),
                            reads=[bank.r, prm.r], writes=[xT.res[c]])

        def load_h(i, ht, slot, src):
            P.dma("sp", ht.t[:], tm(src, i), reads=[R_h[i]], writes=[ht.res[slot]])

        def mm_group(bank, cols, lhs_list, rhs_list, reads, tile_position=None):
            n = len(lhs_list)
            for k in range(n):
                P.op("pe", lambda e, k=k: e.matmul(bank.t[:, cols[0]:cols[1]], lhs_list[k], rhs_list[k],
                                                   start=(k == 0), stop=(k == n - 1)),
                     reads=reads[k], writes=[bank.r], signal=(k == n - 1))

        def load_w_in(l_):
            stw = contextlib.ExitStack()
            uid[0] += 1
            t_ = stw.enter_context(nc.sbuf_tensor("w_in_sb_u%d" % uid[0], [128, 8, INC], BF16, side="right"))
            b_ = Buf(t_, "w_in_sb", 1)
            for c in range(8):
                P.dma("pool", b_.t[:, c, :], w_in_d[l_, c * 128:(c + 1) * 128, :], writes=[b_.r])
            return stw, b_

        st_win, w_in_next = load_w_in(0)
        for l in range(DEPTH):
            lam_init = 0.8 - 0.6 * math.exp(-0.3 * l)
            prm, neglam, pbs = prm_l[l], neglam_l[l], pbs_l[l]
            P.dma("sp", prm.t[:], prm_d[l], writes=[prm.r])
            if True:
                st = es
                lamv = sbuf(st, "lamv%d" % l, [128, 4, 32], F32)
                lt = sbuf(st, "lt%d" % l, [128, 2, 32], F32)
                ls = sbuf(st, "ls%d" % l, [128, 2], F32)
                P.dma("sp", lamv.t[:], lam_d[l], writes=[lamv.r])
                P.op("dve", lambda e: e.tensor_tensor(out=lt.t[:, 0, :], in0=lamv.t[:, 0, :], in1=lamv.t[:, 1, :],
                                                      op=ALU.mult), reads=[lamv.r], writes=[lt.r])
                P.op("dve", lambda e: e.tensor_tensor(out=lt.t[:, 1, :], in0=lamv.t[:, 2, :], in1=lamv.t[:, 3, :],
                                                      op=ALU.mult), reads=[lamv.r, lt.r], writes=[lt.r])
                P.op("dve", lambda e: e.reduce_sum(out=ls.t[:], in_=lt.t[:], axis=AX.X), reads=[lt.r], writes=[ls.r])
                P.op("act", lambda e: e.activation(out=ls.t[:], in_=ls.t[:], func=AF.Exp), reads=[ls.r], writes=[ls.r])
                P.op("dve", lambda e: e.scalar_tensor_tensor(out=neglam.t[:], in0=ls.t[:, 1:2], scalar=-lam_init,
                                                             in1=ls.t[:, 0:1], op0=ALU.add, op1=ALU.subtract),
                     reads=[ls.r], writes=[neglam.r])
                P.op("dve", lambda e: e.tensor_tensor(out=pbs.t[:], in0=prm.t[:, PC_PB:PC_PB + 2],
                                                      in1=prm.t[:, PC_PS:PC_PS + 2], op=ALU.mult),
                     reads=[prm.r], writes=[pbs.r])

        for l in range(DEPTH):
            lam_init = 0.8 - 0.6 * math.exp(-0.3 * l)
            h_src = x_d if l == 0 else h_d

            prm, neglam, pbs = prm_l[l], neglam_l[l], pbs_l[l]
            st_dg = contextlib.ExitStack()
            dg = sbuf(st_dg, "s2_dg", [128, 2, CK, 128], BF16)
            pw_sb = sbuf(st_dg, "s2_pw", [128, 2, 128], BF16)
            with contextlib.ExitStack() as st:
                set_psum(st, 6, 2)
                w_in_sb = w_in_next
                ht = sbuf(st, "s1_ht", [128, 4, D], F32, 2)
                hts = [ht, sbuf(st, "s1_ht2", [128, 4, D], F32, 2)]
                ss2 = [sbuf(st, "s1_ss%d" % k, [128, 4], F32) for k in range(2)]
                rstd2 = [sbuf(st, "s1_rstd%d" % k, [128, 4], F32) for k in range(2)]
                sqj = sbuf(st, "s1_sqj", [128, D], BF16)
                xn2 = [sbuf(st, "s1_xn%d" % k, [128, 4, D], BF16, 4) for k in range(2)]
                nT2 = [sbuf(st, "s1_nT%d" % k, [128, 8, TS], BF16, 8) for k in range(2)]
                sig = sbuf(st, "s1_sig", [128, TS], F32)
                gcs = sbuf(st, "s1_gc", [128, TS], F32)
                glu_st = sbuf(st, "s1_glu", [128, 2, TS], BF16)
                pin_st = sbuf(st, "s1_pin", [128, 2, TS], F32)
                qk_st = sbuf(st, "s1_qk", [128, 4, TS], F32, 4)
                qkr_st = sbuf(st, "s1_qkr", [128, 4, TS], BF16, 4)
                gb_st = sbuf(st, "s1_gb", [128, 2, TS], F32)
                gcv_st = sbuf(st, "s1_gcv", [128, 2, TS], F32)
                v_st = sbuf(st, "s1_v", [128, 4, 512], BF16)
                cos_t = sbuf(st, "s1_cos", [128, TS], F32)
                sin_t = sbuf(st, "s1_sin", [128, TS], F32)
                t1 = sbuf(st, "s1_t1", [128, TS], F32)
                t2 = sbuf(st, "s1_t2", [128, TS], F32)

                P.op("dve", lambda e: e.memset(v_st.t[:], 1.0), writes=[v_st.r])

                def s1_prologue(i_):
                    norm_stats(st, hts[i_ % 2], 0, ss2[i_ % 2], rstd2[i_ % 2], sqj)
                    norm_transpose(hts[i_ % 2], 0, rstd2[i_ % 2], xn2[i_ % 2], nT2[i_ % 2], PC_MIXG)

                P.dma("sp", hts[0].t[:], tm(h_src, 0), reads=[R_h[0]], writes=[hts[0].r])
                P.dma("sp", hts[1].t[:], tm(h_src, 1), reads=[R_h[1]], writes=[hts[1].r])
                s1_prologue(0)
                P.dma("pool", pw_sb.t[:], pw_d[l], writes=[pw_sb.r])
                for cc in range(2):
                    for j in range(CK):
                        col = PC_CW + cc * CK + j
                        P.op("dve", lambda e, cc=cc, j=j, col=col: e.tensor_scalar(
                            out=dg.t[:, cc, j, :], in0=identf.t[:], scalar1=prm.t[:, col:col + 1], scalar2=None,
                            op0=ALU.mult), reads=[identf.r, prm.r], writes=[dg.r])
                for i in range(NT):
                    P.dma("sp", cos_t.t[:], ccos_d[:, i * TS:(i + 1) * TS], writes=[cos_t.r])
                    P.dma("sp", sin_t.t[:], csin_d[:, i * TS:(i + 1) * TS], writes=[sin_t.r])
                    nT = nT2[i % 2]

                    def proj(col0):
                        bank = next_pf()
                        mm_group(bank, (0, TS), [w_in_sb.t[:, c, col0:col0 + 128] for c in range(8)],
                                 [nT.t[:, c, :] for c in range(8)],
                                 [[w_in_sb.r, nT.res[c]] for c in range(8)])
                        return bank

                    for cc in range(2):
                        bg_ = proj(256 + cc * 128)
                        P.op("act", lambda e: e.activation(out=sig.t[:], in_=bg_.t[:], func=AF.Sigmoid),
                             reads=[bg_.r], writes=[sig.r])
                        bv_ = proj(0 + cc * 128)
                        P.op("dve", lambda e: e.tensor_tensor(out=glu_st.t[:, cc, :], in0=bv_.t[:], in1=sig.t[:],
                                                              op=ALU.mult),
                             reads=[bv_.r, sig.r], writes=[glu_st.r])
                    for cc in range(2):
                        bp_ = proj(512 + cc * 128)
                        P.op("act", lambda e: e.copy(out=pin_st.t[:, cc, :], in_=bp_.t[:]),
                             reads=[bp_.r], writes=[pin_st.r])
                    if i + 1 < NT:
                        s1_prologue(i + 1)
                    for m in range(4):
                        bq_ = proj(768 + m * 128)
                        if m % 2 == 0:
                            P.op("act", lambda e: e.copy(out=qk_st.t[:, m, :], in_=bq_.t[:]),
                                 reads=[bq_.r], writes=[qk_st.res[m]])
                        else:
                            P.op("dve", lambda e: e.tensor_copy(out=qk_st.t[:, m, :], in_=bq_.t[:]),
                                 reads=[bq_.r], writes=[qk_st.res[m]])
                    for cc in range(2):
                        bb_ = proj(1536 + cc * 128)
                        P.op("act", lambda e: e.copy(out=gb_st.t[:, cc, :], in_=bb_.t[:]),
                             reads=[bb_.r], writes=[gb_st.r])
                    for cc in range(2):
                        bc_ = proj(1792 + cc * 128)
                        P.op("act", lambda e: e.copy(out=gcs.t[:], in_=bc_.t[:]), reads=[bc_.r], writes=[gcs.r])
                        bs_ = proj(2048 + cc * 128)
                        P.op("dve", lambda e: e.tensor_tensor(out=gcv_st.t[:, cc, :], in0=bs_.t[:], in1=gcs.t[:],
                                                              op=ALU.mult),
                             reads=[bs_.r, gcs.r], writes=[gcv_st.r])
                    for j in range(4):
                        bank = next_pf()
                        mm_group(bank, (0, 256), [nT.t[:, c, j * 128:(j + 1) * 128] for c in range(8)],
                                 [w_in_sb.t[:, c, 1280:1536] for c in range(8)],
                                 [[w_in_sb.r, nT.res[c]] for c in range(8)])
                        for hh in range(4):
                            off = hh * 128 + (0 if hh % 2 == 0 else 64)
                            eng = "act" if hh % 2 == 0 else "dve"
                            if eng == "act":
                                P.op("act", lambda e: e.copy(out=v_st.t[:, j, off:off + 64],
                                                             in_=bank.t[:, hh * 64:(hh + 1) * 64]),
                                     reads=[bank.r], writes=[v_st.r])
                            else:
                                P.op("dve", lambda e: e.tensor_copy(out=v_st.t[:, j, off:off + 64],
                                                                    in_=bank.t[:, hh * 64:(hh + 1) * 64]),
                                     reads=[bank.r], writes=[v_st.r])
                    for m in range(4):
                        bank = next_pf()
                        P.op("pe", lambda e: e.matmul(bank.t[:], rperm.t[:], qk_st.t[:, m, :], start=True, stop=True),
                             reads=[rperm.r, qk_st.res[m]], writes=[bank.r])
                        P.op("dve", lambda e: e.tensor_tensor(out=t1.t[:], in0=qk_st.t[:, m, :], in1=cos_t.t[:],
                                                              op=ALU.mult),
                             reads=[qk_st.res[m], cos_t.r], writes=[t1.r])
                        P.op("dve", lambda e: e.tensor_tensor(out=t2.t[:], in0=bank.t[:], in1=sin_t.t[:],
                                                              op=ALU.mult),
                             reads=[bank.r, sin_t.r], writes=[t2.r])
                        P.op("dve", lambda e: e.tensor_tensor(out=qkr_st.t[:, m, :], in0=t1.t[:], in1=t2.t[:],
                                                              op=ALU.add),
                             reads=[t1.r, t2.r], writes=[qkr_st.res[m]])
                    if i + 2 < NT:
                        P.dma("sp", hts[i % 2].t[:], tm(h_src, i + 2), reads=[R_h[i + 2]], writes=[hts[i % 2].r])
                    P.dma("sp", fm(glu_d, i), glu_st.t[:], reads=[glu_st.r], writes=[R_glu[i]])
                    P.dma("sp", fm(pin_d, i), pin_st.t[:], reads=[pin_st.r], writes=[R_pin[i]])
                    P.dma("sp", fm(q_d, i), qkr_st.t[:, 0:2, :], reads=[qkr_st.res[0], qkr_st.res[1]], writes=[R_q[i]])
                    P.dma("sp", fm(k_d, i), qkr_st.t[:, 2:4, :], reads=[qkr_st.res[2], qkr_st.res[3]], writes=[R_k[i]])
                    P.dma("sp", tm(v_d, i), v_st.t[:], reads=[v_st.r], writes=[R_v[i]])
                    P.dma("sp", fm(gb_d, i), gb_st.t[:], reads=[gb_st.r], writes=[R_gb[i]])
                    P.dma("sp", fm(gcv_d, i), gcv_st.t[:], reads=[gcv_st.r], writes=[R_gcv[i]])
                P.barrier()
            st_win.close()
            if dbg == "s1":
                st_dg.close()
                break

            st_wout = contextlib.ExitStack()
            uid[0] += 1
            w_out_sb = Buf(st_wout.enter_context(nc.sbuf_tensor("w_out_sb_u%d" % uid[0], [128, 8, D], BF16,
                                                                side="right")), "w_out_sb", 1)
            st_kv = contextlib.ExitStack()
            kT = sbuf(st_kv, "at_kT", [128, 2, S], BF16)
            Vs = sbuf(st_kv, "at_V", [128, 32, 512], BF16)
            with contextlib.ExitStack() as st:
                set_psum(st, 8, 0)
                glu_in = [sbuf(st, "s2_glu%d" % k, [128, 2, 30 + TS], BF16) for k in range(2)]
                pin_in = [sbuf(st, "s2_pin%d" % k, [128, 2, 16 + TS], F32) for k in range(2)]
                gcv_in = [sbuf(st, "s2_gcv%d" % k, [128, 2, 2 + TS], F32) for k in range(2)]
                gb_in = [sbuf(st, "s2_gb%d" % k, [128, 2, TS], F32) for k in range(2)]
                yc = sbuf(st, "s2_y", [128, 2, TS], F32, 2)
                ysq = sbuf(st, "s2_ysq", [128, 2, TS], F32, 2)
                m2 = sbuf(st, "s2_m2", [128, TS], F32)
                var = sbuf(st, "s2_var", [128, TS], F32)
                dd = sbuf(st, "s2_dd", [128, TS], F32)
                sA = sbuf(st, "s2_sA", [128, 16 + TS], F32)
                sB = sbuf(st, "s2_sB", [128, 16 + TS], F32)
                pooled = sbuf(st, "s2_pooled", [128, TS], BF16)
                acc3 = sbuf(st, "s2_acc3", [128, TS], F32)
                mixA = [sbuf(st, "s2_mixA%d" % k, [128, 6, TS], BF16) for k in range(2)]


                def s2_load(i):
                    k = i % 2
                    if i == 0:
                        P.op("dve", lambda e: e.memset(glu_in[k].t[:, :, 0:30], 0.0), writes=[glu_in[k].r])
                        P.op("dve", lambda e: e.memset(pin_in[k].t[:, :, 0:16], 0.0), writes=[pin_in[k].r])
                        P.op("dve", lambda e: e.memset(gcv_in[k].t[:, :, 0:2], 0.0), writes=[gcv_in[k].r])
                        P.dma("sp", glu_in[k].t[:, :, 30:30 + TS], fm(glu_d, 0), reads=[R_glu[0]], writes=[glu_in[k].r])
                        P.dma("sp", pin_in[k].t[:, :, 16:16 + TS], fm(pin_d, 0), reads=[R_pin[0]], writes=[pin_in[k].r])
                        P.dma("sp", gcv_in[k].t[:, :, 2:2 + TS], fm(gcv_d, 0), reads=[R_gcv[0]], writes=[gcv_in[k].r])
                    else:
                        P.dma("sp", glu_in[k].t[:], fm(glu_d, i, -30, TS), reads=[R_glu[i - 1], R_glu[i]],
                              writes=[glu_in[k].r])
                        P.dma("sp", pin_in[k].t[:], fm(pin_d, i, -16, TS), reads=[R_pin[i - 1], R_pin[i]],
                              writes=[pin_in[k].r])
                        P.dma("sp", gcv_in[k].t[:], fm(gcv_d, i, -2, TS), reads=[R_gcv[i - 1], R_gcv[i]],
                              writes=[gcv_in[k].r])
                    P.dma("sp", gb_in[k].t[:], fm(gb_d, i), reads=[R_gb[i]], writes=[gb_in[k].r])

                s2_load(0)
                s2_load(1)
                for c in range(8):
                    P.dma("pool", w_out_sb.t[:, c, :], w_out_d[l, c * 128:(c + 1) * 128, :], writes=[w_out_sb.r])
                P.dma("sp", kT.t[:], k_d.rearrange("(c p) t -> p c t", p=128), reads=R_k, writes=[kT.r])
                for i8 in range(NT):
                    P.dma("sp", Vs.t[:, i8 * 4:(i8 + 1) * 4, :], tm(v_d, i8), reads=[R_v[i8]], writes=[Vs.r])
                for i in range(NT):
                    k = i % 2
                    if 1 <= i and i + 1 < NT:
                        s2_load(i + 1)
                    mx = mixA[k]
                    for cc in range(2):
                        bank = next_pf()
                        mm_group(bank, (0, TS), [dg.t[:, cc, j, :] for j in range(CK)],
                                 [glu_in[k].t[:, cc, j:j + TS] for j in range(CK)],
                                 [[dg.r, glu_in[k].r]] * CK)
                        P.op("act", lambda e: e.activation(out=yc.t[:, cc, :], in_=bank.t[:], func=AF.Identity,
                                                           bias=prm.t[:, PC_CB + cc:PC_CB + cc + 1]),
                             reads=[bank.r, prm.r], writes=[yc.res[cc]])
                        P.op("act", lambda e: e.activation(out=ysq.t[:, cc, :], in_=bank.t[:], func=AF.Square,
                                                           bias=prm.t[:, PC_CB + cc:PC_CB + cc + 1]),
                             reads=[bank.r, prm.r], writes=[ysq.res[cc]])
                    bm = next_pf()
                    mm_group(bm, (0, TS), [ones256.t[:], ones256.t[:]], [yc.t[:, 0, :], yc.t[:, 1, :]],
                             [[ones256.r, yc.res[0]], [ones256.r, yc.res[1]]])
                    bq = next_pf()
                    mm_group(bq, (0, TS), [ones256.t[:], ones256.t[:]], [ysq.t[:, 0, :], ysq.t[:, 1, :]],
                             [[ones256.r, ysq.res[0]], [ones256.r, ysq.res[1]]])
                    for cc in range(2):
                        u = pin_in[k].t[:, cc, :]
                        W_ = 16 + TS
                        P.op("dve", lambda e: e.memset(sA.t[:, 0:1], 0.0), writes=[sA.r])
                        P.op("dve", lambda e: e.tensor_tensor(out=sA.t[:, 1:W_], in0=pin_in[k].t[:, cc, 1:W_],
                                                              in1=pin_in[k].t[:, cc, 0:W_ - 1], op=ALU.add),
                             reads=[pin_in[k].r], writes=[sA.r])
                        if cc == 0:
                            P.op("dve", lambda e: e.tensor_tensor(out=sB.t[64:128, 3:W_], in0=sA.t[64:128, 3:W_],
                                                                  in1=sA.t[64:128, 1:W_ - 2], op=ALU.add),
                                 reads=[sA.r], writes=[sB.r])
                            P.op("dve", lambda e: e.tensor_copy(out=sB.t[0:64, 3:W_], in_=sA.t[0:64, 3:W_]),
                                 reads=[sA.r, sB.r], writes=[sB.r])
                            fin = sB
                        else:
                            P.op("dve", lambda e: e.tensor_tensor(out=sB.t[:, 3:W_], in0=sA.t[:, 3:W_],
                                                                  in1=sA.t[:, 1:W_ - 2], op=ALU.add),
                                 reads=[sA.r], writes=[sB.r])
                            P.op("dve", lambda e: e.tensor_tensor(out=sA.t[:, 7:W_], in0=sB.t[:, 7:W_],
                                                                  in1=sB.t[:, 3:W_ - 4], op=ALU.add),
                                 reads=[sB.r, sA.r], writes=[sA.r])
                            P.op("dve", lambda e: e.tensor_tensor(out=sB.t[64:128, 15:W_], in0=sA.t[64:128, 15:W_],
                                                                  in1=sA.t[64:128, 7:W_ - 8], op=ALU.add),
                                 reads=[sA.r, sB.r], writes=[sB.r])
                            P.op("dve", lambda e: e.tensor_copy(out=sB.t[0:64, 15:W_], in_=sA.t[0:64, 15:W_]),
                                 reads=[sA.r, sB.r], writes=[sB.r])
                            fin = sB
                        if i == 0:
                            P.op("dve", lambda e: e.tensor_tensor(out=fin.t[:, 16:32], in0=fin.t[:, 16:32],
                                                                  in1=corr.t[:, cc, :], op=ALU.mult),
                                 reads=[fin.r, corr.r], writes=[fin.r])
                        P.op("dve", lambda e: e.scalar_tensor_tensor(
                            out=pooled.t[:], in0=fin.t[:, 16:16 + TS], scalar=prm.t[:, PC_IW + cc:PC_IW + cc + 1],
                            in1=pin_in[k].t[:, cc, 16:16 + TS], op0=ALU.mult, op1=ALU.subtract),
                            reads=[fin.r, prm.r, pin_in[k].r], writes=[pooled.r])
                        bank = next_pf()
                        P.op("pe", lambda e: e.matmul(bank.t[:], pw_sb.t[:, cc, :], pooled.t[:], start=True, stop=True),
                             reads=[pw_sb.r, pooled.r], writes=[bank.r])
                        P.op("act", lambda e: e.activation(out=mx.t[:, 2 + cc, :], in_=bank.t[:], func=AF.Identity,
                                                           scale=prm.t[:, PC_PS + cc:PC_PS + cc + 1],
                                                           bias=pbs.t[:, cc:cc + 1]),
                             reads=[bank.r, prm.r, pbs.r], writes=[mx.r])
                    for cc in range(2):
                        g_ = gcv_in[k]
                        P.op("dve", lambda e: e.tensor_scalar(out=acc3.t[:], in0=g_.t[:, cc, 0:TS],
                                                              scalar1=prm.t[:, PC_SW + cc * 3:PC_SW + cc * 3 + 1],
                                                              scalar2=None, op0=ALU.mult),
                             reads=[g_.r, prm.r], writes=[acc3.r])
                        for j in (1, 2):
                            P.op("dve", lambda e, j=j: e.scalar_tensor_tensor(
                                out=acc3.t[:], in0=g_.t[:, cc, j:j + TS],
                                scalar=prm.t[:, PC_SW + cc * 3 + j:PC_SW + cc * 3 + j + 1], in1=acc3.t[:],
                                op0=ALU.mult, op1=ALU.add), reads=[g_.r, prm.r, acc3.r], writes=[acc3.r])
                        P.op("dve", lambda e: e.tensor_tensor(out=mx.t[:, 4 + cc, :], in0=acc3.t[:],
                                                              in1=gb_in[k].t[:, cc, :], op=ALU.mult),
                             reads=[acc3.r, gb_in[k].r], writes=[mx.r])
                    P.op("act", lambda e: e.activation(out=m2.t[:], in_=bm.t[:], func=AF.Square),
                         reads=[bm.r], writes=[m2.r])
                    P.op("dve", lambda e: e.tensor_tensor(out=var.t[:], in0=bq.t[:], in1=m2.t[:], op=ALU.subtract),
                         reads=[bq.r, m2.r], writes=[var.r])
                    P.op("dve", lambda e: e.tensor_scalar(out=var.t[:], in0=var.t[:], scalar1=0.0, scalar2=EPS,
                                                          op0=ALU.max, op1=ALU.add), reads=[var.r], writes=[var.r])
                    P.op("act", lambda e: e.activation(out=var.t[:], in_=var.t[:], func=AF.Ln),
                         reads=[var.r], writes=[var.r])
                    P.op("act", lambda e: e.activation(out=var.t[:], in_=var.t[:], func=AF.Exp, scale=-0.5),
                         reads=[var.r], writes=[var.r])
                    for cc in range(2):
                        P.op("dve", lambda e: e.tensor_tensor(out=dd.t[:], in0=bm.t[:], in1=yc.t[:, cc, :],
                                                              op=ALU.subtract),
                             reads=[bm.r, yc.res[cc]], writes=[dd.r])
                        P.op("dve", lambda e: e.tensor_tensor(out=dd.t[:], in0=dd.t[:], in1=var.t[:], op=ALU.mult),
                             reads=[dd.r, var.r], writes=[dd.r])
                        P.op("dve", lambda e: e.tensor_scalar(out=dd.t[:], in0=dd.t[:],
                                                              scalar1=prm.t[:, PC_LNG + cc:PC_LNG + cc + 1],
                                                              scalar2=-1.0, op0=ALU.mult, op1=ALU.mult),
                             reads=[dd.r, prm.r], writes=[dd.r])
                        P.op("act", lambda e: e.activation(out=mx.t[:, cc, :], in_=dd.t[:], func=AF.Silu,
                                                           bias=prm.t[:, PC_LNB + cc:PC_LNB + cc + 1]),
                             reads=[dd.r, prm.r], writes=[mx.r])
                    mv = mix_d.rearrange("(c p) t -> p c t", p=128)
                    P.dma("sp", mv[:, 0:4, i * TS:(i + 1) * TS], mx.t[:, 0:4, :], reads=[mx.r], writes=[R_mixA[i]])
                    P.dma("sp", mv[:, 6:8, i * TS:(i + 1) * TS], mx.t[:, 4:6, :], reads=[mx.r], writes=[R_mixA[i]])
                P.barrier()
            if dbg == "s2a":
                st_kv.close()
                st_wout.close()
                st_dg.close()
                break

            with contextlib.ExitStack() as st:
                qTs = [sbuf(st, "at_q%d" % k, [128, 2, TS], BF16) for k in range(2)]
                pts = [sbuf(st, "at_pt%d" % k, [128, TS], BF16) for k in range(4)]
                rz = [sbuf(st, "at_rz%d" % k, [128, TS], F32) for k in range(2)]
                o12 = [sbuf(st, "at_o%d" % k, [128, TS], F32) for k in range(2)]
                od = sbuf(st, "at_od", [128, TS], F32)
                osq = sbuf(st, "at_osq", [128, TS], F32)
                rs = sbuf(st, "at_rs", [128, TS], F32)
                mixB = [sbuf(st, "at_mix%d" % k, [128, 2, TS], BF16) for k in range(2)]
                set_psum(st, 4, 0)
                accb = pf[0:4]
                sc2 = [psum(st, "sc2_%d_%d" % (k, uid[0]), [128, 2 * TS], F32) for k in range(2)]
                pt2 = [sbuf(st, "at_pt2_%d" % k, [128, 2 * TS], BF16) for k in range(4)]
                P.dma("sp", qTs[0].t[:], fm(q_d, 0), reads=[R_q[0]], writes=[qTs[0].r])
                mv = mix_d.rearrange("(c p) t -> p c t", p=128)

                groups = []
                for i in range(NT):
                    nkt = 4 * i + 4
                    for hp in range(2):
                        for kt in range(nkt):
                            groups.append((i, hp, kt, nkt))

                def front(g):
                    i, hp, kt, nkt = groups[g]
                    if hp == 0 and kt == 0 and i + 1 < NT:
                        P.dma("sp", qTs[(i + 1) % 2].t[:], fm(q_d, i + 1), reads=[R_q[i + 1]],
                              writes=[qTs[(i + 1) % 2].r])
                    qT = qTs[i % 2]
                    jd = kt - 4 * i
                    qs = 128 * jd if jd > 0 else 0
                    n = TS - qs
                    for s_ in range(4):
                        po = s_ * 32
                        sc = sc2[s_ // 2]
                        o_ = (s_ % 2) * TS
                        P.op("pe", lambda e: e.matmul(sc.t[:, o_:o_ + n], kT.t[po:po + 32, hp, kt * 128:(kt + 1) * 128],
                                                      qT.t[po:po + 32, hp, qs:TS], start=True, stop=True,
                                                      tile_position=(po, 0)),
                             reads=[kT.r, qT.r], writes=[sc.r])
                    for pr in range(2):
                        sc = sc2[pr]
                        pt = pt2[(g % 2) * 2 + pr]
                        P.op("act", lambda e: e.activation(
                            out=pt.t[:].rearrange("p (a b) -> p a b", a=2)[:, :, 0:n],
                            in_=sc.t[:].rearrange("p (a b) -> p a b", a=2)[:, :, 0:n], func=AF.Exp, scale=SCALE),
                            reads=[sc.r], writes=[pt.r])
                        if jd >= 0:
                            P.op("dve", lambda e: e.tensor_tensor(
                                out=pt.t[:].rearrange("p (a b) -> p a b", a=2)[:, :, 0:128],
                                in0=pt.t[:].rearrange("p (a b) -> p a b", a=2)[:, :, 0:128],
                                in1=trib.t[:].unsqueeze(1).to_broadcast([128, 2, 128]), op=ALU.mult),
                                reads=[pt.r, trib.r], writes=[pt.r])

                def back(g):
                    i, hp, kt, nkt = groups[g]
                    jd = kt - 4 * i
                    qs = 128 * jd if jd > 0 else 0
                    n = TS - qs
                    for s_ in range(4):
                        h = 2 * hp + s_ // 2
                        pt = pt2[(g % 2) * 2 + s_ // 2]
                        o_ = (s_ % 2) * TS
                        acc = accb[s_]
                        P.op("pe", lambda e: e.matmul(acc.t[:, qs:TS], Vs.t[:, kt, h * 128:(h + 1) * 128],
                                                      pt.t[:, o_:o_ + n], start=(kt == 0), stop=(kt == nkt - 1)),
                             reads=[Vs.r, pt.r], writes=[acc.r])
                    if kt == nkt - 1:
                        finalize(i, 2 * hp)
                        finalize(i, 2 * hp + 1)

                def finalize(i, h):
                    ch = h // 2
                    mb = mixB[i % 2]
                    lo, hi = (0, 64) if h % 2 == 0 else (64, 128)
                    zlo, zhi = (64, 128) if h % 2 == 0 else (0, 64)
                    accs = [accb[(h % 2) * 2], accb[(h % 2) * 2 + 1]]
                    for comp in range(2):
                        P.op("act", lambda e: e.activation(out=rz[comp].t[zlo:zhi, :], in_=accs[comp].t[zlo:zhi, :],
                                                           func=AF.Ln),
                             reads=[accs[comp].r], writes=[rz[comp].r])
                        P.op("act", lambda e: e.activation(out=rz[comp].t[zlo:zhi, :], in_=rz[comp].t[zlo:zhi, :],
                                                           func=AF.Exp, scale=-1.0),
                             reads=[rz[comp].r], writes=[rz[comp].r])
                        P.op("dve", lambda e: e.tensor_tensor(out=o12[comp].t[lo:hi, :], in0=accs[comp].t[lo:hi, :],
                                                              in1=rz[comp].t[zlo:zhi, :], op=ALU.mult),
                             reads=[accs[comp].r, rz[comp].r], writes=[o12[comp].r])
                    P.op("dve", lambda e: e.scalar_tensor_tensor(out=od.t[lo:hi, :], in0=o12[1].t[lo:hi, :],
                                                                 scalar=neglam.t[lo:hi, 0:1], in1=o12[0].t[lo:hi, :],
                                                                 op0=ALU.mult, op1=ALU.add),
                         reads=[o12[0].r, o12[1].r, neglam.r], writes=[od.r])
                    if h % 2 == 1:
                        P.op("act", lambda e: e.activation(out=osq.t[:], in_=od.t[:], func=AF.Square),
                             reads=[od.r], writes=[osq.r])
                        bms = sc2[1]
                        P.op("pe", lambda e: e.matmul(bms.t[:, TS:2 * TS], blk64.t[:], osq.t[:], start=True, stop=True),
                             reads=[blk64.r, osq.r], writes=[bms.r])
                        P.op("act", lambda e: e.activation(out=rs.t[:], in_=bms.t[:, TS:2 * TS], func=AF.Ln,
                                                           bias=epsc.t[:, 0:1]),
                             reads=[bms.r, epsc.r], writes=[rs.r])
                        P.op("act", lambda e: e.activation(out=rs.t[:], in_=rs.t[:], func=AF.Exp, scale=-0.5),
                             reads=[rs.r], writes=[rs.r])
                        P.op("dve", lambda e: e.tensor_tensor(out=rs.t[:], in0=rs.t[:], in1=od.t[:], op=ALU.mult),
                             reads=[rs.r, od.r], writes=[rs.r])
                        P.op("dve", lambda e: e.tensor_scalar(out=mb.t[:, ch, :], in0=rs.t[:],
                                                              scalar1=prm.t[:, PC_SUB:PC_SUB + 1],
                                                              scalar2=1.0 - lam_init, op0=ALU.mult, op1=ALU.mult),
                             reads=[rs.r, prm.r], writes=[mb.r])
                    if h == 3:
                        P.dma("sp", mv[:, 4:6, i * TS:(i + 1) * TS], mb.t[:], reads=[mb.r], writes=[R_mixB[i]])

                LA = 1
                for g in range(len(groups) + LA):
                    if g < len(groups):
                        front(g)
                    if g >= LA:
                        back(g - LA)
                P.barrier()
            st_kv.close()
            st_dg.close()
            if dbg == "s2c":
                st_wout.close()
                break

            st_ple = contextlib.ExitStack()
            wpg = sbuf(st_ple, "s5_wpg", [128, 8, D], BF16)
            wpp = sbuf(st_ple, "s5_wpp", [128, 2, D], BF16)
            stm = contextlib.ExitStack()
            wgs = [sbuf(stm, "wgs0", [128, 4, 8, 256], BF16)]
            wus = [sbuf(stm, "wus0", [128, 4, 8, 256], BF16)]
            wds = [sbuf(stm, "wds0", [128, 4, 2, D], BF16)]
            def load_experts(pz, slot):
                for el in range(4):
                    eidx = pz * 4 + el
                    P.dma("pool", wgs[slot].t[:, el, :, :], wg_d[l, eidx].rearrange("(c p) n -> p c n", p=128),
                          writes=[wgs[slot].r])
                    P.dma("pool", wus[slot].t[:, el, :, :], wu_d[l, eidx].rearrange("(c p) n -> p c n", p=128),
                          writes=[wus[slot].r])
                    P.dma("pool", wds[slot].t[:, el, :, :], wd_d[l, eidx].rearrange("(c p) n -> p c n", p=128),
                          writes=[wds[slot].r])

            with contextlib.ExitStack() as st:
                set_psum(st, 6, 2)
                wrg = sbuf(st, "s3_wrg", [128, 8, 20], F32)
                rbias = sbuf(st, "s3_rb", [128, 20], F32)
                hts = [sbuf(st, "s3_ht%d" % k, [128, 4, D], F32) for k in range(2)]
                mxs = [sbuf(st, "s3_mx%d" % k, [128, 8, TS], BF16) for k in range(2)]
                ss2 = [sbuf(st, "s3_ss%d" % k, [128, 4], F32) for k in range(2)]
                rstd2 = [sbuf(st, "s3_rstd%d" % k, [128, 4], F32) for k in range(2)]
                sqj = sbuf(st, "s3_sqj", [128, D], BF16)
                xn2 = [sbuf(st, "s3_xn%d" % k, [128, 4, D], BF16, 4) for k in range(2)]
                xT2 = [sbuf(st, "s3_xT%d" % k, [128, 8, TS], BF16, 8) for k in range(2)]
                hTf = sbuf(st, "s3_hTf", [128, 8, 128], F32, 2)
                lg = sbuf(st, "s3_lg", [128, 20], F32)
                lg4 = sbuf(st, "s3_lg4", [128, 4, 20], F32)
                r4 = sbuf(st, "s3_r4", [128, 8, 4], F32)
                mg4 = sbuf(st, "s3_mg4", [128, 4, 4], F32)
                t4 = sbuf(st, "s3_t4", [128, 4, 4], F32)
                es4 = sbuf(st, "s3_es4", [128, 4, 4], F32)
                eq4 = sbuf(st, "s3_eq4", [128, 4, 4], F32)
                pr4 = sbuf(st, "s3_pr4", [128, 4, 4, 4], F32)
                sm = sbuf(st, "s3_sm", [128, 16], F32)
                mg = sbuf(st, "s3_mg", [128, 4], F32)
                ge = sbuf(st, "s3_ge", [128, 4], F32)
                esel = sbuf(st, "s3_esel", [128, 4], F32)
                eq = sbuf(st, "s3_eq", [128, 4], F32)
                em2 = sbuf(st, "s3_em2", [128, 4], F32)
                ee = sbuf(st, "s3_ee", [128, 4], F32)
                wsel = sbuf(st, "s3_wsel", [128, 4], F32)
                comb = sbuf(st, "s3_comb", [128, 4, 16], F32)
                cmbT = sbuf(st, "s3_cmbT", [16, TS], BF16)

                P.dma("sp", wrg.t[:], wr_d[l], writes=[wrg.r])
                P.dma("sp", rbias.t[:], rb_d[l], writes=[rbias.r])
                for c in range(8):
                    P.op("dve", lambda e, c=c: e.tensor_scalar(out=wrg.t[:, c, :], in0=wrg.t[:, c, :],
                                                               scalar1=prm.t[:, PC_FFNG + c:PC_FFNG + c + 1],
                                                               scalar2=None, op0=ALU.mult),
                         reads=[wrg.r, prm.r], writes=[wrg.r])

                def s3_load(i):
                    k = i % 2
                    P.dma("sp", hts[k].t[:], tm(h_src, i), reads=[R_h[i]], writes=[hts[k].r])
                    P.dma("sp", mxs[k].t[:], fm(mix_d, i), reads=[R_mixA[i], R_mixB[i]], writes=[mxs[k].r])

                def s3_A(i):
                    k = i % 2
                    ht = hts[k]
                    mx = mxs[k]
                    ss, rstd, xn, xT = ss2[k], rstd2[k], xn2[k], xT2[k]
                    for j in range(4):
                        for half in range(2):
                            bank = next_pf()
                            mm_group(bank, (0, 512), [mx.t[:, c, j * 128:(j + 1) * 128] for c in range(8)],
                                     [w_out_sb.t[:, c, half * 512:(half + 1) * 512] for c in range(8)],
                                     [[mx.r, w_out_sb.r]] * 8)
                            P.op("dve", lambda e: e.tensor_tensor(out=ht.t[:, j, half * 512:(half + 1) * 512],
                                                                  in0=bank.t[:], in1=ht.t[:, j, half * 512:(half + 1) * 512],
                                                                  op=ALU.add),
                                 reads=[bank.r, ht.r], writes=[ht.r])
                    P.dma("sp", tm(h_d, i), ht.t[:], reads=[ht.r], writes=[R_h[i]])
                    norm_stats(st, ht, 0, ss, rstd, sqj)
                    norm_transpose(ht, 0, rstd, xn, xT, PC_FFNG, part=0)

                def s3_A2(i):
                    k = i % 2
                    ht = hts[k]
                    ss, rstd, xn, xT = ss2[k], rstd2[k], xn2[k], xT2[k]
                    norm_transpose(ht, 0, rstd, xn, xT, PC_FFNG, part=1)
                    P.dma("sp", fm(xT_d, i), xT.t[:], reads=xT.res, writes=[R_xT[i]])

                def s3_Ba(i):
                    k = i % 2
                    ht = hts[k]
                    ss, rstd, xn, xT = ss2[k], rstd2[k], xn2[k], xT2[k]
                    for j in range(4):
                        for c2 in range(2):
                            bank = next_pf()
                            while bank is bct:
                                bank = next_pf()
                            for cq in range(4):
                                c = c2 * 4 + cq
                                P.op("pe", lambda e: e.transpose(bank.t[:, cq * 128:(cq + 1) * 128],
                                                                 ht.t[:, j, c * 128:(c + 1) * 128], identf.t[:]),
                                     reads=[ht.r, identf.r], writes=[bank.r], signal=(cq == 3))
                            if c2 == 0:
                                P.op("act", lambda e: e.copy(out=hTf.t[:, 0:4, :], in_=bank.t[:].rearrange("p (a b) -> p a b", a=4)),
                                     reads=[bank.r], writes=[hTf.res[0]])
                            else:
                                P.op("dve", lambda e: e.tensor_copy(out=hTf.t[:, 4:8, :], in_=bank.t[:].rearrange("p (a b) -> p a b", a=4)),
                                     reads=[bank.r], writes=[hTf.res[1]])
                        bl = next_pf()
                        while bl is bct:
                            bl = next_pf()
                        mm_group(bl, (0, 20), [hTf.t[:, c, :] for c in range(8)], [wrg.t[:, c, :] for c in range(8)],
                                 [[hTf.res[c // 4], wrg.r] for c in range(8)])
                        P.op("dve", lambda e: e.scalar_tensor_tensor(out=lg4.t[:, j, :], in0=bl.t[:, 0:20],
                                                                     scalar=rstd.t[:, j:j + 1], in1=rbias.t[:],
                                                                     op0=ALU.mult, op1=ALU.add),
                             reads=[bl.r, rstd.r, rbias.r], writes=[lg4.r])

                def s3_Bb(i):
                    V = lambda fn, rd, wr: P.op("dve", fn, reads=rd, writes=wr)
                    S3 = [128, 4, 4]
                    bc = lambda ap_: ap_.unsqueeze(2).to_broadcast(S3)
                    glg = lg4.t[:, :, 0:4]
                    V(lambda e: e.reduce_max(out=r4.t[:, 0, :], in_=glg, axis=AX.X), [lg4.r], [r4.r])
                    V(lambda e: e.tensor_tensor(out=mg4.t[:], in0=glg, in1=bc(r4.t[:, 0, :]), op=ALU.is_ge),
                      [lg4.r, r4.r], [mg4.r])
                    V(lambda e: e.tensor_tensor(out=t4.t[:], in0=glg, in1=bc(r4.t[:, 0, :]), op=ALU.subtract),
                      [lg4.r, r4.r], [t4.r])
                    P.op("act", lambda e: e.activation(out=t4.t[:], in_=t4.t[:], func=AF.Exp), reads=[t4.r], writes=[t4.r])
                    V(lambda e: e.reduce_sum(out=r4.t[:, 1, :], in_=t4.t[:], axis=AX.X), [t4.r, r4.r], [r4.r])
                    V(lambda e: e.reciprocal(out=r4.t[:, 2, :], in_=r4.t[:, 1, :]), [r4.r], [r4.r])
                    el4 = lg4.t[:, :, 4:20].rearrange("p j (g i) -> p j g i", g=4)
                    V(lambda e: e.tensor_tensor(out=pr4.t[:], in0=el4,
                                                in1=mg4.t[:].unsqueeze(3).to_broadcast([128, 4, 4, 4]), op=ALU.mult),
                      [lg4.r, mg4.r], [pr4.r])
                    V(lambda e: e.reduce_sum(out=es4.t[:], in_=pr4.t[:].rearrange("p j g i -> p j i g"), axis=AX.X),
                      [pr4.r], [es4.r])
                    V(lambda e: e.reduce_max(out=r4.t[:, 3, :], in_=es4.t[:], axis=AX.X), [es4.r, r4.r], [r4.r])
                    V(lambda e: e.tensor_tensor(out=eq4.t[:], in0=es4.t[:], in1=bc(r4.t[:, 3, :]), op=ALU.is_ge),
                      [es4.r, r4.r], [eq4.r])
                    V(lambda e: e.scalar_tensor_tensor(out=t4.t[:], in0=eq4.t[:], scalar=-1e30, in1=es4.t[:],
                                                       op0=ALU.mult, op1=ALU.add), [eq4.r, es4.r, t4.r], [t4.r])
                    V(lambda e: e.reduce_max(out=r4.t[:, 4, :], in_=t4.t[:], axis=AX.X), [t4.r, r4.r], [r4.r])
                    V(lambda e: e.tensor_tensor(out=eq4.t[:], in0=es4.t[:], in1=bc(r4.t[:, 4, :]), op=ALU.is_ge),
                      [es4.r, r4.r, eq4.r], [eq4.r])
                    V(lambda e: e.tensor_tensor(out=t4.t[:], in0=es4.t[:], in1=bc(r4.t[:, 3, :]), op=ALU.subtract),
                      [es4.r, r4.r, t4.r], [t4.r])
                    P.op("act", lambda e: e.activation(out=t4.t[:], in_=t4.t[:], func=AF.Exp), reads=[t4.r], writes=[t4.r])
                    V(lambda e: e.tensor_tensor(out=t4.t[:], in0=t4.t[:], in1=eq4.t[:], op=ALU.mult),
                      [t4.r, eq4.r], [t4.r])
                    V(lambda e: e.reduce_sum(out=r4.t[:, 5, :], in_=t4.t[:], axis=AX.X), [t4.r, r4.r], [r4.r])
                    V(lambda e: e.reciprocal(out=r4.t[:, 6, :], in_=r4.t[:, 5, :]), [r4.r], [r4.r])
                    V(lambda e: e.tensor_tensor(out=r4.t[:, 6, :], in0=r4.t[:, 6, :], in1=r4.t[:, 2, :], op=ALU.mult),
                      [r4.r], [r4.r])
                    V(lambda e: e.tensor_tensor(out=t4.t[:], in0=t4.t[:], in1=bc(r4.t[:, 6, :]), op=ALU.mult),
                      [t4.r, r4.r], [t4.r])
                    V(lambda e: e.tensor_tensor(out=comb.t[:].rearrange("p j (g i) -> p j g i", g=4),
                                                in0=t4.t[:].unsqueeze(2).to_broadcast([128, 4, 4, 4]),
                                                in1=mg4.t[:].unsqueeze(3).to_broadcast([128, 4, 4, 4]), op=ALU.mult),
                      [t4.r, mg4.r], [comb.r])

                def s3_B2(i):
                    for j in range(4):
                        P.op("pe", lambda e: e.transpose(bct.t[0:16, j * 128:(j + 1) * 128], comb.t[:, j, :], identf.t[:]),
                             reads=[comb.r, identf.r], writes=[bct.r])
                    P.op("act", lambda e: e.copy(out=cmbT.t[:], in_=bct.t[0:16, :]), reads=[bct.r], writes=[cmbT.r])
                    P.dma("sp", cmb_d[:, i * TS:(i + 1) * TS], cmbT.t[:], reads=[cmbT.r], writes=[R_cmb[i]])

                bct = pf.pop()
                s3_load(0)
                if NT > 1:
                    s3_load(1)
                s3_A(0)
                s3_A2(0)
                load_experts(0, 0)
                for i in range(NT):
                    if i + 1 < NT:
                        s3_A(i + 1)
                    if i >= 1:
                        s3_B2(i - 1)
                    s3_Ba(i)
                    if i + 2 < NT:
                        s3_load(i + 2)
                    if i + 1 < NT:
                        s3_A2(i + 1)
                    s3_Bb(i)
                s3_B2(NT - 1)
                P.barrier()
            st_wout.close()
            if dbg == "s3":
                stm.close()
                st_ple.close()
                break

            wgs.append(sbuf(stm, "wgs1", [128, 4, 8, 256], BF16))
            wus.append(sbuf(stm, "wus1", [128, 4, 8, 256], BF16))
            wds.append(sbuf(stm, "wds1", [128, 4, 2, D], BF16))
            with contextlib.ExitStack() as st:
                set_psum(st, 8, 0)
                hts = [sbuf(st, "s4_ht%d" % k, [128, 4, D], F32) for k in range(2)]
                xTs = [sbuf(st, "s4_xT%d" % k, [128, 8, TS], BF16) for k in range(2)]
                cms = [sbuf(st, "s4_cm%d" % k, [16, TS], BF16) for k in range(2)]
                cbs = sbuf(st, "s4_cb", [128, TS], F32)
                sg_ = sbuf(st, "s4_sg", [128, TS], F32)
                tt_ = sbuf(st, "s4_tt", [128, TS], F32)
                hdn = sbuf(st, "s4_hdn", [128, 4, 2, TS], BF16)

                def s4_load(i):
                    k = i % 2
                    P.dma("sp", hts[k].t[:], tm(h_d, i), reads=[R_h[i]], writes=[hts[k].r])
                    P.dma("sp", xTs[k].t[:], fm(xT_d, i), reads=[R_xT[i]], writes=[xTs[k].r])
                    P.dma("sp", cms[k].t[:], cmb_d[:, i * TS:(i + 1) * TS], reads=[R_cmb[i]], writes=[cms[k].r])

                s4_load(0)
                for pz in range(4):
                    slot = pz % 2
                    if pz + 1 < 4:
                        load_experts(pz + 1, (pz + 1) % 2)
                    if pz == 0:
                        for c in range(8):
                            P.dma("pool", wpg.t[:, c, :], pgw_d[l, c * 128:(c + 1) * 128, :], writes=[wpg.r])
                        P.dma("pool", wpp.t[:], ppj_d[l].rearrange("(c p) n -> p c n", p=128), writes=[wpp.r])
                    for i in range(NT):
                        k = i % 2
                        if i + 1 < NT:
                            s4_load(i + 1)
                        elif pz + 1 < 4:
                            s4_load(0)
                        ht, xT_, cm = hts[k], xTs[k], cms[k]
                        for el in range(4):
                            eidx = pz * 4 + el
                            bcb = next_pf()
                            P.op("pe", lambda e: e.matmul(bcb.t[:], sel.t[0:16, eidx, :], cm.t[0:16, :],
                                                          start=True, stop=True),
                                 reads=[sel.r, cm.r], writes=[bcb.r])
                            P.op("act", lambda e: e.copy(out=cbs.t[:], in_=bcb.t[:]), reads=[bcb.r], writes=[cbs.r])
                            for hc in range(2):
                                bg_ = next_pf()
                                mm_group(bg_, (0, TS), [wgs[slot].t[:, el, c, hc * 128:(hc + 1) * 128] for c in range(8)],
                                         [xT_.t[:, c, :] for c in range(8)], [[wgs[slot].r, xT_.r]] * 8)
                                bu_ = next_pf()
                                mm_group(bu_, (0, TS), [wus[slot].t[:, el, c, hc * 128:(hc + 1) * 128] for c in range(8)],
                                         [xT_.t[:, c, :] for c in range(8)], [[wus[slot].r, xT_.r]] * 8)
                                P.op("act", lambda e: e.activation(out=sg_.t[:], in_=bg_.t[:], func=AF.Silu),
                                     reads=[bg_.r], writes=[sg_.r])
                                P.op("dve", lambda e: e.tensor_tensor(out=tt_.t[:], in0=bu_.t[:], in1=cbs.t[:],
                                                                      op=ALU.mult),
                                     reads=[bu_.r, cbs.r], writes=[tt_.r])
                                P.op("dve", lambda e: e.tensor_tensor(out=hdn.t[:, el, hc, :], in0=sg_.t[:], in1=tt_.t[:],
                                                                      op=ALU.mult),
                                     reads=[sg_.r, tt_.r], writes=[hdn.r])
                        for j in range(4):
                            for half in range(2):
                                by = next_pf()
                                mm_group(by, (0, 512),
                                         [hdn.t[:, el, hc, j * 128:(j + 1) * 128] for el in range(4) for hc in range(2)],
                                         [wds[slot].t[:, el, hc, half * 512:(half + 1) * 512]
                                          for el in range(4) for hc in range(2)],
                                         [[hdn.r, wds[slot].r]] * 8)
                                P.op("dve", lambda e: e.tensor_tensor(out=ht.t[:, j, half * 512:(half + 1) * 512],
                                                                      in0=by.t[:],
                                                                      in1=ht.t[:, j, half * 512:(half + 1) * 512],
                                                                      op=ALU.add),
                                     reads=[by.r, ht.r], writes=[ht.r])
                        P.dma("sp", tm(h_d, i), ht.t[:], reads=[ht.r], writes=[R_h[i]])
                P.barrier()
            stm.close()
            if dbg == "s4":
                st_ple.close()
                break

            with contextlib.ExitStack() as st:
                set_psum(st, 6, 2)
                gbf = sbuf(st, "s5_gbf", [1, D], F32)
                gbb = sbuf(st, "s5_gbb", [1, D], BF16)
                hts = [sbuf(st, "s5_ht%d" % k, [128, 4, D], F32) for k in range(3)]
                pts_ = [sbuf(st, "s5_p%d" % k, [128, 4, 256], F32) for k in range(3)]
                ss2 = [sbuf(st, "s5_ss%d" % k, [128, 4], F32) for k in range(2)]
                rstd2 = [sbuf(st, "s5_rstd%d" % k, [128, 4], F32) for k in range(2)]
                sqj = sbuf(st, "s5_sqj", [128, D], BF16)
                xn2 = [sbuf(st, "s5_xn%d" % k, [128, 4, D], BF16, 4) for k in range(2)]
                xT2 = [sbuf(st, "s5_xT%d" % k, [128, 8, TS], BF16, 8) for k in range(2)]
                pT2 = [sbuf(st, "s5_pT%d" % k, [128, 2, TS], BF16) for k in range(2)]
                gts = [sbuf(st, "s5_gt%d" % k, [128, 512], F32) for k in range(3)]
                tqs = [sbuf(st, "s5_tq%d" % k, [128, 512], F32) for k in range(3)]
                if l + 1 < DEPTH:
                    st_win, w_in_next = load_w_in(l + 1)
                P.dma("sp", gbf.t[:], pgb_d[l:l + 1, :], writes=[gbf.r])
                P.op("dve", lambda e: e.tensor_copy(out=gbb.t[:], in_=gbf.t[:]), reads=[gbf.r], writes=[gbb.r])

                def s5_load(i):
                    k = i % 3
                    P.dma("sp", hts[k].t[:], tm(h_d, i), reads=[R_h[i]], writes=[hts[k].r])
                    P.dma("sp", pts_[k].t[:], tm(p_d[l], i), writes=[pts_[k].r])

                def s5_P(i):
                    k = i % 2
                    ht, pt_ = hts[i % 3], pts_[i % 3]
                    ss, rstd, xn, xT, pT = ss2[k], rstd2[k], xn2[k], xT2[k], pT2[k]
                    norm_stats(st, ht, 0, ss, rstd, sqj)
                    norm_transpose(ht, 0, rstd, xn, xT, PC_PLEG)
                    for c in range(2):
                        bank = next_pf()
                        for j in range(4):
                            P.op("pe", lambda e: e.transpose(bank.t[:, j * 128:(j + 1) * 128],
                                                             pt_.t[:, j, c * 128:(c + 1) * 128], identf.t[:]),
                                 reads=[pt_.r, identf.r], writes=[bank.r], signal=(j == 3))
                        P.op("act", lambda e: e.copy(out=pT.t[:, c, :], in_=bank.t[:]), reads=[bank.r], writes=[pT.r])

                def s5_M(i):
                    k = i % 2
                    ht, pt_ = hts[i % 3], pts_[i % 3]
                    ss, rstd, xn, xT, pT = ss2[k], rstd2[k], xn2[k], xT2[k], pT2[k]
                    for j in range(4):
                        for half in range(2):
                            hs = slice(half * 512, (half + 1) * 512)
                            gt = gts[(j * 2 + half) % 3]
                            tq = tqs[(j * 2 + half) % 3]
                            bg_ = next_pf()
                            mm_group(bg_, (0, 512),
                                     [xT.t[:, c, j * 128:(j + 1) * 128] for c in range(8)] + [onesrow.t[0:1, :]],
                                     [wpg.t[:, c, hs] for c in range(8)] + [gbb.t[0:1, hs]],
                                     [[xT.res[c], wpg.r] for c in range(8)] + [[onesrow.r, gbb.r]])
                            bp_ = next_pf()
                            mm_group(bp_, (0, 512), [pT.t[:, c, j * 128:(j + 1) * 128] for c in range(2)],
                                     [wpp.t[:, c, hs] for c in range(2)], [[pT.r, wpp.r]] * 2)
                            P.op("act", lambda e: e.activation(out=gt.t[:], in_=bg_.t[:], func=AF.Sigmoid),
                                 reads=[bg_.r], writes=[gt.r])
                            P.op("dve", lambda e: e.tensor_tensor(out=tq.t[:], in0=bp_.t[:], in1=gt.t[:], op=ALU.mult),
                                 reads=[bp_.r, gt.r], writes=[tq.r])
                            P.op("dve", lambda e: e.tensor_tensor(out=ht.t[:, j, hs], in0=ht.t[:, j, hs], in1=tq.t[:],
                                                                  op=ALU.add),
                                 reads=[ht.r, tq.r], writes=[ht.r])
                    if l < DEPTH - 1:
                        P.dma("sp", tm(h_d, i), ht.t[:], reads=[ht.r], writes=[R_h[i]])
                    else:
                        norm_stats(st, ht, 0, ss, rstd, sqj)
                        for j in range(4):
                            P.op("dve", lambda e: e.scalar_tensor_tensor(out=ht.t[:, j, :], in0=ht.t[:, j, :],
                                                                         scalar=rstd.t[:, j:j + 1], in1=gfin.t[:],
                                                                         op0=ALU.mult, op1=ALU.mult),
                                 reads=[ht.r, rstd.r, gfin.r], writes=[ht.r])
                        P.dma("sp", tm(out_d, i), ht.t[:], reads=[ht.r], writes=[R_out[i]])

                s5_load(0)
                if NT > 1:
                    s5_load(1)
                s5_P(0)
                for i in range(NT):
                    if i + 2 < NT:
                        s5_load(i + 2)
                    if i + 1 < NT:
                        s5_P(i + 1)
                    s5_M(i)
                P.barrier()
            st_ple.close()

        P.barrier()
        P.final_wait()
    return P


def _host_consts():
    c = {}
    c["c_identf"] = np.eye(128, dtype=np.float32)
    rp = np.zeros((128, 128), np.float32)
    for b in range(4):
        for d in range(16):
            rp[b * 32 + d + 16, b * 32 + d] = -1.0
            rp[b * 32 + d, b * 32 + d + 16] = 1.0
    c["c_rperm"] = rp
    kk = np.arange(128)[:, None]
    qq = np.arange(128)[None, :]
    c["c_tri"] = (qq >= kk).astype(np.float32)
    b64 = np.zeros((128, 128), np.float32)
    b64[0:64, 0:64] = 1.0 / 64
    b64[64:128, 64:128] = 1.0 / 64
    c["c_blk64"] = b64
    c["c_ones256"] = np.full((128, 128), 1.0 / 256, np.float32)
    sel = np.zeros((16, NE, 128), np.float32)
    for e in range(NE):
        sel[e, e, :] = 1.0
    c["c_sel"] = sel
    pos = np.arange(S, dtype=np.float32)
    inv = (10000.0 ** (-np.arange(0, 32, 2, dtype=np.float32) / np.float32(32))).astype(np.float32)
    ang = (pos[:, None] * inv[None, :]).astype(np.float32)
    ang = np.concatenate([ang, ang], axis=-1)
    cosT = np.cos(ang.astype(np.float64)).astype(np.float32).T
    sinT = np.sin(ang.astype(np.float64)).astype(np.float32).T
    c["c_cos"] = np.ascontiguousarray(np.tile(cosT, (4, 1)))
    c["c_sin"] = np.ascontiguousarray(np.tile(sinT, (4, 1)))
    wins = np.array([2, 4, 8, 16])
    corr = np.zeros((128, 2, 16), np.float32)
    for cc in range(2):
        for p in range(128):
            w = wins[cc * 2 + p // 64]
            for t in range(16):
                corr[p, cc, t] = w / min(t + 1, w)
    c["c_corr"] = corr
    iw = np.zeros((128, 2), np.float32)
    for cc in range(2):
        for p in range(128):
            iw[p, cc] = 1.0 / wins[cc * 2 + p // 64]
    c["_iw"] = iw
    return c


def _fmcols(v, nch):
    return np.ascontiguousarray(np.asarray(v, np.float32).reshape(nch, 128).T)


def _layout_inputs(inp):
    c = _host_consts()
    iw = c.pop("_iw")
    shared = dict(c)
    prm = np.zeros((DEPTH, 128, PC_N), np.float32)
    wr = np.zeros((DEPTH, 128, 8, 20), np.float32)
    rb = np.zeros((DEPTH, 128, 20), np.float32)
    lamv = np.zeros((DEPTH, 128, 4, 32), np.float32)
    pw = np.zeros((DEPTH, 128, 2, 128), np.float32)
    for l in range(DEPTH):
        prm[l, :, PC_MIXG:PC_MIXG + 8] = _fmcols(inp["mix_norm"][l], 8)
        prm[l, :, PC_FFNG:PC_FFNG + 8] = _fmcols(inp["ffn_norm"][l], 8)
        prm[l, :, PC_PLEG:PC_PLEG + 8] = _fmcols(inp["ple_norm"][l], 8)
        prm[l, :, PC_FING:PC_FING + 8] = _fmcols(inp["final_norm"], 8)
        cw = np.asarray(inp["conf_conv_w"][l], np.float32)
        for cc in range(2):
            prm[l, :, PC_CW + cc * CK:PC_CW + (cc + 1) * CK] = cw[:, cc * 128:(cc + 1) * 128].T
        prm[l, :, PC_CB:PC_CB + 2] = _fmcols(inp["conf_conv_b"][l], 2)
        prm[l, :, PC_LNG:PC_LNG + 2] = _fmcols(inp["conf_ln_g"][l], 2)
        prm[l, :, PC_LNB:PC_LNB + 2] = _fmcols(inp["conf_ln_b"][l], 2)
        prm[l, :, PC_PB:PC_PB + 2] = _fmcols(np.asarray(inp["pool_b"][l]).reshape(256), 2)
        prm[l, :, PC_PS:PC_PS + 2] = _fmcols(inp["pool_scale"][l], 2)
        sw = np.asarray(inp["sconv_w"][l], np.float32)
        for cc in range(2):
            prm[l, :, PC_SW + cc * 3:PC_SW + (cc + 1) * 3] = sw[:, cc * 128:(cc + 1) * 128].T
        prm[l, :, PC_SUB] = np.tile(np.asarray(inp["diff_subln_g"][l], np.float32), 2)
        prm[l, :, PC_IW:PC_IW + 2] = iw
        wcat = np.concatenate([np.asarray(inp["router_group_w"][l], np.float32),
                               np.asarray(inp["router_expert_w"][l], np.float32)], axis=1)
        wr[l] = wcat.reshape(8, 128, 20).transpose(1, 0, 2)
        bcat = np.concatenate([np.asarray(inp["router_group_b"][l], np.float32),
                               np.asarray(inp["router_expert_b"][l], np.float32)])
        rb[l] = np.tile(bcat[None, :], (128, 1))
        for n_, key in enumerate(["diff_lam_q1", "diff_lam_k1", "diff_lam_q2", "diff_lam_k2"]):
            lamv[l, :, n_, :] = np.tile(np.asarray(inp[key][l], np.float32)[None, :], (128, 1))
        pwl = np.asarray(inp["pool_w"][l], np.float32)
        for g in range(4):
            cc, hh = g // 2, g % 2
            pw[l, hh * 64:(hh + 1) * 64, cc, hh * 64:(hh + 1) * 64] = pwl[g]
    shared.update({
        "prm": prm, "wr": wr, "rbias": rb, "lamv": lamv, "poolw": pw,
        "gfin": np.ascontiguousarray(np.tile(np.asarray(inp["final_norm"], np.float32)[None, :], (128, 1))),
    })
    for key in ["w_in", "w_out", "expert_w_gate", "expert_w_up", "expert_w_down", "ple_gate_w", "ple_gate_b",
                "ple_proj"]:
        shared[key] = np.ascontiguousarray(np.asarray(inp[key], np.float32))
    return shared


_NC_CACHE = {}


def _get_nc():
    if "nc" not in _NC_CACHE:
        nc = bass.Bass("TRN2", target_bir_lowering=False)
        build(nc)
        _NC_CACHE["nc"] = nc
    return _NC_CACHE["nc"]


def kernel(**inputs):
    shared = _layout_inputs(inputs)
    x = np.asarray(inputs["x"], np.float32)
    p = np.asarray(inputs["p"], np.float32)
    n = x.shape[0]
    in_maps = []
    for b in range(n):
        m = dict(shared)
        m["x"] = np.ascontiguousarray(x[b])
        m["p"] = np.ascontiguousarray(p[:, b])
        in_maps.append(m)
    nc = _get_nc()
    res = run_bass_kernel_spmd(nc, in_maps, core_ids=list(range(n)))
    return np.stack([np.asarray(r["out"], np.float32) for r in res.results], axis=0)
```
